# Optimizing a Trainium2 kernel written in Bass

```python
import math
import jax
import jax.numpy as jnp
from jax import lax
import numpy as np

D_MODEL = 1024
BATCH = 4
SEQ = 8192
DEPTH = 4

NORM_EPS = 1e-6
NEG_INF = -1e30
TINY = 1e-30

DN_HEADS = 4
DN_HEAD_DIM = 128
DN_CONV = 4
DN_CHUNK = 64
DN_W = DN_HEADS * DN_HEAD_DIM

MLA_HEADS = 4
MLA_Q_RANK = 256
MLA_KV_RANK = 128
MLA_NOPE = 128
MLA_ROPE = 64
MLA_V = 128
MLA_QK = MLA_NOPE + MLA_ROPE
MLA_W = MLA_HEADS * MLA_V
ROPE_THETA = 10000.0
Q_BLOCK = 128

EV_IN = 4 * DN_W + 2 * DN_HEADS + MLA_Q_RANK + MLA_KV_RANK + MLA_ROPE
EV_MIX = DN_W + MLA_W

NSA_HEADS = 16
NSA_GROUPS = 4
NSA_HPG = NSA_HEADS // NSA_GROUPS
NSA_HEAD_DIM = 64
NSA_Q_W = NSA_HEADS * NSA_HEAD_DIM
NSA_KV_W = NSA_GROUPS * NSA_HEAD_DIM
CMP_LEN = 32
CMP_STRIDE = 16
CMP_HIDDEN = 256
SLC_LEN = 64
SLC_TOPN = 16
WIN = 512
NSA_Q_BLOCK = 64
FORCE_SCORE = 1e9
OD_IN = NSA_Q_W + 6 * NSA_KV_W + 3 * NSA_HEADS

D_FF = 2816
N_EXPERTS = 8
TOP_K = 2
D_FF_EXPERT = 3584
MOE_BLOCK = 512

kernel_name = 'hybrid_deltanet_mla_nsa_moe_trunk'


def rms_norm(x, gain):
    xf = x.astype(jnp.float32)
    y = xf * lax.rsqrt(jnp.mean(xf * xf, axis=-1, keepdims=True) + NORM_EPS)
    return (y * gain.astype(jnp.float32)).astype(x.dtype)


def l2_normalize(x):
    return x * lax.rsqrt(jnp.sum(x * x, axis=-1, keepdims=True) + NORM_EPS)


def ada_modulation(c, w, b):
    m = jax.nn.silu(c) @ w + b
    shift, scale, gate = jnp.split(m[:, None, :], 3, axis=-1)
    return shift, scale, gate


def masked_softmax(s, mask):
    s = jnp.where(mask, s.astype(jnp.float32), NEG_INF)
    p = jnp.where(mask, jnp.exp(s - jnp.max(s, axis=-1, keepdims=True)), 0.0)
    return p / jnp.maximum(jnp.sum(p, axis=-1, keepdims=True), TINY)


def split_cols(t, sizes):
    return jnp.split(t, [int(v) for v in np.cumsum(sizes)[:-1]], axis=-1)


def causal_depthwise_conv(x, w):
    k_len, ch = w.shape
    return lax.conv_general_dilated(x, w[:, None, :].astype(x.dtype), window_strides=(1,),
                                    padding=[(k_len - 1, 0)],
                                    dimension_numbers=('NWC', 'WIO', 'NWC'),
                                    feature_group_count=ch)


def gated_delta_rule(q, k, v, g, beta):
    out_dtype = v.dtype
    f32 = jnp.float32
    b_, s_len, n_h, dk = q.shape
    dv = v.shape[-1]
    cs = DN_CHUNK
    n_ch = s_len // cs

    def to_chunks(t):
        return t.astype(f32).reshape(b_, n_ch, cs, n_h, -1).transpose(0, 3, 1, 2, 4)

    q = to_chunks(l2_normalize(q.astype(f32)) * (dk ** -0.5))
    k = to_chunks(l2_normalize(k.astype(f32)))
    v = to_chunks(v)
    g = jnp.cumsum(to_chunks(g[..., None])[..., 0], axis=-1)
    beta = to_chunks(beta[..., None])[..., 0]
    incl = jnp.tril(jnp.ones((cs, cs), dtype=bool))
    strict = jnp.tril(jnp.ones((cs, cs), dtype=bool), k=-1)
    gdiff = g[..., :, None] - g[..., None, :]
    decay = jnp.where(incl, jnp.exp(jnp.where(incl, gdiff, 0.0)), 0.0)
    k_beta = k * beta[..., None]
    lower = jnp.where(strict, jnp.einsum('bhnid,bhnjd->bhnij', k_beta, k) * decay, 0.0)
    tmat = lower + jnp.eye(cs, dtype=f32)
    u = lax.linalg.triangular_solve(tmat, v * beta[..., None], left_side=True, lower=True,
                                    unit_diagonal=True)
    w = lax.linalg.triangular_solve(tmat, k_beta * jnp.exp(g)[..., None], left_side=True,
                                    lower=True, unit_diagonal=True)
    qk = jnp.einsum('bhnid,bhnjd->bhnij', q, k) * decay
    q_g = q * jnp.exp(g)[..., None]
    k_tail = k * jnp.exp(g[..., -1:] - g)[..., None]
    a_last = jnp.exp(g[..., -1])

    def step(state, xs):
        u_i, w_i, qg_i, qk_i, kt_i, a_i = xs
        v_new = u_i - jnp.einsum('bhck,bhkv->bhcv', w_i, state)
        o_i = (jnp.einsum('bhck,bhkv->bhcv', qg_i, state)
               + jnp.einsum('bhij,bhjv->bhiv', qk_i, v_new))
        state = state * a_i[..., None, None] + jnp.einsum('bhck,bhcv->bhkv', kt_i, v_new)
        return state, o_i

    xs = tuple(jnp.moveaxis(t, 2, 0) for t in (u, w, q_g, qk, k_tail, a_last))
    state0 = jnp.zeros((b_, n_h, dk, dv), f32)
    _, o = lax.scan(step, state0, xs)
    return o.transpose(1, 0, 3, 2, 4).reshape(b_, s_len, n_h, dv).astype(out_dtype)


def gated_deltanet(p_q, p_k, p_v, p_z, p_b, p_a, conv_w, a_log, dt_bias, norm_gain):
    b_, s_len, _ = p_q.shape
    f32 = jnp.float32
    qkv = jax.nn.silu(causal_depthwise_conv(jnp.concatenate([p_q, p_k, p_v], axis=-1), conv_w))
    q, k, v = jnp.split(qkv, 3, axis=-1)

    def heads(t):
        return t.reshape(b_, s_len, DN_HEADS, DN_HEAD_DIM)

    beta = jax.nn.sigmoid(p_b.astype(f32))
    g = -jnp.exp(a_log.astype(f32)) * jax.nn.softplus(p_a.astype(f32) + dt_bias.astype(f32))
    o = gated_delta_rule(heads(q), heads(k), heads(v), g, beta)
    o = rms_norm(o, norm_gain) * jax.nn.silu(heads(p_z))
    return o.reshape(b_, s_len, DN_W)


def rope_tables(positions):
    inv = 1.0 / (ROPE_THETA ** (jnp.arange(0, MLA_ROPE, 2, dtype=jnp.float32) / MLA_ROPE))
    ang = positions.astype(jnp.float32)[..., None] * inv
    return jnp.cos(ang), jnp.sin(ang)


def apply_rope(x, cos, sin):
    half = x.shape[-1] // 2
    x1, x2 = x[..., :half], x[..., half:]
    cos = cos.astype(x.dtype)
    sin = sin.astype(x.dtype)
    return jnp.concatenate([x1 * cos - x2 * sin, x2 * cos + x1 * sin], axis=-1)


def causal_block_attention(q, k, v, scale):
    b_, s_len, n_h, _ = q.shape
    nb = s_len // Q_BLOCK
    q_b = q.reshape(b_, nb, Q_BLOCK, n_h, -1).swapaxes(0, 1)
    k_pos = jnp.arange(s_len)

    def block(args):
        i, q_i = args
        t = i * Q_BLOCK + jnp.arange(Q_BLOCK)
        s = jnp.einsum('bqhd,bkhd->bhqk', q_i, k).astype(jnp.float32) * scale
        s = jnp.where(k_pos[None, :] <= t[:, None], s, NEG_INF)
        p = jax.nn.softmax(s, axis=-1)
        return jnp.einsum('bhqk,bkhd->bqhd', p.astype(v.dtype), v)

    o = lax.map(block, (jnp.arange(nb), q_b))
    return o.swapaxes(0, 1).reshape(b_, s_len, n_h, v.shape[-1])


def multi_head_latent_attention(c_q, c_kv, k_r, cos, sin, q_norm, kv_norm, w_uq, w_ukv):
    b_, s_len, _ = c_q.shape
    q = (rms_norm(c_q, q_norm) @ w_uq).reshape(b_, s_len, MLA_HEADS, MLA_QK)
    kv = (rms_norm(c_kv, kv_norm) @ w_ukv).reshape(b_, s_len, MLA_HEADS, MLA_NOPE + MLA_V)
    q_nope, q_rope = q[..., :MLA_NOPE], q[..., MLA_NOPE:]
    k_nope, v = kv[..., :MLA_NOPE], kv[..., MLA_NOPE:]
    q_rope = apply_rope(q_rope, cos[:, :, None, :], sin[:, :, None, :])
    k_rope = apply_rope(k_r, cos, sin)[:, :, None, :]
    q = jnp.concatenate([q_nope, q_rope], axis=-1)
    k = jnp.concatenate([k_nope, jnp.broadcast_to(k_rope, (b_, s_len, MLA_HEADS, MLA_ROPE))], axis=-1)
    o = causal_block_attention(q, k, v, MLA_QK ** -0.5)
    return o.reshape(b_, s_len, MLA_W)


def even_token_mixer(h, cos, sin, w_in, conv_w, a_log, dt_bias, dn_norm, q_norm, kv_norm,
                     w_uq, w_ukv, w_out):
    sizes = [DN_W] * 4 + [DN_HEADS] * 2 + [MLA_Q_RANK, MLA_KV_RANK, MLA_ROPE]
    p_q, p_k, p_v, p_z, p_b, p_a, c_q, c_kv, k_r = split_cols(h @ w_in, sizes)
    o_a = gated_deltanet(p_q, p_k, p_v, p_z, p_b, p_a, conv_w, a_log, dt_bias, dn_norm)
    o_b = multi_head_latent_attention(c_q, c_kv, k_r, cos, sin, q_norm, kv_norm, w_uq, w_ukv)
    return jnp.concatenate([o_a, o_b], axis=-1) @ w_out


def compress_blocks(t, pos_emb, w1, w2):
    b_, s_len, n_g, d = t.shape
    r = CMP_LEN // CMP_STRIDE
    n_chunk = s_len // CMP_STRIDE
    n_cmp = n_chunk - r + 1
    ch = t.reshape(b_, n_chunk, CMP_STRIDE, n_g, d)
    blocks = jnp.concatenate([ch[:, j:j + n_cmp] for j in range(r)], axis=2)
    blocks = blocks + pos_emb[:, None, :]
    flat = blocks.transpose(0, 1, 3, 2, 4).reshape(b_, n_cmp, n_g, CMP_LEN * d)
    return jax.nn.silu(flat @ w1) @ w2


def selection_importance(p_cmp, n_slc):
    r = CMP_LEN // CMP_STRIDE
    pad = [(0, 0)] * (p_cmp.ndim - 1)
    chunk = sum(jnp.pad(p_cmp, pad + [(j, r - 1 - j)]) for j in range(r)) / r
    return chunk.reshape(*chunk.shape[:-1], n_slc, SLC_LEN // CMP_STRIDE).sum(axis=-1)


def native_sparse_attention(q, k_cmp, v_cmp, k_slc, v_slc, k_win, v_win, gates):
    b_, s_len, n_g, n_hg, d = q.shape
    qb = NSA_Q_BLOCK
    nb = s_len // qb
    n_slc = s_len // SLC_LEN
    n_top = min(SLC_TOPN, n_slc)
    scale = d ** -0.5
    cmp_end = jnp.arange(k_cmp.shape[1]) * CMP_STRIDE + CMP_LEN - 1
    ks_blk = k_slc.reshape(b_, n_slc, SLC_LEN, n_g, d).transpose(0, 3, 1, 2, 4)
    vs_blk = v_slc.reshape(b_, n_slc, SLC_LEN, n_g, d).transpose(0, 3, 1, 2, 4)
    kw_pad = jnp.pad(k_win, ((0, 0), (WIN, 0), (0, 0), (0, 0)))
    vw_pad = jnp.pad(v_win, ((0, 0), (WIN, 0), (0, 0), (0, 0)))
    b_idx = jnp.arange(b_)[:, None, None, None]
    g_idx = jnp.arange(n_g)[None, :, None, None]
    blk_ids = jnp.arange(n_slc)
    m_sel = n_top * SLC_LEN

    def block(args):
        i, q_i, gate_i = args
        t = i * qb + jnp.arange(qb)
        s_c = jnp.einsum('bqghd,bngd->bghqn', q_i, k_cmp) * scale
        p_c = masked_softmax(s_c, cmp_end[None, :] <= t[:, None])
        o_c = jnp.einsum('bghqn,bngd->bqghd', p_c.astype(v_cmp.dtype), v_cmp)
        imp = selection_importance(jnp.sum(p_c, axis=2), n_slc)
        cur = (t // SLC_LEN)[:, None]
        forced = (blk_ids == 0) | (blk_ids == cur) | (blk_ids == cur - 1)
        imp = jnp.where(forced, FORCE_SCORE, imp)
        imp = jnp.where(blk_ids <= cur, imp, -1.0)
        _, sel = lax.top_k(imp, n_top)
        k_sel = ks_blk[b_idx, g_idx, sel].reshape(b_, n_g, qb, m_sel, d)
        v_sel = vs_blk[b_idx, g_idx, sel].reshape(b_, n_g, qb, m_sel, d)
        key_pos = (sel[..., None] * SLC_LEN + jnp.arange(SLC_LEN)).reshape(b_, n_g, qb, m_sel)
        s_s = jnp.einsum('bqghd,bgqmd->bghqm', q_i, k_sel) * scale
        p_s = masked_softmax(s_s, (key_pos <= t[:, None])[:, :, None])
        o_s = jnp.einsum('bghqm,bgqmd->bqghd', p_s.astype(v_sel.dtype), v_sel)
        kw = lax.dynamic_slice_in_dim(kw_pad, i * qb, qb + WIN, axis=1)
        vw = lax.dynamic_slice_in_dim(vw_pad, i * qb, qb + WIN, axis=1)
        w_pos = i * qb - WIN + jnp.arange(qb + WIN)
        diff = t[:, None] - w_pos[None, :]
        m_w = (diff >= 0) & (diff < WIN) & (w_pos[None, :] >= 0)
        p_w = masked_softmax(jnp.einsum('bqghd,bkgd->bghqk', q_i, kw) * scale, m_w)
        o_w = jnp.einsum('bghqk,bkgd->bqghd', p_w.astype(vw.dtype), vw)
        return gate_i[..., 0:1] * o_c + gate_i[..., 1:2] * o_s + gate_i[..., 2:3] * o_w

    q_b = q.reshape(b_, nb, qb, n_g, n_hg, d).swapaxes(0, 1)
    g_b = gates.reshape(b_, nb, qb, n_g, n_hg, 3).swapaxes(0, 1)
    o = lax.map(block, (jnp.arange(nb), q_b, g_b))
    return o.swapaxes(0, 1).reshape(b_, s_len, n_g * n_hg * d)


def odd_token_mixer(h, w_in, pos_k, pos_v, ck1, ck2, cv1, cv2, w_out):
    b_, s_len, _ = h.shape
    sizes = [NSA_Q_W] + [NSA_KV_W] * 6 + [3 * NSA_HEADS]
    q, kc, vc, ks, vs, kw, vw, gl = split_cols(h @ w_in, sizes)

    def kvh(t):
        return t.reshape(b_, s_len, NSA_GROUPS, NSA_HEAD_DIM)

    o = native_sparse_attention(
        q.reshape(b_, s_len, NSA_GROUPS, NSA_HPG, NSA_HEAD_DIM),
        compress_blocks(kvh(kc), pos_k, ck1, ck2),
        compress_blocks(kvh(vc), pos_v, cv1, cv2),
        kvh(ks), kvh(vs), kvh(kw), kvh(vw),
        jax.nn.sigmoid(gl).reshape(b_, s_len, NSA_GROUPS, NSA_HPG, 3))
    return o @ w_out


def swiglu(h, w_gate, w_up, w_down):
    return (jax.nn.silu(h @ w_gate) * (h @ w_up)) @ w_down


def moe_swiglu(h, w_router, b_router, w1, w3, w2):
    b_, s_len, dm = h.shape
    x = h.reshape(-1, dm)
    n_tok = x.shape[0]
    logits = (x @ w_router).astype(jnp.float32) + b_router.astype(jnp.float32)
    top_val, top_idx = lax.top_k(logits, TOP_K)
    gate_w = jax.nn.softmax(top_val, axis=-1)
    n_assign = n_tok * TOP_K
    flat_e = top_idx.reshape(-1)
    flat_tok = jnp.repeat(jnp.arange(n_tok, dtype=jnp.int32), TOP_K)
    order = jnp.argsort(flat_e)
    s_e = flat_e[order]
    s_tok = flat_tok[order]
    s_w = gate_w.reshape(-1)[order]
    counts = jnp.zeros((N_EXPERTS,), jnp.int32).at[flat_e].add(1)
    padded = ((counts + MOE_BLOCK - 1) // MOE_BLOCK) * MOE_BLOCK
    pad_end = jnp.cumsum(padded)
    pad_start = pad_end - padded
    start = jnp.cumsum(counts) - counts
    dest = pad_start[s_e] + jnp.arange(n_assign, dtype=jnp.int32) - start[s_e]
    n_blk = -(-n_assign // MOE_BLOCK) + N_EXPERTS
    n_rows = n_blk * MOE_BLOCK
    row_tok = jnp.zeros((n_rows,), jnp.int32).at[dest].set(s_tok)
    blk_exp = jnp.minimum(jnp.searchsorted(pad_end, jnp.arange(n_blk, dtype=jnp.int32) * MOE_BLOCK,
                                           side='right'), N_EXPERTS - 1)
    x_rows = x[row_tok].reshape(n_blk, MOE_BLOCK, dm)

    def expert_block(args):
        xb, e = args
        return swiglu(xb, w1[e], w3[e], w2[e])

    y_rows = lax.map(expert_block, (x_rows, blk_exp)).reshape(n_rows, dm)
    y = jnp.zeros_like(x).at[s_tok].add(y_rows[dest] * s_w[:, None].astype(y_rows.dtype))
    return y.reshape(b_, s_len, dm)


def setup_inputs(seed: int = 0) -> dict:
    key = jax.random.key(seed)
    keys = list(jax.random.split(key, 48))
    f32 = jnp.float32
    n_ev = (DEPTH + 1) // 2
    n_od = DEPTH // 2

    def nk():
        return keys.pop()

    def dense(shape, fan_in, gain=1.0):
        return jax.random.normal(nk(), shape, f32) * (gain * fan_in ** -0.5)

    def gain_init(shape):
        return 1.0 + 0.02 * jax.random.normal(nk(), shape, f32)

    x = jax.random.normal(nk(), (BATCH, SEQ, D_MODEL), f32)
    c = jax.random.normal(nk(), (BATCH, D_MODEL), f32)
    offset = jax.random.randint(nk(), (BATCH, 1), 0, 4096, dtype=jnp.int32)
    positions = offset + jnp.arange(SEQ, dtype=jnp.int32)[None, :]
    dt = jnp.exp(jax.random.uniform(nk(), (n_ev, DN_HEADS), f32, math.log(1e-3), math.log(1e-1)))
    return {
        'x': x,
        'c': c,
        'positions': positions,
        'ada_w': dense((DEPTH, 2, D_MODEL, 3 * D_MODEL), D_MODEL, 0.5),
        'ada_b': 0.02 * jax.random.normal(nk(), (DEPTH, 2, 3 * D_MODEL), f32),
        'norm_g': gain_init((DEPTH, 2, D_MODEL)),
        'final_g': gain_init((D_MODEL,)),
        'ev_w_in': dense((n_ev, D_MODEL, EV_IN), D_MODEL),
        'ev_conv_w': dense((n_ev, DN_CONV, 3 * DN_W), DN_CONV),
        'ev_a_log': jnp.log(jax.random.uniform(nk(), (n_ev, DN_HEADS), f32, 1.0, 16.0)),
        'ev_dt_bias': dt + jnp.log(-jnp.expm1(-dt)),
        'ev_dn_norm': gain_init((n_ev, DN_HEAD_DIM)),
        'ev_q_norm': gain_init((n_ev, MLA_Q_RANK)),
        'ev_kv_norm': gain_init((n_ev, MLA_KV_RANK)),
        'ev_w_uq': dense((n_ev, MLA_Q_RANK, MLA_HEADS * MLA_QK), MLA_Q_RANK),
        'ev_w_ukv': dense((n_ev, MLA_KV_RANK, MLA_HEADS * (MLA_NOPE + MLA_V)), MLA_KV_RANK),
        'ev_w_out': dense((n_ev, EV_MIX, D_MODEL), EV_MIX),
        'ev_ff_gate': dense((n_ev, D_MODEL, D_FF), D_MODEL),
        'ev_ff_up': dense((n_ev, D_MODEL, D_FF), D_MODEL),
        'ev_ff_down': dense((n_ev, D_FF, D_MODEL), D_FF),
        'od_w_in': dense((n_od, D_MODEL, OD_IN), D_MODEL),
        'od_cmp_pos_k': 0.1 * jax.random.normal(nk(), (n_od, CMP_LEN, NSA_HEAD_DIM), f32),
        'od_cmp_pos_v': 0.1 * jax.random.normal(nk(), (n_od, CMP_LEN, NSA_HEAD_DIM), f32),
        'od_cmp_k1': dense((n_od, CMP_LEN * NSA_HEAD_DIM, CMP_HIDDEN), CMP_LEN * NSA_HEAD_DIM),
        'od_cmp_k2': dense((n_od, CMP_HIDDEN, NSA_HEAD_DIM), CMP_HIDDEN),
        'od_cmp_v1': dense((n_od, CMP_LEN * NSA_HEAD_DIM, CMP_HIDDEN), CMP_LEN * NSA_HEAD_DIM),
        'od_cmp_v2': dense((n_od, CMP_HIDDEN, NSA_HEAD_DIM), CMP_HIDDEN),
        'od_w_out': dense((n_od, NSA_Q_W, D_MODEL), NSA_Q_W),
        'od_router': dense((n_od, D_MODEL, N_EXPERTS), D_MODEL),
        'od_router_b': 0.01 * jax.random.normal(nk(), (n_od, N_EXPERTS), f32),
        'od_moe_w1': dense((n_od, N_EXPERTS, D_MODEL, D_FF_EXPERT), D_MODEL),
        'od_moe_w3': dense((n_od, N_EXPERTS, D_MODEL, D_FF_EXPERT), D_MODEL),
        'od_moe_w2': dense((n_od, N_EXPERTS, D_FF_EXPERT, D_MODEL), D_FF_EXPERT),
    }


def reference(x, c, positions, ada_w, ada_b, norm_g, final_g,
              ev_w_in, ev_conv_w, ev_a_log, ev_dt_bias, ev_dn_norm, ev_q_norm, ev_kv_norm,
              ev_w_uq, ev_w_ukv, ev_w_out, ev_ff_gate, ev_ff_up, ev_ff_down,
              od_w_in, od_cmp_pos_k, od_cmp_pos_v, od_cmp_k1, od_cmp_k2, od_cmp_v1, od_cmp_v2,
              od_w_out, od_router, od_router_b, od_moe_w1, od_moe_w3, od_moe_w2):
    cos, sin = rope_tables(positions)
    for layer in range(DEPTH):
        j = layer // 2
        shift, scale, gate = ada_modulation(c, ada_w[layer, 0], ada_b[layer, 0])
        h = rms_norm(x, norm_g[layer, 0]) * (1.0 + scale) + shift
        if layer % 2 == 0:
            y = even_token_mixer(h, cos, sin, ev_w_in[j], ev_conv_w[j], ev_a_log[j], ev_dt_bias[j],
                                 ev_dn_norm[j], ev_q_norm[j], ev_kv_norm[j], ev_w_uq[j],
                                 ev_w_ukv[j], ev_w_out[j])
        else:
            y = odd_token_mixer(h, od_w_in[j], od_cmp_pos_k[j], od_cmp_pos_v[j], od_cmp_k1[j],
                                od_cmp_k2[j], od_cmp_v1[j], od_cmp_v2[j], od_w_out[j])
        x = x + gate * y
        shift, scale, gate = ada_modulation(c, ada_w[layer, 1], ada_b[layer, 1])
        h = rms_norm(x, norm_g[layer, 1]) * (1.0 + scale) + shift
        if layer % 2 == 0:
            y = swiglu(h, ev_ff_gate[j], ev_ff_up[j], ev_ff_down[j])
        else:
            y = moe_swiglu(h, od_router[j], od_router_b[j], od_moe_w1[j], od_moe_w3[j], od_moe_w2[j])
        x = x + gate * y
    return rms_norm(x, final_g)
```

```python
import numpy as np
from contextlib import ExitStack
import concourse.bass as bass
import concourse.mybir as mybir
from concourse.bass_utils import run_bass_kernel_spmd

F32 = mybir.dt.float32
BF16 = mybir.dt.bfloat16
I32 = mybir.dt.int32
ALU = mybir.AluOpType
AF = mybir.ActivationFunctionType
AX = mybir.AxisListType

S_LEN = 8192
DM = 1024
NB = S_LEN // 512
NT = S_LEN // 128
EPS = 1e-6
NEG = -30000.0
SES = True


class Buf:
    __slots__ = ("t", "name", "lw", "rd")

    def __init__(self, t, name=""):
        self.t = t
        self.name = name
        self.lw = None
        self.rd = {}

    def __getitem__(self, i):
        return self.t[i]


class Sched:
    def __init__(self, nc, es, ext_in=(), ext_out=()):
        self.nc = nc
        self.es = es
        self.ext_in = set(ext_in)
        self.ext_out = set(ext_out)
        self.eng = {"pe": nc.tensor, "act": nc.scalar, "dve": nc.vector, "pool": nc.gpsimd, "sp": nc.sync}
        self.sems = {}
        self.cnt = {}
        for e in self.eng:
            self.sems[e] = es.enter_context(nc.semaphore("s_" + e))
            self.cnt[e] = 0
        self.waited = {e: {} for e in self.eng}
        self.lanes = {"sp": 12, "pool": 8, "act": 4}
        self.lane_rr = {q: 0 for q in self.lanes}
        for q, n in self.lanes.items():
            for i in range(n):
                k = f"{q}{i}"
                self.sems[k] = es.enter_context(nc.semaphore("l_" + k))
                self.cnt[k] = 0
        self.psb = []
        self.ps_rr = 0
        self.ps4_rr = 0
        self.uid = 0
        self.dram_bufs = {}

    def sb(self, es, shape, dt, name=None):
        self.uid += 1
        name = (name or "t") + f"_{self.uid}"
        return Buf(es.enter_context(self.nc.sbuf_tensor(name, list(shape), dt)), name)

    def dram(self, name, shape, dt):
        if name in self.dram_bufs:
            return self.dram_bufs[name]
        kind = "ExternalInput" if name in self.ext_in else ("ExternalOutput" if name in self.ext_out else "Internal")
        b = Buf(self.nc.dram_tensor(name, list(shape), dt, kind=kind).ap(), name)
        self.dram_bufs[name] = b
        return b

    def init_psum(self):
        for i in range(8):
            self.psb.append(Buf(self.es.enter_context(self.nc.psum_tensor(f"ps{i}", [128, 512], F32)), f"ps{i}"))

    def ps(self):
        b = self.psb[self.ps_rr]
        self.ps_rr = (self.ps_rr + 1) % 8
        return b

    def ps4(self):
        b = self.psb[self.ps4_rr]
        self.ps4_rr = (self.ps4_rr + 1) % 4
        return b

    def _wait(self, e, key, val):
        if val <= 0 or self.waited[e].get(key, 0) >= val:
            return
        self.eng[e].wait_ge(self.sems[key], val)
        self.waited[e][key] = val

    def _deps(self, e, r, w, is_dma=False):
        deps = {}
        for b in r:
            if b.lw is not None:
                k, v = b.lw
                if deps.get(k, 0) < v:
                    deps[k] = v
        for b in w:
            if b.lw is not None:
                k, v = b.lw
                if deps.get(k, 0) < v:
                    deps[k] = v
            for k, v in b.rd.items():
                if deps.get(k, 0) < v:
                    deps[k] = v
        for k, v in deps.items():
            if k == e and not is_dma and not (SES and e != "pe"):
                continue
            self._wait(e, k, v)

    def _commit(self, ev, r, w):
        k, v = ev
        for b in w:
            b.lw = ev
            b.rd = {}
        for b in r:
            if b.rd.get(k, 0) < v:
                b.rd[k] = v

    def op(self, e, fn, r=(), w=()):
        self._deps(e, r, w)
        ins = fn()
        self.cnt[e] += 1
        ins.then_inc(self.sems[e], 1)
        self._commit((e, self.cnt[e]), r, w)

    def pe(self, fn, r=(), w=()):
        self.op("pe", fn, r, w)

    def act(self, fn, r=(), w=()):
        self.op("act", fn, r, w)

    def dve(self, fn, r=(), w=()):
        self.op("dve", fn, r, w)

    def pool(self, fn, r=(), w=()):
        self.op("pool", fn, r, w)

    def dma(self, q, out, in_, r=(), w=(), **kw):
        self._deps(q, r, w, is_dma=True)
        i = self.lane_rr[q]
        self.lane_rr[q] = (i + 1) % self.lanes[q]
        key = f"{q}{i}"
        c = self.cnt[key]
        self._wait(q, key, 16 * c)
        ins = self.eng[q].dma_start(out=out, in_=in_, **kw)
        ins.then_inc(self.sems[key], 16)
        self.cnt[key] = c + 1
        self._commit((key, 16 * (c + 1)), r, w)

    def barrier(self):
        for e in self.eng:
            for k, c in self.cnt.items():
                if k == e or c == 0:
                    continue
                self._wait(e, k, c if k in self.eng else 16 * c)

    def finish(self):
        for k, c in self.cnt.items():
            if k == "sp" or c == 0:
                continue
            self._wait("sp", k, c if k in self.eng else 16 * c)


def mm(S, out_b, out_ap, l_b, l_ap, r_b, r_ap, start, stop):
    S.pe(lambda: S.nc.tensor.matmul(out_ap, l_ap, r_ap, start=start, stop=stop), r=[l_b, r_b], w=[out_b])


class Ctx:
    pass


def setup_consts(S, C):
    nc = S.nc
    es = S.es
    C.ident = S.sb(es, [128, 128], F32, "ident")
    C.ones_bf = S.sb(es, [128, 128], BF16, "ones_bf")
    C.ones_f = S.sb(es, [128, 128], F32, "ones_f")
    S.pool(lambda: nc.gpsimd.memset(C.ident[:], 0.0), w=[C.ident])
    S.pool(lambda: nc.gpsimd.affine_select(out=C.ident[:], in_=C.ident[:], pattern=[[-1, 128]],
                                           compare_op=ALU.not_equal, fill=1.0, base=0, channel_multiplier=1),
           r=[C.ident], w=[C.ident])
    S.pool(lambda: nc.gpsimd.memset(C.ones_bf[:], 1.0), w=[C.ones_bf])
    S.pool(lambda: nc.gpsimd.memset(C.ones_f[:], 1.0), w=[C.ones_f])
    C.eps = S.sb(es, [128, 1], F32, "eps")
    S.pool(lambda: nc.gpsimd.memset(C.eps[:], EPS), w=[C.eps])


def phase_ada(S, C, n_layers):
    nc = S.nc
    c_in = S.dram("cT", [128, 8], F32)
    ada_w = S.dram("ada_w", [4, 2, 1024, 3072], F32)
    ada_b = S.dram("ada_bT", [128, 8 * 24], F32)
    norm_g = S.dram("norm_gT", [128, 8 * 8], F32)
    C.mods = S.sb(S.es, [128, 8 * 24], F32, "mods")
    C.modA = S.sb(S.es, [128, 8 * 8], F32, "modA")
    with ExitStack() as es:
        sc = S.sb(es, [128, 8], F32, "sc")
        bb = S.sb(es, [128, 8 * 24], F32, "bb")
        gg = S.sb(es, [128, 8 * 8], F32, "gg")
        wbuf = [S.sb(es, [128, 8, 3072 // 2], F32, "adaw") for _ in range(2)]
        S.dma("sp", sc[:], c_in[:, :], r=[c_in], w=[sc])
        S.dma("sp", bb[:], ada_b[:, :], r=[ada_b], w=[bb])
        S.dma("sp", gg[:], norm_g[:, :], r=[norm_g], w=[gg])
        S.act(lambda: nc.scalar.activation(out=sc[:], in_=sc[:], func=AF.Silu), r=[sc], w=[sc])
        it = 0
        for l in range(n_layers):
            for s in range(2):
                ls = l * 2 + s
                for half in range(2):
                    wb = wbuf[it % 2]
                    it += 1
                    src = ada_w[l, s, :, half * 1536:(half + 1) * 1536].rearrange("(kc p) n -> p kc n", p=128)
                    for kc in range(8):
                        S.dma("sp", wb[:, kc, :], src[:, kc, :], r=[ada_w], w=[wb])
                    ps = S.ps()
                    for fc in range(12):
                        for kc in range(8):
                            mm(S, ps, ps[:, fc:fc + 1], wb, wb[:, kc, fc * 128:(fc + 1) * 128], sc, sc[:, kc:kc + 1],
                               kc == 0, kc == 7)
                    c0 = ls * 24 + half * 12
                    S.dve(lambda ps=ps, c0=c0: nc.vector.tensor_tensor(out=C.mods[:, c0:c0 + 12], in0=ps[:, 0:12],
                                                                       in1=bb[:, c0:c0 + 12], op=ALU.add),
                          r=[ps, bb], w=[C.mods])
                S.dve(lambda ls=ls: nc.vector.scalar_tensor_tensor(
                    out=C.modA[:, ls * 8:(ls + 1) * 8], in0=C.mods[:, ls * 24 + 8:ls * 24 + 16], scalar=1.0,
                    in1=gg[:, ls * 8:(ls + 1) * 8], op0=ALU.add, op1=ALU.mult), r=[C.mods, gg], w=[C.modA])
        S.barrier()


def modnorm_block(S, C, es_tiles, x_dram, ls, t0, xt, hT, hcol0, want_f32=None):
    nc = S.nc
    sq, rstd, tmp = es_tiles
    src = x_dram[:, t0:t0 + 512].rearrange("(kc p) t -> p kc t", p=128)
    S.dma("sp", xt[:, :, hcol0:hcol0 + 512], src, r=[x_dram], w=[xt])
    for kc in range(8):
        S.pool(lambda kc=kc: nc.gpsimd.tensor_tensor(out=sq[:, kc, :], in0=xt[:, kc, hcol0:hcol0 + 512],
                                                     in1=xt[:, kc, hcol0:hcol0 + 512], op=ALU.mult), r=[xt], w=[sq])
    ps = S.ps()
    for kc in range(8):
        mm(S, ps, ps[:, :], C.ones_bf, C.ones_bf[:, :], sq, sq[:, kc, :], kc == 0, kc == 7)
    S.act(lambda: nc.scalar.activation(out=rstd[:], in_=ps[:, :], func=AF.Sqrt, scale=1.0 / DM, bias=C.eps[:, 0:1]),
          r=[ps, C.eps], w=[rstd])
    S.dve(lambda: nc.vector.reciprocal(out=rstd[:], in_=rstd[:]), r=[rstd], w=[rstd])
    for kc in range(8):
        S.dve(lambda kc=kc: nc.vector.tensor_tensor(out=tmp[:], in0=xt[:, kc, hcol0:hcol0 + 512], in1=rstd[:],
                                                    op=ALU.mult), r=[xt, rstd], w=[tmp])
        a_ap = C.modA[:, ls * 8 + kc:ls * 8 + kc + 1]
        s_ap = C.mods[:, ls * 24 + kc:ls * 24 + kc + 1]
        S.dve(lambda kc=kc, a_ap=a_ap, s_ap=s_ap: nc.vector.tensor_scalar(
            out=hT[:, kc, hcol0:hcol0 + 512], in0=tmp[:], scalar1=a_ap, scalar2=s_ap, op0=ALU.mult, op1=ALU.add),
            r=[tmp, C.modA, C.mods], w=[hT])
        if want_f32 is not None:
            S.dve(lambda kc=kc, a_ap=a_ap, s_ap=s_ap: nc.vector.tensor_scalar(
                out=want_f32[:, kc, 0:512], in0=tmp[:], scalar1=a_ap, scalar2=s_ap, op0=ALU.mult,
                op1=ALU.add), r=[tmp, C.modA, C.mods], w=[want_f32])


def load_w_bf16(S, wt, w_ap, w_dram, K, N, n0=0, ncols=None):
    ncols = ncols or N
    src = w_ap[:, n0:n0 + ncols].rearrange("(kc p) n -> p kc n", p=128)
    for kc in range(K // 128):
        S.dma("pool", wt[:, kc, 0:ncols], src[:, kc, :], r=[w_dram], w=[wt])


def phase_inproj(S, C, x_dram, ls, w_dram, w_ap, ncols, pT_dram, tok_major=()):
    nc = S.nc
    n_ot = (ncols + 127) // 128
    with ExitStack() as es:
        wt = S.sb(es, [128, 8, ncols], BF16, "w_in")
        load_w_bf16(S, wt, w_ap, w_dram, DM, ncols)
        xts = [S.sb(es, [128, 8, 512], F32, "xt") for _ in range(2)]
        hTs = [S.sb(es, [128, 8, 512], BF16, "hT") for _ in range(2)]
        sq = S.sb(es, [128, 8, 512], BF16, "sq")
        rstd = S.sb(es, [128, 512], F32, "rstd")
        tmp = S.sb(es, [128, 512], F32, "tmp")
        stg = [S.sb(es, [128, 512], F32, "stg") for _ in range(4)]
        si = 0
        for tb in range(NB):
            xt = xts[tb % 2]
            hT = hTs[tb % 2]
            modnorm_block(S, C, (sq, rstd, tmp), x_dram, ls, tb * 512, xt, hT, 0)
            for ot in range(n_ot):
                m = min(128, ncols - ot * 128)
                ps = S.ps()
                for kc in range(8):
                    mm(S, ps, ps[0:m, :], wt, wt[:, kc, ot * 128:ot * 128 + m], hT, hT[:, kc, :], kc == 0, kc == 7)
                st = stg[si % 4]
                si += 1
                if ot % 2 == 0:
                    S.act(lambda st=st, ps=ps, m=m: nc.scalar.copy(out=st[0:m, :], in_=ps[0:m, :]), r=[ps], w=[st])
                else:
                    S.dve(lambda st=st, ps=ps, m=m: nc.vector.tensor_copy(out=st[0:m, :], in_=ps[0:m, :]), r=[ps], w=[st])
                S.dma("sp", pT_dram[ot * 128:ot * 128 + m, tb * 512:(tb + 1) * 512], st[0:m, :], r=[st], w=[pT_dram])
            for (c0, n, dst) in tok_major:
                for tt in range(4):
                    ps = S.ps()
                    for kc in range(8):
                        mm(S, ps, ps[:, 0:n], hT, hT[:, kc, tt * 128:(tt + 1) * 128], wt, wt[:, kc, c0:c0 + n],
                           kc == 0, kc == 7)
                    st = stg[si % 4]
                    si += 1
                    S.act(lambda st=st, ps=ps, n=n: nc.scalar.copy(out=st[:, 0:n], in_=ps[:, 0:n]), r=[ps], w=[st])
                    r0 = tb * 512 + tt * 128
                    S.dma("sp", dst[r0:r0 + 128, :], st[:, 0:n], r=[st], w=[dst])
        S.barrier()


def phase_outproj(S, C, x_dram, ls, w_dram, w_ap, oT_dram, xo_dram):
    nc = S.nc
    with ExitStack() as es:
        wt = S.sb(es, [128, 8, DM], BF16, "w_out")
        load_w_bf16(S, wt, w_ap, w_dram, DM, DM)
        xts = [S.sb(es, [128, 8, 512], F32, "xt") for _ in range(2)]
        ots = [S.sb(es, [128, 8, 512], BF16, "oT") for _ in range(2)]
        for tb in range(NB):
            xt = xts[tb % 2]
            ot_ = ots[tb % 2]
            sl = slice(tb * 512, (tb + 1) * 512)
            S.dma("sp", xt[:], x_dram[:, sl].rearrange("(kc p) t -> p kc t", p=128), r=[x_dram], w=[xt])
            S.dma("sp", ot_[:], oT_dram[:, sl].rearrange("(kc p) t -> p kc t", p=128), r=[oT_dram], w=[ot_])
            for oc in range(8):
                ps = S.ps()
                for kc in range(8):
                    mm(S, ps, ps[:, :], wt, wt[:, kc, oc * 128:(oc + 1) * 128], ot_, ot_[:, kc, :], kc == 0, kc == 7)
                g_ap = C.mods[:, ls * 24 + 16 + oc:ls * 24 + 17 + oc]
                S.dve(lambda ps=ps, oc=oc, g_ap=g_ap, xt=xt: nc.vector.scalar_tensor_tensor(
                    out=xt[:, oc, :], in0=ps[:, :], scalar=g_ap, in1=xt[:, oc, :], op0=ALU.mult, op1=ALU.add),
                    r=[ps, C.mods, xt], w=[xt])
            S.dma("sp", xo_dram[:, sl].rearrange("(kc p) t -> p kc t", p=128), xt[:], r=[xt], w=[xo_dram])
        S.barrier()


def phase_ffn(S, C, x_dram, ls, xo_dram, F, n_exp, wg_dram, wg_ap, wu_dram, wu_ap, wd_dram, wd_ap,
              router=None, TB=2048, CT=4):
    nc = S.nc
    n_ft = F // 128
    chunks = [(c0, min(CT, n_ft - c0)) for c0 in range(0, n_ft, CT)]
    nsub = TB // 512
    with ExitStack() as es:
        xt = S.sb(es, [128, 8, TB], F32, "xt")
        hT = S.sb(es, [128, 8, TB], BF16, "hT")
        sq = S.sb(es, [128, 8, 512], BF16, "sq")
        rstd = S.sb(es, [128, 512], F32, "rstd")
        tmp = S.sb(es, [128, 512], F32, "tmp")
        wgs = [S.sb(es, [128, 8, CT * 128], BF16, "wg") for _ in range(2)]
        wus = [S.sb(es, [128, 8, CT * 128], BF16, "wu") for _ in range(2)]
        wds = [S.sb(es, [128, CT, DM], BF16, "wd") for _ in range(2)]
        acts = [S.sb(es, [128, CT, 512], BF16, "act") for _ in range(2)]
        sgs = [S.sb(es, [128, 512], F32, "sg") for _ in range(2)]
        ytm = [S.sb(es, [128, 512], F32, "ytm") for _ in range(2)]
        if router is not None:
            hF = S.sb(es, [128, 8, 512], F32, "hF")
            wr = S.sb(es, [128, 8, 8], F32, "wr")
            rb = S.sb(es, [128, 8], F32, "rb")
            lg = S.sb(es, [128, 8], F32, "lg")
            mx = S.sb(es, [128, 8], F32, "mx")
            ex = S.sb(es, [128, 8], F32, "ex")
            den = S.sb(es, [128, 2], F32, "den")
            gw = S.sb(es, [128, 8], F32, "gw")
            gwT = S.sb(es, [8, TB], F32, "gwT")
            sel = S.sb(es, [8, 8 * 128], F32, "sel")
            Gs = [S.sb(es, [128, 512], F32, "G") for _ in range(nsub)]
            r_dram, r_ap, rb_dram, rb_ap = router
            S.dma("sp", wr[:], r_ap.rearrange("(kc p) n -> p kc n", p=128), r=[r_dram], w=[wr])
            S.dma("sp", rb[:], rb_ap, r=[rb_dram], w=[rb])
            S.pool(lambda: nc.gpsimd.memset(sel[:], 0.0), w=[sel])
            S.pool(lambda: nc.gpsimd.affine_select(out=sel[:].rearrange("k (e m) -> k e m", m=128),
                                                   in_=sel[:].rearrange("k (e m) -> k e m", m=128),
                                                   pattern=[[-1, 8], [0, 128]], compare_op=ALU.not_equal, fill=1.0,
                                                   base=0, channel_multiplier=1), r=[sel], w=[sel])
        wi = 0
        gi = 0
        ai = 0
        for tb in range(S_LEN // TB):
            for sub in range(nsub):
                modnorm_block(S, C, (sq, rstd, tmp), x_dram, ls, tb * TB + sub * 512, xt, hT, sub * 512,
                              want_f32=(hF if router is not None else None))
                if router is not None:
                    for tt in range(4):
                        ps = S.ps()
                        for kc in range(8):
                            mm(S, ps, ps[:, 0:8], hF, hF[:, kc, tt * 128:(tt + 1) * 128], wr, wr[:, kc, :], kc == 0, kc == 7)
                        S.dve(lambda ps=ps: nc.vector.tensor_tensor(out=lg[:], in0=ps[:, 0:8], in1=rb[:], op=ALU.add),
                              r=[ps, rb], w=[lg])
                        S.dve(lambda: nc.vector.max(out=mx[:], in_=lg[:]), r=[lg], w=[mx])
                        S.dve(lambda: nc.vector.tensor_scalar(out=ex[:], in0=lg[:], scalar1=mx[:, 0:1], scalar2=None,
                                                              op0=ALU.subtract), r=[lg, mx], w=[ex])
                        S.act(lambda: nc.scalar.activation(out=ex[:], in_=ex[:], func=AF.Exp), r=[ex], w=[ex])
                        S.dve(lambda: nc.vector.tensor_scalar(out=gw[:], in0=lg[:], scalar1=mx[:, 1:2], scalar2=None,
                                                              op0=ALU.is_ge), r=[lg, mx], w=[gw])
                        S.dve(lambda: nc.vector.tensor_tensor(out=gw[:], in0=gw[:], in1=ex[:], op=ALU.mult),
                              r=[gw, ex], w=[gw])
                        S.dve(lambda: nc.vector.reduce_sum(out=den[:, 0:1], in_=gw[:], axis=AX.X), r=[gw], w=[den])
                        S.dve(lambda: nc.vector.reciprocal(out=den[:, 1:2], in_=den[:, 0:1]), r=[den], w=[den])
                        S.dve(lambda: nc.vector.tensor_scalar(out=gw[:], in0=gw[:], scalar1=den[:, 1:2], scalar2=None,
                                                              op0=ALU.mult), r=[gw, den], w=[gw])
                        pt = S.ps()
                        S.pe(lambda pt=pt: nc.tensor.transpose(out=pt[0:8, 0:128], in_=gw[:, :], identity=C.ident[:, :]),
                             r=[gw, C.ident], w=[pt])
                        c0 = sub * 512 + tt * 128
                        S.act(lambda pt=pt, c0=c0: nc.scalar.copy(out=gwT[:, c0:c0 + 128], in_=pt[0:8, 0:128]),
                              r=[pt], w=[gwT])
            for e in range(n_exp):
                for (c0, ct) in chunks:
                    wg = wgs[wi % 2]
                    wu = wus[wi % 2]
                    wd = wds[wi % 2]
                    wi += 1
                    load_w_bf16(S, wg, wg_ap(e), wg_dram, DM, F, n0=c0 * 128, ncols=ct * 128)
                    load_w_bf16(S, wu, wu_ap(e), wu_dram, DM, F, n0=c0 * 128, ncols=ct * 128)
                    S.dma("pool", wd[:, 0:ct, :], wd_ap(e)[c0 * 128:(c0 + ct) * 128, :].rearrange("(c p) n -> p c n", p=128),
                          r=[wd_dram], w=[wd])
                    for sub in range(nsub):
                        ts = slice(sub * 512, (sub + 1) * 512)
                        G = None
                        if router is not None:
                            G = Gs[sub]
                        if router is not None and c0 == 0:
                            pg = S.ps()
                            mm(S, pg, pg[:, :], sel, sel[:, e * 128:(e + 1) * 128], gwT, gwT[:, ts], True, True)
                            S.act(lambda G=G, pg=pg: nc.scalar.copy(out=G[:], in_=pg[:, :]), r=[pg], w=[G])
                        act_t = acts[ai % 2]
                        ai += 1
                        for f in range(ct):
                            pg = S.ps()
                            pu = S.ps()
                            for kc in range(8):
                                mm(S, pg, pg[:, :], wg, wg[:, kc, f * 128:(f + 1) * 128], hT, hT[:, kc, ts], kc == 0, kc == 7)
                            for kc in range(8):
                                mm(S, pu, pu[:, :], wu, wu[:, kc, f * 128:(f + 1) * 128], hT, hT[:, kc, ts], kc == 0, kc == 7)
                            sg = sgs[f % 2]
                            S.act(lambda sg=sg, pg=pg: nc.scalar.activation(out=sg[:], in_=pg[:, :], func=AF.Silu),
                                  r=[pg], w=[sg])
                            S.dve(lambda sg=sg, pu=pu, f=f, act_t=act_t: nc.vector.tensor_tensor(
                                out=act_t[:, f, :], in0=sg[:], in1=pu[:, :], op=ALU.mult), r=[sg, pu], w=[act_t])
                        for oc in range(8):
                            py = S.ps()
                            for f in range(ct):
                                mm(S, py, py[:, :], wd, wd[:, f, oc * 128:(oc + 1) * 128], act_t, act_t[:, f, :],
                                   f == 0, f == ct - 1)
                            g_ap = C.mods[:, ls * 24 + 16 + oc:ls * 24 + 17 + oc]
                            if G is None:
                                S.dve(lambda py=py, oc=oc, g_ap=g_ap, ts=ts: nc.vector.scalar_tensor_tensor(
                                    out=xt[:, oc, ts], in0=py[:, :], scalar=g_ap, in1=xt[:, oc, ts], op0=ALU.mult,
                                    op1=ALU.add), r=[py, C.mods, xt], w=[xt])
                            else:
                                yt = ytm[oc % 2]
                                S.dve(lambda py=py, yt=yt, G=G: nc.vector.tensor_tensor(out=yt[:], in0=py[:, :], in1=G[:],
                                                                                        op=ALU.mult), r=[py, G], w=[yt])
                                S.dve(lambda yt=yt, oc=oc, g_ap=g_ap, ts=ts: nc.vector.scalar_tensor_tensor(
                                    out=xt[:, oc, ts], in0=yt[:], scalar=g_ap, in1=xt[:, oc, ts], op0=ALU.mult,
                                    op1=ALU.add), r=[yt, C.mods, xt], w=[xt])
            for kc in range(8):
                S.dma("sp", xo_dram[kc * 128:(kc + 1) * 128, tb * TB:(tb + 1) * TB], xt[:, kc, :], r=[xt], w=[xo_dram])
        S.barrier()


def phase_final(S, C, x_dram, out_dram):
    nc = S.nc
    fg = S.dram("final_gT", [128, 8], F32)
    with ExitStack() as es:
        g = S.sb(es, [128, 8], F32, "fg")
        S.dma("sp", g[:], fg[:, :], r=[fg], w=[g])
        xts = [S.sb(es, [128, 8, 512], F32, "xt") for _ in range(2)]
        sq = S.sb(es, [128, 8, 512], BF16, "sq")
        rstd = S.sb(es, [128, 512], F32, "rstd")
        for tb in range(NB):
            xt = xts[tb % 2]
            sl = slice(tb * 512, (tb + 1) * 512)
            S.dma("sp", xt[:], x_dram[:, sl].rearrange("(kc p) t -> p kc t", p=128), r=[x_dram], w=[xt])
            for kc in range(8):
                S.pool(lambda kc=kc, xt=xt: nc.gpsimd.tensor_tensor(out=sq[:, kc, :], in0=xt[:, kc, :], in1=xt[:, kc, :],
                                                                    op=ALU.mult), r=[xt], w=[sq])
            ps = S.ps()
            for kc in range(8):
                mm(S, ps, ps[:, :], C.ones_bf, C.ones_bf[:, :], sq, sq[:, kc, :], kc == 0, kc == 7)
            S.act(lambda ps=ps: nc.scalar.activation(out=rstd[:], in_=ps[:, :], func=AF.Sqrt, scale=1.0 / DM,
                                                     bias=C.eps[:, 0:1]), r=[ps, C.eps], w=[rstd])
            S.dve(lambda: nc.vector.reciprocal(out=rstd[:], in_=rstd[:]), r=[rstd], w=[rstd])
            for kc in range(8):
                S.dve(lambda kc=kc, xt=xt: nc.vector.scalar_tensor_tensor(
                    out=xt[:, kc, :], in0=xt[:, kc, :], scalar=g[:, kc:kc + 1], in1=rstd[:], op0=ALU.mult, op1=ALU.mult),
                    r=[xt, g, rstd], w=[xt])
            S.dma("sp", out_dram[:, sl].rearrange("(kc p) t -> p kc t", p=128), xt[:], r=[xt], w=[out_dram])
        S.barrier()


def common_inputs(inputs, b):
    f = np.float32
    m = {}
    m["xT"] = np.ascontiguousarray(np.asarray(inputs["x"][b], f).T)
    m["cT"] = np.ascontiguousarray(np.asarray(inputs["c"][b], f).reshape(8, 128).T)
    m["ada_w"] = np.asarray(inputs["ada_w"], f)
    m["ada_bT"] = np.ascontiguousarray(np.asarray(inputs["ada_b"], f).reshape(8, 24, 128).transpose(2, 0, 1).reshape(128, 192))
    m["norm_gT"] = np.ascontiguousarray(np.asarray(inputs["norm_g"], f).reshape(8, 8, 128).transpose(2, 0, 1).reshape(128, 64))
    m["final_gT"] = np.ascontiguousarray(np.asarray(inputs["final_g"], f).reshape(8, 128).T)
    m["pos64"] = np.ascontiguousarray(np.broadcast_to(np.asarray(inputs["positions"][b], np.int32)[None, :], (64, 8192)))
    inv = (1.0 / (10000.0 ** (np.arange(0, 64, 2, dtype=np.float32) / 64))).astype(f)
    m["inv_freq2"] = np.concatenate([inv, inv]).reshape(64, 1).astype(f)
    m["rope_sign"] = np.concatenate([-np.ones(32, f), np.ones(32, f)]).reshape(64, 1)
    m["ev_q_normT"] = np.ascontiguousarray(np.asarray(inputs["ev_q_norm"], f).reshape(2, 2, 128).transpose(0, 2, 1))
    m["ev_kv_normT"] = np.ascontiguousarray(np.asarray(inputs["ev_kv_norm"], f).reshape(2, 128, 1))
    m["ev_conv_wT"] = np.ascontiguousarray(np.asarray(inputs["ev_conv_w"], f).reshape(2, 4, 12, 128).transpose(0, 3, 2, 1))
    m["ev_a_log_b"] = np.ascontiguousarray(np.broadcast_to(np.asarray(inputs["ev_a_log"], f)[:, None, :], (2, 128, 4)))
    m["ev_dt_bias_b"] = np.ascontiguousarray(np.broadcast_to(np.asarray(inputs["ev_dt_bias"], f)[:, None, :], (2, 128, 4)))
    m["ev_dn_normT"] = np.ascontiguousarray(np.asarray(inputs["ev_dn_norm"], f).reshape(2, 128, 1))
    m["od_cmp_pos_kT"] = np.ascontiguousarray(np.asarray(inputs["od_cmp_pos_k"], f).transpose(0, 2, 1))
    m["od_cmp_pos_vT"] = np.ascontiguousarray(np.asarray(inputs["od_cmp_pos_v"], f).transpose(0, 2, 1))
    return m


def setup_attn_consts(S, C):
    nc = S.nc
    es = S.es
    C.ident_bf = S.sb(es, [128, 128], BF16, "ident_bf")
    S.dve(lambda: nc.vector.tensor_copy(out=C.ident_bf[:], in_=C.ident[:]), r=[C.ident], w=[C.ident_bf])
    C.tiny = S.sb(es, [128, 1], F32, "tiny")
    S.pool(lambda: nc.gpsimd.memset(C.tiny[:], 1e-30), w=[C.tiny])

    def mask_tile(name, base, cm, step, op=ALU.is_ge):
        t = S.sb(es, [128, 512], BF16, name)
        S.pool(lambda: nc.gpsimd.memset(t[:], 0.0), w=[t])
        S.pool(lambda: nc.gpsimd.affine_select(out=t[:], in_=t[:], pattern=[[step, 512]], compare_op=op, fill=NEG,
                                               base=base, channel_multiplier=cm), r=[t], w=[t])
        return t
    C.Mc = [mask_tile(f"Mc{j}", -128 * j, -1, 1) for j in range(4)]
    C.Mw = [None] + [mask_tile(f"Mw{m}", 511 - 128 * m, 1, -1) for m in range(1, 5)]
    C.Mk = [mask_tile(f"Mk{d}", 512 * d - 31, -16, 1) for d in range(5)]


def attn_chunk(S, C, c, bank, kparts, qparts, V, dv, kts, scale, pts, ot, rden, pi0=0):
    nc = S.nc
    po = S.psb[4 + bank % 2]
    pd = S.psb[6 + bank % 2]
    qs = slice(c * 512, (c + 1) * 512)
    pi = pi0
    for i, (kt, nk, masks) in enumerate(kts):
        pst = S.ps4()
        n_mm = len(kparts) + len(masks)
        j = 0
        for (kb, rows), (qb, _) in zip(kparts, qparts):
            mm(S, pst, pst[0:nk, :], kb, kb[0:rows, kt * 128:kt * 128 + nk], qb, qb[0:rows, qs], j == 0, j == n_mm - 1)
            j += 1
        for (lb, lap, rb, rap) in masks:
            mm(S, pst, pst[0:nk, :], lb, lap, rb, rap, False, j == n_mm - 1)
            j += 1
        pt = pts[pi % len(pts)]
        pi += 1
        S.act(lambda pt=pt, pst=pst, nk=nk: nc.scalar.activation(out=pt[0:nk, :], in_=pst[0:nk, :], func=AF.Exp,
                                                                  scale=scale), r=[pst], w=[pt])
        last = i == len(kts) - 1
        mm(S, po, po[0:dv, :], V, V[0:nk, kt, :], pt, pt[0:nk, :], i == 0, last)
        mm(S, pd, pd[0:dv, :], C.ones_bf, C.ones_bf[0:nk, 0:dv], pt, pt[0:nk, :], i == 0, last)
    S.dve(lambda: nc.vector.tensor_scalar(out=rden[0:dv, :], in0=pd[0:dv, :], scalar1=C.tiny[0:dv, 0:1],
                                          scalar2=None, op0=ALU.max), r=[pd, C.tiny], w=[rden])
    S.dve(lambda: nc.vector.reciprocal(out=rden[0:dv, :], in_=rden[0:dv, :]), r=[rden], w=[rden])
    S.dve(lambda: nc.vector.tensor_tensor(out=ot[0:dv, :], in0=po[0:dv, :], in1=rden[0:dv, :], op=ALU.mult),
          r=[po, rden], w=[ot])
    return pi


def attn_head(S, C, es, kparts, qparts, V, dv, kts_fn, scale, out_cb, pts, otiles, rden):
    pi = 0
    for c in range(16):
        ot = otiles[c % len(otiles)]
        pi = attn_chunk(S, C, c, c, kparts, qparts, V, dv, kts_fn(c), scale, pts, ot, rden, pi)
        out_cb(c, ot)


def phase_mla(S, C, pT, j, oT_dram):
    nc = S.nc
    R_CQ, R_CKV, R_KR = 2056, 2312, 2440
    w_uq = S.dram("ev_w_uq", [2, 256, 768], F32)
    w_ukv = S.dram("ev_w_ukv", [2, 128, 1024], F32)
    qn_d = S.dram("ev_q_normT", [2, 128, 2], F32)
    kvn_d = S.dram("ev_kv_normT", [2, 128, 1], F32)
    pos_d = S.dram("pos64", [64, 8192], I32)
    inv_d = S.dram("inv_freq2", [64, 1], F32)
    sgn_d = S.dram("rope_sign", [64, 1], F32)
    with ExitStack() as es:
        cos2 = S.sb(es, [64, 8192], BF16, "cos2")
        sin2 = S.sb(es, [64, 8192], BF16, "sin2s")
        cqn = S.sb(es, [128, 2, 8192], BF16, "cqn")
        ckvn = S.sb(es, [128, 8192], BF16, "ckvn")
        KR = S.sb(es, [64, 8192], BF16, "KR")
        wq = S.sb(es, [128, 2, 768], BF16, "wq")
        wqs = S.sb(es, [128, 2, 4, 64], BF16, "wqs")
        wkv = S.sb(es, [128, 1024], BF16, "wkv")
        qn = S.sb(es, [128, 2], F32, "qn")
        kvn = S.sb(es, [128, 1], F32, "kvn")
        inv = S.sb(es, [64, 1], F32, "inv")
        sgn = S.sb(es, [64, 1], F32, "sgn")
        negpi = S.sb(es, [64, 1], F32, "negpi")
        S.pool(lambda: nc.gpsimd.memset(negpi[:], -float(np.pi)), w=[negpi])
        S.dma("sp", qn[:], qn_d[j], r=[qn_d], w=[qn])
        S.dma("sp", kvn[:], kvn_d[j], r=[kvn_d], w=[kvn])
        S.dma("sp", inv[:], inv_d[:, :], r=[inv_d], w=[inv])
        S.dma("sp", sgn[:], sgn_d[:, :], r=[sgn_d], w=[sgn])
        for kc in range(2):
            S.dma("pool", wq[:, kc, :], w_uq[j, kc * 128:(kc + 1) * 128, :], r=[w_uq], w=[wq])
            for h in range(4):
                b0 = h * 192 + 128
                S.dma("pool", wqs[:, kc, h, 0:32], w_uq[j, kc * 128:(kc + 1) * 128, b0 + 32:b0 + 64], r=[w_uq], w=[wqs])
                S.dma("pool", wqs[:, kc, h, 32:64], w_uq[j, kc * 128:(kc + 1) * 128, b0:b0 + 32], r=[w_uq], w=[wqs])
        S.dma("pool", wkv[:], w_ukv[j, :, :], r=[w_ukv], w=[wkv])
        with ExitStack() as es2:
            posi = S.sb(es2, [64, 512], I32, "posi")
            ang = S.sb(es2, [64, 512], F32, "ang")
            u = S.sb(es2, [64, 512], F32, "u")
            ni = S.sb(es2, [64, 512], I32, "ni")
            nf = S.sb(es2, [64, 512], F32, "nf")
            cq = S.sb(es2, [128, 2, 512], F32, "cq")
            ckv = S.sb(es2, [128, 512], F32, "ckv")
            kr = S.sb(es2, [64, 512], F32, "kr")
            krs = S.sb(es2, [64, 512], F32, "krs")
            sq = S.sb(es2, [128, 3, 512], BF16, "sq")
            rstd = S.sb(es2, [128, 512], F32, "rstd")
            tmp = S.sb(es2, [128, 512], F32, "tmp")
            for tb in range(NB):
                ts = slice(tb * 512, (tb + 1) * 512)
                S.dma("sp", posi[:], pos_d[:, ts], r=[pos_d], w=[posi])
                S.dve(lambda: nc.vector.tensor_copy(out=ang[:], in_=posi[:]), r=[posi], w=[ang])
                S.dve(lambda: nc.vector.tensor_scalar(out=ang[:], in0=ang[:], scalar1=inv[:, 0:1], scalar2=None,
                                                      op0=ALU.mult), r=[ang, inv], w=[ang])
                for (dst, off, signed) in ((sin2, 0.5, True), (cos2, 0.75, False)):
                    S.dve(lambda off=off: nc.vector.tensor_scalar(out=u[:], in0=ang[:], scalar1=float(1.0 / (2 * np.pi)),
                                                                  scalar2=off, op0=ALU.mult, op1=ALU.add), r=[ang], w=[u])
                    S.dve(lambda: nc.vector.tensor_copy(out=ni[:], in_=u[:]), r=[u], w=[ni])
                    S.dve(lambda: nc.vector.tensor_copy(out=nf[:], in_=ni[:]), r=[ni], w=[nf])
                    S.dve(lambda: nc.vector.tensor_tensor(out=u[:], in0=u[:], in1=nf[:], op=ALU.subtract), r=[u, nf], w=[u])
                    S.dve(lambda: nc.vector.tensor_scalar(out=nf[:], in0=u[:], scalar1=0.0, scalar2=None, op0=ALU.is_lt),
                          r=[u], w=[nf])
                    S.dve(lambda: nc.vector.tensor_tensor(out=u[:], in0=u[:], in1=nf[:], op=ALU.add), r=[u, nf], w=[u])
                    S.act(lambda: nc.scalar.activation(out=u[:], in_=u[:], func=AF.Sin, scale=float(2 * np.pi),
                                                       bias=negpi[:, 0:1]), r=[u, negpi], w=[u])
                    if signed:
                        S.dve(lambda dst=dst, ts=ts: nc.vector.tensor_scalar(out=dst[:, ts], in0=u[:], scalar1=sgn[:, 0:1],
                                                                             scalar2=None, op0=ALU.mult), r=[u, sgn], w=[dst])
                    else:
                        S.dve(lambda dst=dst, ts=ts: nc.vector.tensor_copy(out=dst[:, ts], in_=u[:]), r=[u], w=[dst])
                S.dma("sp", cq[:], pT[R_CQ:R_CQ + 256, ts].rearrange("(kc p) t -> p kc t", p=128), r=[pT], w=[cq])
                S.dma("sp", ckv[:], pT[R_CKV:R_CKV + 128, ts], r=[pT], w=[ckv])
                S.dma("sp", kr[:], pT[R_KR:R_KR + 64, ts], r=[pT], w=[kr])
                S.dma("sp", krs[0:32, :], pT[R_KR + 32:R_KR + 64, ts], r=[pT], w=[krs])
                S.dma("sp", krs[32:64, :], pT[R_KR:R_KR + 32, ts], r=[pT], w=[krs])
                for kc in range(2):
                    S.pool(lambda kc=kc: nc.gpsimd.tensor_tensor(out=sq[:, kc, :], in0=cq[:, kc, :], in1=cq[:, kc, :],
                                                                 op=ALU.mult), r=[cq], w=[sq])
                S.pool(lambda: nc.gpsimd.tensor_tensor(out=sq[:, 2, :], in0=ckv[:], in1=ckv[:], op=ALU.mult), r=[ckv], w=[sq])
                ps = S.ps4()
                for kc in range(2):
                    mm(S, ps, ps[:, :], C.ones_bf, C.ones_bf[:, :], sq, sq[:, kc, :], kc == 0, kc == 1)
                S.act(lambda ps=ps: nc.scalar.activation(out=rstd[:], in_=ps[:, :], func=AF.Sqrt, scale=1.0 / 256,
                                                         bias=C.eps[:, 0:1]), r=[ps, C.eps], w=[rstd])
                S.dve(lambda: nc.vector.reciprocal(out=rstd[:], in_=rstd[:]), r=[rstd], w=[rstd])
                for kc in range(2):
                    S.dve(lambda kc=kc: nc.vector.tensor_tensor(out=tmp[:], in0=cq[:, kc, :], in1=rstd[:], op=ALU.mult),
                          r=[cq, rstd], w=[tmp])
                    S.dve(lambda kc=kc, ts=ts: nc.vector.tensor_scalar(out=cqn[:, kc, ts], in0=tmp[:], scalar1=qn[:, kc:kc + 1],
                                                                       scalar2=None, op0=ALU.mult), r=[tmp, qn], w=[cqn])
                ps = S.ps4()
                mm(S, ps, ps[:, :], C.ones_bf, C.ones_bf[:, :], sq, sq[:, 2, :], True, True)
                S.act(lambda ps=ps: nc.scalar.activation(out=rstd[:], in_=ps[:, :], func=AF.Sqrt, scale=1.0 / 128,
                                                         bias=C.eps[:, 0:1]), r=[ps, C.eps], w=[rstd])
                S.dve(lambda: nc.vector.reciprocal(out=rstd[:], in_=rstd[:]), r=[rstd], w=[rstd])
                S.dve(lambda: nc.vector.tensor_tensor(out=tmp[:], in0=ckv[:], in1=rstd[:], op=ALU.mult), r=[ckv, rstd], w=[tmp])
                S.dve(lambda ts=ts: nc.vector.tensor_scalar(out=ckvn[:, ts], in0=tmp[:], scalar1=kvn[:, 0:1], scalar2=None,
                                                            op0=ALU.mult), r=[tmp, kvn], w=[ckvn])
                S.dve(lambda ts=ts: nc.vector.tensor_tensor(out=kr[:], in0=kr[:], in1=cos2[:, ts], op=ALU.mult), r=[kr, cos2], w=[kr])
                S.dve(lambda ts=ts: nc.vector.tensor_tensor(out=krs[:], in0=krs[:], in1=sin2[:, ts], op=ALU.mult), r=[krs, sin2], w=[krs])
                S.dve(lambda ts=ts: nc.vector.tensor_tensor(out=KR[:, ts], in0=kr[:], in1=krs[:], op=ALU.add), r=[kr, krs], w=[KR])
        S.barrier()
        with ExitStack() as es3:
            QN = S.sb(es3, [128, 8192], BF16, "QN")
            QR = S.sb(es3, [64, 8192], BF16, "QR")
            KN = S.sb(es3, [128, 8192], BF16, "KN")
            V = S.sb(es3, [128, 64, 128], BF16, "V")
            pts = [S.sb(es3, [128, 512], BF16, "pt") for _ in range(3)]
            otiles = [S.sb(es3, [128, 512], BF16, "ot") for _ in range(2)]
            rden = S.sb(es3, [128, 512], F32, "rden")
            t1 = S.sb(es3, [64, 512], F32, "t1")
            t2 = S.sb(es3, [64, 512], F32, "t2")
            for h in range(4):
                for tb in range(NB):
                    ts = slice(tb * 512, (tb + 1) * 512)
                    ps = S.ps4()
                    for kc in range(2):
                        mm(S, ps, ps[:, :], wq, wq[:, kc, h * 192:h * 192 + 128], cqn, cqn[:, kc, ts], kc == 0, kc == 1)
                    S.act(lambda ps=ps, ts=ts: nc.scalar.copy(out=QN[:, ts], in_=ps[:, :]), r=[ps], w=[QN])
                    ps = S.ps4()
                    for kc in range(2):
                        mm(S, ps, ps[0:64, :], wq, wq[:, kc, h * 192 + 128:h * 192 + 192], cqn, cqn[:, kc, ts], kc == 0, kc == 1)
                    ps2 = S.ps4()
                    for kc in range(2):
                        mm(S, ps2, ps2[0:64, :], wqs, wqs[:, kc, h, :], cqn, cqn[:, kc, ts], kc == 0, kc == 1)
                    S.dve(lambda ps=ps, ts=ts: nc.vector.tensor_tensor(out=t1[:], in0=ps[0:64, :], in1=cos2[:, ts], op=ALU.mult),
                          r=[ps, cos2], w=[t1])
                    S.dve(lambda ps2=ps2, ts=ts: nc.vector.tensor_tensor(out=t2[:], in0=ps2[0:64, :], in1=sin2[:, ts], op=ALU.mult),
                          r=[ps2, sin2], w=[t2])
                    S.dve(lambda ts=ts: nc.vector.tensor_tensor(out=QR[:, ts], in0=t1[:], in1=t2[:], op=ALU.add), r=[t1, t2], w=[QR])
                    ps = S.ps4()
                    mm(S, ps, ps[:, :], wkv, wkv[:, h * 256:h * 256 + 128], ckvn, ckvn[:, ts], True, True)
                    S.act(lambda ps=ps, ts=ts: nc.scalar.copy(out=KN[:, ts], in_=ps[:, :]), r=[ps], w=[KN])
                    ps = S.ps4()
                    for tt in range(4):
                        mm(S, ps, ps[:, tt * 128:(tt + 1) * 128], ckvn, ckvn[:, tb * 512 + tt * 128:tb * 512 + (tt + 1) * 128],
                           wkv, wkv[:, h * 256 + 128:h * 256 + 256], True, True)
                    S.act(lambda ps=ps, tb=tb: nc.scalar.copy(out=V[:, tb * 4:(tb + 1) * 4, :],
                                                              in_=ps[:, :].rearrange("p (t d) -> p t d", d=128)), r=[ps], w=[V])

                def kts_fn(c):
                    out = []
                    for kt in range(4 * c + 4):
                        masks = []
                        if kt >= 4 * c:
                            m = C.Mc[kt - 4 * c]
                            masks = [(C.ident_bf, C.ident_bf[:, :], m, m[:, :])]
                        out.append((kt, 128, masks))
                    return out

                def out_cb(c, ot, h=h):
                    S.dma("sp", oT_dram[512 + h * 128:512 + (h + 1) * 128, c * 512:(c + 1) * 512], ot[:, :], r=[ot], w=[oT_dram])

                attn_head(S, C, es3, [(KN, 128), (KR, 64)], [(QN, 128), (QR, 64)], V, 128, kts_fn, 192 ** -0.5, out_cb,
                          pts, otiles, rden)
        S.barrier()


def phase_dn(S, C, pT, j, oT_dram):
    nc = S.nc
    cw_d = S.dram("ev_conv_wT", [2, 128, 12, 4], F32)
    al_d = S.dram("ev_a_log_b", [2, 128, 4], F32)
    dt_d = S.dram("ev_dt_bias_b", [2, 128, 4], F32)
    gn_d = S.dram("ev_dn_normT", [2, 128, 1], F32)
    with ExitStack() as es:
        cw = S.sb(es, [128, 12, 4], F32, "cw")
        al = S.sb(es, [128, 4], F32, "al")
        dtb = S.sb(es, [128, 4], F32, "dtb")
        gn = S.sb(es, [128, 1], F32, "gn")
        S.dma("sp", cw[:], cw_d[j], r=[cw_d], w=[cw])
        S.dma("sp", al[:], al_d[j], r=[al_d], w=[al])
        S.dma("sp", dtb[:], dt_d[j], r=[dt_d], w=[dtb])
        S.dma("sp", gn[:], gn_d[j], r=[gn_d], w=[gn])
        S.act(lambda: nc.scalar.activation(out=al[:], in_=al[:], func=AF.Exp), r=[al], w=[al])
        UT = S.sb(es, [128, 128], F32, "UT")
        S.pool(lambda: nc.gpsimd.memset(UT[:], 1.0), w=[UT])
        S.pool(lambda: nc.gpsimd.affine_select(out=UT[:], in_=UT[:], pattern=[[1, 128]], compare_op=ALU.is_ge, fill=0.0,
                                               base=0, channel_multiplier=-1), r=[UT], w=[UT])
        PM1 = S.sb(es, [128, 128], F32, "PM1")
        S.pool(lambda: nc.gpsimd.memset(PM1[:], 0.0), w=[PM1])
        S.pool(lambda: nc.gpsimd.affine_select(out=PM1[:], in_=PM1[:], pattern=[[-1, 128]], compare_op=ALU.is_gt, fill=1e5,
                                               base=0, channel_multiplier=1), r=[PM1], w=[PM1])
        NM2 = S.sb(es, [128, 128], F32, "NM2")
        S.pool(lambda: nc.gpsimd.memset(NM2[:], 0.0), w=[NM2])
        S.pool(lambda: nc.gpsimd.affine_select(out=NM2[:], in_=NM2[:], pattern=[[1, 128]], compare_op=ALU.is_ge, fill=-1e5,
                                               base=0, channel_multiplier=-1), r=[NM2], w=[NM2])
        braw = S.sb(es, [64, 128], F32, "braw")
        araw = S.sb(es, [64, 128], F32, "araw")
        beta = S.sb(es, [128, 64], F32, "beta")
        g = S.sb(es, [128, 64], F32, "g")
        gc = S.sb(es, [128, 64], F32, "gc")
        ngc = S.sb(es, [128, 64], F32, "ngc")
        glast = S.sb(es, [128, 64], F32, "glast")
        alast = S.sb(es, [128, 64], F32, "alast")
        etail = S.sb(es, [128, 64], F32, "etail")
        bexpg = S.sb(es, [128, 64], F32, "bexpg")
        nbeta = S.sb(es, [128, 64], F32, "nbeta")
        St = S.sb(es, [128, 128], F32, "state")
        NS = 2
        def mk(name, shape=(128, 128), dt=F32):
            return [S.sb(es, list(shape), dt, name) for _ in range(NS)]
        raw = {n: mk("raw" + n, (128, 131)) for n in "qkv"}
        cv = {n: mk("cv" + n) for n in "qkv"}
        rn = mk("rn")
        qT = mk("qT"); kT = mk("kT"); ktok = mk("ktok"); vtok = mk("vtok")
        dgc = mk("dgc"); Dm = mk("Dm"); DiT = mk("DiT"); egr = mk("egr")
        Nm = mk("Nm"); NmT = mk("NmT"); M2 = mk("M2"); M2T = mk("M2T"); RT = mk("RT")
        qkT = mk("qkT"); qgT = mk("qgT"); vb = mk("vb"); kbg = mk("kbg"); ktl = mk("ktl")
        u = mk("u"); wT = mk("wT"); vnew = mk("vnew"); osb = mk("osb"); osq = mk("osq"); zt = mk("zt")
        obf = mk("obf", (128, 128), BF16)

        def evac(dst, ps, eng="act"):
            if eng == "act":
                S.act(lambda: nc.scalar.copy(out=dst[:], in_=ps[:, 0:128]), r=[ps], w=[dst])
            else:
                S.dve(lambda: nc.vector.tensor_copy(out=dst[:], in_=ps[:, 0:128]), r=[ps], w=[dst])

        for h in range(4):
            S.dma("sp", braw[:], pT[2048 + h, :].rearrange("(t p) -> t p", p=128), r=[pT], w=[braw])
            S.dma("sp", araw[:], pT[2052 + h, :].rearrange("(t p) -> t p", p=128), r=[pT], w=[araw])
            ps = S.ps()
            S.pe(lambda ps=ps: nc.tensor.transpose(out=ps[:, 0:64], in_=braw[:, :], identity=C.ident[0:64, 0:64]),
                 r=[braw, C.ident], w=[ps])
            S.act(lambda ps=ps: nc.scalar.activation(out=beta[:], in_=ps[:, 0:64], func=AF.Sigmoid), r=[ps], w=[beta])
            ps = S.ps()
            S.pe(lambda ps=ps: nc.tensor.transpose(out=ps[:, 0:64], in_=araw[:, :], identity=C.ident[0:64, 0:64]),
                 r=[araw, C.ident], w=[ps])
            S.act(lambda ps=ps, h=h: nc.scalar.activation(out=g[:], in_=ps[:, 0:64], func=AF.Exp, bias=dtb[:, h:h + 1]),
                  r=[ps, dtb], w=[g])
            S.dve(lambda: nc.vector.tensor_scalar(out=g[:], in0=g[:], scalar1=1.0, scalar2=None, op0=ALU.add), r=[g], w=[g])
            S.act(lambda: nc.scalar.activation(out=g[:], in_=g[:], func=AF.Ln), r=[g], w=[g])
            S.dve(lambda h=h: nc.vector.tensor_scalar(out=g[:], in0=g[:], scalar1=al[:, h:h + 1], scalar2=-1.0, op0=ALU.mult,
                                                      op1=ALU.mult), r=[g, al], w=[g])
            ps = S.ps()
            mm(S, ps, ps[:, 0:64], UT, UT[:, :], g, g[:, :], True, True)
            S.act(lambda ps=ps: nc.scalar.copy(out=gc[:], in_=ps[:, 0:64]), r=[ps], w=[gc])
            S.dve(lambda: nc.vector.tensor_scalar(out=ngc[:], in0=gc[:], scalar1=-1.0, scalar2=None, op0=ALU.mult), r=[gc], w=[ngc])
            ps = S.ps()
            mm(S, ps, ps[:, 0:64], C.ones_f, C.ones_f[:, :], g, g[:, :], True, True)
            S.act(lambda ps=ps: nc.scalar.copy(out=glast[:], in_=ps[:, 0:64]), r=[ps], w=[glast])
            S.act(lambda: nc.scalar.activation(out=alast[:], in_=glast[:], func=AF.Exp), r=[glast], w=[alast])
            S.dve(lambda: nc.vector.tensor_tensor(out=etail[:], in0=glast[:], in1=gc[:], op=ALU.subtract), r=[glast, gc], w=[etail])
            S.act(lambda: nc.scalar.activation(out=etail[:], in_=etail[:], func=AF.Exp), r=[etail], w=[etail])
            S.act(lambda: nc.scalar.activation(out=bexpg[:], in_=gc[:], func=AF.Exp), r=[gc], w=[bexpg])
            S.dve(lambda: nc.vector.tensor_tensor(out=bexpg[:], in0=bexpg[:], in1=beta[:], op=ALU.mult), r=[bexpg, beta], w=[bexpg])
            S.dve(lambda: nc.vector.tensor_scalar(out=nbeta[:], in0=beta[:], scalar1=-1.0, scalar2=None, op0=ALU.mult),
                  r=[beta], w=[nbeta])
            S.dve(lambda: nc.vector.memset(St[:], 0.0), w=[St])
            for T in range(NT):
                s = T % NS
                t0 = T * 128
                for ci, n in enumerate("qkv"):
                    rw = raw[n][s]
                    row0 = ci * 512 + h * 128
                    if T == 0:
                        S.dve(lambda rw=rw: nc.vector.memset(rw[:, 0:3], 0.0), w=[rw])
                        S.dma("sp", rw[:, 3:131], pT[row0:row0 + 128, 0:128], r=[pT], w=[rw])
                    else:
                        S.dma("sp", rw[:, :], pT[row0:row0 + 128, t0 - 3:t0 + 128], r=[pT], w=[rw])
                    c_ = cv[n][s]
                    ft = ci * 4 + h
                    S.dve(lambda rw=rw, c_=c_, ft=ft: nc.vector.tensor_scalar(out=c_[:], in0=rw[:, 0:128], scalar1=cw[:, ft, 0:1],
                                                                              scalar2=None, op0=ALU.mult), r=[rw, cw], w=[c_])
                    for k in range(1, 4):
                        S.dve(lambda rw=rw, c_=c_, ft=ft, k=k: nc.vector.scalar_tensor_tensor(
                            out=c_[:], in0=rw[:, k:k + 128], scalar=cw[:, ft, k:k + 1], in1=c_[:], op0=ALU.mult, op1=ALU.add),
                            r=[rw, cw, c_], w=[c_])
                    S.act(lambda c_=c_: nc.scalar.activation(out=c_[:], in_=c_[:], func=AF.Silu), r=[c_], w=[c_])
                for n, dst, mul in (("q", qT[s], 128 ** -0.5), ("k", kT[s], 1.0)):
                    c_ = cv[n][s]
                    S.pool(lambda c_=c_: nc.gpsimd.tensor_tensor(out=rn[s][:], in0=c_[:], in1=c_[:], op=ALU.mult), r=[c_], w=[rn[s]])
                    ps = S.ps()
                    mm(S, ps, ps[:, 0:128], C.ones_f, C.ones_f[:, :], rn[s], rn[s][:, :], True, True)
                    S.act(lambda ps=ps: nc.scalar.activation(out=rn[s][:], in_=ps[:, 0:128], func=AF.Sqrt, bias=C.eps[:, 0:1]),
                          r=[ps, C.eps], w=[rn[s]])
                    S.dve(lambda: nc.vector.reciprocal(out=rn[s][:], in_=rn[s][:]), r=[rn[s]], w=[rn[s]])
                    S.dve(lambda c_=c_, dst=dst, mul=mul: nc.vector.scalar_tensor_tensor(
                        out=dst[:], in0=c_[:], scalar=mul, in1=rn[s][:], op0=ALU.mult, op1=ALU.mult), r=[c_, rn[s]], w=[dst])
                ps = S.ps()
                S.pe(lambda ps=ps: nc.tensor.transpose(out=ps[:, 0:128], in_=kT[s][:, :], identity=C.ident[:, :]),
                     r=[kT[s], C.ident], w=[ps])
                evac(ktok[s], ps)
                ps = S.ps()
                S.pe(lambda ps=ps: nc.tensor.transpose(out=ps[:, 0:128], in_=cv["v"][s][:, :], identity=C.ident[:, :]),
                     r=[cv["v"][s], C.ident], w=[ps])
                evac(vtok[s], ps, "dve")
                S.dve(lambda: nc.vector.tensor_scalar(out=dgc[s][:], in0=C.ident[:], scalar1=gc[:, T:T + 1], scalar2=None,
                                                      op0=ALU.mult), r=[C.ident, gc], w=[dgc[s]])
                p1 = S.ps()
                mm(S, p1, p1[:, 0:128], C.ones_f, C.ones_f[:, :], dgc[s], dgc[s][:, :], True, False)
                mm(S, p1, p1[:, 0:128], C.ident, C.ident[:, :], PM1, PM1[:, :], False, True)
                S.act(lambda p1=p1: nc.scalar.activation(out=Dm[s][:], in_=p1[:, 0:128], func=AF.Exp, scale=-1.0,
                                                         bias=gc[:, T:T + 1]), r=[p1, gc], w=[Dm[s]])
                p2 = S.ps()
                mm(S, p2, p2[:, 0:128], C.ones_f, C.ones_f[:, :], dgc[s], dgc[s][:, :], True, False)
                mm(S, p2, p2[:, 0:128], C.ident, C.ident[:, :], NM2, NM2[:, :], False, True)
                S.act(lambda p2=p2: nc.scalar.activation(out=DiT[s][:], in_=p2[:, 0:128], func=AF.Exp, scale=1.0,
                                                         bias=ngc[:, T:T + 1]), r=[p2, ngc], w=[DiT[s]])
                p3 = S.ps()
                mm(S, p3, p3[:, 0:128], C.ones_f, C.ones_f[:, :], dgc[s], dgc[s][:, :], True, True)
                S.act(lambda p3=p3: nc.scalar.activation(out=egr[s][:], in_=p3[:, 0:128], func=AF.Exp), r=[p3], w=[egr[s]])
                pg = S.ps()
                mm(S, pg, pg[:, 0:128], kT[s], kT[s][:, :], kT[s], kT[s][:, :], True, True)
                S.dve(lambda pg=pg: nc.vector.scalar_tensor_tensor(out=Nm[s][:], in0=pg[:, 0:128], scalar=nbeta[:, T:T + 1],
                                                                   in1=Dm[s][:], op0=ALU.mult, op1=ALU.mult),
                      r=[pg, nbeta, Dm[s]], w=[Nm[s]])
                ps = S.ps()
                S.pe(lambda ps=ps: nc.tensor.transpose(out=ps[:, 0:128], in_=Nm[s][:, :], identity=C.ident[:, :]),
                     r=[Nm[s], C.ident], w=[ps])
                evac(NmT[s], ps)
                S.dve(lambda: nc.vector.tensor_tensor(out=RT[s][:], in0=NmT[s][:], in1=C.ident[:], op=ALU.add),
                      r=[NmT[s], C.ident], w=[RT[s]])
                Mc_, McT = Nm[s], NmT[s]
                Mn, MnT = M2[s], M2T[s]
                for lvl in range(6):
                    pa = S.ps()
                    mm(S, pa, pa[:, 0:128], McT, McT[:, :], Mc_, Mc_[:, :], True, True)
                    pb = S.ps()
                    mm(S, pb, pb[:, 0:128], Mc_, Mc_[:, :], McT, McT[:, :], True, True)
                    evac(Mn, pa, "act")
                    evac(MnT, pb, "dve")
                    pr = S.ps()
                    mm(S, pr, pr[:, 0:128], Mn, Mn[:, :], RT[s], RT[s][:, :], True, True)
                    S.dve(lambda pr=pr: nc.vector.tensor_tensor(out=RT[s][:], in0=RT[s][:], in1=pr[:, 0:128], op=ALU.add),
                          r=[RT[s], pr], w=[RT[s]])
                    Mc_, McT, Mn, MnT = Mn, MnT, Mc_, McT
                S.dve(lambda: nc.vector.tensor_scalar(out=vb[s][:], in0=vtok[s][:], scalar1=beta[:, T:T + 1], scalar2=None,
                                                      op0=ALU.mult), r=[vtok[s], beta], w=[vb[s]])
                S.dve(lambda: nc.vector.tensor_scalar(out=kbg[s][:], in0=ktok[s][:], scalar1=bexpg[:, T:T + 1], scalar2=None,
                                                      op0=ALU.mult), r=[ktok[s], bexpg], w=[kbg[s]])
                S.pool(lambda: nc.gpsimd.tensor_scalar(out=ktl[s][:], in0=ktok[s][:], scalar1=etail[:, T:T + 1], scalar2=None,
                                                       op0=ALU.mult), r=[ktok[s], etail], w=[ktl[s]])
                pu = S.ps()
                mm(S, pu, pu[:, 0:128], RT[s], RT[s][:, :], vb[s], vb[s][:, :], True, True)
                evac(u[s], pu, "act")
                pw = S.ps()
                mm(S, pw, pw[:, 0:128], kbg[s], kbg[s][:, :], RT[s], RT[s][:, :], True, True)
                evac(wT[s], pw, "act")
                pq = S.ps()
                mm(S, pq, pq[:, 0:128], kT[s], kT[s][:, :], qT[s], qT[s][:, :], True, True)
                S.dve(lambda pq=pq: nc.vector.tensor_tensor(out=qkT[s][:], in0=pq[:, 0:128], in1=DiT[s][:], op=ALU.mult),
                      r=[pq, DiT[s]], w=[qkT[s]])
                S.pool(lambda: nc.gpsimd.tensor_tensor(out=qgT[s][:], in0=qT[s][:], in1=egr[s][:], op=ALU.mult),
                       r=[qT[s], egr[s]], w=[qgT[s]])
                pv = S.ps()
                mm(S, pv, pv[:, 0:128], wT[s], wT[s][:, :], St, St[:, :], True, True)
                S.dve(lambda pv=pv: nc.vector.tensor_tensor(out=vnew[s][:], in0=u[s][:], in1=pv[:, 0:128], op=ALU.subtract),
                      r=[u[s], pv], w=[vnew[s]])
                po = S.ps()
                mm(S, po, po[:, 0:128], St, St[:, :], qgT[s], qgT[s][:, :], True, False)
                mm(S, po, po[:, 0:128], vnew[s], vnew[s][:, :], qkT[s], qkT[s][:, :], False, True)
                pS = S.ps()
                mm(S, pS, pS[:, 0:128], ktl[s], ktl[s][:, :], vnew[s], vnew[s][:, :], True, True)
                S.dve(lambda pS=pS: nc.vector.scalar_tensor_tensor(out=St[:], in0=St[:], scalar=alast[:, T:T + 1],
                                                                   in1=pS[:, 0:128], op0=ALU.mult, op1=ALU.add),
                      r=[St, alast, pS], w=[St])
                evac(osb[s], po, "act")
                S.pool(lambda: nc.gpsimd.tensor_tensor(out=osq[s][:], in0=osb[s][:], in1=osb[s][:], op=ALU.mult), r=[osb[s]], w=[osq[s]])
                pn = S.ps()
                mm(S, pn, pn[:, 0:128], C.ones_f, C.ones_f[:, :], osq[s], osq[s][:, :], True, True)
                S.act(lambda pn=pn: nc.scalar.activation(out=osq[s][:], in_=pn[:, 0:128], func=AF.Sqrt, scale=1.0 / 128,
                                                         bias=C.eps[:, 0:1]), r=[pn, C.eps], w=[osq[s]])
                S.dve(lambda: nc.vector.reciprocal(out=osq[s][:], in_=osq[s][:]), r=[osq[s]], w=[osq[s]])
                S.dma("sp", zt[s][:], pT[1536 + h * 128:1536 + (h + 1) * 128, t0:t0 + 128], r=[pT], w=[zt[s]])
                S.act(lambda: nc.scalar.activation(out=zt[s][:], in_=zt[s][:], func=AF.Silu), r=[zt[s]], w=[zt[s]])
                S.dve(lambda: nc.vector.scalar_tensor_tensor(out=osb[s][:], in0=osb[s][:], scalar=gn[:, 0:1], in1=osq[s][:],
                                                             op0=ALU.mult, op1=ALU.mult), r=[osb[s], gn, osq[s]], w=[osb[s]])
                S.dve(lambda: nc.vector.tensor_tensor(out=obf[s][:], in0=osb[s][:], in1=zt[s][:], op=ALU.mult),
                      r=[osb[s], zt[s]], w=[obf[s]])
                S.dma("sp", oT_dram[h * 128:(h + 1) * 128, t0:t0 + 128], obf[s][:], r=[obf[s]], w=[oT_dram])
        S.barrier()


def phase_nsa(S, C, pT, vs_tok, vw_tok, j, oT_dram):
    nc = S.nc
    R_Q, R_KC, R_VC, R_KS, R_KW, R_GL = 0, 1024, 1280, 1536, 2048, 2560
    posk_d = S.dram("od_cmp_pos_kT", [2, 64, 32], F32)
    posv_d = S.dram("od_cmp_pos_vT", [2, 64, 32], F32)
    k1_d = S.dram("od_cmp_k1", [2, 2048, 256], F32)
    k2_d = S.dram("od_cmp_k2", [2, 256, 64], F32)
    v1_d = S.dram("od_cmp_v1", [2, 2048, 256], F32)
    v2_d = S.dram("od_cmp_v2", [2, 256, 64], F32)
    SC = 0.125
    with ExitStack() as es:
        KC = [S.sb(es, [64, 512], BF16, "KC") for _ in range(4)]
        for g in range(4):
            S.pool(lambda g=g: nc.gpsimd.memset(KC[g][:], 0.0), w=[KC[g]])
        VC = [S.sb(es, [128, 4, 64], BF16, "VC") for _ in range(4)]
        with ExitStack() as e1:
            src = S.sb(e1, [64, 8192], BF16, "csrc")
            w1 = S.sb(e1, [64, 32, 256], BF16, "w1")
            w2 = S.sb(e1, [128, 2, 64], BF16, "w2")
            posT = S.sb(e1, [64, 32], BF16, "posT")
            c1 = S.sb(e1, [128, 2], F32, "c1")
            h1 = S.sb(e1, [128, 2, 512], BF16, "h1")
            for which, (pos_d, a_d, b_d, row0) in enumerate(((posk_d, k1_d, k2_d, R_KC), (posv_d, v1_d, v2_d, R_VC))):
                S.dma("pool", w1[:], a_d[j].rearrange("(l d) n -> d l n", d=64), r=[a_d], w=[w1])
                S.dma("pool", w2[:], b_d[j].rearrange("(c p) n -> p c n", p=128), r=[b_d], w=[w2])
                S.dma("pool", posT[:], pos_d[j], r=[pos_d], w=[posT])
                for ncx in range(2):
                    ps = S.ps()
                    for l in range(32):
                        mm(S, ps, ps[:, 0:1], w1, w1[:, l, ncx * 128:(ncx + 1) * 128], posT, posT[:, l:l + 1], l == 0, l == 31)
                    S.act(lambda ps=ps, ncx=ncx: nc.scalar.copy(out=c1[:, ncx:ncx + 1], in_=ps[:, 0:1]), r=[ps], w=[c1])
                for g in range(4):
                    S.dma("pool", src[:], pT[row0 + g * 64:row0 + (g + 1) * 64, :], r=[pT], w=[src])
                    for ncx in range(2):
                        ps = S.ps()
                        for l in range(32):
                            mm(S, ps, ps[:, 0:511], w1, w1[:, l, ncx * 128:(ncx + 1) * 128], src,
                               src[:, l:l + 16 * 510 + 1:16], l == 0, l == 31)
                        S.act(lambda ps=ps, ncx=ncx: nc.scalar.activation(out=h1[:, ncx, 0:511], in_=ps[:, 0:511], func=AF.Silu,
                                                                          bias=c1[:, ncx:ncx + 1]), r=[ps, c1], w=[h1])
                    if which == 0:
                        ps = S.ps()
                        for ncx in range(2):
                            mm(S, ps, ps[0:64, 0:511], w2, w2[:, ncx, :], h1, h1[:, ncx, 0:511], ncx == 0, ncx == 1)
                        S.act(lambda ps=ps, g=g: nc.scalar.copy(out=KC[g][:, 0:511], in_=ps[0:64, 0:511]), r=[ps], w=[KC[g]])
                    else:
                        for nt in range(4):
                            rows = 128 if nt < 3 else 127
                            ps = S.ps()
                            for ncx in range(2):
                                mm(S, ps, ps[0:rows, 0:64], h1, h1[:, ncx, nt * 128:nt * 128 + rows], w2, w2[:, ncx, :],
                                   ncx == 0, ncx == 1)
                            S.act(lambda ps=ps, g=g, nt=nt, rows=rows: nc.scalar.copy(out=VC[g][0:rows, nt, :], in_=ps[0:rows, 0:64]),
                                  r=[ps], w=[VC[g]])
        S.barrier()
        negselT = S.sb(es, [128, 8192], BF16, "negselT")
        Wm = S.sb(es, [128, 16], F32, "Wm")
        Wm0 = S.sb(es, [128, 16], F32, "Wm0")
        for (t_, b_) in ((Wm, 97), (Wm0, -31)):
            S.pool(lambda t_=t_: nc.gpsimd.memset(t_[:], 0.0), w=[t_])
            S.pool(lambda t_=t_, b_=b_: nc.gpsimd.affine_select(out=t_[:], in_=t_[:], pattern=[[-16, 16]], compare_op=ALU.is_ge,
                                                                fill=NEG, base=b_, channel_multiplier=1), r=[t_], w=[t_])
        Ebig = S.sb(es, [128, 8192], BF16, "Ebig")
        Sel48 = S.sb(es, [48, 48, 64], F32, "Sel48")
        S.pool(lambda: nc.gpsimd.memset(Sel48[:], 0.0), w=[Sel48])
        S.pool(lambda: nc.gpsimd.affine_select(out=Sel48[:], in_=Sel48[:], pattern=[[-1, 48], [0, 64]],
                                               compare_op=ALU.not_equal, fill=1.0, base=0, channel_multiplier=1),
               r=[Sel48], w=[Sel48])
        S.pool(lambda: nc.gpsimd.memset(Ebig[:], 0.0), w=[Ebig])
        ebv = Ebig[:].rearrange("p (b x) -> p b x", x=64)
        S.pool(lambda: nc.gpsimd.affine_select(out=ebv, in_=ebv, pattern=[[-1, 128], [0, 64]], compare_op=ALU.not_equal,
                                               fill=1.0, base=0, channel_multiplier=1), r=[Ebig], w=[Ebig])
        for g in range(4):
            with ExitStack() as e2:
                QT4 = [S.sb(e2, [64, 8192], BF16, "QT4") for _ in range(4)]
                for hg in range(4):
                    r0 = R_Q + (g * 4 + hg) * 64
                    S.dma("pool", QT4[hg][:], pT[r0:r0 + 64, :], r=[pT], w=[QT4[hg]])
                scs = [S.sb(e2, [128, 512], F32, "sc") for _ in range(2)]
                pp = S.sb(e2, [128, 516], F32, "pp")
                rs = S.sb(e2, [128, 2], F32, "rs")
                imp = S.sb(e2, [128, 128], F32, "imp")
                imp2 = S.sb(e2, [128, 128], F32, "imp2")
                mx = S.sb(e2, [128, 8], F32, "mx")
                S.dve(lambda: nc.vector.memset(pp[:], 0.0), w=[pp])
                for T in range(NT):
                    for hg in range(4):
                        ps = S.ps()
                        mm(S, ps, ps[:, 0:512], QT4[hg], QT4[hg][:, T * 128:(T + 1) * 128], KC[g], KC[g][:, 0:512], True, True)
                        sc = scs[hg % 2]
                        S.act(lambda ps=ps, sc=sc: nc.scalar.activation(out=sc[:], in_=ps[:, 0:512], func=AF.Copy, scale=SC),
                              r=[ps], w=[sc])
                        w0 = max(8 * T - 8, 0)
                        w1_ = min(8 * T + 8, 512)
                        wm_ap = Wm0[:, 0:8] if T == 0 else Wm[:, 0:w1_ - w0]
                        S.pool(lambda sc=sc, w0=w0, w1_=w1_, wm_ap=wm_ap: nc.gpsimd.tensor_tensor(
                            out=sc[:, w0:w1_], in0=sc[:, w0:w1_], in1=wm_ap, op=ALU.add), r=[sc, Wm, Wm0], w=[sc])
                        if w1_ < 512:
                            S.pool(lambda sc=sc, w1_=w1_: nc.gpsimd.memset(sc[:, w1_:512], NEG), r=[sc], w=[sc])
                        S.act(lambda sc=sc: nc.scalar.activation(out=sc[:], in_=sc[:], func=AF.Exp, accum_out=rs[:, 0:1]),
                              r=[sc], w=[sc, rs])
                        S.dve(lambda: nc.vector.tensor_scalar(out=rs[:, 1:2], in0=rs[:, 0:1], scalar1=C.tiny[:, 0:1], scalar2=None,
                                                              op0=ALU.max), r=[rs, C.tiny], w=[rs])
                        S.dve(lambda: nc.vector.reciprocal(out=rs[:, 1:2], in_=rs[:, 1:2]), r=[rs], w=[rs])
                        if hg == 0:
                            S.dve(lambda sc=sc: nc.vector.tensor_scalar(out=pp[:, 1:513], in0=sc[:], scalar1=rs[:, 1:2], scalar2=None,
                                                                        op0=ALU.mult), r=[sc, rs], w=[pp])
                        else:
                            S.dve(lambda sc=sc: nc.vector.scalar_tensor_tensor(out=pp[:, 1:513], in0=sc[:], scalar=rs[:, 1:2],
                                                                               in1=pp[:, 1:513], op0=ALU.mult, op1=ALU.add),
                                  r=[sc, rs, pp], w=[pp])
                    a = pp[:, 0:512].rearrange("p (j f) -> p j f", f=4)
                    e_ = pp[:, 4:516].rearrange("p (j f) -> p j f", f=4)
                    S.dve(lambda a=a: nc.vector.tensor_scalar(out=imp[:], in0=a[:, :, 0], scalar1=0.5, scalar2=None, op0=ALU.mult),
                          r=[pp], w=[imp])
                    for f in (1, 2, 3):
                        S.dve(lambda a=a, f=f: nc.vector.tensor_tensor(out=imp[:], in0=imp[:], in1=a[:, :, f], op=ALU.add),
                              r=[pp, imp], w=[imp])
                    S.dve(lambda e_=e_: nc.vector.scalar_tensor_tensor(out=imp[:], in0=e_[:, :, 0], scalar=0.5, in1=imp[:],
                                                                       op0=ALU.mult, op1=ALU.add), r=[pp, imp], w=[imp])
                    for half in range(2):
                        cur = 2 * T + half
                        hs = slice(half * 64, half * 64 + 64)
                        if cur + 1 < 128:
                            S.pool(lambda hs=hs, cur=cur: nc.gpsimd.memset(imp[hs, cur + 1:128], -1.0), r=[imp], w=[imp])
                        lo = max(cur - 1, 0)
                        S.pool(lambda hs=hs, lo=lo, cur=cur: nc.gpsimd.memset(imp[hs, lo:cur + 1], 1e9), r=[imp], w=[imp])
                    S.pool(lambda: nc.gpsimd.memset(imp[:, 0:1], 1e9), r=[imp], w=[imp])
                    S.dve(lambda: nc.vector.max(out=mx[:], in_=imp[:]), r=[imp], w=[mx])
                    S.dve(lambda: nc.vector.match_replace(out=imp2[:], in_to_replace=mx[:], in_values=imp[:], imm_value=-2.0),
                          r=[mx, imp], w=[imp2])
                    S.dve(lambda: nc.vector.max(out=mx[:], in_=imp2[:]), r=[imp2], w=[mx])
                    S.dve(lambda: nc.vector.tensor_scalar(out=imp2[:], in0=imp[:], scalar1=mx[:, 7:8], scalar2=None, op0=ALU.is_ge),
                          r=[imp, mx], w=[imp2])
                    S.dve(lambda: nc.vector.tensor_scalar(out=imp2[:], in0=imp2[:], scalar1=-1.0, scalar2=-NEG, op0=ALU.add,
                                                          op1=ALU.mult), r=[imp2], w=[imp2])
                    ps = S.ps()
                    S.pe(lambda ps=ps: nc.tensor.transpose(out=ps[:, 0:128], in_=imp2[:, :], identity=C.ident[:, :]),
                         r=[imp2, C.ident], w=[ps])
                    S.act(lambda ps=ps, T=T: nc.scalar.copy(out=negselT[:, T * 128:(T + 1) * 128], in_=ps[:, 0:128]),
                          r=[ps], w=[negselT])
            S.barrier()
            with ExitStack() as e3:
                ksT = S.sb(e3, [64, 8192], BF16, "ksT")
                kwT = S.sb(e3, [64, 8192], BF16, "kwT")
                VS = S.sb(e3, [128, 64, 64], BF16, "VS")
                VW = S.sb(e3, [128, 64, 64], BF16, "VW")
                QT = S.sb(e3, [64, 8192], BF16, "QT")
                gates = S.sb(e3, [48, 8192], F32, "gates")
                pts = [S.sb(e3, [128, 512], BF16, "pt") for _ in range(3)]
                ots = [S.sb(e3, [64, 512], F32, "ot") for _ in range(2)]
                rden = S.sb(e3, [64, 512], F32, "rden")
                acc = S.sb(e3, [64, 512], F32, "acc")
                tmpo = S.sb(e3, [64, 512], F32, "tmpo")
                obf = [S.sb(e3, [64, 512], BF16, "obf") for _ in range(2)]
                S.dma("pool", ksT[:], pT[R_KS + g * 64:R_KS + (g + 1) * 64, :], r=[pT], w=[ksT])
                S.dma("pool", kwT[:], pT[R_KW + g * 64:R_KW + (g + 1) * 64, :], r=[pT], w=[kwT])
                for q4 in range(4):
                    tsl = slice(q4 * 16, (q4 + 1) * 16)
                    rsl = slice(q4 * 2048, (q4 + 1) * 2048)
                    S.dma("pool", VS[:, tsl, :], vs_tok[rsl, g * 64:(g + 1) * 64].rearrange("(t p) d -> p t d", p=128),
                          r=[vs_tok], w=[VS])
                    S.dma("pool", VW[:, tsl, :], vw_tok[rsl, g * 64:(g + 1) * 64].rearrange("(t p) d -> p t d", p=128),
                          r=[vw_tok], w=[VW])
                S.dma("sp", gates[:], pT[R_GL:R_GL + 48, :], r=[pT], w=[gates])
                S.act(lambda: nc.scalar.activation(out=gates[:], in_=gates[:], func=AF.Sigmoid), r=[gates], w=[gates])
                pi = 0
                bank = 0
                for hg in range(4):
                    head = g * 4 + hg
                    S.dma("pool", QT[:], pT[R_Q + head * 64:R_Q + (head + 1) * 64, :], r=[pT], w=[QT])
                    for c in range(16):
                        qs = slice(c * 512, (c + 1) * 512)
                        for br in range(3):
                            if br == 0:
                                kts = []
                                for nt in range(4):
                                    D = c - 4 * nt
                                    if D < 0:
                                        continue
                                    nk = 128 if nt < 3 else 127
                                    masks = [(C.ident_bf, C.ident_bf[:, 0:nk], C.Mk[D], C.Mk[D][:, :])] if D <= 4 else []
                                    kts.append((nt, nk, masks))
                                kp, V_ = [(KC[g], 64)], VC[g]
                            elif br == 1:
                                kts = []
                                for kt in range(4 * c + 4):
                                    masks = [(Ebig, Ebig[:, kt * 128:(kt + 1) * 128], negselT, negselT[:, qs])]
                                    if kt >= 4 * c:
                                        m_ = C.Mc[kt - 4 * c]
                                        masks.append((C.ident_bf, C.ident_bf[:, :], m_, m_[:, :]))
                                    kts.append((kt, 128, masks))
                                kp, V_ = [(ksT, 64)], VS
                            else:
                                kts = []
                                for jj in range(-4, 4):
                                    kt = 4 * c + jj
                                    if kt < 0:
                                        continue
                                    m_ = C.Mw[-jj] if jj < 0 else C.Mc[jj]
                                    kts.append((kt, 128, [(C.ident_bf, C.ident_bf[:, :], m_, m_[:, :])]))
                                kp, V_ = [(kwT, 64)], VW
                            ot = ots[bank % 2]
                            pi = attn_chunk(S, C, c, bank, kp, [(QT, 64)], V_, 64, kts, SC, pts, ot, rden, pi)
                            bank += 1
                            pg = S.ps4()
                            mm(S, pg, pg[0:64, :], Sel48, Sel48[:, head * 3 + br, :], gates, gates[:, qs], True, True)
                            if br == 0:
                                S.dve(lambda ot=ot, pg=pg: nc.vector.tensor_tensor(out=acc[:], in0=ot[:], in1=pg[0:64, :], op=ALU.mult),
                                      r=[ot, pg], w=[acc])
                            else:
                                S.dve(lambda ot=ot, pg=pg: nc.vector.tensor_tensor(out=tmpo[:], in0=ot[:], in1=pg[0:64, :], op=ALU.mult),
                                      r=[ot, pg], w=[tmpo])
                                S.pool(lambda: nc.gpsimd.tensor_tensor(out=acc[:], in0=acc[:], in1=tmpo[:], op=ALU.add),
                                       r=[acc, tmpo], w=[acc])
                        ob = obf[c % 2]
                        S.act(lambda ob=ob: nc.scalar.copy(out=ob[:], in_=acc[:]), r=[acc], w=[ob])
                        S.dma("sp", oT_dram[head * 64:(head + 1) * 64, qs], ob[:], r=[ob], w=[oT_dram])
            S.barrier()
        S.barrier()


W_NAMES = ["ev_w_in", "ev_w_uq", "ev_w_ukv", "ev_w_out", "ev_ff_gate", "ev_ff_up", "ev_ff_down",
           "od_w_in", "od_cmp_k1", "od_cmp_k2", "od_cmp_v1", "od_cmp_v2", "od_w_out", "od_router",
           "od_moe_w1", "od_moe_w3", "od_moe_w2"]
L_NAMES = ["xT", "cT", "ada_w", "ada_bT", "norm_gT", "final_gT", "pos64", "inv_freq2", "rope_sign", "ev_q_normT",
           "ev_kv_normT", "ev_conv_wT", "ev_a_log_b", "ev_dt_bias_b", "ev_dn_normT", "od_cmp_pos_kT", "od_cmp_pos_vT",
           "od_router_b_b"]


def build_program(n_layers=4):
    nc = bass.Bass("TRN2", target_bir_lowering=False)
    with ExitStack() as es:
        S = Sched(nc, es, ext_in=W_NAMES + L_NAMES, ext_out=["outT"])
        C = Ctx()
        S.init_psum()
        setup_consts(S, C)
        setup_attn_consts(S, C)
        phase_ada(S, C, 4)
        xT = S.dram("xT", [1024, 8192], F32)
        XA = S.dram("XA", [1024, 8192], F32)
        XB = S.dram("XB", [1024, 8192], F32)
        PT = S.dram("PT", [2608, 8192], F32)
        OT = S.dram("OT", [1024, 8192], BF16)
        VSd = S.dram("VS", [8192, 256], F32)
        VWd = S.dram("VW", [8192, 256], F32)
        outT = S.dram("outT", [1024, 8192], F32)
        ev_w_in = S.dram("ev_w_in", [2, 1024, 2504], F32)
        ev_w_out = S.dram("ev_w_out", [2, 1024, 1024], F32)
        wg = S.dram("ev_ff_gate", [2, 1024, 2816], F32)
        wu = S.dram("ev_ff_up", [2, 1024, 2816], F32)
        wd = S.dram("ev_ff_down", [2, 2816, 1024], F32)
        od_w_in = S.dram("od_w_in", [2, 1024, 2608], F32)
        od_w_out = S.dram("od_w_out", [2, 1024, 1024], F32)
        rt = S.dram("od_router", [2, 1024, 8], F32)
        rtb = S.dram("od_router_b_b", [2, 128, 8], F32)
        m1 = S.dram("od_moe_w1", [2, 8, 1024, 3584], F32)
        m3 = S.dram("od_moe_w3", [2, 8, 1024, 3584], F32)
        m2 = S.dram("od_moe_w2", [2, 8, 3584, 1024], F32)
        x_cur = xT
        for layer in range(n_layers):
            j = layer // 2
            ls = layer * 2
            if layer % 2 == 0:
                phase_inproj(S, C, x_cur, ls, ev_w_in, ev_w_in[j], 2504, PT)
                phase_dn(S, C, PT, j, OT)
                phase_mla(S, C, PT, j, OT)
                phase_outproj(S, C, x_cur, ls, ev_w_out, ev_w_out[j], OT, XA)
                phase_ffn(S, C, XA, ls + 1, XB, 2816, 1, wg, lambda e, j=j: wg[j], wu, lambda e, j=j: wu[j],
                          wd, lambda e, j=j: wd[j], TB=1024)
            else:
                phase_inproj(S, C, x_cur, ls, od_w_in, od_w_in[j], 2608, PT, tok_major=[(1792, 256, VSd), (2304, 256, VWd)])
                phase_nsa(S, C, PT, VSd, VWd, j, OT)
                phase_outproj(S, C, x_cur, ls, od_w_out, od_w_out[j], OT, XA)
                phase_ffn(S, C, XA, ls + 1, XB, 3584, 8, m1, lambda e, j=j: m1[j, e], m3, lambda e, j=j: m3[j, e],
                          m2, lambda e, j=j: m2[j, e], router=(rt, rt[j], rtb, rtb[j]), TB=1024)
            x_cur = XB
        phase_final(S, C, x_cur, outT)
        S.finish()
    return nc, S


def kernel(**inputs):
    f = np.float32
    nc, _ = build_program()
    shared = {k: np.ascontiguousarray(np.asarray(inputs[k], f)) for k in W_NAMES}
    in_maps = []
    for core in range(8):
        b = core % 4
        m = common_inputs(inputs, b)
        m["od_router_b_b"] = np.ascontiguousarray(np.broadcast_to(np.asarray(inputs["od_router_b"], f)[:, None, :], (2, 128, 8)))
        d = dict(shared)
        for k in L_NAMES:
            d[k] = m[k]
        in_maps.append(d)
    res = run_bass_kernel_spmd(nc, in_maps, core_ids=list(range(8)))
    out = np.stack([np.ascontiguousarray(res.results[b]["outT"].T) for b in range(4)], axis=0)
    return out.astype(np.float32)
```

```python
import numpy as np
from contextlib import ExitStack
import concourse.bass as bass
import concourse.mybir as mybir
from concourse.bass_utils import run_bass_kernel_spmd

F32 = mybir.dt.float32
BF16 = mybir.dt.bfloat16
I32 = mybir.dt.int32
ALU = mybir.AluOpType
AF = mybir.ActivationFunctionType
AX = mybir.AxisListType

S_LEN = 8192
DM = 1024
NB = S_LEN // 512
NT = S_LEN // 128
EPS = 1e-6
NEG = -30000.0
SES = True


class Buf:
    __slots__ = ("t", "name", "lw", "rd")

    def __init__(self, t, name=""):
        self.t = t
        self.name = name
        self.lw = None
        self.rd = {}

    def __getitem__(self, i):
        return self.t[i]


class Sched:
    def __init__(self, nc, es, ext_in=(), ext_out=()):
        self.nc = nc
        self.es = es
        self.ext_in = set(ext_in)
        self.ext_out = set(ext_out)
        self.eng = {"pe": nc.tensor, "act": nc.scalar, "dve": nc.vector, "pool": nc.gpsimd, "sp": nc.sync}
        self.sems = {}
        self.cnt = {}
        for e in self.eng:
            self.sems[e] = es.enter_context(nc.semaphore("s_" + e))
            self.cnt[e] = 0
        self.waited = {e: {} for e in self.eng}
        self.lanes = {"sp": 12, "pool": 8, "act": 4}
        self.lane_rr = {q: 0 for q in self.lanes}
        for q, n in self.lanes.items():
            for i in range(n):
                k = f"{q}{i}"
                self.sems[k] = es.enter_context(nc.semaphore("l_" + k))
                self.cnt[k] = 0
        self.psb = []
        self.ps_rr = 0
        self.ps4_rr = 0
        self.uid = 0
        self.dram_bufs = {}

    def sb(self, es, shape, dt, name=None):
        self.uid += 1
        name = (name or "t") + f"_{self.uid}"
        return Buf(es.enter_context(self.nc.sbuf_tensor(name, list(shape), dt)), name)

    def dram(self, name, shape, dt):
        if name in self.dram_bufs:
            return self.dram_bufs[name]
        kind = "ExternalInput" if name in self.ext_in else ("ExternalOutput" if name in self.ext_out else "Internal")
        b = Buf(self.nc.dram_tensor(name, list(shape), dt, kind=kind).ap(), name)
        self.dram_bufs[name] = b
        return b

    def init_psum(self):
        for i in range(8):
            self.psb.append(Buf(self.es.enter_context(self.nc.psum_tensor(f"ps{i}", [128, 512], F32)), f"ps{i}"))

    def ps(self):
        b = self.psb[self.ps_rr]
        self.ps_rr = (self.ps_rr + 1) % 8
        return b

    def ps4(self):
        b = self.psb[self.ps4_rr]
        self.ps4_rr = (self.ps4_rr + 1) % 4
        return b

    def _wait(self, e, key, val):
        if val <= 0 or self.waited[e].get(key, 0) >= val:
            return
        self.eng[e].wait_ge(self.sems[key], val)
        self.waited[e][key] = val

    def _deps(self, e, r, w, is_dma=False):
        deps = {}
        for b in r:
            if b.lw is not None:
                k, v = b.lw
                if deps.get(k, 0) < v:
                    deps[k] = v
        for b in w:
            if b.lw is not None:
                k, v = b.lw
                if deps.get(k, 0) < v:
                    deps[k] = v
            for k, v in b.rd.items():
                if deps.get(k, 0) < v:
                    deps[k] = v
        for k, v in deps.items():
            if k == e and not is_dma and not (SES and e != "pe"):
                continue
            self._wait(e, k, v)

    def _commit(self, ev, r, w):
        k, v = ev
        for b in w:
            b.lw = ev
            b.rd = {}
        for b in r:
            if b.rd.get(k, 0) < v:
                b.rd[k] = v

    def op(self, e, fn, r=(), w=()):
        self._deps(e, r, w)
        ins = fn()
        self.cnt[e] += 1
        ins.then_inc(self.sems[e], 1)
        self._commit((e, self.cnt[e]), r, w)

    def pe(self, fn, r=(), w=()):
        self.op("pe", fn, r, w)

    def act(self, fn, r=(), w=()):
        self.op("act", fn, r, w)

    def dve(self, fn, r=(), w=()):
        self.op("dve", fn, r, w)

    def pool(self, fn, r=(), w=()):
        self.op("pool", fn, r, w)

    def dma(self, q, out, in_, r=(), w=(), **kw):
        self._deps(q, r, w, is_dma=True)
        i = self.lane_rr[q]
        self.lane_rr[q] = (i + 1) % self.lanes[q]
        key = f"{q}{i}"
        c = self.cnt[key]
        self._wait(q, key, 16 * c)
        ins = self.eng[q].dma_start(out=out, in_=in_, **kw)
        ins.then_inc(self.sems[key], 16)
        self.cnt[key] = c + 1
        self._commit((key, 16 * (c + 1)), r, w)

    def barrier(self):
        for e in self.eng:
            for k, c in self.cnt.items():
                if k == e or c == 0:
                    continue
                self._wait(e, k, c if k in self.eng else 16 * c)

    def finish(self):
        for k, c in self.cnt.items():
            if k == "sp" or c == 0:
                continue
            self._wait("sp", k, c if k in self.eng else 16 * c)


def mm(S, out_b, out_ap, l_b, l_ap, r_b, r_ap, start, stop):
    S.pe(lambda: S.nc.tensor.matmul(out_ap, l_ap, r_ap, start=start, stop=stop), r=[l_b, r_b], w=[out_b])


class Ctx:
    pass


def setup_consts(S, C):
    nc = S.nc
    es = S.es
    C.ident = S.sb(es, [128, 128], F32, "ident")
    C.ones_bf = S.sb(es, [128, 128], BF16, "ones_bf")
    C.ones_f = S.sb(es, [128, 128], F32, "ones_f")
    S.pool(lambda: nc.gpsimd.memset(C.ident[:], 0.0), w=[C.ident])
    S.pool(lambda: nc.gpsimd.affine_select(out=C.ident[:], in_=C.ident[:], pattern=[[-1, 128]],
                                           compare_op=ALU.not_equal, fill=1.0, base=0, channel_multiplier=1),
           r=[C.ident], w=[C.ident])
    S.pool(lambda: nc.gpsimd.memset(C.ones_bf[:], 1.0), w=[C.ones_bf])
    S.pool(lambda: nc.gpsimd.memset(C.ones_f[:], 1.0), w=[C.ones_f])
    C.eps = S.sb(es, [128, 1], F32, "eps")
    S.pool(lambda: nc.gpsimd.memset(C.eps[:], EPS), w=[C.eps])


def phase_ada(S, C, n_layers):
    nc = S.nc
    c_in = S.dram("cT", [128, 8], F32)
    ada_w = S.dram("ada_w", [4, 2, 1024, 3072], F32)
    ada_b = S.dram("ada_bT", [128, 8 * 24], F32)
    norm_g = S.dram("norm_gT", [128, 8 * 8], F32)
    C.mods = S.sb(S.es, [128, 8 * 24], F32, "mods")
    C.modA = S.sb(S.es, [128, 8 * 8], F32, "modA")
    with ExitStack() as es:
        sc = S.sb(es, [128, 8], F32, "sc")
        bb = S.sb(es, [128, 8 * 24], F32, "bb")
        gg = S.sb(es, [128, 8 * 8], F32, "gg")
        wbuf = [S.sb(es, [128, 8, 3072 // 2], F32, "adaw") for _ in range(2)]
        S.dma("sp", sc[:], c_in[:, :], r=[c_in], w=[sc])
        S.dma("sp", bb[:], ada_b[:, :], r=[ada_b], w=[bb])
        S.dma("sp", gg[:], norm_g[:, :], r=[norm_g], w=[gg])
        S.act(lambda: nc.scalar.activation(out=sc[:], in_=sc[:], func=AF.Silu), r=[sc], w=[sc])
        it = 0
        for l in range(n_layers):
            for s in range(2):
                ls = l * 2 + s
                for half in range(2):
                    wb = wbuf[it % 2]
                    it += 1
                    src = ada_w[l, s, :, half * 1536:(half + 1) * 1536].rearrange("(kc p) n -> p kc n", p=128)
                    for kc in range(8):
                        S.dma("sp", wb[:, kc, :], src[:, kc, :], r=[ada_w], w=[wb])
                    ps = S.ps()
                    for fc in range(12):
                        for kc in range(8):
                            mm(S, ps, ps[:, fc:fc + 1], wb, wb[:, kc, fc * 128:(fc + 1) * 128], sc, sc[:, kc:kc + 1],
                               kc == 0, kc == 7)
                    c0 = ls * 24 + half * 12
                    S.dve(lambda ps=ps, c0=c0: nc.vector.tensor_tensor(out=C.mods[:, c0:c0 + 12], in0=ps[:, 0:12],
                                                                       in1=bb[:, c0:c0 + 12], op=ALU.add),
                          r=[ps, bb], w=[C.mods])
                S.dve(lambda ls=ls: nc.vector.scalar_tensor_tensor(
                    out=C.modA[:, ls * 8:(ls + 1) * 8], in0=C.mods[:, ls * 24 + 8:ls * 24 + 16], scalar=1.0,
                    in1=gg[:, ls * 8:(ls + 1) * 8], op0=ALU.add, op1=ALU.mult), r=[C.mods, gg], w=[C.modA])
        S.barrier()


def modnorm_block(S, C, es_tiles, x_dram, ls, t0, xt, hT, hcol0, want_f32=None):
    nc = S.nc
    sq, rstd, tmp = es_tiles
    src = x_dram[:, t0:t0 + 512].rearrange("(kc p) t -> p kc t", p=128)
    S.dma("sp", xt[:, :, hcol0:hcol0 + 512], src, r=[x_dram], w=[xt])
    for kc in range(8):
        S.pool(lambda kc=kc: nc.gpsimd.tensor_tensor(out=sq[:, kc, :], in0=xt[:, kc, hcol0:hcol0 + 512],
                                                     in1=xt[:, kc, hcol0:hcol0 + 512], op=ALU.mult), r=[xt], w=[sq])
    ps = S.ps()
    for kc in range(8):
        mm(S, ps, ps[:, :], C.ones_bf, C.ones_bf[:, :], sq, sq[:, kc, :], kc == 0, kc == 7)
    S.act(lambda: nc.scalar.activation(out=rstd[:], in_=ps[:, :], func=AF.Sqrt, scale=1.0 / DM, bias=C.eps[:, 0:1]),
          r=[ps, C.eps], w=[rstd])
    S.dve(lambda: nc.vector.reciprocal(out=rstd[:], in_=rstd[:]), r=[rstd], w=[rstd])
    for kc in range(8):
        S.dve(lambda kc=kc: nc.vector.tensor_tensor(out=tmp[:], in0=xt[:, kc, hcol0:hcol0 + 512], in1=rstd[:],
                                                    op=ALU.mult), r=[xt, rstd], w=[tmp])
        a_ap = C.modA[:, ls * 8 + kc:ls * 8 + kc + 1]
        s_ap = C.mods[:, ls * 24 + kc:ls * 24 + kc + 1]
        S.dve(lambda kc=kc, a_ap=a_ap, s_ap=s_ap: nc.vector.tensor_scalar(
            out=hT[:, kc, hcol0:hcol0 + 512], in0=tmp[:], scalar1=a_ap, scalar2=s_ap, op0=ALU.mult, op1=ALU.add),
            r=[tmp, C.modA, C.mods], w=[hT])
        if want_f32 is not None:
            S.dve(lambda kc=kc, a_ap=a_ap, s_ap=s_ap: nc.vector.tensor_scalar(
                out=want_f32[:, kc, 0:512], in0=tmp[:], scalar1=a_ap, scalar2=s_ap, op0=ALU.mult,
                op1=ALU.add), r=[tmp, C.modA, C.mods], w=[want_f32])


def load_w_bf16(S, wt, w_ap, w_dram, K, N, n0=0, ncols=None):
    ncols = ncols or N
    src = w_ap[:, n0:n0 + ncols].rearrange("(kc p) n -> p kc n", p=128)
    for kc in range(K // 128):
        S.dma("pool", wt[:, kc, 0:ncols], src[:, kc, :], r=[w_dram], w=[wt])


def phase_inproj(S, C, x_dram, ls, w_dram, w_ap, ncols, pT_dram, tok_major=()):
    nc = S.nc
    n_ot = (ncols + 127) // 128
    with ExitStack() as es:
        wt = S.sb(es, [128, 8, ncols], BF16, "w_in")
        load_w_bf16(S, wt, w_ap, w_dram, DM, ncols)
        xts = [S.sb(es, [128, 8, 512], F32, "xt") for _ in range(2)]
        hTs = [S.sb(es, [128, 8, 512], BF16, "hT") for _ in range(2)]
        sq = S.sb(es, [128, 8, 512], BF16, "sq")
        rstd = S.sb(es, [128, 512], F32, "rstd")
        tmp = S.sb(es, [128, 512], F32, "tmp")
        stg = [S.sb(es, [128, 512], F32, "stg") for _ in range(4)]
        si = 0
        for tb in range(NB):
            xt = xts[tb % 2]
            hT = hTs[tb % 2]
            modnorm_block(S, C, (sq, rstd, tmp), x_dram, ls, tb * 512, xt, hT, 0)
            for ot in range(n_ot):
                m = min(128, ncols - ot * 128)
                ps = S.ps()
                for kc in range(8):
                    mm(S, ps, ps[0:m, :], wt, wt[:, kc, ot * 128:ot * 128 + m], hT, hT[:, kc, :], kc == 0, kc == 7)
                st = stg[si % 4]
                si += 1
                if ot % 2 == 0:
                    S.act(lambda st=st, ps=ps, m=m: nc.scalar.copy(out=st[0:m, :], in_=ps[0:m, :]), r=[ps], w=[st])
                else:
                    S.dve(lambda st=st, ps=ps, m=m: nc.vector.tensor_copy(out=st[0:m, :], in_=ps[0:m, :]), r=[ps], w=[st])
                S.dma("sp", pT_dram[ot * 128:ot * 128 + m, tb * 512:(tb + 1) * 512], st[0:m, :], r=[st], w=[pT_dram])
            for (c0, n, dst) in tok_major:
                for tt in range(4):
                    ps = S.ps()
                    for kc in range(8):
                        mm(S, ps, ps[:, 0:n], hT, hT[:, kc, tt * 128:(tt + 1) * 128], wt, wt[:, kc, c0:c0 + n],
                           kc == 0, kc == 7)
                    st = stg[si % 4]
                    si += 1
                    S.act(lambda st=st, ps=ps, n=n: nc.scalar.copy(out=st[:, 0:n], in_=ps[:, 0:n]), r=[ps], w=[st])
                    r0 = tb * 512 + tt * 128
                    S.dma("sp", dst[r0:r0 + 128, :], st[:, 0:n], r=[st], w=[dst])
        S.barrier()


def phase_outproj(S, C, x_dram, ls, w_dram, w_ap, oT_dram, xo_dram):
    nc = S.nc
    with ExitStack() as es:
        wt = S.sb(es, [128, 8, DM], BF16, "w_out")
        load_w_bf16(S, wt, w_ap, w_dram, DM, DM)
        xts = [S.sb(es, [128, 8, 512], F32, "xt") for _ in range(2)]
        ots = [S.sb(es, [128, 8, 512], BF16, "oT") for _ in range(2)]
        for tb in range(NB):
            xt = xts[tb % 2]
            ot_ = ots[tb % 2]
            sl = slice(tb * 512, (tb + 1) * 512)
            S.dma("sp", xt[:], x_dram[:, sl].rearrange("(kc p) t -> p kc t", p=128), r=[x_dram], w=[xt])
            S.dma("sp", ot_[:], oT_dram[:, sl].rearrange("(kc p) t -> p kc t", p=128), r=[oT_dram], w=[ot_])
            for oc in range(8):
                ps = S.ps()
                for kc in range(8):
                    mm(S, ps, ps[:, :], wt, wt[:, kc, oc * 128:(oc + 1) * 128], ot_, ot_[:, kc, :], kc == 0, kc == 7)
                g_ap = C.mods[:, ls * 24 + 16 + oc:ls * 24 + 17 + oc]
                S.dve(lambda ps=ps, oc=oc, g_ap=g_ap, xt=xt: nc.vector.scalar_tensor_tensor(
                    out=xt[:, oc, :], in0=ps[:, :], scalar=g_ap, in1=xt[:, oc, :], op0=ALU.mult, op1=ALU.add),
                    r=[ps, C.mods, xt], w=[xt])
            S.dma("sp", xo_dram[:, sl].rearrange("(kc p) t -> p kc t", p=128), xt[:], r=[xt], w=[xo_dram])
        S.barrier()


def phase_ffn(S, C, x_dram, ls, xo_dram, F, n_exp, wg_dram, wg_ap, wu_dram, wu_ap, wd_dram, wd_ap,
              router=None, TB=2048, CT=4):
    nc = S.nc
    n_ft = F // 128
    chunks = [(c0, min(CT, n_ft - c0)) for c0 in range(0, n_ft, CT)]
    nsub = TB // 512
    with ExitStack() as es:
        xt = S.sb(es, [128, 8, TB], F32, "xt")
        hT = S.sb(es, [128, 8, TB], BF16, "hT")
        sq = S.sb(es, [128, 8, 512], BF16, "sq")
        rstd = S.sb(es, [128, 512], F32, "rstd")
        tmp = S.sb(es, [128, 512], F32, "tmp")
        wgs = [S.sb(es, [128, 8, CT * 128], BF16, "wg") for _ in range(2)]
        wus = [S.sb(es, [128, 8, CT * 128], BF16, "wu") for _ in range(2)]
        wds = [S.sb(es, [128, CT, DM], BF16, "wd") for _ in range(2)]
        acts = [S.sb(es, [128, CT, 512], BF16, "act") for _ in range(2)]
        sgs = [S.sb(es, [128, 512], F32, "sg") for _ in range(2)]
        ytm = [S.sb(es, [128, 512], F32, "ytm") for _ in range(2)]
        if router is not None:
            hF = S.sb(es, [128, 8, 512], F32, "hF")
            wr = S.sb(es, [128, 8, 8], F32, "wr")
            rb = S.sb(es, [128, 8], F32, "rb")
            lg = S.sb(es, [128, 8], F32, "lg")
            mx = S.sb(es, [128, 8], F32, "mx")
            ex = S.sb(es, [128, 8], F32, "ex")
            den = S.sb(es, [128, 2], F32, "den")
            gw = S.sb(es, [128, 8], F32, "gw")
            gwT = S.sb(es, [8, TB], F32, "gwT")
            sel = S.sb(es, [8, 8 * 128], F32, "sel")
            Gs = [S.sb(es, [128, 512], F32, "G") for _ in range(nsub)]
            r_dram, r_ap, rb_dram, rb_ap = router
            S.dma("sp", wr[:], r_ap.rearrange("(kc p) n -> p kc n", p=128), r=[r_dram], w=[wr])
            S.dma("sp", rb[:], rb_ap, r=[rb_dram], w=[rb])
            S.pool(lambda: nc.gpsimd.memset(sel[:], 0.0), w=[sel])
            S.pool(lambda: nc.gpsimd.affine_select(out=sel[:].rearrange("k (e m) -> k e m", m=128),
                                                   in_=sel[:].rearrange("k (e m) -> k e m", m=128),
                                                   pattern=[[-1, 8], [0, 128]], compare_op=ALU.not_equal, fill=1.0,
                                                   base=0, channel_multiplier=1), r=[sel], w=[sel])
        wi = 0
        gi = 0
        ai = 0
        for tb in range(S_LEN // TB):
            for sub in range(nsub):
                modnorm_block(S, C, (sq, rstd, tmp), x_dram, ls, tb * TB + sub * 512, xt, hT, sub * 512,
                              want_f32=(hF if router is not None else None))
                if router is not None:
                    for tt in range(4):
                        ps = S.ps()
                        for kc in range(8):
                            mm(S, ps, ps[:, 0:8], hF, hF[:, kc, tt * 128:(tt + 1) * 128], wr, wr[:, kc, :], kc == 0, kc == 7)
                        S.dve(lambda ps=ps: nc.vector.tensor_tensor(out=lg[:], in0=ps[:, 0:8], in1=rb[:], op=ALU.add),
                              r=[ps, rb], w=[lg])
                        S.dve(lambda: nc.vector.max(out=mx[:], in_=lg[:]), r=[lg], w=[mx])
                        S.dve(lambda: nc.vector.tensor_scalar(out=ex[:], in0=lg[:], scalar1=mx[:, 0:1], scalar2=None,
                                                              op0=ALU.subtract), r=[lg, mx], w=[ex])
                        S.act(lambda: nc.scalar.activation(out=ex[:], in_=ex[:], func=AF.Exp), r=[ex], w=[ex])
                        S.dve(lambda: nc.vector.tensor_scalar(out=gw[:], in0=lg[:], scalar1=mx[:, 1:2], scalar2=None,
                                                              op0=ALU.is_ge), r=[lg, mx], w=[gw])
                        S.dve(lambda: nc.vector.tensor_tensor(out=gw[:], in0=gw[:], in1=ex[:], op=ALU.mult),
                              r=[gw, ex], w=[gw])
                        S.dve(lambda: nc.vector.reduce_sum(out=den[:, 0:1], in_=gw[:], axis=AX.X), r=[gw], w=[den])
                        S.dve(lambda: nc.vector.reciprocal(out=den[:, 1:2], in_=den[:, 0:1]), r=[den], w=[den])
                        S.dve(lambda: nc.vector.tensor_scalar(out=gw[:], in0=gw[:], scalar1=den[:, 1:2], scalar2=None,
                                                              op0=ALU.mult), r=[gw, den], w=[gw])
                        pt = S.ps()
                        S.pe(lambda pt=pt: nc.tensor.transpose(out=pt[0:8, 0:128], in_=gw[:, :], identity=C.ident[:, :]),
                             r=[gw, C.ident], w=[pt])
                        c0 = sub * 512 + tt * 128
                        S.act(lambda pt=pt, c0=c0: nc.scalar.copy(out=gwT[:, c0:c0 + 128], in_=pt[0:8, 0:128]),
                              r=[pt], w=[gwT])
            for e in range(n_exp):
                for (c0, ct) in chunks:
                    wg = wgs[wi % 2]
                    wu = wus[wi % 2]
                    wd = wds[wi % 2]
                    wi += 1
                    load_w_bf16(S, wg, wg_ap(e), wg_dram, DM, F, n0=c0 * 128, ncols=ct * 128)
                    load_w_bf16(S, wu, wu_ap(e), wu_dram, DM, F, n0=c0 * 128, ncols=ct * 128)
                    S.dma("pool", wd[:, 0:ct, :], wd_ap(e)[c0 * 128:(c0 + ct) * 128, :].rearrange("(c p) n -> p c n", p=128),
                          r=[wd_dram], w=[wd])
                    for sub in range(nsub):
                        ts = slice(sub * 512, (sub + 1) * 512)
                        G = None
                        if router is not None:
                            G = Gs[sub]
                        if router is not None and c0 == 0:
                            pg = S.ps()
                            mm(S, pg, pg[:, :], sel, sel[:, e * 128:(e + 1) * 128], gwT, gwT[:, ts], True, True)
                            S.act(lambda G=G, pg=pg: nc.scalar.copy(out=G[:], in_=pg[:, :]), r=[pg], w=[G])
                        act_t = acts[ai % 2]
                        ai += 1
                        for f in range(ct):
                            pg = S.ps()
                            pu = S.ps()
                            for kc in range(8):
                                mm(S, pg, pg[:, :], wg, wg[:, kc, f * 128:(f + 1) * 128], hT, hT[:, kc, ts], kc == 0, kc == 7)
                            for kc in range(8):
                                mm(S, pu, pu[:, :], wu, wu[:, kc, f * 128:(f + 1) * 128], hT, hT[:, kc, ts], kc == 0, kc == 7)
                            sg = sgs[f % 2]
                            S.act(lambda sg=sg, pg=pg: nc.scalar.activation(out=sg[:], in_=pg[:, :], func=AF.Silu),
                                  r=[pg], w=[sg])
                            S.dve(lambda sg=sg, pu=pu, f=f, act_t=act_t: nc.vector.tensor_tensor(
                                out=act_t[:, f, :], in0=sg[:], in1=pu[:, :], op=ALU.mult), r=[sg, pu], w=[act_t])
                        for oc in range(8):
                            py = S.ps()
                            for f in range(ct):
                                mm(S, py, py[:, :], wd, wd[:, f, oc * 128:(oc + 1) * 128], act_t, act_t[:, f, :],
                                   f == 0, f == ct - 1)
                            g_ap = C.mods[:, ls * 24 + 16 + oc:ls * 24 + 17 + oc]
                            if G is None:
                                S.dve(lambda py=py, oc=oc, g_ap=g_ap, ts=ts: nc.vector.scalar_tensor_tensor(
                                    out=xt[:, oc, ts], in0=py[:, :], scalar=g_ap, in1=xt[:, oc, ts], op0=ALU.mult,
                                    op1=ALU.add), r=[py, C.mods, xt], w=[xt])
                            else:
                                yt = ytm[oc % 2]
                                S.dve(lambda py=py, yt=yt, G=G: nc.vector.tensor_tensor(out=yt[:], in0=py[:, :], in1=G[:],
                                                                                        op=ALU.mult), r=[py, G], w=[yt])
                                S.dve(lambda yt=yt, oc=oc, g_ap=g_ap, ts=ts: nc.vector.scalar_tensor_tensor(
                                    out=xt[:, oc, ts], in0=yt[:], scalar=g_ap, in1=xt[:, oc, ts], op0=ALU.mult,
                                    op1=ALU.add), r=[yt, C.mods, xt], w=[xt])
            for kc in range(8):
                S.dma("sp", xo_dram[kc * 128:(kc + 1) * 128, tb * TB:(tb + 1) * TB], xt[:, kc, :], r=[xt], w=[xo_dram])
        S.barrier()


def phase_final(S, C, x_dram, out_dram):
    nc = S.nc
    fg = S.dram("final_gT", [128, 8], F32)
    with ExitStack() as es:
        g = S.sb(es, [128, 8], F32, "fg")
        S.dma("sp", g[:], fg[:, :], r=[fg], w=[g])
        xts = [S.sb(es, [128, 8, 512], F32, "xt") for _ in range(2)]
        sq = S.sb(es, [128, 8, 512], BF16, "sq")
        rstd = S.sb(es, [128, 512], F32, "rstd")
        for tb in range(NB):
            xt = xts[tb % 2]
            sl = slice(tb * 512, (tb + 1) * 512)
            S.dma("sp", xt[:], x_dram[:, sl].rearrange("(kc p) t -> p kc t", p=128), r=[x_dram], w=[xt])
            for kc in range(8):
                S.pool(lambda kc=kc, xt=xt: nc.gpsimd.tensor_tensor(out=sq[:, kc, :], in0=xt[:, kc, :], in1=xt[:, kc, :],
                                                                    op=ALU.mult), r=[xt], w=[sq])
            ps = S.ps()
            for kc in range(8):
                mm(S, ps, ps[:, :], C.ones_bf, C.ones_bf[:, :], sq, sq[:, kc, :], kc == 0, kc == 7)
            S.act(lambda ps=ps: nc.scalar.activation(out=rstd[:], in_=ps[:, :], func=AF.Sqrt, scale=1.0 / DM,
                                                     bias=C.eps[:, 0:1]), r=[ps, C.eps], w=[rstd])
            S.dve(lambda: nc.vector.reciprocal(out=rstd[:], in_=rstd[:]), r=[rstd], w=[rstd])
            for kc in range(8):
                S.dve(lambda kc=kc, xt=xt: nc.vector.scalar_tensor_tensor(
                    out=xt[:, kc, :], in0=xt[:, kc, :], scalar=g[:, kc:kc + 1], in1=rstd[:], op0=ALU.mult, op1=ALU.mult),
                    r=[xt, g, rstd], w=[xt])
            S.dma("sp", out_dram[:, sl].rearrange("(kc p) t -> p kc t", p=128), xt[:], r=[xt], w=[out_dram])
        S.barrier()


def common_inputs(inputs, b):
    f = np.float32
    m = {}
    m["xT"] = np.ascontiguousarray(np.asarray(inputs["x"][b], f).T)
    m["cT"] = np.ascontiguousarray(np.asarray(inputs["c"][b], f).reshape(8, 128).T)
    m["ada_w"] = np.asarray(inputs["ada_w"], f)
    m["ada_bT"] = np.ascontiguousarray(np.asarray(inputs["ada_b"], f).reshape(8, 24, 128).transpose(2, 0, 1).reshape(128, 192))
    m["norm_gT"] = np.ascontiguousarray(np.asarray(inputs["norm_g"], f).reshape(8, 8, 128).transpose(2, 0, 1).reshape(128, 64))
    m["final_gT"] = np.ascontiguousarray(np.asarray(inputs["final_g"], f).reshape(8, 128).T)
    m["pos64"] = np.ascontiguousarray(np.broadcast_to(np.asarray(inputs["positions"][b], np.int32)[None, :], (64, 8192)))
    inv = (1.0 / (10000.0 ** (np.arange(0, 64, 2, dtype=np.float32) / 64))).astype(f)
    m["inv_freq2"] = np.concatenate([inv, inv]).reshape(64, 1).astype(f)
    m["rope_sign"] = np.concatenate([-np.ones(32, f), np.ones(32, f)]).reshape(64, 1)
    m["ev_q_normT"] = np.ascontiguousarray(np.asarray(inputs["ev_q_norm"], f).reshape(2, 2, 128).transpose(0, 2, 1))
    m["ev_kv_normT"] = np.ascontiguousarray(np.asarray(inputs["ev_kv_norm"], f).reshape(2, 128, 1))
    m["ev_conv_wT"] = np.ascontiguousarray(np.asarray(inputs["ev_conv_w"], f).reshape(2, 4, 12, 128).transpose(0, 3, 2, 1))
    m["ev_a_log_b"] = np.ascontiguousarray(np.broadcast_to(np.asarray(inputs["ev_a_log"], f)[:, None, :], (2, 128, 4)))
    m["ev_dt_bias_b"] = np.ascontiguousarray(np.broadcast_to(np.asarray(inputs["ev_dt_bias"], f)[:, None, :], (2, 128, 4)))
    m["ev_dn_normT"] = np.ascontiguousarray(np.asarray(inputs["ev_dn_norm"], f).reshape(2, 128, 1))
    m["od_cmp_pos_kT"] = np.ascontiguousarray(np.asarray(inputs["od_cmp_pos_k"], f).transpose(0, 2, 1))
    m["od_cmp_pos_vT"] = np.ascontiguousarray(np.asarray(inputs["od_cmp_pos_v"], f).transpose(0, 2, 1))
    return m


def setup_attn_consts(S, C):
    nc = S.nc
    es = S.es
    C.ident_bf = S.sb(es, [128, 128], BF16, "ident_bf")
    S.dve(lambda: nc.vector.tensor_copy(out=C.ident_bf[:], in_=C.ident[:]), r=[C.ident], w=[C.ident_bf])
    C.tiny = S.sb(es, [128, 1], F32, "tiny")
    S.pool(lambda: nc.gpsimd.memset(C.tiny[:], 1e-30), w=[C.tiny])

    def mask_tile(name, base, cm, step, op=ALU.is_ge):
        t = S.sb(es, [128, 512], BF16, name)
        S.pool(lambda: nc.gpsimd.memset(t[:], 0.0), w=[t])
        S.pool(lambda: nc.gpsimd.affine_select(out=t[:], in_=t[:], pattern=[[step, 512]], compare_op=op, fill=NEG,
                                               base=base, channel_multiplier=cm), r=[t], w=[t])
        return t
    C.Mc = [mask_tile(f"Mc{j}", -128 * j, -1, 1) for j in range(4)]
    C.Mw = [None] + [mask_tile(f"Mw{m}", 511 - 128 * m, 1, -1) for m in range(1, 5)]
    C.Mk = [mask_tile(f"Mk{d}", 512 * d - 31, -16, 1) for d in range(5)]


def attn_chunk(S, C, c, bank, kparts, qparts, V, dv, kts, scale, pts, ot, rden, pi0=0):
    nc = S.nc
    po = S.psb[4 + bank % 2]
    pd = S.psb[6 + bank % 2]
    qs = slice(c * 512, (c + 1) * 512)
    pi = pi0
    for i, (kt, nk, masks) in enumerate(kts):
        pst = S.ps4()
        n_mm = len(kparts) + len(masks)
        j = 0
        for (kb, rows), (qb, _) in zip(kparts, qparts):
            mm(S, pst, pst[0:nk, :], kb, kb[0:rows, kt * 128:kt * 128 + nk], qb, qb[0:rows, qs], j == 0, j == n_mm - 1)
            j += 1
        for (lb, lap, rb, rap) in masks:
            mm(S, pst, pst[0:nk, :], lb, lap, rb, rap, False, j == n_mm - 1)
            j += 1
        pt = pts[pi % len(pts)]
        pi += 1
        S.act(lambda pt=pt, pst=pst, nk=nk: nc.scalar.activation(out=pt[0:nk, :], in_=pst[0:nk, :], func=AF.Exp,
                                                                  scale=scale), r=[pst], w=[pt])
        last = i == len(kts) - 1
        mm(S, po, po[0:dv, :], V, V[0:nk, kt, :], pt, pt[0:nk, :], i == 0, last)
        mm(S, pd, pd[0:dv, :], C.ones_bf, C.ones_bf[0:nk, 0:dv], pt, pt[0:nk, :], i == 0, last)
    S.dve(lambda: nc.vector.tensor_scalar(out=rden[0:dv, :], in0=pd[0:dv, :], scalar1=C.tiny[0:dv, 0:1],
                                          scalar2=None, op0=ALU.max), r=[pd, C.tiny], w=[rden])
    S.dve(lambda: nc.vector.reciprocal(out=rden[0:dv, :], in_=rden[0:dv, :]), r=[rden], w=[rden])
    S.dve(lambda: nc.vector.tensor_tensor(out=ot[0:dv, :], in0=po[0:dv, :], in1=rden[0:dv, :], op=ALU.mult),
          r=[po, rden], w=[ot])
    return pi


def attn_head(S, C, es, kparts, qparts, V, dv, kts_fn, scale, out_cb, pts, otiles, rden):
    pi = 0
    for c in range(16):
        ot = otiles[c % len(otiles)]
        pi = attn_chunk(S, C, c, c, kparts, qparts, V, dv, kts_fn(c), scale, pts, ot, rden, pi)
        out_cb(c, ot)


def phase_mla(S, C, pT, j, oT_dram):
    nc = S.nc
    R_CQ, R_CKV, R_KR = 2056, 2312, 2440
    w_uq = S.dram("ev_w_uq", [2, 256, 768], F32)
    w_ukv = S.dram("ev_w_ukv", [2, 128, 1024], F32)
    qn_d = S.dram("ev_q_normT", [2, 128, 2], F32)
    kvn_d = S.dram("ev_kv_normT", [2, 128, 1], F32)
    pos_d = S.dram("pos64", [64, 8192], I32)
    inv_d = S.dram("inv_freq2", [64, 1], F32)
    sgn_d = S.dram("rope_sign", [64, 1], F32)
    with ExitStack() as es:
        cos2 = S.sb(es, [64, 8192], BF16, "cos2")
        sin2 = S.sb(es, [64, 8192], BF16, "sin2s")
        cqn = S.sb(es, [128, 2, 8192], BF16, "cqn")
        ckvn = S.sb(es, [128, 8192], BF16, "ckvn")
        KR = S.sb(es, [64, 8192], BF16, "KR")
        wq = S.sb(es, [128, 2, 768], BF16, "wq")
        wqs = S.sb(es, [128, 2, 4, 64], BF16, "wqs")
        wkv = S.sb(es, [128, 1024], BF16, "wkv")
        qn = S.sb(es, [128, 2], F32, "qn")
        kvn = S.sb(es, [128, 1], F32, "kvn")
        inv = S.sb(es, [64, 1], F32, "inv")
        sgn = S.sb(es, [64, 1], F32, "sgn")
        negpi = S.sb(es, [64, 1], F32, "negpi")
        S.pool(lambda: nc.gpsimd.memset(negpi[:], -float(np.pi)), w=[negpi])
        S.dma("sp", qn[:], qn_d[j], r=[qn_d], w=[qn])
        S.dma("sp", kvn[:], kvn_d[j], r=[kvn_d], w=[kvn])
        S.dma("sp", inv[:], inv_d[:, :], r=[inv_d], w=[inv])
        S.dma("sp", sgn[:], sgn_d[:, :], r=[sgn_d], w=[sgn])
        for kc in range(2):
            S.dma("pool", wq[:, kc, :], w_uq[j, kc * 128:(kc + 1) * 128, :], r=[w_uq], w=[wq])
            for h in range(4):
                b0 = h * 192 + 128
                S.dma("pool", wqs[:, kc, h, 0:32], w_uq[j, kc * 128:(kc + 1) * 128, b0 + 32:b0 + 64], r=[w_uq], w=[wqs])
                S.dma("pool", wqs[:, kc, h, 32:64], w_uq[j, kc * 128:(kc + 1) * 128, b0:b0 + 32], r=[w_uq], w=[wqs])
        S.dma("pool", wkv[:], w_ukv[j, :, :], r=[w_ukv], w=[wkv])
        with ExitStack() as es2:
            posi = S.sb(es2, [64, 512], I32, "posi")
            ang = S.sb(es2, [64, 512], F32, "ang")
            u = S.sb(es2, [64, 512], F32, "u")
            ni = S.sb(es2, [64, 512], I32, "ni")
            nf = S.sb(es2, [64, 512], F32, "nf")
            cq = S.sb(es2, [128, 2, 512], F32, "cq")
            ckv = S.sb(es2, [128, 512], F32, "ckv")
            kr = S.sb(es2, [64, 512], F32, "kr")
            krs = S.sb(es2, [64, 512], F32, "krs")
            sq = S.sb(es2, [128, 3, 512], BF16, "sq")
            rstd = S.sb(es2, [128, 512], F32, "rstd")
            tmp = S.sb(es2, [128, 512], F32, "tmp")
            for tb in range(NB):
                ts = slice(tb * 512, (tb + 1) * 512)
                S.dma("sp", posi[:], pos_d[:, ts], r=[pos_d], w=[posi])
                S.dve(lambda: nc.vector.tensor_copy(out=ang[:], in_=posi[:]), r=[posi], w=[ang])
                S.dve(lambda: nc.vector.tensor_scalar(out=ang[:], in0=ang[:], scalar1=inv[:, 0:1], scalar2=None,
                                                      op0=ALU.mult), r=[ang, inv], w=[ang])
                for (dst, off, signed) in ((sin2, 0.5, True), (cos2, 0.75, False)):
                    S.dve(lambda off=off: nc.vector.tensor_scalar(out=u[:], in0=ang[:], scalar1=float(1.0 / (2 * np.pi)),
                                                                  scalar2=off, op0=ALU.mult, op1=ALU.add), r=[ang], w=[u])
                    S.dve(lambda: nc.vector.tensor_copy(out=ni[:], in_=u[:]), r=[u], w=[ni])
                    S.dve(lambda: nc.vector.tensor_copy(out=nf[:], in_=ni[:]), r=[ni], w=[nf])
                    S.dve(lambda: nc.vector.tensor_tensor(out=u[:], in0=u[:], in1=nf[:], op=ALU.subtract), r=[u, nf], w=[u])
                    S.dve(lambda: nc.vector.tensor_scalar(out=nf[:], in0=u[:], scalar1=0.0, scalar2=None, op0=ALU.is_lt),
                          r=[u], w=[nf])
                    S.dve(lambda: nc.vector.tensor_tensor(out=u[:], in0=u[:], in1=nf[:], op=ALU.add), r=[u, nf], w=[u])
                    S.act(lambda: nc.scalar.activation(out=u[:], in_=u[:], func=AF.Sin, scale=float(2 * np.pi),
                                                       bias=negpi[:, 0:1]), r=[u, negpi], w=[u])
                    if signed:
                        S.dve(lambda dst=dst, ts=ts: nc.vector.tensor_scalar(out=dst[:, ts], in0=u[:], scalar1=sgn[:, 0:1],
                                                                             scalar2=None, op0=ALU.mult), r=[u, sgn], w=[dst])
                    else:
                        S.dve(lambda dst=dst, ts=ts: nc.vector.tensor_copy(out=dst[:, ts], in_=u[:]), r=[u], w=[dst])
                S.dma("sp", cq[:], pT[R_CQ:R_CQ + 256, ts].rearrange("(kc p) t -> p kc t", p=128), r=[pT], w=[cq])
                S.dma("sp", ckv[:], pT[R_CKV:R_CKV + 128, ts], r=[pT], w=[ckv])
                S.dma("sp", kr[:], pT[R_KR:R_KR + 64, ts], r=[pT], w=[kr])
                S.dma("sp", krs[0:32, :], pT[R_KR + 32:R_KR + 64, ts], r=[pT], w=[krs])
                S.dma("sp", krs[32:64, :], pT[R_KR:R_KR + 32, ts], r=[pT], w=[krs])
                for kc in range(2):
                    S.pool(lambda kc=kc: nc.gpsimd.tensor_tensor(out=sq[:, kc, :], in0=cq[:, kc, :], in1=cq[:, kc, :],
                                                                 op=ALU.mult), r=[cq], w=[sq])
                S.pool(lambda: nc.gpsimd.tensor_tensor(out=sq[:, 2, :], in0=ckv[:], in1=ckv[:], op=ALU.mult), r=[ckv], w=[sq])
                ps = S.ps4()
                for kc in range(2):
                    mm(S, ps, ps[:, :], C.ones_bf, C.ones_bf[:, :], sq, sq[:, kc, :], kc == 0, kc == 1)
                S.act(lambda ps=ps: nc.scalar.activation(out=rstd[:], in_=ps[:, :], func=AF.Sqrt, scale=1.0 / 256,
                                                         bias=C.eps[:, 0:1]), r=[ps, C.eps], w=[rstd])
                S.dve(lambda: nc.vector.reciprocal(out=rstd[:], in_=rstd[:]), r=[rstd], w=[rstd])
                for kc in range(2):
                    S.dve(lambda kc=kc: nc.vector.tensor_tensor(out=tmp[:], in0=cq[:, kc, :], in1=rstd[:], op=ALU.mult),
                          r=[cq, rstd], w=[tmp])
                    S.dve(lambda kc=kc, ts=ts: nc.vector.tensor_scalar(out=cqn[:, kc, ts], in0=tmp[:], scalar1=qn[:, kc:kc + 1],
                                                                       scalar2=None, op0=ALU.mult), r=[tmp, qn], w=[cqn])
                ps = S.ps4()
                mm(S, ps, ps[:, :], C.ones_bf, C.ones_bf[:, :], sq, sq[:, 2, :], True, True)
                S.act(lambda ps=ps: nc.scalar.activation(out=rstd[:], in_=ps[:, :], func=AF.Sqrt, scale=1.0 / 128,
                                                         bias=C.eps[:, 0:1]), r=[ps, C.eps], w=[rstd])
                S.dve(lambda: nc.vector.reciprocal(out=rstd[:], in_=rstd[:]), r=[rstd], w=[rstd])
                S.dve(lambda: nc.vector.tensor_tensor(out=tmp[:], in0=ckv[:], in1=rstd[:], op=ALU.mult), r=[ckv, rstd], w=[tmp])
                S.dve(lambda ts=ts: nc.vector.tensor_scalar(out=ckvn[:, ts], in0=tmp[:], scalar1=kvn[:, 0:1], scalar2=None,
                                                            op0=ALU.mult), r=[tmp, kvn], w=[ckvn])
                S.dve(lambda ts=ts: nc.vector.tensor_tensor(out=kr[:], in0=kr[:], in1=cos2[:, ts], op=ALU.mult), r=[kr, cos2], w=[kr])
                S.dve(lambda ts=ts: nc.vector.tensor_tensor(out=krs[:], in0=krs[:], in1=sin2[:, ts], op=ALU.mult), r=[krs, sin2], w=[krs])
                S.dve(lambda ts=ts: nc.vector.tensor_tensor(out=KR[:, ts], in0=kr[:], in1=krs[:], op=ALU.add), r=[kr, krs], w=[KR])
        S.barrier()
        with ExitStack() as es3:
            QN = S.sb(es3, [128, 8192], BF16, "QN")
            QR = S.sb(es3, [64, 8192], BF16, "QR")
            KN = S.sb(es3, [128, 8192], BF16, "KN")
            V = S.sb(es3, [128, 64, 128], BF16, "V")
            pts = [S.sb(es3, [128, 512], BF16, "pt") for _ in range(3)]
            otiles = [S.sb(es3, [128, 512], BF16, "ot") for _ in range(2)]
            rden = S.sb(es3, [128, 512], F32, "rden")
            t1 = S.sb(es3, [64, 512], F32, "t1")
            t2 = S.sb(es3, [64, 512], F32, "t2")
            for h in range(4):
                for tb in range(NB):
                    ts = slice(tb * 512, (tb + 1) * 512)
                    ps = S.ps4()
                    for kc in range(2):
                        mm(S, ps, ps[:, :], wq, wq[:, kc, h * 192:h * 192 + 128], cqn, cqn[:, kc, ts], kc == 0, kc == 1)
                    S.act(lambda ps=ps, ts=ts: nc.scalar.copy(out=QN[:, ts], in_=ps[:, :]), r=[ps], w=[QN])
                    ps = S.ps4()
                    for kc in range(2):
                        mm(S, ps, ps[0:64, :], wq, wq[:, kc, h * 192 + 128:h * 192 + 192], cqn, cqn[:, kc, ts], kc == 0, kc == 1)
                    ps2 = S.ps4()
                    for kc in range(2):
                        mm(S, ps2, ps2[0:64, :], wqs, wqs[:, kc, h, :], cqn, cqn[:, kc, ts], kc == 0, kc == 1)
                    S.dve(lambda ps=ps, ts=ts: nc.vector.tensor_tensor(out=t1[:], in0=ps[0:64, :], in1=cos2[:, ts], op=ALU.mult),
                          r=[ps, cos2], w=[t1])
                    S.dve(lambda ps2=ps2, ts=ts: nc.vector.tensor_tensor(out=t2[:], in0=ps2[0:64, :], in1=sin2[:, ts], op=ALU.mult),
                          r=[ps2, sin2], w=[t2])
                    S.dve(lambda ts=ts: nc.vector.tensor_tensor(out=QR[:, ts], in0=t1[:], in1=t2[:], op=ALU.add), r=[t1, t2], w=[QR])
                    ps = S.ps4()
                    mm(S, ps, ps[:, :], wkv, wkv[:, h * 256:h * 256 + 128], ckvn, ckvn[:, ts], True, True)
                    S.act(lambda ps=ps, ts=ts: nc.scalar.copy(out=KN[:, ts], in_=ps[:, :]), r=[ps], w=[KN])
                    ps = S.ps4()
                    for tt in range(4):
                        mm(S, ps, ps[:, tt * 128:(tt + 1) * 128], ckvn, ckvn[:, tb * 512 + tt * 128:tb * 512 + (tt + 1) * 128],
                           wkv, wkv[:, h * 256 + 128:h * 256 + 256], True, True)
                    S.act(lambda ps=ps, tb=tb: nc.scalar.copy(out=V[:, tb * 4:(tb + 1) * 4, :],
                                                              in_=ps[:, :].rearrange("p (t d) -> p t d", d=128)), r=[ps], w=[V])

                def kts_fn(c):
                    out = []
                    for kt in range(4 * c + 4):
                        masks = []
                        if kt >= 4 * c:
                            m = C.Mc[kt - 4 * c]
                            masks = [(C.ident_bf, C.ident_bf[:, :], m, m[:, :])]
                        out.append((kt, 128, masks))
                    return out

                def out_cb(c, ot, h=h):
                    S.dma("sp", oT_dram[512 + h * 128:512 + (h + 1) * 128, c * 512:(c + 1) * 512], ot[:, :], r=[ot], w=[oT_dram])

                attn_head(S, C, es3, [(KN, 128), (KR, 64)], [(QN, 128), (QR, 64)], V, 128, kts_fn, 192 ** -0.5, out_cb,
                          pts, otiles, rden)
        S.barrier()


def phase_dn(S, C, pT, j, oT_dram):
    nc = S.nc
    cw_d = S.dram("ev_conv_wT", [2, 128, 12, 4], F32)
    al_d = S.dram("ev_a_log_b", [2, 128, 4], F32)
    dt_d = S.dram("ev_dt_bias_b", [2, 128, 4], F32)
    gn_d = S.dram("ev_dn_normT", [2, 128, 1], F32)
    with ExitStack() as es:
        cw = S.sb(es, [128, 12, 4], F32, "cw")
        al = S.sb(es, [128, 4], F32, "al")
        dtb = S.sb(es, [128, 4], F32, "dtb")
        gn = S.sb(es, [128, 1], F32, "gn")
        S.dma("sp", cw[:], cw_d[j], r=[cw_d], w=[cw])
        S.dma("sp", al[:], al_d[j], r=[al_d], w=[al])
        S.dma("sp", dtb[:], dt_d[j], r=[dt_d], w=[dtb])
        S.dma("sp", gn[:], gn_d[j], r=[gn_d], w=[gn])
        S.act(lambda: nc.scalar.activation(out=al[:], in_=al[:], func=AF.Exp), r=[al], w=[al])
        UT = S.sb(es, [128, 128], F32, "UT")
        S.pool(lambda: nc.gpsimd.memset(UT[:], 1.0), w=[UT])
        S.pool(lambda: nc.gpsimd.affine_select(out=UT[:], in_=UT[:], pattern=[[1, 128]], compare_op=ALU.is_ge, fill=0.0,
                                               base=0, channel_multiplier=-1), r=[UT], w=[UT])
        PM1 = S.sb(es, [128, 128], F32, "PM1")
        S.pool(lambda: nc.gpsimd.memset(PM1[:], 0.0), w=[PM1])
        S.pool(lambda: nc.gpsimd.affine_select(out=PM1[:], in_=PM1[:], pattern=[[-1, 128]], compare_op=ALU.is_gt, fill=1e5,
                                               base=0, channel_multiplier=1), r=[PM1], w=[PM1])
        NM2 = S.sb(es, [128, 128], F32, "NM2")
        S.pool(lambda: nc.gpsimd.memset(NM2[:], 0.0), w=[NM2])
        S.pool(lambda: nc.gpsimd.affine_select(out=NM2[:], in_=NM2[:], pattern=[[1, 128]], compare_op=ALU.is_ge, fill=-1e5,
                                               base=0, channel_multiplier=-1), r=[NM2], w=[NM2])
        braw = S.sb(es, [64, 128], F32, "braw")
        araw = S.sb(es, [64, 128], F32, "araw")
        beta = S.sb(es, [128, 64], F32, "beta")
        g = S.sb(es, [128, 64], F32, "g")
        gc = S.sb(es, [128, 64], F32, "gc")
        ngc = S.sb(es, [128, 64], F32, "ngc")
        glast = S.sb(es, [128, 64], F32, "glast")
        alast = S.sb(es, [128, 64], F32, "alast")
        etail = S.sb(es, [128, 64], F32, "etail")
        bexpg = S.sb(es, [128, 64], F32, "bexpg")
        nbeta = S.sb(es, [128, 64], F32, "nbeta")
        St = S.sb(es, [128, 128], F32, "state")
        NS = 2
        def mk(name, shape=(128, 128), dt=F32):
            return [S.sb(es, list(shape), dt, name) for _ in range(NS)]
        raw = {n: mk("raw" + n, (128, 131)) for n in "qkv"}
        cv = {n: mk("cv" + n) for n in "qkv"}
        rn = mk("rn")
        qT = mk("qT"); kT = mk("kT"); ktok = mk("ktok"); vtok = mk("vtok")
        dgc = mk("dgc"); Dm = mk("Dm"); DiT = mk("DiT"); egr = mk("egr")
        Nm = mk("Nm"); NmT = mk("NmT"); M2 = mk("M2"); M2T = mk("M2T"); RT = mk("RT")
        qkT = mk("qkT"); qgT = mk("qgT"); vb = mk("vb"); kbg = mk("kbg"); ktl = mk("ktl")
        u = mk("u"); wT = mk("wT"); vnew = mk("vnew"); osb = mk("osb"); osq = mk("osq"); zt = mk("zt")
        obf = mk("obf", (128, 128), BF16)

        def evac(dst, ps, eng="act"):
            if eng == "act":
                S.act(lambda: nc.scalar.copy(out=dst[:], in_=ps[:, 0:128]), r=[ps], w=[dst])
            else:
                S.dve(lambda: nc.vector.tensor_copy(out=dst[:], in_=ps[:, 0:128]), r=[ps], w=[dst])

        for h in range(4):
            S.dma("sp", braw[:], pT[2048 + h, :].rearrange("(t p) -> t p", p=128), r=[pT], w=[braw])
            S.dma("sp", araw[:], pT[2052 + h, :].rearrange("(t p) -> t p", p=128), r=[pT], w=[araw])
            ps = S.ps()
            S.pe(lambda ps=ps: nc.tensor.transpose(out=ps[:, 0:64], in_=braw[:, :], identity=C.ident[0:64, 0:64]),
                 r=[braw, C.ident], w=[ps])
            S.act(lambda ps=ps: nc.scalar.activation(out=beta[:], in_=ps[:, 0:64], func=AF.Sigmoid), r=[ps], w=[beta])
            ps = S.ps()
            S.pe(lambda ps=ps: nc.tensor.transpose(out=ps[:, 0:64], in_=araw[:, :], identity=C.ident[0:64, 0:64]),
                 r=[araw, C.ident], w=[ps])
            S.act(lambda ps=ps, h=h: nc.scalar.activation(out=g[:], in_=ps[:, 0:64], func=AF.Exp, bias=dtb[:, h:h + 1]),
                  r=[ps, dtb], w=[g])
            S.dve(lambda: nc.vector.tensor_scalar(out=g[:], in0=g[:], scalar1=1.0, scalar2=None, op0=ALU.add), r=[g], w=[g])
            S.act(lambda: nc.scalar.activation(out=g[:], in_=g[:], func=AF.Ln), r=[g], w=[g])
            S.dve(lambda h=h: nc.vector.tensor_scalar(out=g[:], in0=g[:], scalar1=al[:, h:h + 1], scalar2=-1.0, op0=ALU.mult,
                                                      op1=ALU.mult), r=[g, al], w=[g])
            ps = S.ps()
            mm(S, ps, ps[:, 0:64], UT, UT[:, :], g, g[:, :], True, True)
            S.act(lambda ps=ps: nc.scalar.copy(out=gc[:], in_=ps[:, 0:64]), r=[ps], w=[gc])
            S.dve(lambda: nc.vector.tensor_scalar(out=ngc[:], in0=gc[:], scalar1=-1.0, scalar2=None, op0=ALU.mult), r=[gc], w=[ngc])
            ps = S.ps()
            mm(S, ps, ps[:, 0:64], C.ones_f, C.ones_f[:, :], g, g[:, :], True, True)
            S.act(lambda ps=ps: nc.scalar.copy(out=glast[:], in_=ps[:, 0:64]), r=[ps], w=[glast])
            S.act(lambda: nc.scalar.activation(out=alast[:], in_=glast[:], func=AF.Exp), r=[glast], w=[alast])
            S.dve(lambda: nc.vector.tensor_tensor(out=etail[:], in0=glast[:], in1=gc[:], op=ALU.subtract), r=[glast, gc], w=[etail])
            S.act(lambda: nc.scalar.activation(out=etail[:], in_=etail[:], func=AF.Exp), r=[etail], w=[etail])
            S.act(lambda: nc.scalar.activation(out=bexpg[:], in_=gc[:], func=AF.Exp), r=[gc], w=[bexpg])
            S.dve(lambda: nc.vector.tensor_tensor(out=bexpg[:], in0=bexpg[:], in1=beta[:], op=ALU.mult), r=[bexpg, beta], w=[bexpg])
            S.dve(lambda: nc.vector.tensor_scalar(out=nbeta[:], in0=beta[:], scalar1=-1.0, scalar2=None, op0=ALU.mult),
                  r=[beta], w=[nbeta])
            S.dve(lambda: nc.vector.memset(St[:], 0.0), w=[St])
            for T in range(NT):
                s = T % NS
                t0 = T * 128
                for ci, n in enumerate("qkv"):
                    rw = raw[n][s]
                    row0 = ci * 512 + h * 128
                    if T == 0:
                        S.dve(lambda rw=rw: nc.vector.memset(rw[:, 0:3], 0.0), w=[rw])
                        S.dma("sp", rw[:, 3:131], pT[row0:row0 + 128, 0:128], r=[pT], w=[rw])
                    else:
                        S.dma("sp", rw[:, :], pT[row0:row0 + 128, t0 - 3:t0 + 128], r=[pT], w=[rw])
                    c_ = cv[n][s]
                    ft = ci * 4 + h
                    S.dve(lambda rw=rw, c_=c_, ft=ft: nc.vector.tensor_scalar(out=c_[:], in0=rw[:, 0:128], scalar1=cw[:, ft, 0:1],
                                                                              scalar2=None, op0=ALU.mult), r=[rw, cw], w=[c_])
                    for k in range(1, 4):
                        S.dve(lambda rw=rw, c_=c_, ft=ft, k=k: nc.vector.scalar_tensor_tensor(
                            out=c_[:], in0=rw[:, k:k + 128], scalar=cw[:, ft, k:k + 1], in1=c_[:], op0=ALU.mult, op1=ALU.add),
                            r=[rw, cw, c_], w=[c_])
                    S.act(lambda c_=c_: nc.scalar.activation(out=c_[:], in_=c_[:], func=AF.Silu), r=[c_], w=[c_])
                for n, dst, mul in (("q", qT[s], 128 ** -0.5), ("k", kT[s], 1.0)):
                    c_ = cv[n][s]
                    S.pool(lambda c_=c_: nc.gpsimd.tensor_tensor(out=rn[s][:], in0=c_[:], in1=c_[:], op=ALU.mult), r=[c_], w=[rn[s]])
                    ps = S.ps()
                    mm(S, ps, ps[:, 0:128], C.ones_f, C.ones_f[:, :], rn[s], rn[s][:, :], True, True)
                    S.act(lambda ps=ps: nc.scalar.activation(out=rn[s][:], in_=ps[:, 0:128], func=AF.Sqrt, bias=C.eps[:, 0:1]),
                          r=[ps, C.eps], w=[rn[s]])
                    S.dve(lambda: nc.vector.reciprocal(out=rn[s][:], in_=rn[s][:]), r=[rn[s]], w=[rn[s]])
                    S.dve(lambda c_=c_, dst=dst, mul=mul: nc.vector.scalar_tensor_tensor(
                        out=dst[:], in0=c_[:], scalar=mul, in1=rn[s][:], op0=ALU.mult, op1=ALU.mult), r=[c_, rn[s]], w=[dst])
                ps = S.ps()
                S.pe(lambda ps=ps: nc.tensor.transpose(out=ps[:, 0:128], in_=kT[s][:, :], identity=C.ident[:, :]),
                     r=[kT[s], C.ident], w=[ps])
                evac(ktok[s], ps)
                ps = S.ps()
                S.pe(lambda ps=ps: nc.tensor.transpose(out=ps[:, 0:128], in_=cv["v"][s][:, :], identity=C.ident[:, :]),
                     r=[cv["v"][s], C.ident], w=[ps])
                evac(vtok[s], ps, "dve")
                S.dve(lambda: nc.vector.tensor_scalar(out=dgc[s][:], in0=C.ident[:], scalar1=gc[:, T:T + 1], scalar2=None,
                                                      op0=ALU.mult), r=[C.ident, gc], w=[dgc[s]])
                p1 = S.ps()
                mm(S, p1, p1[:, 0:128], C.ones_f, C.ones_f[:, :], dgc[s], dgc[s][:, :], True, False)
                mm(S, p1, p1[:, 0:128], C.ident, C.ident[:, :], PM1, PM1[:, :], False, True)
                S.act(lambda p1=p1: nc.scalar.activation(out=Dm[s][:], in_=p1[:, 0:128], func=AF.Exp, scale=-1.0,
                                                         bias=gc[:, T:T + 1]), r=[p1, gc], w=[Dm[s]])
                p2 = S.ps()
                mm(S, p2, p2[:, 0:128], C.ones_f, C.ones_f[:, :], dgc[s], dgc[s][:, :], True, False)
                mm(S, p2, p2[:, 0:128], C.ident, C.ident[:, :], NM2, NM2[:, :], False, True)
                S.act(lambda p2=p2: nc.scalar.activation(out=DiT[s][:], in_=p2[:, 0:128], func=AF.Exp, scale=1.0,
                                                         bias=ngc[:, T:T + 1]), r=[p2, ngc], w=[DiT[s]])
                p3 = S.ps()
                mm(S, p3, p3[:, 0:128], C.ones_f, C.ones_f[:, :], dgc[s], dgc[s][:, :], True, True)
                S.act(lambda p3=p3: nc.scalar.activation(out=egr[s][:], in_=p3[:, 0:128], func=AF.Exp), r=[p3], w=[egr[s]])
                pg = S.ps()
                mm(S, pg, pg[:, 0:128], kT[s], kT[s][:, :], kT[s], kT[s][:, :], True, True)
                S.dve(lambda pg=pg: nc.vector.scalar_tensor_tensor(out=Nm[s][:], in0=pg[:, 0:128], scalar=nbeta[:, T:T + 1],
                                                                   in1=Dm[s][:], op0=ALU.mult, op1=ALU.mult),
                      r=[pg, nbeta, Dm[s]], w=[Nm[s]])
                ps = S.ps()
                S.pe(lambda ps=ps: nc.tensor.transpose(out=ps[:, 0:128], in_=Nm[s][:, :], identity=C.ident[:, :]),
                     r=[Nm[s], C.ident], w=[ps])
                evac(NmT[s], ps)
                S.dve(lambda: nc.vector.tensor_tensor(out=RT[s][:], in0=NmT[s][:], in1=C.ident[:], op=ALU.add),
                      r=[NmT[s], C.ident], w=[RT[s]])
                Mc_, McT = Nm[s], NmT[s]
                Mn, MnT = M2[s], M2T[s]
                for lvl in range(6):
                    pa = S.ps()
                    mm(S, pa, pa[:, 0:128], McT, McT[:, :], Mc_, Mc_[:, :], True, True)
                    pb = S.ps()
                    mm(S, pb, pb[:, 0:128], Mc_, Mc_[:, :], McT, McT[:, :], True, True)
                    evac(Mn, pa, "act")
                    evac(MnT, pb, "dve")
                    pr = S.ps()
                    mm(S, pr, pr[:, 0:128], Mn, Mn[:, :], RT[s], RT[s][:, :], True, True)
                    S.dve(lambda pr=pr: nc.vector.tensor_tensor(out=RT[s][:], in0=RT[s][:], in1=pr[:, 0:128], op=ALU.add),
                          r=[RT[s], pr], w=[RT[s]])
                    Mc_, McT, Mn, MnT = Mn, MnT, Mc_, McT
                S.dve(lambda: nc.vector.tensor_scalar(out=vb[s][:], in0=vtok[s][:], scalar1=beta[:, T:T + 1], scalar2=None,
                                                      op0=ALU.mult), r=[vtok[s], beta], w=[vb[s]])
                S.dve(lambda: nc.vector.tensor_scalar(out=kbg[s][:], in0=ktok[s][:], scalar1=bexpg[:, T:T + 1], scalar2=None,
                                                      op0=ALU.mult), r=[ktok[s], bexpg], w=[kbg[s]])
                S.pool(lambda: nc.gpsimd.tensor_scalar(out=ktl[s][:], in0=ktok[s][:], scalar1=etail[:, T:T + 1], scalar2=None,
                                                       op0=ALU.mult), r=[ktok[s], etail], w=[ktl[s]])
                pu = S.ps()
                mm(S, pu, pu[:, 0:128], RT[s], RT[s][:, :], vb[s], vb[s][:, :], True, True)
                evac(u[s], pu, "act")
                pw = S.ps()
                mm(S, pw, pw[:, 0:128], kbg[s], kbg[s][:, :], RT[s], RT[s][:, :], True, True)
                evac(wT[s], pw, "act")
                pq = S.ps()
                mm(S, pq, pq[:, 0:128], kT[s], kT[s][:, :], qT[s], qT[s][:, :], True, True)
                S.dve(lambda pq=pq: nc.vector.tensor_tensor(out=qkT[s][:], in0=pq[:, 0:128], in1=DiT[s][:], op=ALU.mult),
                      r=[pq, DiT[s]], w=[qkT[s]])
                S.pool(lambda: nc.gpsimd.tensor_tensor(out=qgT[s][:], in0=qT[s][:], in1=egr[s][:], op=ALU.mult),
                       r=[qT[s], egr[s]], w=[qgT[s]])
                pv = S.ps()
                mm(S, pv, pv[:, 0:128], wT[s], wT[s][:, :], St, St[:, :], True, True)
                S.dve(lambda pv=pv: nc.vector.tensor_tensor(out=vnew[s][:], in0=u[s][:], in1=pv[:, 0:128], op=ALU.subtract),
                      r=[u[s], pv], w=[vnew[s]])
                po = S.ps()
                mm(S, po, po[:, 0:128], St, St[:, :], qgT[s], qgT[s][:, :], True, False)
                mm(S, po, po[:, 0:128], vnew[s], vnew[s][:, :], qkT[s], qkT[s][:, :], False, True)
                pS = S.ps()
                mm(S, pS, pS[:, 0:128], ktl[s], ktl[s][:, :], vnew[s], vnew[s][:, :], True, True)
                S.dve(lambda pS=pS: nc.vector.scalar_tensor_tensor(out=St[:], in0=St[:], scalar=alast[:, T:T + 1],
                                                                   in1=pS[:, 0:128], op0=ALU.mult, op1=ALU.add),
                      r=[St, alast, pS], w=[St])
                evac(osb[s], po, "act")
                S.pool(lambda: nc.gpsimd.tensor_tensor(out=osq[s][:], in0=osb[s][:], in1=osb[s][:], op=ALU.mult), r=[osb[s]], w=[osq[s]])
                pn = S.ps()
                mm(S, pn, pn[:, 0:128], C.ones_f, C.ones_f[:, :], osq[s], osq[s][:, :], True, True)
                S.act(lambda pn=pn: nc.scalar.activation(out=osq[s][:], in_=pn[:, 0:128], func=AF.Sqrt, scale=1.0 / 128,
                                                         bias=C.eps[:, 0:1]), r=[pn, C.eps], w=[osq[s]])
                S.dve(lambda: nc.vector.reciprocal(out=osq[s][:], in_=osq[s][:]), r=[osq[s]], w=[osq[s]])
                S.dma("sp", zt[s][:], pT[1536 + h * 128:1536 + (h + 1) * 128, t0:t0 + 128], r=[pT], w=[zt[s]])
                S.act(lambda: nc.scalar.activation(out=zt[s][:], in_=zt[s][:], func=AF.Silu), r=[zt[s]], w=[zt[s]])
                S.dve(lambda: nc.vector.scalar_tensor_tensor(out=osb[s][:], in0=osb[s][:], scalar=gn[:, 0:1], in1=osq[s][:],
                                                             op0=ALU.mult, op1=ALU.mult), r=[osb[s], gn, osq[s]], w=[osb[s]])
                S.dve(lambda: nc.vector.tensor_tensor(out=obf[s][:], in0=osb[s][:], in1=zt[s][:], op=ALU.mult),
                      r=[osb[s], zt[s]], w=[obf[s]])
                S.dma("sp", oT_dram[h * 128:(h + 1) * 128, t0:t0 + 128], obf[s][:], r=[obf[s]], w=[oT_dram])
        S.barrier()


def phase_nsa(S, C, pT, vs_tok, vw_tok, j, oT_dram):
    nc = S.nc
    R_Q, R_KC, R_VC, R_KS, R_KW, R_GL = 0, 1024, 1280, 1536, 2048, 2560
    posk_d = S.dram("od_cmp_pos_kT", [2, 64, 32], F32)
    posv_d = S.dram("od_cmp_pos_vT", [2, 64, 32], F32)
    k1_d = S.dram("od_cmp_k1", [2, 2048, 256], F32)
    k2_d = S.dram("od_cmp_k2", [2, 256, 64], F32)
    v1_d = S.dram("od_cmp_v1", [2, 2048, 256], F32)
    v2_d = S.dram("od_cmp_v2", [2, 256, 64], F32)
    SC = 0.125
    with ExitStack() as es:
        KC = [S.sb(es, [64, 512], BF16, "KC") for _ in range(4)]
        for g in range(4):
            S.pool(lambda g=g: nc.gpsimd.memset(KC[g][:], 0.0), w=[KC[g]])
        VC = [S.sb(es, [128, 4, 64], BF16, "VC") for _ in range(4)]
        with ExitStack() as e1:
            src = S.sb(e1, [64, 8192], BF16, "csrc")
            w1 = S.sb(e1, [64, 32, 256], BF16, "w1")
            w2 = S.sb(e1, [128, 2, 64], BF16, "w2")
            posT = S.sb(e1, [64, 32], BF16, "posT")
            c1 = S.sb(e1, [128, 2], F32, "c1")
            h1 = S.sb(e1, [128, 2, 512], BF16, "h1")
            for which, (pos_d, a_d, b_d, row0) in enumerate(((posk_d, k1_d, k2_d, R_KC), (posv_d, v1_d, v2_d, R_VC))):
                S.dma("pool", w1[:], a_d[j].rearrange("(l d) n -> d l n", d=64), r=[a_d], w=[w1])
                S.dma("pool", w2[:], b_d[j].rearrange("(c p) n -> p c n", p=128), r=[b_d], w=[w2])
                S.dma("pool", posT[:], pos_d[j], r=[pos_d], w=[posT])
                for ncx in range(2):
                    ps = S.ps()
                    for l in range(32):
                        mm(S, ps, ps[:, 0:1], w1, w1[:, l, ncx * 128:(ncx + 1) * 128], posT, posT[:, l:l + 1], l == 0, l == 31)
                    S.act(lambda ps=ps, ncx=ncx: nc.scalar.copy(out=c1[:, ncx:ncx + 1], in_=ps[:, 0:1]), r=[ps], w=[c1])
                for g in range(4):
                    S.dma("pool", src[:], pT[row0 + g * 64:row0 + (g + 1) * 64, :], r=[pT], w=[src])
                    for ncx in range(2):
                        ps = S.ps()
                        for l in range(32):
                            mm(S, ps, ps[:, 0:511], w1, w1[:, l, ncx * 128:(ncx + 1) * 128], src,
                               src[:, l:l + 16 * 510 + 1:16], l == 0, l == 31)
                        S.act(lambda ps=ps, ncx=ncx: nc.scalar.activation(out=h1[:, ncx, 0:511], in_=ps[:, 0:511], func=AF.Silu,
                                                                          bias=c1[:, ncx:ncx + 1]), r=[ps, c1], w=[h1])
                    if which == 0:
                        ps = S.ps()
                        for ncx in range(2):
                            mm(S, ps, ps[0:64, 0:511], w2, w2[:, ncx, :], h1, h1[:, ncx, 0:511], ncx == 0, ncx == 1)
                        S.act(lambda ps=ps, g=g: nc.scalar.copy(out=KC[g][:, 0:511], in_=ps[0:64, 0:511]), r=[ps], w=[KC[g]])
                    else:
                        for nt in range(4):
                            rows = 128 if nt < 3 else 127
                            ps = S.ps()
                            for ncx in range(2):
                                mm(S, ps, ps[0:rows, 0:64], h1, h1[:, ncx, nt * 128:nt * 128 + rows], w2, w2[:, ncx, :],
                                   ncx == 0, ncx == 1)
                            S.act(lambda ps=ps, g=g, nt=nt, rows=rows: nc.scalar.copy(out=VC[g][0:rows, nt, :], in_=ps[0:rows, 0:64]),
                                  r=[ps], w=[VC[g]])
        S.barrier()
        negselT = S.sb(es, [128, 8192], BF16, "negselT")
        Wm = S.sb(es, [128, 16], F32, "Wm")
        Wm0 = S.sb(es, [128, 16], F32, "Wm0")
        for (t_, b_) in ((Wm, 97), (Wm0, -31)):
            S.pool(lambda t_=t_: nc.gpsimd.memset(t_[:], 0.0), w=[t_])
            S.pool(lambda t_=t_, b_=b_: nc.gpsimd.affine_select(out=t_[:], in_=t_[:], pattern=[[-16, 16]], compare_op=ALU.is_ge,
                                                                fill=NEG, base=b_, channel_multiplier=1), r=[t_], w=[t_])
        Ebig = S.sb(es, [128, 8192], BF16, "Ebig")
        Sel48 = S.sb(es, [48, 48, 64], F32, "Sel48")
        S.pool(lambda: nc.gpsimd.memset(Sel48[:], 0.0), w=[Sel48])
        S.pool(lambda: nc.gpsimd.affine_select(out=Sel48[:], in_=Sel48[:], pattern=[[-1, 48], [0, 64]],
                                               compare_op=ALU.not_equal, fill=1.0, base=0, channel_multiplier=1),
               r=[Sel48], w=[Sel48])
        S.pool(lambda: nc.gpsimd.memset(Ebig[:], 0.0), w=[Ebig])
        ebv = Ebig[:].rearrange("p (b x) -> p b x", x=64)
        S.pool(lambda: nc.gpsimd.affine_select(out=ebv, in_=ebv, pattern=[[-1, 128], [0, 64]], compare_op=ALU.not_equal,
                                               fill=1.0, base=0, channel_multiplier=1), r=[Ebig], w=[Ebig])
        for g in range(4):
            with ExitStack() as e2:
                QT4 = [S.sb(e2, [64, 8192], BF16, "QT4") for _ in range(4)]
                for hg in range(4):
                    r0 = R_Q + (g * 4 + hg) * 64
                    S.dma("pool", QT4[hg][:], pT[r0:r0 + 64, :], r=[pT], w=[QT4[hg]])
                scs = [S.sb(e2, [128, 512], F32, "sc") for _ in range(2)]
                pp = S.sb(e2, [128, 516], F32, "pp")
                rs = S.sb(e2, [128, 2], F32, "rs")
                imp = S.sb(e2, [128, 128], F32, "imp")
                imp2 = S.sb(e2, [128, 128], F32, "imp2")
                mx = S.sb(e2, [128, 8], F32, "mx")
                S.dve(lambda: nc.vector.memset(pp[:], 0.0), w=[pp])
                for T in range(NT):
                    for hg in range(4):
                        ps = S.ps()
                        mm(S, ps, ps[:, 0:512], QT4[hg], QT4[hg][:, T * 128:(T + 1) * 128], KC[g], KC[g][:, 0:512], True, True)
                        sc = scs[hg % 2]
                        S.act(lambda ps=ps, sc=sc: nc.scalar.activation(out=sc[:], in_=ps[:, 0:512], func=AF.Copy, scale=SC),
                              r=[ps], w=[sc])
                        w0 = max(8 * T - 8, 0)
                        w1_ = min(8 * T + 8, 512)
                        wm_ap = Wm0[:, 0:8] if T == 0 else Wm[:, 0:w1_ - w0]
                        S.pool(lambda sc=sc, w0=w0, w1_=w1_, wm_ap=wm_ap: nc.gpsimd.tensor_tensor(
                            out=sc[:, w0:w1_], in0=sc[:, w0:w1_], in1=wm_ap, op=ALU.add), r=[sc, Wm, Wm0], w=[sc])
                        if w1_ < 512:
                            S.pool(lambda sc=sc, w1_=w1_: nc.gpsimd.memset(sc[:, w1_:512], NEG), r=[sc], w=[sc])
                        S.act(lambda sc=sc: nc.scalar.activation(out=sc[:], in_=sc[:], func=AF.Exp, accum_out=rs[:, 0:1]),
                              r=[sc], w=[sc, rs])
                        S.dve(lambda: nc.vector.tensor_scalar(out=rs[:, 1:2], in0=rs[:, 0:1], scalar1=C.tiny[:, 0:1], scalar2=None,
                                                              op0=ALU.max), r=[rs, C.tiny], w=[rs])
                        S.dve(lambda: nc.vector.reciprocal(out=rs[:, 1:2], in_=rs[:, 1:2]), r=[rs], w=[rs])
                        if hg == 0:
                            S.dve(lambda sc=sc: nc.vector.tensor_scalar(out=pp[:, 1:513], in0=sc[:], scalar1=rs[:, 1:2], scalar2=None,
                                                                        op0=ALU.mult), r=[sc, rs], w=[pp])
                        else:
                            S.dve(lambda sc=sc: nc.vector.scalar_tensor_tensor(out=pp[:, 1:513], in0=sc[:], scalar=rs[:, 1:2],
                                                                               in1=pp[:, 1:513], op0=ALU.mult, op1=ALU.add),
                                  r=[sc, rs, pp], w=[pp])
                    a = pp[:, 0:512].rearrange("p (j f) -> p j f", f=4)
                    e_ = pp[:, 4:516].rearrange("p (j f) -> p j f", f=4)
                    S.dve(lambda a=a: nc.vector.tensor_scalar(out=imp[:], in0=a[:, :, 0], scalar1=0.5, scalar2=None, op0=ALU.mult),
                          r=[pp], w=[imp])
                    for f in (1, 2, 3):
                        S.dve(lambda a=a, f=f: nc.vector.tensor_tensor(out=imp[:], in0=imp[:], in1=a[:, :, f], op=ALU.add),
                              r=[pp, imp], w=[imp])
                    S.dve(lambda e_=e_: nc.vector.scalar_tensor_tensor(out=imp[:], in0=e_[:, :, 0], scalar=0.5, in1=imp[:],
                                                                       op0=ALU.mult, op1=ALU.add), r=[pp, imp], w=[imp])
                    for half in range(2):
                        cur = 2 * T + half
                        hs = slice(half * 64, half * 64 + 64)
                        if cur + 1 < 128:
                            S.pool(lambda hs=hs, cur=cur: nc.gpsimd.memset(imp[hs, cur + 1:128], -1.0), r=[imp], w=[imp])
                        lo = max(cur - 1, 0)
                        S.pool(lambda hs=hs, lo=lo, cur=cur: nc.gpsimd.memset(imp[hs, lo:cur + 1], 1e9), r=[imp], w=[imp])
                    S.pool(lambda: nc.gpsimd.memset(imp[:, 0:1], 1e9), r=[imp], w=[imp])
                    S.dve(lambda: nc.vector.max(out=mx[:], in_=imp[:]), r=[imp], w=[mx])
                    S.dve(lambda: nc.vector.match_replace(out=imp2[:], in_to_replace=mx[:], in_values=imp[:], imm_value=-2.0),
                          r=[mx, imp], w=[imp2])
                    S.dve(lambda: nc.vector.max(out=mx[:], in_=imp2[:]), r=[imp2], w=[mx])
                    S.dve(lambda: nc.vector.tensor_scalar(out=imp2[:], in0=imp[:], scalar1=mx[:, 7:8], scalar2=None, op0=ALU.is_ge),
                          r=[imp, mx], w=[imp2])
                    S.dve(lambda: nc.vector.tensor_scalar(out=imp2[:], in0=imp2[:], scalar1=-1.0, scalar2=-NEG, op0=ALU.add,
                                                          op1=ALU.mult), r=[imp2], w=[imp2])
                    ps = S.ps()
                    S.pe(lambda ps=ps: nc.tensor.transpose(out=ps[:, 0:128], in_=imp2[:, :], identity=C.ident[:, :]),
                         r=[imp2, C.ident], w=[ps])
                    S.act(lambda ps=ps, T=T: nc.scalar.copy(out=negselT[:, T * 128:(T + 1) * 128], in_=ps[:, 0:128]),
                          r=[ps], w=[negselT])
            S.barrier()
            with ExitStack() as e3:
                ksT = S.sb(e3, [64, 8192], BF16, "ksT")
                kwT = S.sb(e3, [64, 8192], BF16, "kwT")
                VS = S.sb(e3, [128, 64, 64], BF16, "VS")
                VW = S.sb(e3, [128, 64, 64], BF16, "VW")
                QT = S.sb(e3, [64, 8192], BF16, "QT")
                gates = S.sb(e3, [48, 8192], F32, "gates")
                pts = [S.sb(e3, [128, 512], BF16, "pt") for _ in range(3)]
                ots = [S.sb(e3, [64, 512], F32, "ot") for _ in range(2)]
                rden = S.sb(e3, [64, 512], F32, "rden")
                acc = S.sb(e3, [64, 512], F32, "acc")
                tmpo = S.sb(e3, [64, 512], F32, "tmpo")
                obf = [S.sb(e3, [64, 512], BF16, "obf") for _ in range(2)]
                S.dma("pool", ksT[:], pT[R_KS + g * 64:R_KS + (g + 1) * 64, :], r=[pT], w=[ksT])
                S.dma("pool", kwT[:], pT[R_KW + g * 64:R_KW + (g + 1) * 64, :], r=[pT], w=[kwT])
                for q4 in range(4):
                    tsl = slice(q4 * 16, (q4 + 1) * 16)
                    rsl = slice(q4 * 2048, (q4 + 1) * 2048)
                    S.dma("pool", VS[:, tsl, :], vs_tok[rsl, g * 64:(g + 1) * 64].rearrange("(t p) d -> p t d", p=128),
                          r=[vs_tok], w=[VS])
                    S.dma("pool", VW[:, tsl, :], vw_tok[rsl, g * 64:(g + 1) * 64].rearrange("(t p) d -> p t d", p=128),
                          r=[vw_tok], w=[VW])
                S.dma("sp", gates[:], pT[R_GL:R_GL + 48, :], r=[pT], w=[gates])
                S.act(lambda: nc.scalar.activation(out=gates[:], in_=gates[:], func=AF.Sigmoid), r=[gates], w=[gates])
                pi = 0
                bank = 0
                for hg in range(4):
                    head = g * 4 + hg
                    S.dma("pool", QT[:], pT[R_Q + head * 64:R_Q + (head + 1) * 64, :], r=[pT], w=[QT])
                    for c in range(16):
                        qs = slice(c * 512, (c + 1) * 512)
                        for br in range(3):
                            if br == 0:
                                kts = []
                                for nt in range(4):
                                    D = c - 4 * nt
                                    if D < 0:
                                        continue
                                    nk = 128 if nt < 3 else 127
                                    masks = [(C.ident_bf, C.ident_bf[:, 0:nk], C.Mk[D], C.Mk[D][:, :])] if D <= 4 else []
                                    kts.append((nt, nk, masks))
                                kp, V_ = [(KC[g], 64)], VC[g]
                            elif br == 1:
                                kts = []
                                for kt in range(4 * c + 4):
                                    masks = [(Ebig, Ebig[:, kt * 128:(kt + 1) * 128], negselT, negselT[:, qs])]
                                    if kt >= 4 * c:
                                        m_ = C.Mc[kt - 4 * c]
                                        masks.append((C.ident_bf, C.ident_bf[:, :], m_, m_[:, :]))
                                    kts.append((kt, 128, masks))
                                kp, V_ = [(ksT, 64)], VS
                            else:
                                kts = []
                                for jj in range(-4, 4):
                                    kt = 4 * c + jj
                                    if kt < 0:
                                        continue
                                    m_ = C.Mw[-jj] if jj < 0 else C.Mc[jj]
                                    kts.append((kt, 128, [(C.ident_bf, C.ident_bf[:, :], m_, m_[:, :])]))
                                kp, V_ = [(kwT, 64)], VW
                            ot = ots[bank % 2]
                            pi = attn_chunk(S, C, c, bank, kp, [(QT, 64)], V_, 64, kts, SC, pts, ot, rden, pi)
                            bank += 1
                            pg = S.ps4()
                            mm(S, pg, pg[0:64, :], Sel48, Sel48[:, head * 3 + br, :], gates, gates[:, qs], True, True)
                            if br == 0:
                                S.dve(lambda ot=ot, pg=pg: nc.vector.tensor_tensor(out=acc[:], in0=ot[:], in1=pg[0:64, :], op=ALU.mult),
                                      r=[ot, pg], w=[acc])
                            else:
                                S.dve(lambda ot=ot, pg=pg: nc.vector.tensor_tensor(out=tmpo[:], in0=ot[:], in1=pg[0:64, :], op=ALU.mult),
                                      r=[ot, pg], w=[tmpo])
                                S.pool(lambda: nc.gpsimd.tensor_tensor(out=acc[:], in0=acc[:], in1=tmpo[:], op=ALU.add),
                                       r=[acc, tmpo], w=[acc])
                        ob = obf[c % 2]
                        S.act(lambda ob=ob: nc.scalar.copy(out=ob[:], in_=acc[:]), r=[acc], w=[ob])
                        S.dma("sp", oT_dram[head * 64:(head + 1) * 64, qs], ob[:], r=[ob], w=[oT_dram])
            S.barrier()
        S.barrier()


def phase_nsa2(S, C, pT, vs_tok, vw_tok, j, oT_dram):
    nc = S.nc
    R_Q, R_KC, R_VC, R_KS, R_KW, R_GL = 0, 1024, 1280, 1536, 2048, 2560
    posk_d = S.dram("od_cmp_pos_kT", [2, 64, 32], F32)
    posv_d = S.dram("od_cmp_pos_vT", [2, 64, 32], F32)
    k1_d = S.dram("od_cmp_k1", [2, 2048, 256], F32)
    k2_d = S.dram("od_cmp_k2", [2, 256, 64], F32)
    v1_d = S.dram("od_cmp_v1", [2, 2048, 256], F32)
    v2_d = S.dram("od_cmp_v2", [2, 256, 64], F32)
    SC = 0.125
    with ExitStack() as es:
        KC = [S.sb(es, [64, 512], BF16, "KC") for _ in range(4)]
        for g in range(4):
            S.pool(lambda g=g: nc.gpsimd.memset(KC[g][:], 0.0), w=[KC[g]])
        VC = [S.sb(es, [128, 4, 64], BF16, "VC") for _ in range(4)]
        with ExitStack() as e1:
            src = S.sb(e1, [64, 8192], BF16, "csrc")
            w1 = S.sb(e1, [64, 32, 256], BF16, "w1")
            w2 = S.sb(e1, [128, 2, 64], BF16, "w2")
            posT = S.sb(e1, [64, 32], BF16, "posT")
            c1 = S.sb(e1, [128, 2], F32, "c1")
            h1 = S.sb(e1, [128, 2, 512], BF16, "h1")
            for which, (pos_d, a_d, b_d, row0) in enumerate(((posk_d, k1_d, k2_d, R_KC), (posv_d, v1_d, v2_d, R_VC))):
                S.dma("pool", w1[:], a_d[j].rearrange("(l d) n -> d l n", d=64), r=[a_d], w=[w1])
                S.dma("pool", w2[:], b_d[j].rearrange("(c p) n -> p c n", p=128), r=[b_d], w=[w2])
                S.dma("pool", posT[:], pos_d[j], r=[pos_d], w=[posT])
                for ncx in range(2):
                    ps = S.ps()
                    for l in range(32):
                        mm(S, ps, ps[:, 0:1], w1, w1[:, l, ncx * 128:(ncx + 1) * 128], posT, posT[:, l:l + 1], l == 0, l == 31)
                    S.act(lambda ps=ps, ncx=ncx: nc.scalar.copy(out=c1[:, ncx:ncx + 1], in_=ps[:, 0:1]), r=[ps], w=[c1])
                for g in range(4):
                    S.dma("pool", src[:], pT[row0 + g * 64:row0 + (g + 1) * 64, :], r=[pT], w=[src])
                    for ncx in range(2):
                        ps = S.ps()
                        for l in range(32):
                            mm(S, ps, ps[:, 0:511], w1, w1[:, l, ncx * 128:(ncx + 1) * 128], src,
                               src[:, l:l + 16 * 510 + 1:16], l == 0, l == 31)
                        S.act(lambda ps=ps, ncx=ncx: nc.scalar.activation(out=h1[:, ncx, 0:511], in_=ps[:, 0:511], func=AF.Silu,
                                                                          bias=c1[:, ncx:ncx + 1]), r=[ps, c1], w=[h1])
                    if which == 0:
                        ps = S.ps()
                        for ncx in range(2):
                            mm(S, ps, ps[0:64, 0:511], w2, w2[:, ncx, :], h1, h1[:, ncx, 0:511], ncx == 0, ncx == 1)
                        S.act(lambda ps=ps, g=g: nc.scalar.copy(out=KC[g][:, 0:511], in_=ps[0:64, 0:511]), r=[ps], w=[KC[g]])
                    else:
                        for nt in range(4):
                            rows = 128 if nt < 3 else 127
                            ps = S.ps()
                            for ncx in range(2):
                                mm(S, ps, ps[0:rows, 0:64], h1, h1[:, ncx, nt * 128:nt * 128 + rows], w2, w2[:, ncx, :],
                                   ncx == 0, ncx == 1)
                            S.act(lambda ps=ps, g=g, nt=nt, rows=rows: nc.scalar.copy(out=VC[g][0:rows, nt, :], in_=ps[0:rows, 0:64]),
                                  r=[ps], w=[VC[g]])
        S.barrier()
        sel01T = S.sb(es, [128, 8192], BF16, "sel01T")
        Wm = S.sb(es, [128, 16], F32, "Wm")
        Wm0 = S.sb(es, [128, 16], F32, "Wm0")
        for (t_, b_) in ((Wm, 97), (Wm0, -31)):
            S.pool(lambda t_=t_: nc.gpsimd.memset(t_[:], 0.0), w=[t_])
            S.pool(lambda t_=t_, b_=b_: nc.gpsimd.affine_select(out=t_[:], in_=t_[:], pattern=[[-16, 16]], compare_op=ALU.is_ge,
                                                                fill=NEG, base=b_, channel_multiplier=1), r=[t_], w=[t_])
        Ebig = S.sb(es, [128, 8192], BF16, "Ebig")
        def m01(name, base, cm, step):
            t = S.sb(es, [128, 512], BF16, name)
            S.pool(lambda: nc.gpsimd.memset(t[:], 1.0), w=[t])
            S.pool(lambda: nc.gpsimd.affine_select(out=t[:], in_=t[:], pattern=[[step, 512]], compare_op=ALU.is_ge, fill=0.0,
                                                   base=base, channel_multiplier=cm), r=[t], w=[t])
            return t
        Mc01 = [m01(f"Mc01{j_}", -128 * j_, -1, 1) for j_ in range(4)]
        Mw01 = [None] + [m01(f"Mw01{m_}", 511 - 128 * m_, 1, -1) for m_ in range(1, 5)]
        Mk01 = [m01(f"Mk01{d_}", 512 * d_ - 31, -16, 1) for d_ in range(5)]
        GS = S.dram("GS", [48, 8192], F32)
        with ExitStack() as eg:
            gt = S.sb(eg, [48, 8192], F32, "gt")
            S.dma("sp", gt[:], pT[R_GL:R_GL + 48, :], r=[pT], w=[gt])
            S.act(lambda: nc.scalar.activation(out=gt[:], in_=gt[:], func=AF.Sigmoid), r=[gt], w=[gt])
            S.dma("sp", GS[:, :], gt[:], r=[gt], w=[GS])
            S.barrier()
        S.pool(lambda: nc.gpsimd.memset(Ebig[:], 0.0), w=[Ebig])
        ebv = Ebig[:].rearrange("p (b x) -> p b x", x=64)
        S.pool(lambda: nc.gpsimd.affine_select(out=ebv, in_=ebv, pattern=[[-1, 128], [0, 64]], compare_op=ALU.not_equal,
                                               fill=1.0, base=0, channel_multiplier=1), r=[Ebig], w=[Ebig])
        for g in range(4):
            with ExitStack() as e2:
                QT4 = [S.sb(e2, [64, 8192], BF16, "QT4") for _ in range(4)]
                for hg in range(4):
                    r0 = R_Q + (g * 4 + hg) * 64
                    S.dma("pool", QT4[hg][:], pT[r0:r0 + 64, :], r=[pT], w=[QT4[hg]])
                NTI = 4
                BS = []
                for _ in range(NTI):
                    BS.append(dict(scs=[S.sb(e2, [128, 512], F32, "sc") for _ in range(2)],
                                   pp=S.sb(e2, [128, 516], F32, "pp"), rs=S.sb(e2, [128, 2], F32, "rs"),
                                   imp=S.sb(e2, [128, 128], F32, "imp"), imp2=S.sb(e2, [128, 128], F32, "imp2"),
                                   mx=S.sb(e2, [128, 8], F32, "mx")))
                    S.dve(lambda: nc.vector.memset(BS[-1]["pp"][:], 0.0), w=[BS[-1]["pp"]])

                def sel_gen(T, B):
                    scs, pp, rs, imp, imp2, mx = B["scs"], B["pp"], B["rs"], B["imp"], B["imp2"], B["mx"]
                    for hg in range(4):
                        ps = S.ps()
                        mm(S, ps, ps[:, 0:512], QT4[hg], QT4[hg][:, T * 128:(T + 1) * 128], KC[g], KC[g][:, 0:512], True, True)
                        sc = scs[hg % 2]
                        S.act(lambda: nc.scalar.activation(out=sc[:], in_=ps[:, 0:512], func=AF.Copy, scale=SC), r=[ps], w=[sc])
                        yield
                        w0 = max(8 * T - 8, 0)
                        w1_ = min(8 * T + 8, 512)
                        wm_ap = Wm0[:, 0:8] if T == 0 else Wm[:, 0:w1_ - w0]
                        S.pool(lambda: nc.gpsimd.tensor_tensor(out=sc[:, w0:w1_], in0=sc[:, w0:w1_], in1=wm_ap, op=ALU.add),
                               r=[sc, Wm, Wm0], w=[sc])
                        if w1_ < 512:
                            S.pool(lambda: nc.gpsimd.memset(sc[:, w1_:512], NEG), r=[sc], w=[sc])
                        yield
                        S.act(lambda: nc.scalar.activation(out=sc[:], in_=sc[:], func=AF.Exp, accum_out=rs[:, 0:1]),
                              r=[sc], w=[sc, rs])
                        yield
                        S.dve(lambda: nc.vector.tensor_scalar(out=rs[:, 1:2], in0=rs[:, 0:1], scalar1=C.tiny[:, 0:1], scalar2=None,
                                                              op0=ALU.max), r=[rs, C.tiny], w=[rs])
                        S.dve(lambda: nc.vector.reciprocal(out=rs[:, 1:2], in_=rs[:, 1:2]), r=[rs], w=[rs])
                        if hg == 0:
                            S.dve(lambda: nc.vector.tensor_scalar(out=pp[:, 1:513], in0=sc[:], scalar1=rs[:, 1:2], scalar2=None,
                                                                  op0=ALU.mult), r=[sc, rs], w=[pp])
                        else:
                            S.dve(lambda: nc.vector.scalar_tensor_tensor(out=pp[:, 1:513], in0=sc[:], scalar=rs[:, 1:2],
                                                                         in1=pp[:, 1:513], op0=ALU.mult, op1=ALU.add),
                                  r=[sc, rs, pp], w=[pp])
                        yield
                    a_ = pp[:, 0:512].rearrange("p (j f) -> p j f", f=4)
                    e_ = pp[:, 4:516].rearrange("p (j f) -> p j f", f=4)
                    S.dve(lambda: nc.vector.tensor_scalar(out=imp[:], in0=a_[:, :, 0], scalar1=0.5, scalar2=None, op0=ALU.mult),
                          r=[pp], w=[imp])
                    for f in (1, 2, 3):
                        S.dve(lambda f=f: nc.vector.tensor_tensor(out=imp[:], in0=imp[:], in1=a_[:, :, f], op=ALU.add),
                              r=[pp, imp], w=[imp])
                    S.dve(lambda: nc.vector.scalar_tensor_tensor(out=imp[:], in0=e_[:, :, 0], scalar=0.5, in1=imp[:],
                                                                 op0=ALU.mult, op1=ALU.add), r=[pp, imp], w=[imp])
                    yield
                    for half in range(2):
                        cur = 2 * T + half
                        hs = slice(half * 64, half * 64 + 64)
                        if cur + 1 < 128:
                            S.pool(lambda hs=hs, cur=cur: nc.gpsimd.memset(imp[hs, cur + 1:128], -1.0), r=[imp], w=[imp])
                        S.pool(lambda hs=hs, cur=cur: nc.gpsimd.memset(imp[hs, cur:cur + 1], 2e9), r=[imp], w=[imp])
                        if cur >= 1:
                            S.pool(lambda hs=hs, cur=cur: nc.gpsimd.memset(imp[hs, cur - 1:cur], 1e9), r=[imp], w=[imp])
                    S.pool(lambda: nc.gpsimd.memset(imp[:, 0:1], 3e9), r=[imp], w=[imp])
                    yield
                    S.dve(lambda: nc.vector.max(out=mx[:], in_=imp[:]), r=[imp], w=[mx])
                    S.dve(lambda: nc.vector.match_replace(out=imp2[:], in_to_replace=mx[:], in_values=imp[:], imm_value=-2.0),
                          r=[mx, imp], w=[imp2])
                    S.dve(lambda: nc.vector.max(out=mx[:], in_=imp2[:]), r=[imp2], w=[mx])
                    S.dve(lambda: nc.vector.tensor_scalar(out=imp2[:], in0=imp[:], scalar1=mx[:, 7:8], scalar2=None, op0=ALU.is_ge),
                          r=[imp, mx], w=[imp2])
                    yield
                    ps = S.ps()
                    S.pe(lambda: nc.tensor.transpose(out=ps[:, 0:128], in_=imp2[:, :], identity=C.ident[:, :]),
                         r=[imp2, C.ident], w=[ps])
                    S.act(lambda: nc.scalar.copy(out=sel01T[:, T * 128:(T + 1) * 128], in_=ps[:, 0:128]), r=[ps], w=[sel01T])
                    yield

                for T0 in range(0, NT, NTI):
                    gens = [sel_gen(T0 + i_, BS[i_]) for i_ in range(NTI)]
                    alive = list(gens)
                    while alive:
                        nxt = []
                        for g_ in alive:
                            try:
                                next(g_)
                                nxt.append(g_)
                            except StopIteration:
                                pass
                        alive = nxt
            S.barrier()
            with ExitStack() as e3:
                ksT2 = S.sb(e3, [128, 8192], BF16, "ksT2")
                kwT2 = S.sb(e3, [128, 8192], BF16, "kwT2")
                KC2 = S.sb(e3, [128, 512], BF16, "KC2")
                VSa = S.sb(e3, [128, 64, 65], BF16, "VSa")
                VWa = S.sb(e3, [128, 64, 65], BF16, "VWa")
                VCa = S.sb(e3, [128, 4, 65], BF16, "VCa")
                QTp = S.sb(e3, [128, 2, 8192], BF16, "QTp")
                Sel12 = S.sb(e3, [48, 12, 64], F32, "Sel12")
                NPT = 6
                pts = [S.sb(e3, [128, 512], BF16, "pt") for _ in range(NPT)]
                mks = [S.sb(e3, [128, 512], BF16, "mk") for _ in range(3)]
                gch = [S.sb(e3, [48, 512], F32, "gch") for _ in range(2)]
                acc = [S.sb(e3, [64, 512], F32, "acc") for _ in range(4)]
                rdn = S.sb(e3, [65, 512], F32, "rdn")
                rb = S.sb(e3, [64, 512], F32, "rb")
                t1 = S.sb(e3, [64, 512], F32, "t1")
                t2 = S.sb(e3, [64, 512], F32, "t2")
                obf = [S.sb(e3, [64, 512], BF16, "obf") for _ in range(2)]
                S.pool(lambda: nc.gpsimd.memset(Sel12[:], 0.0), w=[Sel12])
                S.pool(lambda g=g: nc.gpsimd.affine_select(out=Sel12[:], in_=Sel12[:], pattern=[[-1, 12], [0, 64]],
                                                           compare_op=ALU.not_equal, fill=1.0, base=-12 * g, channel_multiplier=1),
                       r=[Sel12], w=[Sel12])
                for hf in range(2):
                    ps_ = slice(hf * 64, hf * 64 + 64)
                    S.dma("pool", ksT2[ps_, :], pT[R_KS + g * 64:R_KS + (g + 1) * 64, :], r=[pT], w=[ksT2])
                    S.dma("pool", kwT2[ps_, :], pT[R_KW + g * 64:R_KW + (g + 1) * 64, :], r=[pT], w=[kwT2])
                    S.dma("sp", KC2[ps_, :], KC[g][:, :], r=[KC[g]], w=[KC2])
                for hg in range(4):
                    r0 = R_Q + (g * 4 + hg) * 64
                    S.dma("pool", QTp[(hg % 2) * 64:(hg % 2) * 64 + 64, hg // 2, :], pT[r0:r0 + 64, :], r=[pT], w=[QTp])
                for va in (VSa, VWa, VCa):
                    S.pool(lambda va=va: nc.gpsimd.memset(va[:], 1.0), w=[va])
                S.pool(lambda: nc.gpsimd.tensor_copy(out=VCa[:, :, 0:64], in_=VC[g][:, :, :]), r=[VC[g], VCa], w=[VCa])
                for q4 in range(4):
                    tsl = slice(q4 * 16, (q4 + 1) * 16)
                    rsl = slice(q4 * 2048, (q4 + 1) * 2048)
                    S.dma("pool", VSa[:, tsl, 0:64], vs_tok[rsl, g * 64:(g + 1) * 64].rearrange("(t p) d -> p t d", p=128),
                          r=[vs_tok], w=[VSa])
                    S.dma("pool", VWa[:, tsl, 0:64], vw_tok[rsl, g * 64:(g + 1) * 64].rearrange("(t p) d -> p t d", p=128),
                          r=[vw_tok], w=[VWa])
                cnt = {"pt": 0, "mk": 0, "ps": 0, "mul": 0}

                def ps3():
                    b_ = S.psb[cnt["ps"] % 4]
                    cnt["ps"] += 1
                    return b_

                for c in range(16):
                    qs = slice(c * 512, (c + 1) * 512)
                    gc_ = gch[c % 2]
                    S.dma("sp", gc_[:], GS[:, qs], r=[GS], w=[gc_])
                    for br in range(3):
                        items = []
                        if br == 0:
                            for nt in range(4):
                                D = c - 4 * nt
                                if D < 0:
                                    continue
                                items.append((nt, 128 if nt < 3 else 127, "const", Mk01[D] if D <= 4 else None))
                            kk, Va = KC2, VCa
                        elif br == 1:
                            for kt in range(4 * c + 4):
                                items.append((kt, 128, "sel", Mc01[kt - 4 * c] if kt >= 4 * c else None))
                            kk, Va = ksT2, VSa
                        else:
                            for jj in range(-4, 4):
                                kt = 4 * c + jj
                                if kt < 0:
                                    continue
                                items.append((kt, 128, "const", Mw01[-jj] if jj < 0 else Mc01[jj]))
                            kk, Va = kwT2, VWa
                        work = [(ii, hg) for ii in range(len(items)) for hg in range(4)]
                        state = {}

                        def stage1(w_):
                            ii, hg = w_
                            kt, nk, kind, arg = items[ii]
                            if kind == "sel" and hg == 0:
                                pm = ps3()
                                mm(S, pm, pm[0:nk, :], Ebig, Ebig[:, kt * 128:kt * 128 + nk], sel01T, sel01T[:, qs], True, True)
                                mk = mks[cnt["mk"] % 3]
                                cnt["mk"] += 1
                                if arg is None:
                                    S.act(lambda: nc.scalar.copy(out=mk[0:nk, :], in_=pm[0:nk, :]), r=[pm], w=[mk])
                                else:
                                    S.dve(lambda: nc.vector.tensor_tensor(out=mk[0:nk, :], in0=pm[0:nk, :], in1=arg[0:nk, :],
                                                                          op=ALU.mult), r=[pm, arg], w=[mk])
                                state[("mk", ii)] = mk
                            mask = state[("mk", ii)] if kind == "sel" else arg
                            pb_ = (hg % 2) * 64
                            pst = ps3()
                            mm(S, pst, pst[0:nk, :], kk, kk[pb_:pb_ + 64, kt * 128:kt * 128 + nk], QTp, QTp[pb_:pb_ + 64, hg // 2, qs],
                               True, True)
                            pt = pts[cnt["pt"] % NPT]
                            cnt["pt"] += 1
                            S.act(lambda: nc.scalar.activation(out=pt[0:nk, :], in_=pst[0:nk, :], func=AF.Exp, scale=SC),
                                  r=[pst], w=[pt])
                            if mask is not None:
                                cnt["mul"] += 1
                                if cnt["mul"] % 2 == 0:
                                    S.dve(lambda: nc.vector.tensor_tensor(out=pt[0:nk, :], in0=pt[0:nk, :], in1=mask[0:nk, :],
                                                                          op=ALU.mult), r=[pt, mask], w=[pt])
                                else:
                                    S.pool(lambda: nc.gpsimd.tensor_tensor(out=pt[0:nk, :], in0=pt[0:nk, :], in1=mask[0:nk, :],
                                                                           op=ALU.mult), r=[pt, mask], w=[pt])
                            state[("pt", ii, hg)] = pt

                        def stage2(w_):
                            ii, hg = w_
                            kt, nk, kind, arg = items[ii]
                            pt = state.pop(("pt", ii, hg))
                            po = S.psb[4 + hg]
                            mm(S, po, po[0:65, :], Va, Va[0:nk, kt, :], pt, pt[0:nk, :], ii == 0, ii == len(items) - 1)

                        DEPTH = 3
                        for i_ in range(len(work) + DEPTH):
                            if i_ < len(work):
                                stage1(work[i_])
                            if i_ >= DEPTH:
                                stage2(work[i_ - DEPTH])
                        for hg in range(4):
                            po = S.psb[4 + hg]
                            S.dve(lambda po=po: nc.vector.tensor_scalar(out=rdn[64:65, :], in0=po[64:65, :], scalar1=C.tiny[64:65, 0:1],
                                                                        scalar2=None, op0=ALU.max), r=[po, C.tiny], w=[rdn])
                            S.dve(lambda: nc.vector.reciprocal(out=rdn[64:65, :], in_=rdn[64:65, :]), r=[rdn], w=[rdn])
                            pbc = ps3()
                            mm(S, pbc, pbc[0:64, :], C.ones_f, C.ones_f[64:65, 0:64], rdn, rdn[64:65, :], True, True)
                            S.act(lambda pbc=pbc: nc.scalar.copy(out=rb[:], in_=pbc[0:64, :]), r=[pbc], w=[rb])
                            S.dve(lambda po=po: nc.vector.tensor_tensor(out=t1[:], in0=po[0:64, :], in1=rb[:], op=ALU.mult),
                                  r=[po, rb], w=[t1])
                            pg = ps3()
                            mm(S, pg, pg[0:64, :], Sel12, Sel12[:, hg * 3 + br, :], gc_, gc_[:, :], True, True)
                            if br == 0:
                                S.dve(lambda pg=pg, hg=hg: nc.vector.tensor_tensor(out=acc[hg][:], in0=t1[:], in1=pg[0:64, :], op=ALU.mult),
                                      r=[t1, pg], w=[acc[hg]])
                            else:
                                S.dve(lambda pg=pg: nc.vector.tensor_tensor(out=t2[:], in0=t1[:], in1=pg[0:64, :], op=ALU.mult),
                                      r=[t1, pg], w=[t2])
                                S.pool(lambda hg=hg: nc.gpsimd.tensor_tensor(out=acc[hg][:], in0=acc[hg][:], in1=t2[:], op=ALU.add),
                                       r=[acc[hg], t2], w=[acc[hg]])
                    for hg in range(4):
                        head_ = g * 4 + hg
                        ob = obf[hg % 2]
                        S.act(lambda ob=ob, hg=hg: nc.scalar.copy(out=ob[:], in_=acc[hg][:]), r=[acc[hg]], w=[ob])
                        S.dma("sp", oT_dram[head_ * 64:(head_ + 1) * 64, qs], ob[:], r=[ob], w=[oT_dram])
            S.barrier()
        S.barrier()


W_NAMES = ["ev_w_in", "ev_w_uq", "ev_w_ukv", "ev_w_out", "ev_ff_gate", "ev_ff_up", "ev_ff_down",
           "od_w_in", "od_cmp_k1", "od_cmp_k2", "od_cmp_v1", "od_cmp_v2", "od_w_out", "od_router",
           "od_moe_w1", "od_moe_w3", "od_moe_w2"]
L_NAMES = ["xT", "cT", "ada_w", "ada_bT", "norm_gT", "final_gT", "pos64", "inv_freq2", "rope_sign", "ev_q_normT",
           "ev_kv_normT", "ev_conv_wT", "ev_a_log_b", "ev_dt_bias_b", "ev_dn_normT", "od_cmp_pos_kT", "od_cmp_pos_vT",
           "od_router_b_b"]


def build_program(n_layers=4):
    nc = bass.Bass("TRN2", target_bir_lowering=False)
    with ExitStack() as es:
        S = Sched(nc, es, ext_in=W_NAMES + L_NAMES, ext_out=["outT"])
        C = Ctx()
        S.init_psum()
        setup_consts(S, C)
        setup_attn_consts(S, C)
        phase_ada(S, C, 4)
        xT = S.dram("xT", [1024, 8192], F32)
        XA = S.dram("XA", [1024, 8192], F32)
        XB = S.dram("XB", [1024, 8192], F32)
        PT = S.dram("PT", [2608, 8192], F32)
        OT = S.dram("OT", [1024, 8192], BF16)
        VSd = S.dram("VS", [8192, 256], F32)
        VWd = S.dram("VW", [8192, 256], F32)
        outT = S.dram("outT", [1024, 8192], F32)
        ev_w_in = S.dram("ev_w_in", [2, 1024, 2504], F32)
        ev_w_out = S.dram("ev_w_out", [2, 1024, 1024], F32)
        wg = S.dram("ev_ff_gate", [2, 1024, 2816], F32)
        wu = S.dram("ev_ff_up", [2, 1024, 2816], F32)
        wd = S.dram("ev_ff_down", [2, 2816, 1024], F32)
        od_w_in = S.dram("od_w_in", [2, 1024, 2608], F32)
        od_w_out = S.dram("od_w_out", [2, 1024, 1024], F32)
        rt = S.dram("od_router", [2, 1024, 8], F32)
        rtb = S.dram("od_router_b_b", [2, 128, 8], F32)
        m1 = S.dram("od_moe_w1", [2, 8, 1024, 3584], F32)
        m3 = S.dram("od_moe_w3", [2, 8, 1024, 3584], F32)
        m2 = S.dram("od_moe_w2", [2, 8, 3584, 1024], F32)
        x_cur = xT
        for layer in range(n_layers):
            j = layer // 2
            ls = layer * 2
            if layer % 2 == 0:
                phase_inproj(S, C, x_cur, ls, ev_w_in, ev_w_in[j], 2504, PT)
                phase_dn(S, C, PT, j, OT)
                phase_mla(S, C, PT, j, OT)
                phase_outproj(S, C, x_cur, ls, ev_w_out, ev_w_out[j], OT, XA)
                phase_ffn(S, C, XA, ls + 1, XB, 2816, 1, wg, lambda e, j=j: wg[j], wu, lambda e, j=j: wu[j],
                          wd, lambda e, j=j: wd[j], TB=1024)
            else:
                phase_inproj(S, C, x_cur, ls, od_w_in, od_w_in[j], 2608, PT, tok_major=[(1792, 256, VSd), (2304, 256, VWd)])
                phase_nsa2(S, C, PT, VSd, VWd, j, OT)
                phase_outproj(S, C, x_cur, ls, od_w_out, od_w_out[j], OT, XA)
                phase_ffn(S, C, XA, ls + 1, XB, 3584, 8, m1, lambda e, j=j: m1[j, e], m3, lambda e, j=j: m3[j, e],
                          m2, lambda e, j=j: m2[j, e], router=(rt, rt[j], rtb, rtb[j]), TB=1024)
            x_cur = XB
        phase_final(S, C, x_cur, outT)
        S.finish()
    return nc, S


def kernel(**inputs):
    f = np.float32
    nc, _ = build_program()
    shared = {k: np.ascontiguousarray(np.asarray(inputs[k], f)) for k in W_NAMES}
    in_maps = []
    for core in range(8):
        b = core % 4
        m = common_inputs(inputs, b)
        m["od_router_b_b"] = np.ascontiguousarray(np.broadcast_to(np.asarray(inputs["od_router_b"], f)[:, None, :], (2, 128, 8)))
        d = dict(shared)
        for k in L_NAMES:
            d[k] = m[k]
        in_maps.append(d)
    res = run_bass_kernel_spmd(nc, in_maps, core_ids=list(range(8)))
    out = np.stack([np.ascontiguousarray(res.results[b]["outT"].T) for b in range(4)], axis=0)
    return out.astype(np.float32)
```

```python
import threading
import numpy as np
from contextlib import ExitStack
import concourse.bass as bass
import concourse.mybir as mybir
from concourse.bass_utils import run_bass_kernel_spmd

F32 = mybir.dt.float32
BF16 = mybir.dt.bfloat16
I32 = mybir.dt.int32
ALU = mybir.AluOpType
AF = mybir.ActivationFunctionType
AX = mybir.AxisListType

S_LEN = 8192
DM = 1024
NB = S_LEN // 512
NT = S_LEN // 128
EPS = 1e-6
NEG = -30000.0
SES = True


class Buf:
    __slots__ = ("t", "name", "lw", "rd")

    def __init__(self, t, name=""):
        self.t = t
        self.name = name
        self.lw = None
        self.rd = {}

    def __getitem__(self, i):
        return self.t[i]


class Interleaver:
    def __init__(self, S):
        self.S = S

    def run(self, fns):
        n = len(fns)
        self.n = n
        self.sems = [threading.Semaphore(0) for _ in range(n)]
        self.alive = [True] * n
        self.err = None
        self.tls = threading.local()
        threads = [threading.Thread(target=self._wrap, args=(i, fns[i])) for i in range(n)]
        self.S.coop = self
        for t in threads:
            t.start()
        self.sems[0].release()
        for t in threads:
            t.join()
        self.S.coop = None
        if self.err is not None:
            raise self.err

    def idx(self):
        return getattr(self.tls, "i", None)

    def _next(self, i):
        for k in range(1, self.n + 1):
            j = (i + k) % self.n
            if self.alive[j] and j != i:
                return j
        return None

    def _wrap(self, i, fn):
        self.sems[i].acquire()
        self.tls.i = i
        try:
            if self.err is None:
                fn()
        except BaseException as e:
            if self.err is None:
                self.err = e
        finally:
            self.alive[i] = False
            j = self._next(i)
            if j is not None:
                self.sems[j].release()

    def step(self):
        i = self.idx()
        if i is None:
            return
        if self.err is not None:
            raise RuntimeError("interleave abort")
        j = self._next(i)
        if j is None:
            return
        self.sems[j].release()
        self.sems[i].acquire()
        if self.err is not None:
            raise RuntimeError("interleave abort")


class Sched:
    def __init__(self, nc, es, ext_in=(), ext_out=()):
        self.nc = nc
        self.es = es
        self.ext_in = set(ext_in)
        self.ext_out = set(ext_out)
        self.eng = {"pe": nc.tensor, "act": nc.scalar, "dve": nc.vector, "pool": nc.gpsimd, "sp": nc.sync}
        self.sems = {}
        self.cnt = {}
        for e in self.eng:
            self.sems[e] = es.enter_context(nc.semaphore("s_" + e))
            self.cnt[e] = 0
        self.waited = {e: {} for e in self.eng}
        self.lanes = {"sp": 12, "pool": 8, "act": 4}
        self.lane_rr = {q: 0 for q in self.lanes}
        for q, n in self.lanes.items():
            for i in range(n):
                k = f"{q}{i}"
                self.sems[k] = es.enter_context(nc.semaphore("l_" + k))
                self.cnt[k] = 0
        self.coop = None
        self.coop_ps = {}
        self.psb = []
        self.ps_rr = 0
        self.ps4_rr = 0
        self.uid = 0
        self.dram_bufs = {}

    def sb(self, es, shape, dt, name=None):
        self.uid += 1
        name = (name or "t") + f"_{self.uid}"
        return Buf(es.enter_context(self.nc.sbuf_tensor(name, list(shape), dt)), name)

    def dram(self, name, shape, dt):
        if name in self.dram_bufs:
            return self.dram_bufs[name]
        kind = "ExternalInput" if name in self.ext_in else ("ExternalOutput" if name in self.ext_out else "Internal")
        b = Buf(self.nc.dram_tensor(name, list(shape), dt, kind=kind).ap(), name)
        self.dram_bufs[name] = b
        return b

    def init_psum(self):
        for i in range(8):
            self.psb.append(Buf(self.es.enter_context(self.nc.psum_tensor(f"ps{i}", [128, 512], F32)), f"ps{i}"))

    def ps(self):
        if self.coop is not None and self.coop.idx() is not None:
            i = self.coop.idx()
            k = self.coop_ps.get(i, 0)
            self.coop_ps[i] = k + 1
            return self.psb[2 * i + (k % 2)]
        b = self.psb[self.ps_rr]
        self.ps_rr = (self.ps_rr + 1) % 8
        return b

    def ps4(self):
        b = self.psb[self.ps4_rr]
        self.ps4_rr = (self.ps4_rr + 1) % 4
        return b

    def _wait(self, e, key, val):
        if val <= 0 or self.waited[e].get(key, 0) >= val:
            return
        self.eng[e].wait_ge(self.sems[key], val)
        self.waited[e][key] = val

    def _deps(self, e, r, w, is_dma=False):
        deps = {}
        for b in r:
            if b.lw is not None:
                k, v = b.lw
                if deps.get(k, 0) < v:
                    deps[k] = v
        for b in w:
            if b.lw is not None:
                k, v = b.lw
                if deps.get(k, 0) < v:
                    deps[k] = v
            for k, v in b.rd.items():
                if deps.get(k, 0) < v:
                    deps[k] = v
        for k, v in deps.items():
            if k == e and not is_dma and not (SES and e != "pe"):
                continue
            self._wait(e, k, v)

    def _commit(self, ev, r, w):
        k, v = ev
        for b in w:
            b.lw = ev
            b.rd = {}
        for b in r:
            if b.rd.get(k, 0) < v:
                b.rd[k] = v

    def op(self, e, fn, r=(), w=()):
        self._deps(e, r, w)
        ins = fn()
        self.cnt[e] += 1
        ins.then_inc(self.sems[e], 1)
        self._commit((e, self.cnt[e]), r, w)
        if self.coop is not None:
            self.coop.step()

    def pe(self, fn, r=(), w=()):
        self.op("pe", fn, r, w)

    def act(self, fn, r=(), w=()):
        self.op("act", fn, r, w)

    def dve(self, fn, r=(), w=()):
        self.op("dve", fn, r, w)

    def pool(self, fn, r=(), w=()):
        self.op("pool", fn, r, w)

    def dma(self, q, out, in_, r=(), w=(), **kw):
        self._deps(q, r, w, is_dma=True)
        i = self.lane_rr[q]
        self.lane_rr[q] = (i + 1) % self.lanes[q]
        key = f"{q}{i}"
        c = self.cnt[key]
        self._wait(q, key, 16 * c)
        ins = self.eng[q].dma_start(out=out, in_=in_, **kw)
        ins.then_inc(self.sems[key], 16)
        self.cnt[key] = c + 1
        self._commit((key, 16 * (c + 1)), r, w)
        if self.coop is not None:
            self.coop.step()

    def barrier(self):
        for e in self.eng:
            for k, c in self.cnt.items():
                if k == e or c == 0:
                    continue
                self._wait(e, k, c if k in self.eng else 16 * c)

    def finish(self):
        for k, c in self.cnt.items():
            if k == "sp" or c == 0:
                continue
            self._wait("sp", k, c if k in self.eng else 16 * c)


def mm(S, out_b, out_ap, l_b, l_ap, r_b, r_ap, start, stop):
    S.pe(lambda: S.nc.tensor.matmul(out_ap, l_ap, r_ap, start=start, stop=stop), r=[l_b, r_b], w=[out_b])


class Ctx:
    pass


def setup_consts(S, C):
    nc = S.nc
    es = S.es
    C.ident = S.sb(es, [128, 128], F32, "ident")
    C.ones_bf = S.sb(es, [128, 128], BF16, "ones_bf")
    C.ones_f = S.sb(es, [128, 128], F32, "ones_f")
    S.pool(lambda: nc.gpsimd.memset(C.ident[:], 0.0), w=[C.ident])
    S.pool(lambda: nc.gpsimd.affine_select(out=C.ident[:], in_=C.ident[:], pattern=[[-1, 128]],
                                           compare_op=ALU.not_equal, fill=1.0, base=0, channel_multiplier=1),
           r=[C.ident], w=[C.ident])
    S.pool(lambda: nc.gpsimd.memset(C.ones_bf[:], 1.0), w=[C.ones_bf])
    S.pool(lambda: nc.gpsimd.memset(C.ones_f[:], 1.0), w=[C.ones_f])
    C.eps = S.sb(es, [128, 1], F32, "eps")
    S.pool(lambda: nc.gpsimd.memset(C.eps[:], EPS), w=[C.eps])


def phase_ada(S, C, n_layers):
    nc = S.nc
    c_in = S.dram("cT", [128, 8], F32)
    ada_w = S.dram("ada_w", [4, 2, 1024, 3072], F32)
    ada_b = S.dram("ada_bT", [128, 8 * 24], F32)
    norm_g = S.dram("norm_gT", [128, 8 * 8], F32)
    C.mods = S.sb(S.es, [128, 8 * 24], F32, "mods")
    C.modA = S.sb(S.es, [128, 8 * 8], F32, "modA")
    with ExitStack() as es:
        sc = S.sb(es, [128, 8], F32, "sc")
        bb = S.sb(es, [128, 8 * 24], F32, "bb")
        gg = S.sb(es, [128, 8 * 8], F32, "gg")
        wbuf = [S.sb(es, [128, 8, 3072 // 2], F32, "adaw") for _ in range(2)]
        S.dma("sp", sc[:], c_in[:, :], r=[c_in], w=[sc])
        S.dma("sp", bb[:], ada_b[:, :], r=[ada_b], w=[bb])
        S.dma("sp", gg[:], norm_g[:, :], r=[norm_g], w=[gg])
        S.act(lambda: nc.scalar.activation(out=sc[:], in_=sc[:], func=AF.Silu), r=[sc], w=[sc])
        it = 0
        for l in range(n_layers):
            for s in range(2):
                ls = l * 2 + s
                for half in range(2):
                    wb = wbuf[it % 2]
                    it += 1
                    src = ada_w[l, s, :, half * 1536:(half + 1) * 1536].rearrange("(kc p) n -> p kc n", p=128)
                    for kc in range(8):
                        S.dma("sp", wb[:, kc, :], src[:, kc, :], r=[ada_w], w=[wb])
                    ps = S.ps()
                    for fc in range(12):
                        for kc in range(8):
                            mm(S, ps, ps[:, fc:fc + 1], wb, wb[:, kc, fc * 128:(fc + 1) * 128], sc, sc[:, kc:kc + 1],
                               kc == 0, kc == 7)
                    c0 = ls * 24 + half * 12
                    S.dve(lambda ps=ps, c0=c0: nc.vector.tensor_tensor(out=C.mods[:, c0:c0 + 12], in0=ps[:, 0:12],
                                                                       in1=bb[:, c0:c0 + 12], op=ALU.add),
                          r=[ps, bb], w=[C.mods])
                S.dve(lambda ls=ls: nc.vector.scalar_tensor_tensor(
                    out=C.modA[:, ls * 8:(ls + 1) * 8], in0=C.mods[:, ls * 24 + 8:ls * 24 + 16], scalar=1.0,
                    in1=gg[:, ls * 8:(ls + 1) * 8], op0=ALU.add, op1=ALU.mult), r=[C.mods, gg], w=[C.modA])
        S.barrier()


def modnorm_block(S, C, es_tiles, x_dram, ls, t0, xt, hT, hcol0, want_f32=None):
    nc = S.nc
    sq, rstd, tmp = es_tiles
    src = x_dram[:, t0:t0 + 512].rearrange("(kc p) t -> p kc t", p=128)
    S.dma("sp", xt[:, :, hcol0:hcol0 + 512], src, r=[x_dram], w=[xt])
    for kc in range(8):
        S.pool(lambda kc=kc: nc.gpsimd.tensor_tensor(out=sq[:, kc, :], in0=xt[:, kc, hcol0:hcol0 + 512],
                                                     in1=xt[:, kc, hcol0:hcol0 + 512], op=ALU.mult), r=[xt], w=[sq])
    ps = S.ps()
    for kc in range(8):
        mm(S, ps, ps[:, :], C.ones_bf, C.ones_bf[:, :], sq, sq[:, kc, :], kc == 0, kc == 7)
    S.act(lambda: nc.scalar.activation(out=rstd[:], in_=ps[:, :], func=AF.Sqrt, scale=1.0 / DM, bias=C.eps[:, 0:1]),
          r=[ps, C.eps], w=[rstd])
    S.dve(lambda: nc.vector.reciprocal(out=rstd[:], in_=rstd[:]), r=[rstd], w=[rstd])
    for kc in range(8):
        S.dve(lambda kc=kc: nc.vector.tensor_tensor(out=tmp[:], in0=xt[:, kc, hcol0:hcol0 + 512], in1=rstd[:],
                                                    op=ALU.mult), r=[xt, rstd], w=[tmp])
        a_ap = C.modA[:, ls * 8 + kc:ls * 8 + kc + 1]
        s_ap = C.mods[:, ls * 24 + kc:ls * 24 + kc + 1]
        S.dve(lambda kc=kc, a_ap=a_ap, s_ap=s_ap: nc.vector.tensor_scalar(
            out=hT[:, kc, hcol0:hcol0 + 512], in0=tmp[:], scalar1=a_ap, scalar2=s_ap, op0=ALU.mult, op1=ALU.add),
            r=[tmp, C.modA, C.mods], w=[hT])
        if want_f32 is not None:
            S.dve(lambda kc=kc, a_ap=a_ap, s_ap=s_ap: nc.vector.tensor_scalar(
                out=want_f32[:, kc, 0:512], in0=tmp[:], scalar1=a_ap, scalar2=s_ap, op0=ALU.mult,
                op1=ALU.add), r=[tmp, C.modA, C.mods], w=[want_f32])


def load_w_bf16(S, wt, w_ap, w_dram, K, N, n0=0, ncols=None):
    ncols = ncols or N
    src = w_ap[:, n0:n0 + ncols].rearrange("(kc p) n -> p kc n", p=128)
    for kc in range(K // 128):
        S.dma("pool", wt[:, kc, 0:ncols], src[:, kc, :], r=[w_dram], w=[wt])


def phase_inproj(S, C, x_dram, ls, w_dram, w_ap, ncols, pT_dram, tok_major=()):
    nc = S.nc
    n_ot = (ncols + 127) // 128
    with ExitStack() as es:
        wt = S.sb(es, [128, 8, ncols], BF16, "w_in")
        load_w_bf16(S, wt, w_ap, w_dram, DM, ncols)
        xts = [S.sb(es, [128, 8, 512], F32, "xt") for _ in range(2)]
        hTs = [S.sb(es, [128, 8, 512], BF16, "hT") for _ in range(2)]
        sq = S.sb(es, [128, 8, 512], BF16, "sq")
        rstd = S.sb(es, [128, 512], F32, "rstd")
        tmp = S.sb(es, [128, 512], F32, "tmp")
        stg = [S.sb(es, [128, 512], F32, "stg") for _ in range(4)]
        si = 0
        for tb in range(NB):
            xt = xts[tb % 2]
            hT = hTs[tb % 2]
            modnorm_block(S, C, (sq, rstd, tmp), x_dram, ls, tb * 512, xt, hT, 0)
            for ot in range(n_ot):
                m = min(128, ncols - ot * 128)
                ps = S.ps()
                for kc in range(8):
                    mm(S, ps, ps[0:m, :], wt, wt[:, kc, ot * 128:ot * 128 + m], hT, hT[:, kc, :], kc == 0, kc == 7)
                st = stg[si % 4]
                si += 1
                if ot % 2 == 0:
                    S.act(lambda st=st, ps=ps, m=m: nc.scalar.copy(out=st[0:m, :], in_=ps[0:m, :]), r=[ps], w=[st])
                else:
                    S.dve(lambda st=st, ps=ps, m=m: nc.vector.tensor_copy(out=st[0:m, :], in_=ps[0:m, :]), r=[ps], w=[st])
                S.dma("sp", pT_dram[ot * 128:ot * 128 + m, tb * 512:(tb + 1) * 512], st[0:m, :], r=[st], w=[pT_dram])
            for (c0, n, dst) in tok_major:
                for tt in range(4):
                    ps = S.ps()
                    for kc in range(8):
                        mm(S, ps, ps[:, 0:n], hT, hT[:, kc, tt * 128:(tt + 1) * 128], wt, wt[:, kc, c0:c0 + n],
                           kc == 0, kc == 7)
                    st = stg[si % 4]
                    si += 1
                    S.act(lambda st=st, ps=ps, n=n: nc.scalar.copy(out=st[:, 0:n], in_=ps[:, 0:n]), r=[ps], w=[st])
                    r0 = tb * 512 + tt * 128
                    S.dma("sp", dst[r0:r0 + 128, :], st[:, 0:n], r=[st], w=[dst])
        S.barrier()


def phase_outproj(S, C, x_dram, ls, w_dram, w_ap, oT_dram, xo_dram):
    nc = S.nc
    with ExitStack() as es:
        wt = S.sb(es, [128, 8, DM], BF16, "w_out")
        load_w_bf16(S, wt, w_ap, w_dram, DM, DM)
        xts = [S.sb(es, [128, 8, 512], F32, "xt") for _ in range(2)]
        ots = [S.sb(es, [128, 8, 512], BF16, "oT") for _ in range(2)]
        for tb in range(NB):
            xt = xts[tb % 2]
            ot_ = ots[tb % 2]
            sl = slice(tb * 512, (tb + 1) * 512)
            S.dma("sp", xt[:], x_dram[:, sl].rearrange("(kc p) t -> p kc t", p=128), r=[x_dram], w=[xt])
            S.dma("sp", ot_[:], oT_dram[:, sl].rearrange("(kc p) t -> p kc t", p=128), r=[oT_dram], w=[ot_])
            for oc in range(8):
                ps = S.ps()
                for kc in range(8):
                    mm(S, ps, ps[:, :], wt, wt[:, kc, oc * 128:(oc + 1) * 128], ot_, ot_[:, kc, :], kc == 0, kc == 7)
                g_ap = C.mods[:, ls * 24 + 16 + oc:ls * 24 + 17 + oc]
                S.dve(lambda ps=ps, oc=oc, g_ap=g_ap, xt=xt: nc.vector.scalar_tensor_tensor(
                    out=xt[:, oc, :], in0=ps[:, :], scalar=g_ap, in1=xt[:, oc, :], op0=ALU.mult, op1=ALU.add),
                    r=[ps, C.mods, xt], w=[xt])
            S.dma("sp", xo_dram[:, sl].rearrange("(kc p) t -> p kc t", p=128), xt[:], r=[xt], w=[xo_dram])
        S.barrier()


def phase_ffn(S, C, x_dram, ls, xo_dram, F, n_exp, wg_dram, wg_ap, wu_dram, wu_ap, wd_dram, wd_ap,
              router=None, TB=2048, CT=4, conv=True):
    nc = S.nc
    n_ft = F // 128
    chunks = [(c0, min(CT, n_ft - c0)) for c0 in range(0, n_ft, CT)]
    nsub = TB // 512
    WB = {}
    if conv:
        nq = 4 if n_ft % 4 == 0 else 2
        rq = DM // nq
        tq = n_ft // nq
        for e in range(n_exp):
            for q in range(nq):
                for nm, ap_fn, dr, shape, rows in (("g", wg_ap, wg_dram, [rq, F], slice(q * rq, (q + 1) * rq)),
                                                   ("u", wu_ap, wu_dram, [rq, F], slice(q * rq, (q + 1) * rq)),
                                                   ("d", wd_ap, wd_dram, [tq * 128, DM], slice(q * tq * 128, (q + 1) * tq * 128))):
                    b_ = S.dram(f"WB{nm}_{F}_{e}_{q}", shape, BF16)
                    WB[(nm, e, q)] = b_
                    S.dma("pool", b_[:, :], ap_fn(e)[rows, :], r=[dr], w=[b_])
    with ExitStack() as es:
        xt = S.sb(es, [128, 8, TB], F32, "xt")
        hT = S.sb(es, [128, 8, TB], BF16, "hT")
        sq = S.sb(es, [128, 8, 512], BF16, "sq")
        rstd = S.sb(es, [128, 512], F32, "rstd")
        tmp = S.sb(es, [128, 512], F32, "tmp")
        wgs = [S.sb(es, [128, 8, CT * 128], BF16, "wg") for _ in range(2)]
        wus = [S.sb(es, [128, 8, CT * 128], BF16, "wu") for _ in range(2)]
        wds = [S.sb(es, [128, CT, DM], BF16, "wd") for _ in range(2)]
        acts = [S.sb(es, [128, CT, 512], BF16, "act") for _ in range(2)]
        sgs = [S.sb(es, [128, 512], F32, "sg") for _ in range(2)]
        ytm = [S.sb(es, [128, 512], F32, "ytm") for _ in range(2)]
        if router is not None:
            hF = S.sb(es, [128, 8, 512], F32, "hF")
            wr = S.sb(es, [128, 8, 8], F32, "wr")
            rb = S.sb(es, [128, 8], F32, "rb")
            lg = S.sb(es, [128, 8], F32, "lg")
            mx = S.sb(es, [128, 8], F32, "mx")
            ex = S.sb(es, [128, 8], F32, "ex")
            den = S.sb(es, [128, 2], F32, "den")
            gw = S.sb(es, [128, 8], F32, "gw")
            gwT = S.sb(es, [8, TB], F32, "gwT")
            sel = S.sb(es, [8, 8 * 128], F32, "sel")
            Gs = [S.sb(es, [128, 512], F32, "G") for _ in range(nsub)]
            r_dram, r_ap, rb_dram, rb_ap = router
            S.dma("sp", wr[:], r_ap.rearrange("(kc p) n -> p kc n", p=128), r=[r_dram], w=[wr])
            S.dma("sp", rb[:], rb_ap, r=[rb_dram], w=[rb])
            S.pool(lambda: nc.gpsimd.memset(sel[:], 0.0), w=[sel])
            S.pool(lambda: nc.gpsimd.affine_select(out=sel[:].rearrange("k (e m) -> k e m", m=128),
                                                   in_=sel[:].rearrange("k (e m) -> k e m", m=128),
                                                   pattern=[[-1, 8], [0, 128]], compare_op=ALU.not_equal, fill=1.0,
                                                   base=0, channel_multiplier=1), r=[sel], w=[sel])
        wi = 0
        gi = 0
        ai = 0
        for tb in range(S_LEN // TB):
            for sub in range(nsub):
                modnorm_block(S, C, (sq, rstd, tmp), x_dram, ls, tb * TB + sub * 512, xt, hT, sub * 512,
                              want_f32=(hF if router is not None else None))
                if router is not None:
                    for tt in range(4):
                        ps = S.ps()
                        for kc in range(8):
                            mm(S, ps, ps[:, 0:8], hF, hF[:, kc, tt * 128:(tt + 1) * 128], wr, wr[:, kc, :], kc == 0, kc == 7)
                        S.dve(lambda ps=ps: nc.vector.tensor_tensor(out=lg[:], in0=ps[:, 0:8], in1=rb[:], op=ALU.add),
                              r=[ps, rb], w=[lg])
                        S.dve(lambda: nc.vector.max(out=mx[:], in_=lg[:]), r=[lg], w=[mx])
                        S.dve(lambda: nc.vector.tensor_scalar(out=ex[:], in0=lg[:], scalar1=mx[:, 0:1], scalar2=None,
                                                              op0=ALU.subtract), r=[lg, mx], w=[ex])
                        S.act(lambda: nc.scalar.activation(out=ex[:], in_=ex[:], func=AF.Exp), r=[ex], w=[ex])
                        S.dve(lambda: nc.vector.tensor_scalar(out=gw[:], in0=lg[:], scalar1=mx[:, 1:2], scalar2=None,
                                                              op0=ALU.is_ge), r=[lg, mx], w=[gw])
                        S.dve(lambda: nc.vector.tensor_tensor(out=gw[:], in0=gw[:], in1=ex[:], op=ALU.mult),
                              r=[gw, ex], w=[gw])
                        S.dve(lambda: nc.vector.reduce_sum(out=den[:, 0:1], in_=gw[:], axis=AX.X), r=[gw], w=[den])
                        S.dve(lambda: nc.vector.reciprocal(out=den[:, 1:2], in_=den[:, 0:1]), r=[den], w=[den])
                        S.dve(lambda: nc.vector.tensor_scalar(out=gw[:], in0=gw[:], scalar1=den[:, 1:2], scalar2=None,
                                                              op0=ALU.mult), r=[gw, den], w=[gw])
                        pt = S.ps()
                        S.pe(lambda pt=pt: nc.tensor.transpose(out=pt[0:8, 0:128], in_=gw[:, :], identity=C.ident[:, :]),
                             r=[gw, C.ident], w=[pt])
                        c0 = sub * 512 + tt * 128
                        S.act(lambda pt=pt, c0=c0: nc.scalar.copy(out=gwT[:, c0:c0 + 128], in_=pt[0:8, 0:128]),
                              r=[pt], w=[gwT])
            for e in range(n_exp):
                for (c0, ct) in chunks:
                    wg = wgs[wi % 2]
                    wu = wus[wi % 2]
                    wd = wds[wi % 2]
                    wi += 1
                    if conv:
                        kpq = rq // 128
                        for kc in range(8):
                            q, lk = kc // kpq, kc % kpq
                            for nm, wt_ in (("g", wg), ("u", wu)):
                                b_ = WB[(nm, e, q)]
                                S.dma("sp", wt_[:, kc, 0:ct * 128], b_[lk * 128:(lk + 1) * 128, c0 * 128:(c0 + ct) * 128],
                                      r=[b_], w=[wt_])
                        for f in range(ct):
                            q, lt = (c0 + f) // tq, (c0 + f) % tq
                            b_ = WB[("d", e, q)]
                            S.dma("sp", wd[:, f, :], b_[lt * 128:(lt + 1) * 128, :], r=[b_], w=[wd])
                    else:
                        load_w_bf16(S, wg, wg_ap(e), wg_dram, DM, F, n0=c0 * 128, ncols=ct * 128)
                        load_w_bf16(S, wu, wu_ap(e), wu_dram, DM, F, n0=c0 * 128, ncols=ct * 128)
                        S.dma("pool", wd[:, 0:ct, :], wd_ap(e)[c0 * 128:(c0 + ct) * 128, :].rearrange("(c p) n -> p c n", p=128),
                              r=[wd_dram], w=[wd])
                    for sub in range(nsub):
                        ts = slice(sub * 512, (sub + 1) * 512)
                        G = None
                        if router is not None:
                            G = Gs[sub]
                        if router is not None and c0 == 0:
                            pg = S.ps()
                            mm(S, pg, pg[:, :], sel, sel[:, e * 128:(e + 1) * 128], gwT, gwT[:, ts], True, True)
                            S.act(lambda G=G, pg=pg: nc.scalar.copy(out=G[:], in_=pg[:, :]), r=[pg], w=[G])
                        act_t = acts[ai % 2]
                        ai += 1
                        for f in range(ct):
                            pg = S.ps()
                            pu = S.ps()
                            for kc in range(8):
                                mm(S, pg, pg[:, :], wg, wg[:, kc, f * 128:(f + 1) * 128], hT, hT[:, kc, ts], kc == 0, kc == 7)
                            for kc in range(8):
                                mm(S, pu, pu[:, :], wu, wu[:, kc, f * 128:(f + 1) * 128], hT, hT[:, kc, ts], kc == 0, kc == 7)
                            sg = sgs[f % 2]
                            S.act(lambda sg=sg, pg=pg: nc.scalar.activation(out=sg[:], in_=pg[:, :], func=AF.Silu),
                                  r=[pg], w=[sg])
                            S.dve(lambda sg=sg, pu=pu, f=f, act_t=act_t: nc.vector.tensor_tensor(
                                out=act_t[:, f, :], in0=sg[:], in1=pu[:, :], op=ALU.mult), r=[sg, pu], w=[act_t])
                        for oc in range(8):
                            py = S.ps()
                            for f in range(ct):
                                mm(S, py, py[:, :], wd, wd[:, f, oc * 128:(oc + 1) * 128], act_t, act_t[:, f, :],
                                   f == 0, f == ct - 1)
                            g_ap = C.mods[:, ls * 24 + 16 + oc:ls * 24 + 17 + oc]
                            if G is None:
                                S.dve(lambda py=py, oc=oc, g_ap=g_ap, ts=ts: nc.vector.scalar_tensor_tensor(
                                    out=xt[:, oc, ts], in0=py[:, :], scalar=g_ap, in1=xt[:, oc, ts], op0=ALU.mult,
                                    op1=ALU.add), r=[py, C.mods, xt], w=[xt])
                            else:
                                yt = ytm[oc % 2]
                                S.dve(lambda py=py, yt=yt, G=G: nc.vector.tensor_tensor(out=yt[:], in0=py[:, :], in1=G[:],
                                                                                        op=ALU.mult), r=[py, G], w=[yt])
                                S.dve(lambda yt=yt, oc=oc, g_ap=g_ap, ts=ts: nc.vector.scalar_tensor_tensor(
                                    out=xt[:, oc, ts], in0=yt[:], scalar=g_ap, in1=xt[:, oc, ts], op0=ALU.mult,
                                    op1=ALU.add), r=[yt, C.mods, xt], w=[xt])
            for kc in range(8):
                S.dma("sp", xo_dram[kc * 128:(kc + 1) * 128, tb * TB:(tb + 1) * TB], xt[:, kc, :], r=[xt], w=[xo_dram])
        S.barrier()


def phase_final(S, C, x_dram, out_dram):
    nc = S.nc
    fg = S.dram("final_gT", [128, 8], F32)
    with ExitStack() as es:
        g = S.sb(es, [128, 8], F32, "fg")
        S.dma("sp", g[:], fg[:, :], r=[fg], w=[g])
        xts = [S.sb(es, [128, 8, 512], F32, "xt") for _ in range(2)]
        sq = S.sb(es, [128, 8, 512], BF16, "sq")
        rstd = S.sb(es, [128, 512], F32, "rstd")
        for tb in range(NB):
            xt = xts[tb % 2]
            sl = slice(tb * 512, (tb + 1) * 512)
            S.dma("sp", xt[:], x_dram[:, sl].rearrange("(kc p) t -> p kc t", p=128), r=[x_dram], w=[xt])
            for kc in range(8):
                S.pool(lambda kc=kc, xt=xt: nc.gpsimd.tensor_tensor(out=sq[:, kc, :], in0=xt[:, kc, :], in1=xt[:, kc, :],
                                                                    op=ALU.mult), r=[xt], w=[sq])
            ps = S.ps()
            for kc in range(8):
                mm(S, ps, ps[:, :], C.ones_bf, C.ones_bf[:, :], sq, sq[:, kc, :], kc == 0, kc == 7)
            S.act(lambda ps=ps: nc.scalar.activation(out=rstd[:], in_=ps[:, :], func=AF.Sqrt, scale=1.0 / DM,
                                                     bias=C.eps[:, 0:1]), r=[ps, C.eps], w=[rstd])
            S.dve(lambda: nc.vector.reciprocal(out=rstd[:], in_=rstd[:]), r=[rstd], w=[rstd])
            for kc in range(8):
                S.dve(lambda kc=kc, xt=xt: nc.vector.scalar_tensor_tensor(
                    out=xt[:, kc, :], in0=xt[:, kc, :], scalar=g[:, kc:kc + 1], in1=rstd[:], op0=ALU.mult, op1=ALU.mult),
                    r=[xt, g, rstd], w=[xt])
            S.dma("sp", out_dram[:, sl].rearrange("(kc p) t -> p kc t", p=128), xt[:], r=[xt], w=[out_dram])
        S.barrier()


def common_inputs(inputs, b):
    f = np.float32
    m = {}
    m["xT"] = np.ascontiguousarray(np.asarray(inputs["x"][b], f).T)
    m["cT"] = np.ascontiguousarray(np.asarray(inputs["c"][b], f).reshape(8, 128).T)
    m["ada_w"] = np.asarray(inputs["ada_w"], f)
    m["ada_bT"] = np.ascontiguousarray(np.asarray(inputs["ada_b"], f).reshape(8, 24, 128).transpose(2, 0, 1).reshape(128, 192))
    m["norm_gT"] = np.ascontiguousarray(np.asarray(inputs["norm_g"], f).reshape(8, 8, 128).transpose(2, 0, 1).reshape(128, 64))
    m["final_gT"] = np.ascontiguousarray(np.asarray(inputs["final_g"], f).reshape(8, 128).T)
    m["pos64"] = np.ascontiguousarray(np.broadcast_to(np.asarray(inputs["positions"][b], np.int32)[None, :], (64, 8192)))
    inv = (1.0 / (10000.0 ** (np.arange(0, 64, 2, dtype=np.float32) / 64))).astype(f)
    m["inv_freq2"] = np.concatenate([inv, inv]).reshape(64, 1).astype(f)
    m["rope_sign"] = np.concatenate([-np.ones(32, f), np.ones(32, f)]).reshape(64, 1)
    m["ev_q_normT"] = np.ascontiguousarray(np.asarray(inputs["ev_q_norm"], f).reshape(2, 2, 128).transpose(0, 2, 1))
    m["ev_kv_normT"] = np.ascontiguousarray(np.asarray(inputs["ev_kv_norm"], f).reshape(2, 128, 1))
    m["ev_conv_wT"] = np.ascontiguousarray(np.asarray(inputs["ev_conv_w"], f).reshape(2, 4, 12, 128).transpose(0, 3, 2, 1))
    m["ev_a_log_b"] = np.ascontiguousarray(np.broadcast_to(np.asarray(inputs["ev_a_log"], f)[:, None, :], (2, 128, 4)))
    m["ev_dt_bias_b"] = np.ascontiguousarray(np.broadcast_to(np.asarray(inputs["ev_dt_bias"], f)[:, None, :], (2, 128, 4)))
    m["ev_dn_normT"] = np.ascontiguousarray(np.asarray(inputs["ev_dn_norm"], f).reshape(2, 128, 1))
    m["od_cmp_pos_kT"] = np.ascontiguousarray(np.asarray(inputs["od_cmp_pos_k"], f).transpose(0, 2, 1))
    m["od_cmp_pos_vT"] = np.ascontiguousarray(np.asarray(inputs["od_cmp_pos_v"], f).transpose(0, 2, 1))
    return m


def setup_attn_consts(S, C):
    nc = S.nc
    es = S.es
    C.ident_bf = S.sb(es, [128, 128], BF16, "ident_bf")
    S.dve(lambda: nc.vector.tensor_copy(out=C.ident_bf[:], in_=C.ident[:]), r=[C.ident], w=[C.ident_bf])
    C.tiny = S.sb(es, [128, 1], F32, "tiny")
    S.pool(lambda: nc.gpsimd.memset(C.tiny[:], 1e-30), w=[C.tiny])

    def mask_tile(name, base, cm, step, op=ALU.is_ge):
        t = S.sb(es, [128, 512], BF16, name)
        S.pool(lambda: nc.gpsimd.memset(t[:], 0.0), w=[t])
        S.pool(lambda: nc.gpsimd.affine_select(out=t[:], in_=t[:], pattern=[[step, 512]], compare_op=op, fill=NEG,
                                               base=base, channel_multiplier=cm), r=[t], w=[t])
        return t
    C.Mc = [mask_tile(f"Mc{j}", -128 * j, -1, 1) for j in range(4)]


def attn_chunk(S, C, c, bank, kparts, qparts, V, dv, kts, scale, pts, ot, rden, pi0=0):
    nc = S.nc
    po = S.psb[4 + bank % 2]
    pd = S.psb[6 + bank % 2]
    qs = slice(c * 512, (c + 1) * 512)
    n = len(kts)
    held = {}

    def st1(i):
        kt, nk, masks = kts[i]
        pst = S.ps4()
        n_mm = len(kparts) + len(masks)
        j = 0
        for (kb, rows), (qb, _) in zip(kparts, qparts):
            mm(S, pst, pst[0:nk, :], kb, kb[0:rows, kt * 128:kt * 128 + nk], qb, qb[0:rows, qs], j == 0, j == n_mm - 1)
            j += 1
        for (lb, lap, rb, rap) in masks:
            mm(S, pst, pst[0:nk, :], lb, lap, rb, rap, False, j == n_mm - 1)
            j += 1
        pt = pts[(pi0 + i) % len(pts)]
        S.act(lambda: nc.scalar.activation(out=pt[0:nk, :], in_=pst[0:nk, :], func=AF.Exp, scale=scale), r=[pst], w=[pt])
        held[i] = pt

    def st2(i):
        kt, nk, masks = kts[i]
        pt = held.pop(i)
        mm(S, po, po[0:dv, :], V, V[0:nk, kt, :], pt, pt[0:nk, :], i == 0, i == n - 1)
        mm(S, pd, pd[0:dv, :], C.ones_bf, C.ones_bf[0:nk, 0:dv], pt, pt[0:nk, :], i == 0, i == n - 1)

    D = min(2, len(pts) - 1)
    for i in range(n + D):
        if i < n:
            st1(i)
        if i >= D:
            st2(i - D)
    S.dve(lambda: nc.vector.tensor_scalar(out=rden[0:dv, :], in0=pd[0:dv, :], scalar1=C.tiny[0:dv, 0:1],
                                          scalar2=None, op0=ALU.max), r=[pd, C.tiny], w=[rden])
    S.dve(lambda: nc.vector.reciprocal(out=rden[0:dv, :], in_=rden[0:dv, :]), r=[rden], w=[rden])
    S.dve(lambda: nc.vector.tensor_tensor(out=ot[0:dv, :], in0=po[0:dv, :], in1=rden[0:dv, :], op=ALU.mult),
          r=[po, rden], w=[ot])
    return pi0 + n


def attn_head(S, C, es, kparts, qparts, V, dv, kts_fn, scale, out_cb, pts, otiles, rden):
    pi = 0
    for c in range(16):
        ot = otiles[c % len(otiles)]
        pi = attn_chunk(S, C, c, c, kparts, qparts, V, dv, kts_fn(c), scale, pts, ot, rden, pi)
        out_cb(c, ot)


def phase_mla(S, C, pT, j, oT_dram):
    nc = S.nc
    R_CQ, R_CKV, R_KR = 2056, 2312, 2440
    w_uq = S.dram("ev_w_uq", [2, 256, 768], F32)
    w_ukv = S.dram("ev_w_ukv", [2, 128, 1024], F32)
    qn_d = S.dram("ev_q_normT", [2, 128, 2], F32)
    kvn_d = S.dram("ev_kv_normT", [2, 128, 1], F32)
    pos_d = S.dram("pos64", [64, 8192], I32)
    inv_d = S.dram("inv_freq2", [64, 1], F32)
    sgn_d = S.dram("rope_sign", [64, 1], F32)
    with ExitStack() as es:
        cos2 = S.sb(es, [64, 8192], BF16, "cos2")
        sin2 = S.sb(es, [64, 8192], BF16, "sin2s")
        cqn = S.sb(es, [128, 2, 8192], BF16, "cqn")
        ckvn = S.sb(es, [128, 8192], BF16, "ckvn")
        KR = S.sb(es, [64, 8192], BF16, "KR")
        wq = S.sb(es, [128, 2, 768], BF16, "wq")
        wqs = S.sb(es, [128, 2, 4, 64], BF16, "wqs")
        wkv = S.sb(es, [128, 1024], BF16, "wkv")
        qn = S.sb(es, [128, 2], F32, "qn")
        kvn = S.sb(es, [128, 1], F32, "kvn")
        inv = S.sb(es, [64, 1], F32, "inv")
        sgn = S.sb(es, [64, 1], F32, "sgn")
        negpi = S.sb(es, [64, 1], F32, "negpi")
        S.pool(lambda: nc.gpsimd.memset(negpi[:], -float(np.pi)), w=[negpi])
        S.dma("sp", qn[:], qn_d[j], r=[qn_d], w=[qn])
        S.dma("sp", kvn[:], kvn_d[j], r=[kvn_d], w=[kvn])
        S.dma("sp", inv[:], inv_d[:, :], r=[inv_d], w=[inv])
        S.dma("sp", sgn[:], sgn_d[:, :], r=[sgn_d], w=[sgn])
        for kc in range(2):
            S.dma("pool", wq[:, kc, :], w_uq[j, kc * 128:(kc + 1) * 128, :], r=[w_uq], w=[wq])
            for h in range(4):
                b0 = h * 192 + 128
                S.dma("pool", wqs[:, kc, h, 0:32], w_uq[j, kc * 128:(kc + 1) * 128, b0 + 32:b0 + 64], r=[w_uq], w=[wqs])
                S.dma("pool", wqs[:, kc, h, 32:64], w_uq[j, kc * 128:(kc + 1) * 128, b0:b0 + 32], r=[w_uq], w=[wqs])
        S.dma("pool", wkv[:], w_ukv[j, :, :], r=[w_ukv], w=[wkv])
        with ExitStack() as es2:
            posi = S.sb(es2, [64, 512], I32, "posi")
            ang = S.sb(es2, [64, 512], F32, "ang")
            u = S.sb(es2, [64, 512], F32, "u")
            ni = S.sb(es2, [64, 512], I32, "ni")
            nf = S.sb(es2, [64, 512], F32, "nf")
            cq = S.sb(es2, [128, 2, 512], F32, "cq")
            ckv = S.sb(es2, [128, 512], F32, "ckv")
            kr = S.sb(es2, [64, 512], F32, "kr")
            krs = S.sb(es2, [64, 512], F32, "krs")
            sq = S.sb(es2, [128, 3, 512], BF16, "sq")
            rstd = S.sb(es2, [128, 512], F32, "rstd")
            tmp = S.sb(es2, [128, 512], F32, "tmp")
            for tb in range(NB):
                ts = slice(tb * 512, (tb + 1) * 512)
                S.dma("sp", posi[:], pos_d[:, ts], r=[pos_d], w=[posi])
                S.dve(lambda: nc.vector.tensor_copy(out=ang[:], in_=posi[:]), r=[posi], w=[ang])
                S.dve(lambda: nc.vector.tensor_scalar(out=ang[:], in0=ang[:], scalar1=inv[:, 0:1], scalar2=None,
                                                      op0=ALU.mult), r=[ang, inv], w=[ang])
                for (dst, off, signed) in ((sin2, 0.5, True), (cos2, 0.75, False)):
                    S.dve(lambda off=off: nc.vector.tensor_scalar(out=u[:], in0=ang[:], scalar1=float(1.0 / (2 * np.pi)),
                                                                  scalar2=off, op0=ALU.mult, op1=ALU.add), r=[ang], w=[u])
                    S.dve(lambda: nc.vector.tensor_copy(out=ni[:], in_=u[:]), r=[u], w=[ni])
                    S.dve(lambda: nc.vector.tensor_copy(out=nf[:], in_=ni[:]), r=[ni], w=[nf])
                    S.dve(lambda: nc.vector.tensor_tensor(out=u[:], in0=u[:], in1=nf[:], op=ALU.subtract), r=[u, nf], w=[u])
                    S.dve(lambda: nc.vector.tensor_scalar(out=nf[:], in0=u[:], scalar1=0.0, scalar2=None, op0=ALU.is_lt),
                          r=[u], w=[nf])
                    S.dve(lambda: nc.vector.tensor_tensor(out=u[:], in0=u[:], in1=nf[:], op=ALU.add), r=[u, nf], w=[u])
                    S.act(lambda: nc.scalar.activation(out=u[:], in_=u[:], func=AF.Sin, scale=float(2 * np.pi),
                                                       bias=negpi[:, 0:1]), r=[u, negpi], w=[u])
                    if signed:
                        S.dve(lambda dst=dst, ts=ts: nc.vector.tensor_scalar(out=dst[:, ts], in0=u[:], scalar1=sgn[:, 0:1],
                                                                             scalar2=None, op0=ALU.mult), r=[u, sgn], w=[dst])
                    else:
                        S.dve(lambda dst=dst, ts=ts: nc.vector.tensor_copy(out=dst[:, ts], in_=u[:]), r=[u], w=[dst])
                S.dma("sp", cq[:], pT[R_CQ:R_CQ + 256, ts].rearrange("(kc p) t -> p kc t", p=128), r=[pT], w=[cq])
                S.dma("sp", ckv[:], pT[R_CKV:R_CKV + 128, ts], r=[pT], w=[ckv])
                S.dma("sp", kr[:], pT[R_KR:R_KR + 64, ts], r=[pT], w=[kr])
                S.dma("sp", krs[0:32, :], pT[R_KR + 32:R_KR + 64, ts], r=[pT], w=[krs])
                S.dma("sp", krs[32:64, :], pT[R_KR:R_KR + 32, ts], r=[pT], w=[krs])
                for kc in range(2):
                    S.pool(lambda kc=kc: nc.gpsimd.tensor_tensor(out=sq[:, kc, :], in0=cq[:, kc, :], in1=cq[:, kc, :],
                                                                 op=ALU.mult), r=[cq], w=[sq])
                S.pool(lambda: nc.gpsimd.tensor_tensor(out=sq[:, 2, :], in0=ckv[:], in1=ckv[:], op=ALU.mult), r=[ckv], w=[sq])
                ps = S.ps4()
                for kc in range(2):
                    mm(S, ps, ps[:, :], C.ones_bf, C.ones_bf[:, :], sq, sq[:, kc, :], kc == 0, kc == 1)
                S.act(lambda ps=ps: nc.scalar.activation(out=rstd[:], in_=ps[:, :], func=AF.Sqrt, scale=1.0 / 256,
                                                         bias=C.eps[:, 0:1]), r=[ps, C.eps], w=[rstd])
                S.dve(lambda: nc.vector.reciprocal(out=rstd[:], in_=rstd[:]), r=[rstd], w=[rstd])
                for kc in range(2):
                    S.dve(lambda kc=kc: nc.vector.tensor_tensor(out=tmp[:], in0=cq[:, kc, :], in1=rstd[:], op=ALU.mult),
                          r=[cq, rstd], w=[tmp])
                    S.dve(lambda kc=kc, ts=ts: nc.vector.tensor_scalar(out=cqn[:, kc, ts], in0=tmp[:], scalar1=qn[:, kc:kc + 1],
                                                                       scalar2=None, op0=ALU.mult), r=[tmp, qn], w=[cqn])
                ps = S.ps4()
                mm(S, ps, ps[:, :], C.ones_bf, C.ones_bf[:, :], sq, sq[:, 2, :], True, True)
                S.act(lambda ps=ps: nc.scalar.activation(out=rstd[:], in_=ps[:, :], func=AF.Sqrt, scale=1.0 / 128,
                                                         bias=C.eps[:, 0:1]), r=[ps, C.eps], w=[rstd])
                S.dve(lambda: nc.vector.reciprocal(out=rstd[:], in_=rstd[:]), r=[rstd], w=[rstd])
                S.dve(lambda: nc.vector.tensor_tensor(out=tmp[:], in0=ckv[:], in1=rstd[:], op=ALU.mult), r=[ckv, rstd], w=[tmp])
                S.dve(lambda ts=ts: nc.vector.tensor_scalar(out=ckvn[:, ts], in0=tmp[:], scalar1=kvn[:, 0:1], scalar2=None,
                                                            op0=ALU.mult), r=[tmp, kvn], w=[ckvn])
                S.dve(lambda ts=ts: nc.vector.tensor_tensor(out=kr[:], in0=kr[:], in1=cos2[:, ts], op=ALU.mult), r=[kr, cos2], w=[kr])
                S.dve(lambda ts=ts: nc.vector.tensor_tensor(out=krs[:], in0=krs[:], in1=sin2[:, ts], op=ALU.mult), r=[krs, sin2], w=[krs])
                S.dve(lambda ts=ts: nc.vector.tensor_tensor(out=KR[:, ts], in0=kr[:], in1=krs[:], op=ALU.add), r=[kr, krs], w=[KR])
        S.barrier()
        with ExitStack() as es3:
            QN = S.sb(es3, [128, 8192], BF16, "QN")
            QR = S.sb(es3, [64, 8192], BF16, "QR")
            KN = S.sb(es3, [128, 8192], BF16, "KN")
            V = S.sb(es3, [128, 64, 128], BF16, "V")
            pts = [S.sb(es3, [128, 512], BF16, "pt") for _ in range(4)]
            otiles = [S.sb(es3, [128, 512], BF16, "ot") for _ in range(2)]
            rden = S.sb(es3, [128, 512], F32, "rden")
            t1 = S.sb(es3, [64, 512], F32, "t1")
            t2 = S.sb(es3, [64, 512], F32, "t2")
            for h in range(4):
                for tb in range(NB):
                    ts = slice(tb * 512, (tb + 1) * 512)
                    ps = S.ps4()
                    for kc in range(2):
                        mm(S, ps, ps[:, :], wq, wq[:, kc, h * 192:h * 192 + 128], cqn, cqn[:, kc, ts], kc == 0, kc == 1)
                    S.act(lambda ps=ps, ts=ts: nc.scalar.copy(out=QN[:, ts], in_=ps[:, :]), r=[ps], w=[QN])
                    ps = S.ps4()
                    for kc in range(2):
                        mm(S, ps, ps[0:64, :], wq, wq[:, kc, h * 192 + 128:h * 192 + 192], cqn, cqn[:, kc, ts], kc == 0, kc == 1)
                    ps2 = S.ps4()
                    for kc in range(2):
                        mm(S, ps2, ps2[0:64, :], wqs, wqs[:, kc, h, :], cqn, cqn[:, kc, ts], kc == 0, kc == 1)
                    S.dve(lambda ps=ps, ts=ts: nc.vector.tensor_tensor(out=t1[:], in0=ps[0:64, :], in1=cos2[:, ts], op=ALU.mult),
                          r=[ps, cos2], w=[t1])
                    S.dve(lambda ps2=ps2, ts=ts: nc.vector.tensor_tensor(out=t2[:], in0=ps2[0:64, :], in1=sin2[:, ts], op=ALU.mult),
                          r=[ps2, sin2], w=[t2])
                    S.dve(lambda ts=ts: nc.vector.tensor_tensor(out=QR[:, ts], in0=t1[:], in1=t2[:], op=ALU.add), r=[t1, t2], w=[QR])
                    ps = S.ps4()
                    mm(S, ps, ps[:, :], wkv, wkv[:, h * 256:h * 256 + 128], ckvn, ckvn[:, ts], True, True)
                    S.act(lambda ps=ps, ts=ts: nc.scalar.copy(out=KN[:, ts], in_=ps[:, :]), r=[ps], w=[KN])
                    ps = S.ps4()
                    for tt in range(4):
                        mm(S, ps, ps[:, tt * 128:(tt + 1) * 128], ckvn, ckvn[:, tb * 512 + tt * 128:tb * 512 + (tt + 1) * 128],
                           wkv, wkv[:, h * 256 + 128:h * 256 + 256], True, True)
                    S.act(lambda ps=ps, tb=tb: nc.scalar.copy(out=V[:, tb * 4:(tb + 1) * 4, :],
                                                              in_=ps[:, :].rearrange("p (t d) -> p t d", d=128)), r=[ps], w=[V])

                def kts_fn(c):
                    out = []
                    for kt in range(4 * c + 4):
                        masks = []
                        if kt >= 4 * c:
                            m = C.Mc[kt - 4 * c]
                            masks = [(C.ident_bf, C.ident_bf[:, :], m, m[:, :])]
                        out.append((kt, 128, masks))
                    return out

                def out_cb(c, ot, h=h):
                    S.dma("sp", oT_dram[512 + h * 128:512 + (h + 1) * 128, c * 512:(c + 1) * 512], ot[:, :], r=[ot], w=[oT_dram])

                attn_head(S, C, es3, [(KN, 128), (KR, 64)], [(QN, 128), (QR, 64)], V, 128, kts_fn, 192 ** -0.5, out_cb,
                          pts, otiles, rden)
        S.barrier()


def phase_dn(S, C, pT, j, oT_dram):
    nc = S.nc
    cw_d = S.dram("ev_conv_wT", [2, 128, 12, 4], F32)
    al_d = S.dram("ev_a_log_b", [2, 128, 4], F32)
    dt_d = S.dram("ev_dt_bias_b", [2, 128, 4], F32)
    gn_d = S.dram("ev_dn_normT", [2, 128, 1], F32)
    with ExitStack() as es:
        cw = S.sb(es, [128, 12, 4], F32, "cw")
        al = S.sb(es, [128, 4], F32, "al")
        dtb = S.sb(es, [128, 4], F32, "dtb")
        gn = S.sb(es, [128, 1], F32, "gn")
        S.dma("sp", cw[:], cw_d[j], r=[cw_d], w=[cw])
        S.dma("sp", al[:], al_d[j], r=[al_d], w=[al])
        S.dma("sp", dtb[:], dt_d[j], r=[dt_d], w=[dtb])
        S.dma("sp", gn[:], gn_d[j], r=[gn_d], w=[gn])
        S.act(lambda: nc.scalar.activation(out=al[:], in_=al[:], func=AF.Exp), r=[al], w=[al])
        UT = S.sb(es, [128, 128], F32, "UT")
        S.pool(lambda: nc.gpsimd.memset(UT[:], 1.0), w=[UT])
        S.pool(lambda: nc.gpsimd.affine_select(out=UT[:], in_=UT[:], pattern=[[1, 128]], compare_op=ALU.is_ge, fill=0.0,
                                               base=0, channel_multiplier=-1), r=[UT], w=[UT])
        PM1 = S.sb(es, [128, 128], F32, "PM1")
        S.pool(lambda: nc.gpsimd.memset(PM1[:], 0.0), w=[PM1])
        S.pool(lambda: nc.gpsimd.affine_select(out=PM1[:], in_=PM1[:], pattern=[[-1, 128]], compare_op=ALU.is_gt, fill=1e5,
                                               base=0, channel_multiplier=1), r=[PM1], w=[PM1])
        NM2 = S.sb(es, [128, 128], F32, "NM2")
        S.pool(lambda: nc.gpsimd.memset(NM2[:], 0.0), w=[NM2])
        S.pool(lambda: nc.gpsimd.affine_select(out=NM2[:], in_=NM2[:], pattern=[[1, 128]], compare_op=ALU.is_ge, fill=-1e5,
                                               base=0, channel_multiplier=-1), r=[NM2], w=[NM2])
        GV = {n_: [S.sb(es, [64, 128] if n_ in ("braw", "araw") else [128, 64], F32, n_) for _ in range(4)]
              for n_ in ("braw", "araw", "beta", "g", "gc", "ngc", "glast", "alast", "etail", "bexpg", "nbeta")}
        Sts = [S.sb(es, [128, 128], F32, "state") for _ in range(4)]
        NS = 4
        def mk(name, shape=(128, 128), dt=F32):
            return [S.sb(es, list(shape), dt, name) for _ in range(NS)]
        raw = {n: mk("raw" + n, (128, 131)) for n in "qkv"}
        cv = {n: mk("cv" + n) for n in "qkv"}
        rn = mk("rn")
        qT = mk("qT"); kT = mk("kT"); ktok = mk("ktok"); vtok = mk("vtok")
        dgc = mk("dgc"); Dm = mk("Dm"); DiT = mk("DiT"); egr = mk("egr")
        Nm = mk("Nm"); NmT = mk("NmT"); M2 = mk("M2"); M2T = mk("M2T"); RT = mk("RT")
        qkT = mk("qkT"); qgT = mk("qgT"); vb = mk("vb"); kbg = mk("kbg"); ktl = mk("ktl")
        u = mk("u"); wT = mk("wT"); vnew = mk("vnew"); osb = mk("osb"); osq = mk("osq"); zt = mk("zt")
        obf = mk("obf", (128, 128), BF16)

        def evac(dst, ps, eng="act"):
            if eng == "act":
                S.act(lambda: nc.scalar.copy(out=dst[:], in_=ps[:, 0:128]), r=[ps], w=[dst])
            else:
                S.dve(lambda: nc.vector.tensor_copy(out=dst[:], in_=ps[:, 0:128]), r=[ps], w=[dst])

        def head_fn(h):
            braw, araw, beta, g, gc, ngc, glast, alast, etail, bexpg, nbeta = (GV[n_][h] for n_ in (
                "braw", "araw", "beta", "g", "gc", "ngc", "glast", "alast", "etail", "bexpg", "nbeta"))
            St = Sts[h]
            S.dma("sp", braw[:], pT[2048 + h, :].rearrange("(t p) -> t p", p=128), r=[pT], w=[braw])
            S.dma("sp", araw[:], pT[2052 + h, :].rearrange("(t p) -> t p", p=128), r=[pT], w=[araw])
            ps = S.ps()
            S.pe(lambda ps=ps: nc.tensor.transpose(out=ps[:, 0:64], in_=braw[:, :], identity=C.ident[0:64, 0:64]),
                 r=[braw, C.ident], w=[ps])
            S.act(lambda ps=ps: nc.scalar.activation(out=beta[:], in_=ps[:, 0:64], func=AF.Sigmoid), r=[ps], w=[beta])
            ps = S.ps()
            S.pe(lambda ps=ps: nc.tensor.transpose(out=ps[:, 0:64], in_=araw[:, :], identity=C.ident[0:64, 0:64]),
                 r=[araw, C.ident], w=[ps])
            S.act(lambda ps=ps, h=h: nc.scalar.activation(out=g[:], in_=ps[:, 0:64], func=AF.Exp, bias=dtb[:, h:h + 1]),
                  r=[ps, dtb], w=[g])
            S.dve(lambda: nc.vector.tensor_scalar(out=g[:], in0=g[:], scalar1=1.0, scalar2=None, op0=ALU.add), r=[g], w=[g])
            S.act(lambda: nc.scalar.activation(out=g[:], in_=g[:], func=AF.Ln), r=[g], w=[g])
            S.dve(lambda h=h: nc.vector.tensor_scalar(out=g[:], in0=g[:], scalar1=al[:, h:h + 1], scalar2=-1.0, op0=ALU.mult,
                                                      op1=ALU.mult), r=[g, al], w=[g])
            ps = S.ps()
            mm(S, ps, ps[:, 0:64], UT, UT[:, :], g, g[:, :], True, True)
            S.act(lambda ps=ps: nc.scalar.copy(out=gc[:], in_=ps[:, 0:64]), r=[ps], w=[gc])
            S.dve(lambda: nc.vector.tensor_scalar(out=ngc[:], in0=gc[:], scalar1=-1.0, scalar2=None, op0=ALU.mult), r=[gc], w=[ngc])
            ps = S.ps()
            mm(S, ps, ps[:, 0:64], C.ones_f, C.ones_f[:, :], g, g[:, :], True, True)
            S.act(lambda ps=ps: nc.scalar.copy(out=glast[:], in_=ps[:, 0:64]), r=[ps], w=[glast])
            S.act(lambda: nc.scalar.activation(out=alast[:], in_=glast[:], func=AF.Exp), r=[glast], w=[alast])
            S.dve(lambda: nc.vector.tensor_tensor(out=etail[:], in0=glast[:], in1=gc[:], op=ALU.subtract), r=[glast, gc], w=[etail])
            S.act(lambda: nc.scalar.activation(out=etail[:], in_=etail[:], func=AF.Exp), r=[etail], w=[etail])
            S.act(lambda: nc.scalar.activation(out=bexpg[:], in_=gc[:], func=AF.Exp), r=[gc], w=[bexpg])
            S.dve(lambda: nc.vector.tensor_tensor(out=bexpg[:], in0=bexpg[:], in1=beta[:], op=ALU.mult), r=[bexpg, beta], w=[bexpg])
            S.dve(lambda: nc.vector.tensor_scalar(out=nbeta[:], in0=beta[:], scalar1=-1.0, scalar2=None, op0=ALU.mult),
                  r=[beta], w=[nbeta])
            S.dve(lambda: nc.vector.memset(St[:], 0.0), w=[St])
            for T in range(NT):
                s = h
                t0 = T * 128
                for ci, n in enumerate("qkv"):
                    rw = raw[n][s]
                    row0 = ci * 512 + h * 128
                    if T == 0:
                        S.dve(lambda rw=rw: nc.vector.memset(rw[:, 0:3], 0.0), w=[rw])
                        S.dma("sp", rw[:, 3:131], pT[row0:row0 + 128, 0:128], r=[pT], w=[rw])
                    else:
                        S.dma("sp", rw[:, :], pT[row0:row0 + 128, t0 - 3:t0 + 128], r=[pT], w=[rw])
                    c_ = cv[n][s]
                    ft = ci * 4 + h
                    S.dve(lambda rw=rw, c_=c_, ft=ft: nc.vector.tensor_scalar(out=c_[:], in0=rw[:, 0:128], scalar1=cw[:, ft, 0:1],
                                                                              scalar2=None, op0=ALU.mult), r=[rw, cw], w=[c_])
                    for k in range(1, 4):
                        S.dve(lambda rw=rw, c_=c_, ft=ft, k=k: nc.vector.scalar_tensor_tensor(
                            out=c_[:], in0=rw[:, k:k + 128], scalar=cw[:, ft, k:k + 1], in1=c_[:], op0=ALU.mult, op1=ALU.add),
                            r=[rw, cw, c_], w=[c_])
                    S.act(lambda c_=c_: nc.scalar.activation(out=c_[:], in_=c_[:], func=AF.Silu), r=[c_], w=[c_])
                for n, dst, mul in (("q", qT[s], 128 ** -0.5), ("k", kT[s], 1.0)):
                    c_ = cv[n][s]
                    S.pool(lambda c_=c_: nc.gpsimd.tensor_tensor(out=rn[s][:], in0=c_[:], in1=c_[:], op=ALU.mult), r=[c_], w=[rn[s]])
                    ps = S.ps()
                    mm(S, ps, ps[:, 0:128], C.ones_f, C.ones_f[:, :], rn[s], rn[s][:, :], True, True)
                    S.act(lambda ps=ps: nc.scalar.activation(out=rn[s][:], in_=ps[:, 0:128], func=AF.Sqrt, bias=C.eps[:, 0:1]),
                          r=[ps, C.eps], w=[rn[s]])
                    S.dve(lambda: nc.vector.reciprocal(out=rn[s][:], in_=rn[s][:]), r=[rn[s]], w=[rn[s]])
                    S.dve(lambda c_=c_, dst=dst, mul=mul: nc.vector.scalar_tensor_tensor(
                        out=dst[:], in0=c_[:], scalar=mul, in1=rn[s][:], op0=ALU.mult, op1=ALU.mult), r=[c_, rn[s]], w=[dst])
                ps = S.ps()
                S.pe(lambda ps=ps: nc.tensor.transpose(out=ps[:, 0:128], in_=kT[s][:, :], identity=C.ident[:, :]),
                     r=[kT[s], C.ident], w=[ps])
                evac(ktok[s], ps)
                ps = S.ps()
                S.pe(lambda ps=ps: nc.tensor.transpose(out=ps[:, 0:128], in_=cv["v"][s][:, :], identity=C.ident[:, :]),
                     r=[cv["v"][s], C.ident], w=[ps])
                evac(vtok[s], ps, "dve")
                S.dve(lambda: nc.vector.tensor_scalar(out=dgc[s][:], in0=C.ident[:], scalar1=gc[:, T:T + 1], scalar2=None,
                                                      op0=ALU.mult), r=[C.ident, gc], w=[dgc[s]])
                p1 = S.ps()
                mm(S, p1, p1[:, 0:128], C.ones_f, C.ones_f[:, :], dgc[s], dgc[s][:, :], True, False)
                mm(S, p1, p1[:, 0:128], C.ident, C.ident[:, :], PM1, PM1[:, :], False, True)
                S.act(lambda p1=p1: nc.scalar.activation(out=Dm[s][:], in_=p1[:, 0:128], func=AF.Exp, scale=-1.0,
                                                         bias=gc[:, T:T + 1]), r=[p1, gc], w=[Dm[s]])
                p2 = S.ps()
                mm(S, p2, p2[:, 0:128], C.ones_f, C.ones_f[:, :], dgc[s], dgc[s][:, :], True, False)
                mm(S, p2, p2[:, 0:128], C.ident, C.ident[:, :], NM2, NM2[:, :], False, True)
                S.act(lambda p2=p2: nc.scalar.activation(out=DiT[s][:], in_=p2[:, 0:128], func=AF.Exp, scale=1.0,
                                                         bias=ngc[:, T:T + 1]), r=[p2, ngc], w=[DiT[s]])
                p3 = S.ps()
                mm(S, p3, p3[:, 0:128], C.ones_f, C.ones_f[:, :], dgc[s], dgc[s][:, :], True, True)
                S.act(lambda p3=p3: nc.scalar.activation(out=egr[s][:], in_=p3[:, 0:128], func=AF.Exp), r=[p3], w=[egr[s]])
                pg = S.ps()
                mm(S, pg, pg[:, 0:128], kT[s], kT[s][:, :], kT[s], kT[s][:, :], True, True)
                S.dve(lambda pg=pg: nc.vector.scalar_tensor_tensor(out=Nm[s][:], in0=pg[:, 0:128], scalar=nbeta[:, T:T + 1],
                                                                   in1=Dm[s][:], op0=ALU.mult, op1=ALU.mult),
                      r=[pg, nbeta, Dm[s]], w=[Nm[s]])
                ps = S.ps()
                S.pe(lambda ps=ps: nc.tensor.transpose(out=ps[:, 0:128], in_=Nm[s][:, :], identity=C.ident[:, :]),
                     r=[Nm[s], C.ident], w=[ps])
                evac(NmT[s], ps)
                S.dve(lambda: nc.vector.tensor_tensor(out=RT[s][:], in0=NmT[s][:], in1=C.ident[:], op=ALU.add),
                      r=[NmT[s], C.ident], w=[RT[s]])
                Mc_, McT = Nm[s], NmT[s]
                Mn, MnT = M2[s], M2T[s]
                for lvl in range(6):
                    pa = S.ps()
                    mm(S, pa, pa[:, 0:128], McT, McT[:, :], Mc_, Mc_[:, :], True, True)
                    pb = S.ps()
                    mm(S, pb, pb[:, 0:128], Mc_, Mc_[:, :], McT, McT[:, :], True, True)
                    evac(Mn, pa, "act")
                    evac(MnT, pb, "dve")
                    pr = S.ps()
                    mm(S, pr, pr[:, 0:128], Mn, Mn[:, :], RT[s], RT[s][:, :], True, True)
                    S.dve(lambda pr=pr: nc.vector.tensor_tensor(out=RT[s][:], in0=RT[s][:], in1=pr[:, 0:128], op=ALU.add),
                          r=[RT[s], pr], w=[RT[s]])
                    Mc_, McT, Mn, MnT = Mn, MnT, Mc_, McT
                S.dve(lambda: nc.vector.tensor_scalar(out=vb[s][:], in0=vtok[s][:], scalar1=beta[:, T:T + 1], scalar2=None,
                                                      op0=ALU.mult), r=[vtok[s], beta], w=[vb[s]])
                S.dve(lambda: nc.vector.tensor_scalar(out=kbg[s][:], in0=ktok[s][:], scalar1=bexpg[:, T:T + 1], scalar2=None,
                                                      op0=ALU.mult), r=[ktok[s], bexpg], w=[kbg[s]])
                S.pool(lambda: nc.gpsimd.tensor_scalar(out=ktl[s][:], in0=ktok[s][:], scalar1=etail[:, T:T + 1], scalar2=None,
                                                       op0=ALU.mult), r=[ktok[s], etail], w=[ktl[s]])
                pu = S.ps()
                mm(S, pu, pu[:, 0:128], RT[s], RT[s][:, :], vb[s], vb[s][:, :], True, True)
                evac(u[s], pu, "act")
                pw = S.ps()
                mm(S, pw, pw[:, 0:128], kbg[s], kbg[s][:, :], RT[s], RT[s][:, :], True, True)
                evac(wT[s], pw, "act")
                pq = S.ps()
                mm(S, pq, pq[:, 0:128], kT[s], kT[s][:, :], qT[s], qT[s][:, :], True, True)
                S.dve(lambda pq=pq: nc.vector.tensor_tensor(out=qkT[s][:], in0=pq[:, 0:128], in1=DiT[s][:], op=ALU.mult),
                      r=[pq, DiT[s]], w=[qkT[s]])
                S.pool(lambda: nc.gpsimd.tensor_tensor(out=qgT[s][:], in0=qT[s][:], in1=egr[s][:], op=ALU.mult),
                       r=[qT[s], egr[s]], w=[qgT[s]])
                pv = S.ps()
                mm(S, pv, pv[:, 0:128], wT[s], wT[s][:, :], St, St[:, :], True, True)
                S.dve(lambda pv=pv: nc.vector.tensor_tensor(out=vnew[s][:], in0=u[s][:], in1=pv[:, 0:128], op=ALU.subtract),
                      r=[u[s], pv], w=[vnew[s]])
                po = S.ps()
                mm(S, po, po[:, 0:128], St, St[:, :], qgT[s], qgT[s][:, :], True, False)
                mm(S, po, po[:, 0:128], vnew[s], vnew[s][:, :], qkT[s], qkT[s][:, :], False, True)
                pS = S.ps()
                mm(S, pS, pS[:, 0:128], ktl[s], ktl[s][:, :], vnew[s], vnew[s][:, :], True, True)
                S.dve(lambda pS=pS: nc.vector.scalar_tensor_tensor(out=St[:], in0=St[:], scalar=alast[:, T:T + 1],
                                                                   in1=pS[:, 0:128], op0=ALU.mult, op1=ALU.add),
                      r=[St, alast, pS], w=[St])
                evac(osb[s], po, "act")
                S.pool(lambda: nc.gpsimd.tensor_tensor(out=osq[s][:], in0=osb[s][:], in1=osb[s][:], op=ALU.mult), r=[osb[s]], w=[osq[s]])
                pn = S.ps()
                mm(S, pn, pn[:, 0:128], C.ones_f, C.ones_f[:, :], osq[s], osq[s][:, :], True, True)
                S.act(lambda pn=pn: nc.scalar.activation(out=osq[s][:], in_=pn[:, 0:128], func=AF.Sqrt, scale=1.0 / 128,
                                                         bias=C.eps[:, 0:1]), r=[pn, C.eps], w=[osq[s]])
                S.dve(lambda: nc.vector.reciprocal(out=osq[s][:], in_=osq[s][:]), r=[osq[s]], w=[osq[s]])
                S.dma("sp", zt[s][:], pT[1536 + h * 128:1536 + (h + 1) * 128, t0:t0 + 128], r=[pT], w=[zt[s]])
                S.act(lambda: nc.scalar.activation(out=zt[s][:], in_=zt[s][:], func=AF.Silu), r=[zt[s]], w=[zt[s]])
                S.dve(lambda: nc.vector.scalar_tensor_tensor(out=osb[s][:], in0=osb[s][:], scalar=gn[:, 0:1], in1=osq[s][:],
                                                             op0=ALU.mult, op1=ALU.mult), r=[osb[s], gn, osq[s]], w=[osb[s]])
                S.dve(lambda: nc.vector.tensor_tensor(out=obf[s][:], in0=osb[s][:], in1=zt[s][:], op=ALU.mult),
                      r=[osb[s], zt[s]], w=[obf[s]])
                S.dma("sp", oT_dram[h * 128:(h + 1) * 128, t0:t0 + 128], obf[s][:], r=[obf[s]], w=[oT_dram])
        Interleaver(S).run([(lambda h=h: head_fn(h)) for h in range(4)])
        S.barrier()


def phase_nsa(S, C, pT, vs_tok, vw_tok, j, oT_dram):
    nc = S.nc
    R_Q, R_KC, R_VC, R_KS, R_KW, R_GL = 0, 1024, 1280, 1536, 2048, 2560
    posk_d = S.dram("od_cmp_pos_kT", [2, 64, 32], F32)
    posv_d = S.dram("od_cmp_pos_vT", [2, 64, 32], F32)
    k1_d = S.dram("od_cmp_k1", [2, 2048, 256], F32)
    k2_d = S.dram("od_cmp_k2", [2, 256, 64], F32)
    v1_d = S.dram("od_cmp_v1", [2, 2048, 256], F32)
    v2_d = S.dram("od_cmp_v2", [2, 256, 64], F32)
    SC = 0.125
    with ExitStack() as es:
        KC = [S.sb(es, [64, 512], BF16, "KC") for _ in range(4)]
        for g in range(4):
            S.pool(lambda g=g: nc.gpsimd.memset(KC[g][:], 0.0), w=[KC[g]])
        VC = [S.sb(es, [128, 4, 64], BF16, "VC") for _ in range(4)]
        with ExitStack() as e1:
            src = S.sb(e1, [64, 8192], BF16, "csrc")
            w1 = S.sb(e1, [64, 32, 256], BF16, "w1")
            w2 = S.sb(e1, [128, 2, 64], BF16, "w2")
            posT = S.sb(e1, [64, 32], BF16, "posT")
            c1 = S.sb(e1, [128, 2], F32, "c1")
            h1 = S.sb(e1, [128, 2, 512], BF16, "h1")
            for which, (pos_d, a_d, b_d, row0) in enumerate(((posk_d, k1_d, k2_d, R_KC), (posv_d, v1_d, v2_d, R_VC))):
                S.dma("pool", w1[:], a_d[j].rearrange("(l d) n -> d l n", d=64), r=[a_d], w=[w1])
                S.dma("pool", w2[:], b_d[j].rearrange("(c p) n -> p c n", p=128), r=[b_d], w=[w2])
                S.dma("pool", posT[:], pos_d[j], r=[pos_d], w=[posT])
                for ncx in range(2):
                    ps = S.ps()
                    for l in range(32):
                        mm(S, ps, ps[:, 0:1], w1, w1[:, l, ncx * 128:(ncx + 1) * 128], posT, posT[:, l:l + 1], l == 0, l == 31)
                    S.act(lambda ps=ps, ncx=ncx: nc.scalar.copy(out=c1[:, ncx:ncx + 1], in_=ps[:, 0:1]), r=[ps], w=[c1])
                for g in range(4):
                    S.dma("pool", src[:], pT[row0 + g * 64:row0 + (g + 1) * 64, :], r=[pT], w=[src])
                    for ncx in range(2):
                        ps = S.ps()
                        for l in range(32):
                            mm(S, ps, ps[:, 0:511], w1, w1[:, l, ncx * 128:(ncx + 1) * 128], src,
                               src[:, l:l + 16 * 510 + 1:16], l == 0, l == 31)
                        S.act(lambda ps=ps, ncx=ncx: nc.scalar.activation(out=h1[:, ncx, 0:511], in_=ps[:, 0:511], func=AF.Silu,
                                                                          bias=c1[:, ncx:ncx + 1]), r=[ps, c1], w=[h1])
                    if which == 0:
                        ps = S.ps()
                        for ncx in range(2):
                            mm(S, ps, ps[0:64, 0:511], w2, w2[:, ncx, :], h1, h1[:, ncx, 0:511], ncx == 0, ncx == 1)
                        S.act(lambda ps=ps, g=g: nc.scalar.copy(out=KC[g][:, 0:511], in_=ps[0:64, 0:511]), r=[ps], w=[KC[g]])
                    else:
                        for nt in range(4):
                            rows = 128 if nt < 3 else 127
                            ps = S.ps()
                            for ncx in range(2):
                                mm(S, ps, ps[0:rows, 0:64], h1, h1[:, ncx, nt * 128:nt * 128 + rows], w2, w2[:, ncx, :],
                                   ncx == 0, ncx == 1)
                            S.act(lambda ps=ps, g=g, nt=nt, rows=rows: nc.scalar.copy(out=VC[g][0:rows, nt, :], in_=ps[0:rows, 0:64]),
                                  r=[ps], w=[VC[g]])
        S.barrier()
        negselT = S.sb(es, [128, 8192], BF16, "negselT")
        Wm = S.sb(es, [128, 16], F32, "Wm")
        Wm0 = S.sb(es, [128, 16], F32, "Wm0")
        for (t_, b_) in ((Wm, 97), (Wm0, -31)):
            S.pool(lambda t_=t_: nc.gpsimd.memset(t_[:], 0.0), w=[t_])
            S.pool(lambda t_=t_, b_=b_: nc.gpsimd.affine_select(out=t_[:], in_=t_[:], pattern=[[-16, 16]], compare_op=ALU.is_ge,
                                                                fill=NEG, base=b_, channel_multiplier=1), r=[t_], w=[t_])
        Ebig = S.sb(es, [128, 8192], BF16, "Ebig")
        Sel48 = S.sb(es, [48, 48, 64], F32, "Sel48")
        S.pool(lambda: nc.gpsimd.memset(Sel48[:], 0.0), w=[Sel48])
        S.pool(lambda: nc.gpsimd.affine_select(out=Sel48[:], in_=Sel48[:], pattern=[[-1, 48], [0, 64]],
                                               compare_op=ALU.not_equal, fill=1.0, base=0, channel_multiplier=1),
               r=[Sel48], w=[Sel48])
        S.pool(lambda: nc.gpsimd.memset(Ebig[:], 0.0), w=[Ebig])
        ebv = Ebig[:].rearrange("p (b x) -> p b x", x=64)
        S.pool(lambda: nc.gpsimd.affine_select(out=ebv, in_=ebv, pattern=[[-1, 128], [0, 64]], compare_op=ALU.not_equal,
                                               fill=1.0, base=0, channel_multiplier=1), r=[Ebig], w=[Ebig])
        for g in range(4):
            with ExitStack() as e2:
                QT4 = [S.sb(e2, [64, 8192], BF16, "QT4") for _ in range(4)]
                for hg in range(4):
                    r0 = R_Q + (g * 4 + hg) * 64
                    S.dma("pool", QT4[hg][:], pT[r0:r0 + 64, :], r=[pT], w=[QT4[hg]])
                scs = [S.sb(e2, [128, 512], F32, "sc") for _ in range(2)]
                pp = S.sb(e2, [128, 516], F32, "pp")
                rs = S.sb(e2, [128, 2], F32, "rs")
                imp = S.sb(e2, [128, 128], F32, "imp")
                imp2 = S.sb(e2, [128, 128], F32, "imp2")
                mx = S.sb(e2, [128, 8], F32, "mx")
                S.dve(lambda: nc.vector.memset(pp[:], 0.0), w=[pp])
                for T in range(NT):
                    for hg in range(4):
                        ps = S.ps()
                        mm(S, ps, ps[:, 0:512], QT4[hg], QT4[hg][:, T * 128:(T + 1) * 128], KC[g], KC[g][:, 0:512], True, True)
                        sc = scs[hg % 2]
                        S.act(lambda ps=ps, sc=sc: nc.scalar.activation(out=sc[:], in_=ps[:, 0:512], func=AF.Copy, scale=SC),
                              r=[ps], w=[sc])
                        w0 = max(8 * T - 8, 0)
                        w1_ = min(8 * T + 8, 512)
                        wm_ap = Wm0[:, 0:8] if T == 0 else Wm[:, 0:w1_ - w0]
                        S.pool(lambda sc=sc, w0=w0, w1_=w1_, wm_ap=wm_ap: nc.gpsimd.tensor_tensor(
                            out=sc[:, w0:w1_], in0=sc[:, w0:w1_], in1=wm_ap, op=ALU.add), r=[sc, Wm, Wm0], w=[sc])
                        if w1_ < 512:
                            S.pool(lambda sc=sc, w1_=w1_: nc.gpsimd.memset(sc[:, w1_:512], NEG), r=[sc], w=[sc])
                        S.act(lambda sc=sc: nc.scalar.activation(out=sc[:], in_=sc[:], func=AF.Exp, accum_out=rs[:, 0:1]),
                              r=[sc], w=[sc, rs])
                        S.dve(lambda: nc.vector.tensor_scalar(out=rs[:, 1:2], in0=rs[:, 0:1], scalar1=C.tiny[:, 0:1], scalar2=None,
                                                              op0=ALU.max), r=[rs, C.tiny], w=[rs])
                        S.dve(lambda: nc.vector.reciprocal(out=rs[:, 1:2], in_=rs[:, 1:2]), r=[rs], w=[rs])
                        if hg == 0:
                            S.dve(lambda sc=sc: nc.vector.tensor_scalar(out=pp[:, 1:513], in0=sc[:], scalar1=rs[:, 1:2], scalar2=None,
                                                                        op0=ALU.mult), r=[sc, rs], w=[pp])
                        else:
                            S.dve(lambda sc=sc: nc.vector.scalar_tensor_tensor(out=pp[:, 1:513], in0=sc[:], scalar=rs[:, 1:2],
                                                                               in1=pp[:, 1:513], op0=ALU.mult, op1=ALU.add),
                                  r=[sc, rs, pp], w=[pp])
                    a = pp[:, 0:512].rearrange("p (j f) -> p j f", f=4)
                    e_ = pp[:, 4:516].rearrange("p (j f) -> p j f", f=4)
                    S.dve(lambda a=a: nc.vector.tensor_scalar(out=imp[:], in0=a[:, :, 0], scalar1=0.5, scalar2=None, op0=ALU.mult),
                          r=[pp], w=[imp])
                    for f in (1, 2, 3):
                        S.dve(lambda a=a, f=f: nc.vector.tensor_tensor(out=imp[:], in0=imp[:], in1=a[:, :, f], op=ALU.add),
                              r=[pp, imp], w=[imp])
                    S.dve(lambda e_=e_: nc.vector.scalar_tensor_tensor(out=imp[:], in0=e_[:, :, 0], scalar=0.5, in1=imp[:],
                                                                       op0=ALU.mult, op1=ALU.add), r=[pp, imp], w=[imp])
                    for half in range(2):
                        cur = 2 * T + half
                        hs = slice(half * 64, half * 64 + 64)
                        if cur + 1 < 128:
                            S.pool(lambda hs=hs, cur=cur: nc.gpsimd.memset(imp[hs, cur + 1:128], -1.0), r=[imp], w=[imp])
                        lo = max(cur - 1, 0)
                        S.pool(lambda hs=hs, lo=lo, cur=cur: nc.gpsimd.memset(imp[hs, lo:cur + 1], 1e9), r=[imp], w=[imp])
                    S.pool(lambda: nc.gpsimd.memset(imp[:, 0:1], 1e9), r=[imp], w=[imp])
                    S.dve(lambda: nc.vector.max(out=mx[:], in_=imp[:]), r=[imp], w=[mx])
                    S.dve(lambda: nc.vector.match_replace(out=imp2[:], in_to_replace=mx[:], in_values=imp[:], imm_value=-2.0),
                          r=[mx, imp], w=[imp2])
                    S.dve(lambda: nc.vector.max(out=mx[:], in_=imp2[:]), r=[imp2], w=[mx])
                    S.dve(lambda: nc.vector.tensor_scalar(out=imp2[:], in0=imp[:], scalar1=mx[:, 7:8], scalar2=None, op0=ALU.is_ge),
                          r=[imp, mx], w=[imp2])
                    S.dve(lambda: nc.vector.tensor_scalar(out=imp2[:], in0=imp2[:], scalar1=-1.0, scalar2=-NEG, op0=ALU.add,
                                                          op1=ALU.mult), r=[imp2], w=[imp2])
                    ps = S.ps()
                    S.pe(lambda ps=ps: nc.tensor.transpose(out=ps[:, 0:128], in_=imp2[:, :], identity=C.ident[:, :]),
                         r=[imp2, C.ident], w=[ps])
                    S.act(lambda ps=ps, T=T: nc.scalar.copy(out=negselT[:, T * 128:(T + 1) * 128], in_=ps[:, 0:128]),
                          r=[ps], w=[negselT])
            S.barrier()
            with ExitStack() as e3:
                ksT = S.sb(e3, [64, 8192], BF16, "ksT")
                kwT = S.sb(e3, [64, 8192], BF16, "kwT")
                VS = S.sb(e3, [128, 64, 64], BF16, "VS")
                VW = S.sb(e3, [128, 64, 64], BF16, "VW")
                QT = S.sb(e3, [64, 8192], BF16, "QT")
                gates = S.sb(e3, [48, 8192], F32, "gates")
                pts = [S.sb(e3, [128, 512], BF16, "pt") for _ in range(3)]
                ots = [S.sb(e3, [64, 512], F32, "ot") for _ in range(2)]
                rden = S.sb(e3, [64, 512], F32, "rden")
                acc = S.sb(e3, [64, 512], F32, "acc")
                tmpo = S.sb(e3, [64, 512], F32, "tmpo")
                obf = [S.sb(e3, [64, 512], BF16, "obf") for _ in range(2)]
                S.dma("pool", ksT[:], pT[R_KS + g * 64:R_KS + (g + 1) * 64, :], r=[pT], w=[ksT])
                S.dma("pool", kwT[:], pT[R_KW + g * 64:R_KW + (g + 1) * 64, :], r=[pT], w=[kwT])
                for q4 in range(4):
                    tsl = slice(q4 * 16, (q4 + 1) * 16)
                    rsl = slice(q4 * 2048, (q4 + 1) * 2048)
                    S.dma("pool", VS[:, tsl, :], vs_tok[rsl, g * 64:(g + 1) * 64].rearrange("(t p) d -> p t d", p=128),
                          r=[vs_tok], w=[VS])
                    S.dma("pool", VW[:, tsl, :], vw_tok[rsl, g * 64:(g + 1) * 64].rearrange("(t p) d -> p t d", p=128),
                          r=[vw_tok], w=[VW])
                S.dma("sp", gates[:], pT[R_GL:R_GL + 48, :], r=[pT], w=[gates])
                S.act(lambda: nc.scalar.activation(out=gates[:], in_=gates[:], func=AF.Sigmoid), r=[gates], w=[gates])
                pi = 0
                bank = 0
                for hg in range(4):
                    head = g * 4 + hg
                    S.dma("pool", QT[:], pT[R_Q + head * 64:R_Q + (head + 1) * 64, :], r=[pT], w=[QT])
                    for c in range(16):
                        qs = slice(c * 512, (c + 1) * 512)
                        for br in range(3):
                            if br == 0:
                                kts = []
                                for nt in range(4):
                                    D = c - 4 * nt
                                    if D < 0:
                                        continue
                                    nk = 128 if nt < 3 else 127
                                    masks = [(C.ident_bf, C.ident_bf[:, 0:nk], C.Mk[D], C.Mk[D][:, :])] if D <= 4 else []
                                    kts.append((nt, nk, masks))
                                kp, V_ = [(KC[g], 64)], VC[g]
                            elif br == 1:
                                kts = []
                                for kt in range(4 * c + 4):
                                    masks = [(Ebig, Ebig[:, kt * 128:(kt + 1) * 128], negselT, negselT[:, qs])]
                                    if kt >= 4 * c:
                                        m_ = C.Mc[kt - 4 * c]
                                        masks.append((C.ident_bf, C.ident_bf[:, :], m_, m_[:, :]))
                                    kts.append((kt, 128, masks))
                                kp, V_ = [(ksT, 64)], VS
                            else:
                                kts = []
                                for jj in range(-4, 4):
                                    kt = 4 * c + jj
                                    if kt < 0:
                                        continue
                                    m_ = C.Mw[-jj] if jj < 0 else C.Mc[jj]
                                    kts.append((kt, 128, [(C.ident_bf, C.ident_bf[:, :], m_, m_[:, :])]))
                                kp, V_ = [(kwT, 64)], VW
                            ot = ots[bank % 2]
                            pi = attn_chunk(S, C, c, bank, kp, [(QT, 64)], V_, 64, kts, SC, pts, ot, rden, pi)
                            bank += 1
                            pg = S.ps4()
                            mm(S, pg, pg[0:64, :], Sel48, Sel48[:, head * 3 + br, :], gates, gates[:, qs], True, True)
                            if br == 0:
                                S.dve(lambda ot=ot, pg=pg: nc.vector.tensor_tensor(out=acc[:], in0=ot[:], in1=pg[0:64, :], op=ALU.mult),
                                      r=[ot, pg], w=[acc])
                            else:
                                S.dve(lambda ot=ot, pg=pg: nc.vector.tensor_tensor(out=tmpo[:], in0=ot[:], in1=pg[0:64, :], op=ALU.mult),
                                      r=[ot, pg], w=[tmpo])
                                S.pool(lambda: nc.gpsimd.tensor_tensor(out=acc[:], in0=acc[:], in1=tmpo[:], op=ALU.add),
                                       r=[acc, tmpo], w=[acc])
                        ob = obf[c % 2]
                        S.act(lambda ob=ob: nc.scalar.copy(out=ob[:], in_=acc[:]), r=[acc], w=[ob])
                        S.dma("sp", oT_dram[head * 64:(head + 1) * 64, qs], ob[:], r=[ob], w=[oT_dram])
            S.barrier()
        S.barrier()


def phase_nsa2(S, C, pT, vs_tok, vw_tok, j, oT_dram):
    nc = S.nc
    R_Q, R_KC, R_VC, R_KS, R_KW, R_GL = 0, 1024, 1280, 1536, 2048, 2560
    posk_d = S.dram("od_cmp_pos_kT", [2, 64, 32], F32)
    posv_d = S.dram("od_cmp_pos_vT", [2, 64, 32], F32)
    k1_d = S.dram("od_cmp_k1", [2, 2048, 256], F32)
    k2_d = S.dram("od_cmp_k2", [2, 256, 64], F32)
    v1_d = S.dram("od_cmp_v1", [2, 2048, 256], F32)
    v2_d = S.dram("od_cmp_v2", [2, 256, 64], F32)
    SC = 0.125
    with ExitStack() as es:
        KC = [S.sb(es, [64, 512], BF16, "KC") for _ in range(4)]
        for g in range(4):
            S.pool(lambda g=g: nc.gpsimd.memset(KC[g][:], 0.0), w=[KC[g]])
        VC = [S.sb(es, [128, 4, 64], BF16, "VC") for _ in range(4)]
        with ExitStack() as e1:
            src = S.sb(e1, [64, 8192], BF16, "csrc")
            w1 = S.sb(e1, [64, 32, 256], BF16, "w1")
            w2 = S.sb(e1, [128, 2, 64], BF16, "w2")
            posT = S.sb(e1, [64, 32], BF16, "posT")
            c1 = S.sb(e1, [128, 2], F32, "c1")
            h1 = S.sb(e1, [128, 2, 512], BF16, "h1")
            for which, (pos_d, a_d, b_d, row0) in enumerate(((posk_d, k1_d, k2_d, R_KC), (posv_d, v1_d, v2_d, R_VC))):
                S.dma("pool", w1[:], a_d[j].rearrange("(l d) n -> d l n", d=64), r=[a_d], w=[w1])
                S.dma("pool", w2[:], b_d[j].rearrange("(c p) n -> p c n", p=128), r=[b_d], w=[w2])
                S.dma("pool", posT[:], pos_d[j], r=[pos_d], w=[posT])
                for ncx in range(2):
                    ps = S.ps()
                    for l in range(32):
                        mm(S, ps, ps[:, 0:1], w1, w1[:, l, ncx * 128:(ncx + 1) * 128], posT, posT[:, l:l + 1], l == 0, l == 31)
                    S.act(lambda ps=ps, ncx=ncx: nc.scalar.copy(out=c1[:, ncx:ncx + 1], in_=ps[:, 0:1]), r=[ps], w=[c1])
                for g in range(4):
                    S.dma("pool", src[:], pT[row0 + g * 64:row0 + (g + 1) * 64, :], r=[pT], w=[src])
                    for ncx in range(2):
                        ps = S.ps()
                        for l in range(32):
                            mm(S, ps, ps[:, 0:511], w1, w1[:, l, ncx * 128:(ncx + 1) * 128], src,
                               src[:, l:l + 16 * 510 + 1:16], l == 0, l == 31)
                        S.act(lambda ps=ps, ncx=ncx: nc.scalar.activation(out=h1[:, ncx, 0:511], in_=ps[:, 0:511], func=AF.Silu,
                                                                          bias=c1[:, ncx:ncx + 1]), r=[ps, c1], w=[h1])
                    if which == 0:
                        ps = S.ps()
                        for ncx in range(2):
                            mm(S, ps, ps[0:64, 0:511], w2, w2[:, ncx, :], h1, h1[:, ncx, 0:511], ncx == 0, ncx == 1)
                        S.act(lambda ps=ps, g=g: nc.scalar.copy(out=KC[g][:, 0:511], in_=ps[0:64, 0:511]), r=[ps], w=[KC[g]])
                    else:
                        for nt in range(4):
                            rows = 128 if nt < 3 else 127
                            ps = S.ps()
                            for ncx in range(2):
                                mm(S, ps, ps[0:rows, 0:64], h1, h1[:, ncx, nt * 128:nt * 128 + rows], w2, w2[:, ncx, :],
                                   ncx == 0, ncx == 1)
                            S.act(lambda ps=ps, g=g, nt=nt, rows=rows: nc.scalar.copy(out=VC[g][0:rows, nt, :], in_=ps[0:rows, 0:64]),
                                  r=[ps], w=[VC[g]])
        S.barrier()
        sel01T = S.sb(es, [128, 8192], BF16, "sel01T")
        Wm = S.sb(es, [128, 16], F32, "Wm")
        Wm0 = S.sb(es, [128, 16], F32, "Wm0")
        for (t_, b_) in ((Wm, 97), (Wm0, -31)):
            S.pool(lambda t_=t_: nc.gpsimd.memset(t_[:], 0.0), w=[t_])
            S.pool(lambda t_=t_, b_=b_: nc.gpsimd.affine_select(out=t_[:], in_=t_[:], pattern=[[-16, 16]], compare_op=ALU.is_ge,
                                                                fill=NEG, base=b_, channel_multiplier=1), r=[t_], w=[t_])
        Ebig = S.sb(es, [128, 8192], BF16, "Ebig")
        def m01(name, base, cm, step):
            t = S.sb(es, [128, 512], BF16, name)
            S.pool(lambda: nc.gpsimd.memset(t[:], 1.0), w=[t])
            S.pool(lambda: nc.gpsimd.affine_select(out=t[:], in_=t[:], pattern=[[step, 512]], compare_op=ALU.is_ge, fill=0.0,
                                                   base=base, channel_multiplier=cm), r=[t], w=[t])
            return t
        Mc01 = [m01(f"Mc01{j_}", -128 * j_, -1, 1) for j_ in range(4)]
        Mw01 = [None] + [m01(f"Mw01{m_}", 511 - 128 * m_, 1, -1) for m_ in range(1, 5)]
        Mk01 = [m01(f"Mk01{d_}", 512 * d_ - 31, -16, 1) for d_ in range(5)]
        GS = S.dram("GS", [48, 8192], F32)
        with ExitStack() as eg:
            gt = S.sb(eg, [48, 8192], F32, "gt")
            S.dma("sp", gt[:], pT[R_GL:R_GL + 48, :], r=[pT], w=[gt])
            S.act(lambda: nc.scalar.activation(out=gt[:], in_=gt[:], func=AF.Sigmoid), r=[gt], w=[gt])
            S.dma("sp", GS[:, :], gt[:], r=[gt], w=[GS])
            S.barrier()
        S.pool(lambda: nc.gpsimd.memset(Ebig[:], 0.0), w=[Ebig])
        ebv = Ebig[:].rearrange("p (b x) -> p b x", x=64)
        S.pool(lambda: nc.gpsimd.affine_select(out=ebv, in_=ebv, pattern=[[-1, 128], [0, 64]], compare_op=ALU.not_equal,
                                               fill=1.0, base=0, channel_multiplier=1), r=[Ebig], w=[Ebig])
        for g in range(4):
            with ExitStack() as e2:
                QT4 = [S.sb(e2, [64, 8192], BF16, "QT4") for _ in range(4)]
                for hg in range(4):
                    r0 = R_Q + (g * 4 + hg) * 64
                    S.dma("pool", QT4[hg][:], pT[r0:r0 + 64, :], r=[pT], w=[QT4[hg]])
                NTI = 4
                BS = []
                for _ in range(NTI):
                    BS.append(dict(scs=[S.sb(e2, [128, 512], F32, "sc") for _ in range(2)],
                                   pp=S.sb(e2, [128, 516], F32, "pp"), rs=S.sb(e2, [128, 2], F32, "rs"),
                                   imp=S.sb(e2, [128, 128], F32, "imp"), imp2=S.sb(e2, [128, 128], F32, "imp2"),
                                   mx=S.sb(e2, [128, 8], F32, "mx")))
                    S.dve(lambda: nc.vector.memset(BS[-1]["pp"][:], 0.0), w=[BS[-1]["pp"]])

                def sel_gen(T, B):
                    scs, pp, rs, imp, imp2, mx = B["scs"], B["pp"], B["rs"], B["imp"], B["imp2"], B["mx"]
                    for hg in range(4):
                        ps = S.ps()
                        mm(S, ps, ps[:, 0:512], QT4[hg], QT4[hg][:, T * 128:(T + 1) * 128], KC[g], KC[g][:, 0:512], True, True)
                        sc = scs[hg % 2]
                        S.act(lambda: nc.scalar.activation(out=sc[:], in_=ps[:, 0:512], func=AF.Copy, scale=SC), r=[ps], w=[sc])
                        yield
                        w0 = max(8 * T - 8, 0)
                        w1_ = min(8 * T + 8, 512)
                        wm_ap = Wm0[:, 0:8] if T == 0 else Wm[:, 0:w1_ - w0]
                        S.pool(lambda: nc.gpsimd.tensor_tensor(out=sc[:, w0:w1_], in0=sc[:, w0:w1_], in1=wm_ap, op=ALU.add),
                               r=[sc, Wm, Wm0], w=[sc])
                        if w1_ < 512:
                            S.pool(lambda: nc.gpsimd.memset(sc[:, w1_:512], NEG), r=[sc], w=[sc])
                        yield
                        S.act(lambda: nc.scalar.activation(out=sc[:], in_=sc[:], func=AF.Exp, accum_out=rs[:, 0:1]),
                              r=[sc], w=[sc, rs])
                        yield
                        S.dve(lambda: nc.vector.tensor_scalar(out=rs[:, 1:2], in0=rs[:, 0:1], scalar1=C.tiny[:, 0:1], scalar2=None,
                                                              op0=ALU.max), r=[rs, C.tiny], w=[rs])
                        S.dve(lambda: nc.vector.reciprocal(out=rs[:, 1:2], in_=rs[:, 1:2]), r=[rs], w=[rs])
                        if hg == 0:
                            S.dve(lambda: nc.vector.tensor_scalar(out=pp[:, 1:513], in0=sc[:], scalar1=rs[:, 1:2], scalar2=None,
                                                                  op0=ALU.mult), r=[sc, rs], w=[pp])
                        else:
                            S.dve(lambda: nc.vector.scalar_tensor_tensor(out=pp[:, 1:513], in0=sc[:], scalar=rs[:, 1:2],
                                                                         in1=pp[:, 1:513], op0=ALU.mult, op1=ALU.add),
                                  r=[sc, rs, pp], w=[pp])
                        yield
                    a_ = pp[:, 0:512].rearrange("p (j f) -> p j f", f=4)
                    e_ = pp[:, 4:516].rearrange("p (j f) -> p j f", f=4)
                    S.dve(lambda: nc.vector.tensor_scalar(out=imp[:], in0=a_[:, :, 0], scalar1=0.5, scalar2=None, op0=ALU.mult),
                          r=[pp], w=[imp])
                    for f in (1, 2, 3):
                        S.dve(lambda f=f: nc.vector.tensor_tensor(out=imp[:], in0=imp[:], in1=a_[:, :, f], op=ALU.add),
                              r=[pp, imp], w=[imp])
                    S.dve(lambda: nc.vector.scalar_tensor_tensor(out=imp[:], in0=e_[:, :, 0], scalar=0.5, in1=imp[:],
                                                                 op0=ALU.mult, op1=ALU.add), r=[pp, imp], w=[imp])
                    yield
                    for half in range(2):
                        cur = 2 * T + half
                        hs = slice(half * 64, half * 64 + 64)
                        if cur + 1 < 128:
                            S.pool(lambda hs=hs, cur=cur: nc.gpsimd.memset(imp[hs, cur + 1:128], -1.0), r=[imp], w=[imp])
                        S.pool(lambda hs=hs, cur=cur: nc.gpsimd.memset(imp[hs, cur:cur + 1], 2e9), r=[imp], w=[imp])
                        if cur >= 1:
                            S.pool(lambda hs=hs, cur=cur: nc.gpsimd.memset(imp[hs, cur - 1:cur], 1e9), r=[imp], w=[imp])
                    S.pool(lambda: nc.gpsimd.memset(imp[:, 0:1], 3e9), r=[imp], w=[imp])
                    yield
                    S.dve(lambda: nc.vector.max(out=mx[:], in_=imp[:]), r=[imp], w=[mx])
                    S.dve(lambda: nc.vector.match_replace(out=imp2[:], in_to_replace=mx[:], in_values=imp[:], imm_value=-2.0),
                          r=[mx, imp], w=[imp2])
                    S.dve(lambda: nc.vector.max(out=mx[:], in_=imp2[:]), r=[imp2], w=[mx])
                    S.dve(lambda: nc.vector.tensor_scalar(out=imp2[:], in0=imp[:], scalar1=mx[:, 7:8], scalar2=None, op0=ALU.is_ge),
                          r=[imp, mx], w=[imp2])
                    yield
                    ps = S.ps()
                    S.pe(lambda: nc.tensor.transpose(out=ps[:, 0:128], in_=imp2[:, :], identity=C.ident[:, :]),
                         r=[imp2, C.ident], w=[ps])
                    S.act(lambda: nc.scalar.copy(out=sel01T[:, T * 128:(T + 1) * 128], in_=ps[:, 0:128]), r=[ps], w=[sel01T])
                    yield

                for T0 in range(0, NT, NTI):
                    gens = [sel_gen(T0 + i_, BS[i_]) for i_ in range(NTI)]
                    alive = list(gens)
                    while alive:
                        nxt = []
                        for g_ in alive:
                            try:
                                next(g_)
                                nxt.append(g_)
                            except StopIteration:
                                pass
                        alive = nxt
            S.barrier()
            with ExitStack() as e3:
                ksT2 = S.sb(e3, [128, 8192], BF16, "ksT2")
                kwT2 = S.sb(e3, [128, 8192], BF16, "kwT2")
                KC2 = S.sb(e3, [128, 512], BF16, "KC2")
                VSa = S.sb(e3, [128, 64, 65], BF16, "VSa")
                VWa = S.sb(e3, [128, 64, 65], BF16, "VWa")
                VCa = S.sb(e3, [128, 4, 65], BF16, "VCa")
                QTp = S.sb(e3, [128, 2, 8192], BF16, "QTp")
                Sel12 = S.sb(e3, [48, 12, 64], F32, "Sel12")
                NPT = 6
                pts = [S.sb(e3, [128, 512], BF16, "pt") for _ in range(NPT)]
                mks = [S.sb(e3, [128, 512], BF16, "mk") for _ in range(3)]
                gch = [S.sb(e3, [48, 512], F32, "gch") for _ in range(2)]
                acc = [S.sb(e3, [64, 512], F32, "acc") for _ in range(4)]
                T1 = [S.sb(e3, [64, 512], F32, "t1") for _ in range(4)]
                T2 = [S.sb(e3, [65, 512], F32, "t2") for _ in range(4)]
                obf = [S.sb(e3, [64, 512], BF16, "obf") for _ in range(2)]
                S.pool(lambda: nc.gpsimd.memset(Sel12[:], 0.0), w=[Sel12])
                S.pool(lambda g=g: nc.gpsimd.affine_select(out=Sel12[:], in_=Sel12[:], pattern=[[-1, 12], [0, 64]],
                                                           compare_op=ALU.not_equal, fill=1.0, base=-12 * g, channel_multiplier=1),
                       r=[Sel12], w=[Sel12])
                for hf in range(2):
                    ps_ = slice(hf * 64, hf * 64 + 64)
                    S.dma("pool", ksT2[ps_, :], pT[R_KS + g * 64:R_KS + (g + 1) * 64, :], r=[pT], w=[ksT2])
                    S.dma("pool", kwT2[ps_, :], pT[R_KW + g * 64:R_KW + (g + 1) * 64, :], r=[pT], w=[kwT2])
                    S.dma("sp", KC2[ps_, :], KC[g][:, :], r=[KC[g]], w=[KC2])
                for hg in range(4):
                    r0 = R_Q + (g * 4 + hg) * 64
                    S.dma("pool", QTp[(hg % 2) * 64:(hg % 2) * 64 + 64, hg // 2, :], pT[r0:r0 + 64, :], r=[pT], w=[QTp])
                for va in (VSa, VWa, VCa):
                    S.pool(lambda va=va: nc.gpsimd.memset(va[:], 1.0), w=[va])
                S.pool(lambda: nc.gpsimd.tensor_copy(out=VCa[:, :, 0:64], in_=VC[g][:, :, :]), r=[VC[g], VCa], w=[VCa])
                for q4 in range(4):
                    tsl = slice(q4 * 16, (q4 + 1) * 16)
                    rsl = slice(q4 * 2048, (q4 + 1) * 2048)
                    S.dma("pool", VSa[:, tsl, 0:64], vs_tok[rsl, g * 64:(g + 1) * 64].rearrange("(t p) d -> p t d", p=128),
                          r=[vs_tok], w=[VSa])
                    S.dma("pool", VWa[:, tsl, 0:64], vw_tok[rsl, g * 64:(g + 1) * 64].rearrange("(t p) d -> p t d", p=128),
                          r=[vw_tok], w=[VWa])
                cnt = {"pt": 0, "mk": 0, "ps": 0, "mul": 0}

                def ps3():
                    b_ = S.psb[cnt["ps"] % 4]
                    cnt["ps"] += 1
                    return b_

                for c in range(16):
                    qs = slice(c * 512, (c + 1) * 512)
                    gc_ = gch[c % 2]
                    S.dma("sp", gc_[:], GS[:, qs], r=[GS], w=[gc_])
                    for br in range(3):
                        items = []
                        if br == 0:
                            for nt in range(4):
                                D = c - 4 * nt
                                if D < 0:
                                    continue
                                items.append((nt, 128 if nt < 3 else 127, "const", Mk01[D] if D <= 4 else None))
                            kk, Va = KC2, VCa
                        elif br == 1:
                            for kt in range(4 * c + 4):
                                items.append((kt, 128, "sel", Mc01[kt - 4 * c] if kt >= 4 * c else None))
                            kk, Va = ksT2, VSa
                        else:
                            for jj in range(-4, 4):
                                kt = 4 * c + jj
                                if kt < 0:
                                    continue
                                items.append((kt, 128, "const", Mw01[-jj] if jj < 0 else Mc01[jj]))
                            kk, Va = kwT2, VWa
                        work = [(ii, hg) for ii in range(len(items)) for hg in range(4)]
                        state = {}

                        def stage1(w_):
                            ii, hg = w_
                            kt, nk, kind, arg = items[ii]
                            if kind == "sel" and hg == 0:
                                pm = ps3()
                                mm(S, pm, pm[0:nk, :], Ebig, Ebig[:, kt * 128:kt * 128 + nk], sel01T, sel01T[:, qs], True, True)
                                mk = mks[cnt["mk"] % 3]
                                cnt["mk"] += 1
                                if arg is None:
                                    S.act(lambda: nc.scalar.copy(out=mk[0:nk, :], in_=pm[0:nk, :]), r=[pm], w=[mk])
                                else:
                                    S.dve(lambda: nc.vector.tensor_tensor(out=mk[0:nk, :], in0=pm[0:nk, :], in1=arg[0:nk, :],
                                                                          op=ALU.mult), r=[pm, arg], w=[mk])
                                state[("mk", ii)] = mk
                            mask = state[("mk", ii)] if kind == "sel" else arg
                            pb_ = (hg % 2) * 64
                            pst = ps3()
                            mm(S, pst, pst[0:nk, :], kk, kk[pb_:pb_ + 64, kt * 128:kt * 128 + nk], QTp, QTp[pb_:pb_ + 64, hg // 2, qs],
                               True, True)
                            pt = pts[cnt["pt"] % NPT]
                            cnt["pt"] += 1
                            S.act(lambda: nc.scalar.activation(out=pt[0:nk, :], in_=pst[0:nk, :], func=AF.Exp, scale=SC),
                                  r=[pst], w=[pt])
                            if mask is not None:
                                cnt["mul"] += 1
                                if cnt["mul"] % 2 == 0:
                                    S.dve(lambda: nc.vector.tensor_tensor(out=pt[0:nk, :], in0=pt[0:nk, :], in1=mask[0:nk, :],
                                                                          op=ALU.mult), r=[pt, mask], w=[pt])
                                else:
                                    S.pool(lambda: nc.gpsimd.tensor_tensor(out=pt[0:nk, :], in0=pt[0:nk, :], in1=mask[0:nk, :],
                                                                           op=ALU.mult), r=[pt, mask], w=[pt])
                            state[("pt", ii, hg)] = pt

                        def stage2(w_):
                            ii, hg = w_
                            kt, nk, kind, arg = items[ii]
                            pt = state.pop(("pt", ii, hg))
                            po = S.psb[4 + hg]
                            mm(S, po, po[0:65, :], Va, Va[0:nk, kt, :], pt, pt[0:nk, :], ii == 0, ii == len(items) - 1)

                        DEPTH = 3
                        for i_ in range(len(work) + DEPTH):
                            if i_ < len(work):
                                stage1(work[i_])
                            if i_ >= DEPTH:
                                stage2(work[i_ - DEPTH])
                        def norm_fn(hg, br=br):
                            po = S.psb[4 + hg]
                            pb_ = S.psb[hg]
                            t1, t2 = T1[hg], T2[hg]
                            S.dve(lambda: nc.vector.tensor_scalar(out=t2[64:65, :], in0=po[64:65, :], scalar1=C.tiny[64:65, 0:1],
                                                                  scalar2=None, op0=ALU.max), r=[po, C.tiny], w=[t2])
                            S.dve(lambda: nc.vector.reciprocal(out=t2[64:65, :], in_=t2[64:65, :]), r=[t2], w=[t2])
                            mm(S, pb_, pb_[0:64, :], C.ones_f, C.ones_f[64:65, 0:64], t2, t2[64:65, :], True, True)
                            S.act(lambda: nc.scalar.copy(out=t1[:], in_=po[0:64, :]), r=[po], w=[t1])
                            S.dve(lambda: nc.vector.tensor_tensor(out=t1[:], in0=t1[:], in1=pb_[0:64, :], op=ALU.mult),
                                  r=[t1, pb_], w=[t1])
                            mm(S, pb_, pb_[0:64, :], Sel12, Sel12[:, hg * 3 + br, :], gc_, gc_[:, :], True, True)
                            if br == 0:
                                S.dve(lambda: nc.vector.tensor_tensor(out=acc[hg][:], in0=t1[:], in1=pb_[0:64, :], op=ALU.mult),
                                      r=[t1, pb_], w=[acc[hg]])
                            else:
                                S.dve(lambda: nc.vector.tensor_tensor(out=t2[0:64, :], in0=t1[:], in1=pb_[0:64, :], op=ALU.mult),
                                      r=[t1, pb_], w=[t2])
                                S.pool(lambda: nc.gpsimd.tensor_tensor(out=acc[hg][:], in0=acc[hg][:], in1=t2[0:64, :], op=ALU.add),
                                       r=[acc[hg], t2], w=[acc[hg]])

                        Interleaver(S).run([(lambda hg=hg: norm_fn(hg)) for hg in range(4)])
                    for hg in range(4):
                        head_ = g * 4 + hg
                        ob = obf[hg % 2]
                        S.act(lambda ob=ob, hg=hg: nc.scalar.copy(out=ob[:], in_=acc[hg][:]), r=[acc[hg]], w=[ob])
                        S.dma("sp", oT_dram[head_ * 64:(head_ + 1) * 64, qs], ob[:], r=[ob], w=[oT_dram])
            S.barrier()
        S.barrier()


W_NAMES = ["ev_w_in", "ev_w_uq", "ev_w_ukv", "ev_w_out", "ev_ff_gate", "ev_ff_up", "ev_ff_down",
           "od_w_in", "od_cmp_k1", "od_cmp_k2", "od_cmp_v1", "od_cmp_v2", "od_w_out", "od_router",
           "od_moe_w1", "od_moe_w3", "od_moe_w2"]
L_NAMES = ["xT", "cT", "ada_w", "ada_bT", "norm_gT", "final_gT", "pos64", "inv_freq2", "rope_sign", "ev_q_normT",
           "ev_kv_normT", "ev_conv_wT", "ev_a_log_b", "ev_dt_bias_b", "ev_dn_normT", "od_cmp_pos_kT", "od_cmp_pos_vT",
           "od_router_b_b"]


def build_program(n_layers=4):
    nc = bass.Bass("TRN2", target_bir_lowering=False)
    with ExitStack() as es:
        S = Sched(nc, es, ext_in=W_NAMES + L_NAMES, ext_out=["outT"])
        C = Ctx()
        S.init_psum()
        setup_consts(S, C)
        setup_attn_consts(S, C)
        phase_ada(S, C, 4)
        xT = S.dram("xT", [1024, 8192], F32)
        XA = S.dram("XA", [1024, 8192], F32)
        XB = S.dram("XB", [1024, 8192], F32)
        PT = S.dram("PT", [2608, 8192], F32)
        OT = S.dram("OT", [1024, 8192], BF16)
        VSd = S.dram("VS", [8192, 256], F32)
        VWd = S.dram("VW", [8192, 256], F32)
        outT = S.dram("outT", [1024, 8192], F32)
        ev_w_in = S.dram("ev_w_in", [2, 1024, 2504], F32)
        ev_w_out = S.dram("ev_w_out", [2, 1024, 1024], F32)
        wg = S.dram("ev_ff_gate", [2, 1024, 2816], F32)
        wu = S.dram("ev_ff_up", [2, 1024, 2816], F32)
        wd = S.dram("ev_ff_down", [2, 2816, 1024], F32)
        od_w_in = S.dram("od_w_in", [2, 1024, 2608], F32)
        od_w_out = S.dram("od_w_out", [2, 1024, 1024], F32)
        rt = S.dram("od_router", [2, 1024, 8], F32)
        rtb = S.dram("od_router_b_b", [2, 128, 8], F32)
        m1 = S.dram("od_moe_w1", [2, 8, 1024, 3584], F32)
        m3 = S.dram("od_moe_w3", [2, 8, 1024, 3584], F32)
        m2 = S.dram("od_moe_w2", [2, 8, 3584, 1024], F32)
        x_cur = xT
        for layer in range(n_layers):
            j = layer // 2
            ls = layer * 2
            if layer % 2 == 0:
                phase_inproj(S, C, x_cur, ls, ev_w_in, ev_w_in[j], 2504, PT)
                phase_dn(S, C, PT, j, OT)
                phase_mla(S, C, PT, j, OT)
                phase_outproj(S, C, x_cur, ls, ev_w_out, ev_w_out[j], OT, XA)
                phase_ffn(S, C, XA, ls + 1, XB, 2816, 1, wg, lambda e, j=j: wg[j], wu, lambda e, j=j: wu[j],
                          wd, lambda e, j=j: wd[j], TB=1024)
            else:
                phase_inproj(S, C, x_cur, ls, od_w_in, od_w_in[j], 2608, PT, tok_major=[(1792, 256, VSd), (2304, 256, VWd)])
                phase_nsa2(S, C, PT, VSd, VWd, j, OT)
                phase_outproj(S, C, x_cur, ls, od_w_out, od_w_out[j], OT, XA)
                phase_ffn(S, C, XA, ls + 1, XB, 3584, 8, m1, lambda e, j=j: m1[j, e], m3, lambda e, j=j: m3[j, e],
                          m2, lambda e, j=j: m2[j, e], router=(rt, rt[j], rtb, rtb[j]), TB=1024)
            x_cur = XB
        phase_final(S, C, x_cur, outT)
        S.finish()
    return nc, S


def kernel(**inputs):
    f = np.float32
    nc, _ = build_program()
    shared = {k: np.ascontiguousarray(np.asarray(inputs[k], f)) for k in W_NAMES}
    in_maps = []
    for core in range(8):
        b = core % 4
        m = common_inputs(inputs, b)
        m["od_router_b_b"] = np.ascontiguousarray(np.broadcast_to(np.asarray(inputs["od_router_b"], f)[:, None, :], (2, 128, 8)))
        d = dict(shared)
        for k in L_NAMES:
            d[k] = m[k]
        in_maps.append(d)
    res = run_bass_kernel_spmd(nc, in_maps, core_ids=list(range(8)))
    out = np.stack([np.ascontiguousarray(res.results[b]["outT"].T) for b in range(4)], axis=0)
    return out.astype(np.float32)
```

```python
import threading
import numpy as np
from contextlib import ExitStack
import concourse.bass as bass
import concourse.mybir as mybir
from concourse.bass_utils import run_bass_kernel_spmd

F32 = mybir.dt.float32
BF16 = mybir.dt.bfloat16
I32 = mybir.dt.int32
ALU = mybir.AluOpType
AF = mybir.ActivationFunctionType
AX = mybir.AxisListType

S_LEN = 8192
DM = 1024
NB = S_LEN // 512
NT = S_LEN // 128
EPS = 1e-6
NEG = -30000.0
SES = True


class Buf:
    __slots__ = ("t", "name", "lw", "rd")

    def __init__(self, t, name=""):
        self.t = t
        self.name = name
        self.lw = None
        self.rd = {}

    def __getitem__(self, i):
        return self.t[i]


class Interleaver:
    def __init__(self, S):
        self.S = S

    def run(self, fns):
        n = len(fns)
        self.n = n
        self.sems = [threading.Semaphore(0) for _ in range(n)]
        self.alive = [True] * n
        self.err = None
        self.tls = threading.local()
        threads = [threading.Thread(target=self._wrap, args=(i, fns[i])) for i in range(n)]
        self.S.coop = self
        for t in threads:
            t.start()
        self.sems[0].release()
        for t in threads:
            t.join()
        self.S.coop = None
        if self.err is not None:
            raise self.err

    def idx(self):
        return getattr(self.tls, "i", None)

    def _next(self, i):
        for k in range(1, self.n + 1):
            j = (i + k) % self.n
            if self.alive[j] and j != i:
                return j
        return None

    def _wrap(self, i, fn):
        self.sems[i].acquire()
        self.tls.i = i
        try:
            if self.err is None:
                fn()
        except BaseException as e:
            if self.err is None:
                self.err = e
        finally:
            self.alive[i] = False
            j = self._next(i)
            if j is not None:
                self.sems[j].release()

    def step(self):
        i = self.idx()
        if i is None:
            return
        if self.err is not None:
            raise RuntimeError("interleave abort")
        j = self._next(i)
        if j is None:
            return
        self.sems[j].release()
        self.sems[i].acquire()
        if self.err is not None:
            raise RuntimeError("interleave abort")


class Sched:
    def __init__(self, nc, es, ext_in=(), ext_out=()):
        self.nc = nc
        self.es = es
        self.ext_in = set(ext_in)
        self.ext_out = set(ext_out)
        self.eng = {"pe": nc.tensor, "act": nc.scalar, "dve": nc.vector, "pool": nc.gpsimd, "sp": nc.sync}
        self.sems = {}
        self.cnt = {}
        for e in self.eng:
            self.sems[e] = es.enter_context(nc.semaphore("s_" + e))
            self.cnt[e] = 0
        self.waited = {e: {} for e in self.eng}
        self.lanes = {"sp": 12, "pool": 8, "act": 4}
        self.lane_rr = {q: 0 for q in self.lanes}
        for q, n in self.lanes.items():
            for i in range(n):
                k = f"{q}{i}"
                self.sems[k] = es.enter_context(nc.semaphore("l_" + k))
                self.cnt[k] = 0
        self.coop = None
        self.coop_ps = {}
        self.psb = []
        self.ps_rr = 0
        self.ps4_rr = 0
        self.uid = 0
        self.dram_bufs = {}

    def sb(self, es, shape, dt, name=None):
        self.uid += 1
        name = (name or "t") + f"_{self.uid}"
        return Buf(es.enter_context(self.nc.sbuf_tensor(name, list(shape), dt)), name)

    def dram(self, name, shape, dt):
        if name in self.dram_bufs:
            return self.dram_bufs[name]
        kind = "ExternalInput" if name in self.ext_in else ("ExternalOutput" if name in self.ext_out else "Internal")
        b = Buf(self.nc.dram_tensor(name, list(shape), dt, kind=kind).ap(), name)
        self.dram_bufs[name] = b
        return b

    def init_psum(self):
        for i in range(8):
            self.psb.append(Buf(self.es.enter_context(self.nc.psum_tensor(f"ps{i}", [128, 512], F32)), f"ps{i}"))

    def ps(self):
        if self.coop is not None and self.coop.idx() is not None:
            i = self.coop.idx()
            k = self.coop_ps.get(i, 0)
            self.coop_ps[i] = k + 1
            return self.psb[2 * i + (k % 2)]
        b = self.psb[self.ps_rr]
        self.ps_rr = (self.ps_rr + 1) % 8
        return b

    def ps4(self):
        b = self.psb[self.ps4_rr]
        self.ps4_rr = (self.ps4_rr + 1) % 4
        return b

    def _wait(self, e, key, val):
        if val <= 0 or self.waited[e].get(key, 0) >= val:
            return
        self.eng[e].wait_ge(self.sems[key], val)
        self.waited[e][key] = val

    def _deps(self, e, r, w, is_dma=False):
        deps = {}
        for b in r:
            if b.lw is not None:
                k, v = b.lw
                if deps.get(k, 0) < v:
                    deps[k] = v
        for b in w:
            if b.lw is not None:
                k, v = b.lw
                if deps.get(k, 0) < v:
                    deps[k] = v
            for k, v in b.rd.items():
                if deps.get(k, 0) < v:
                    deps[k] = v
        for k, v in deps.items():
            if k == e and not is_dma and not (SES and e != "pe"):
                continue
            self._wait(e, k, v)

    def _commit(self, ev, r, w):
        k, v = ev
        for b in w:
            b.lw = ev
            b.rd = {}
        for b in r:
            if b.rd.get(k, 0) < v:
                b.rd[k] = v

    def op(self, e, fn, r=(), w=()):
        self._deps(e, r, w)
        ins = fn()
        self.cnt[e] += 1
        ins.then_inc(self.sems[e], 1)
        self._commit((e, self.cnt[e]), r, w)
        if self.coop is not None:
            self.coop.step()

    def pe(self, fn, r=(), w=()):
        self.op("pe", fn, r, w)

    def act(self, fn, r=(), w=()):
        self.op("act", fn, r, w)

    def dve(self, fn, r=(), w=()):
        self.op("dve", fn, r, w)

    def pool(self, fn, r=(), w=()):
        self.op("pool", fn, r, w)

    def dma(self, q, out, in_, r=(), w=(), **kw):
        self._deps(q, r, w, is_dma=True)
        i = self.lane_rr[q]
        self.lane_rr[q] = (i + 1) % self.lanes[q]
        key = f"{q}{i}"
        c = self.cnt[key]
        self._wait(q, key, 16 * c)
        ins = self.eng[q].dma_start(out=out, in_=in_, **kw)
        ins.then_inc(self.sems[key], 16)
        self.cnt[key] = c + 1
        self._commit((key, 16 * (c + 1)), r, w)
        if self.coop is not None:
            self.coop.step()

    def barrier(self):
        for e in self.eng:
            for k, c in self.cnt.items():
                if k == e or c == 0:
                    continue
                self._wait(e, k, c if k in self.eng else 16 * c)

    def finish(self):
        for k, c in self.cnt.items():
            if k == "sp" or c == 0:
                continue
            self._wait("sp", k, c if k in self.eng else 16 * c)


def mm(S, out_b, out_ap, l_b, l_ap, r_b, r_ap, start, stop):
    S.pe(lambda: S.nc.tensor.matmul(out_ap, l_ap, r_ap, start=start, stop=stop), r=[l_b, r_b], w=[out_b])


class Ctx:
    pass


def setup_consts(S, C):
    nc = S.nc
    es = S.es
    C.ident = S.sb(es, [128, 128], F32, "ident")
    C.ones_bf = S.sb(es, [128, 128], BF16, "ones_bf")
    C.ones_f = S.sb(es, [128, 128], F32, "ones_f")
    S.pool(lambda: nc.gpsimd.memset(C.ident[:], 0.0), w=[C.ident])
    S.pool(lambda: nc.gpsimd.affine_select(out=C.ident[:], in_=C.ident[:], pattern=[[-1, 128]],
                                           compare_op=ALU.not_equal, fill=1.0, base=0, channel_multiplier=1),
           r=[C.ident], w=[C.ident])
    S.pool(lambda: nc.gpsimd.memset(C.ones_bf[:], 1.0), w=[C.ones_bf])
    S.pool(lambda: nc.gpsimd.memset(C.ones_f[:], 1.0), w=[C.ones_f])
    C.eps = S.sb(es, [128, 1], F32, "eps")
    S.pool(lambda: nc.gpsimd.memset(C.eps[:], EPS), w=[C.eps])


def phase_ada(S, C, n_layers):
    nc = S.nc
    c_in = S.dram("cT", [128, 8], F32)
    ada_w = S.dram("ada_w", [4, 2, 1024, 3072], F32)
    ada_b = S.dram("ada_bT", [128, 8 * 24], F32)
    norm_g = S.dram("norm_gT", [128, 8 * 8], F32)
    C.mods = S.sb(S.es, [128, 8 * 24], F32, "mods")
    C.modA = S.sb(S.es, [128, 8 * 8], F32, "modA")
    with ExitStack() as es:
        sc = S.sb(es, [128, 8], F32, "sc")
        bb = S.sb(es, [128, 8 * 24], F32, "bb")
        gg = S.sb(es, [128, 8 * 8], F32, "gg")
        wbuf = [S.sb(es, [128, 8, 3072 // 2], F32, "adaw") for _ in range(2)]
        S.dma("sp", sc[:], c_in[:, :], r=[c_in], w=[sc])
        S.dma("sp", bb[:], ada_b[:, :], r=[ada_b], w=[bb])
        S.dma("sp", gg[:], norm_g[:, :], r=[norm_g], w=[gg])
        S.act(lambda: nc.scalar.activation(out=sc[:], in_=sc[:], func=AF.Silu), r=[sc], w=[sc])
        it = 0
        for l in range(n_layers):
            for s in range(2):
                ls = l * 2 + s
                for half in range(2):
                    wb = wbuf[it % 2]
                    it += 1
                    src = ada_w[l, s, :, half * 1536:(half + 1) * 1536].rearrange("(kc p) n -> p kc n", p=128)
                    for kc in range(8):
                        S.dma("sp", wb[:, kc, :], src[:, kc, :], r=[ada_w], w=[wb])
                    ps = S.ps()
                    for fc in range(12):
                        for kc in range(8):
                            mm(S, ps, ps[:, fc:fc + 1], wb, wb[:, kc, fc * 128:(fc + 1) * 128], sc, sc[:, kc:kc + 1],
                               kc == 0, kc == 7)
                    c0 = ls * 24 + half * 12
                    S.dve(lambda ps=ps, c0=c0: nc.vector.tensor_tensor(out=C.mods[:, c0:c0 + 12], in0=ps[:, 0:12],
                                                                       in1=bb[:, c0:c0 + 12], op=ALU.add),
                          r=[ps, bb], w=[C.mods])
                S.dve(lambda ls=ls: nc.vector.scalar_tensor_tensor(
                    out=C.modA[:, ls * 8:(ls + 1) * 8], in0=C.mods[:, ls * 24 + 8:ls * 24 + 16], scalar=1.0,
                    in1=gg[:, ls * 8:(ls + 1) * 8], op0=ALU.add, op1=ALU.mult), r=[C.mods, gg], w=[C.modA])
        S.barrier()


def modnorm_block(S, C, es_tiles, x_dram, ls, t0, xt, hT, hcol0, want_f32=None):
    nc = S.nc
    sq, rstd, tmp = es_tiles
    src = x_dram[:, t0:t0 + 512].rearrange("(kc p) t -> p kc t", p=128)
    S.dma("sp", xt[:, :, hcol0:hcol0 + 512], src, r=[x_dram], w=[xt])
    for kc in range(8):
        S.pool(lambda kc=kc: nc.gpsimd.tensor_tensor(out=sq[:, kc, :], in0=xt[:, kc, hcol0:hcol0 + 512],
                                                     in1=xt[:, kc, hcol0:hcol0 + 512], op=ALU.mult), r=[xt], w=[sq])
    ps = S.ps()
    for kc in range(8):
        mm(S, ps, ps[:, :], C.ones_bf, C.ones_bf[:, :], sq, sq[:, kc, :], kc == 0, kc == 7)
    S.act(lambda: nc.scalar.activation(out=rstd[:], in_=ps[:, :], func=AF.Sqrt, scale=1.0 / DM, bias=C.eps[:, 0:1]),
          r=[ps, C.eps], w=[rstd])
    S.dve(lambda: nc.vector.reciprocal(out=rstd[:], in_=rstd[:]), r=[rstd], w=[rstd])
    for kc in range(8):
        S.dve(lambda kc=kc: nc.vector.tensor_tensor(out=tmp[:], in0=xt[:, kc, hcol0:hcol0 + 512], in1=rstd[:],
                                                    op=ALU.mult), r=[xt, rstd], w=[tmp])
        a_ap = C.modA[:, ls * 8 + kc:ls * 8 + kc + 1]
        s_ap = C.mods[:, ls * 24 + kc:ls * 24 + kc + 1]
        S.dve(lambda kc=kc, a_ap=a_ap, s_ap=s_ap: nc.vector.tensor_scalar(
            out=hT[:, kc, hcol0:hcol0 + 512], in0=tmp[:], scalar1=a_ap, scalar2=s_ap, op0=ALU.mult, op1=ALU.add),
            r=[tmp, C.modA, C.mods], w=[hT])
        if want_f32 is not None:
            S.dve(lambda kc=kc, a_ap=a_ap, s_ap=s_ap: nc.vector.tensor_scalar(
                out=want_f32[:, kc, 0:512], in0=tmp[:], scalar1=a_ap, scalar2=s_ap, op0=ALU.mult,
                op1=ALU.add), r=[tmp, C.modA, C.mods], w=[want_f32])


def load_w_bf16(S, wt, w_ap, w_dram, K, N, n0=0, ncols=None):
    ncols = ncols or N
    src = w_ap[:, n0:n0 + ncols].rearrange("(kc p) n -> p kc n", p=128)
    for kc in range(K // 128):
        S.dma("pool", wt[:, kc, 0:ncols], src[:, kc, :], r=[w_dram], w=[wt])


def phase_inproj(S, C, x_dram, ls, w_dram, w_ap, ncols, pT_dram, tok_major=()):
    nc = S.nc
    n_ot = (ncols + 127) // 128
    with ExitStack() as es:
        wt = S.sb(es, [128, 8, ncols], BF16, "w_in")
        load_w_bf16(S, wt, w_ap, w_dram, DM, ncols)
        xts = [S.sb(es, [128, 8, 512], F32, "xt") for _ in range(2)]
        hTs = [S.sb(es, [128, 8, 512], BF16, "hT") for _ in range(2)]
        sq = S.sb(es, [128, 8, 512], BF16, "sq")
        rstd = S.sb(es, [128, 512], F32, "rstd")
        tmp = S.sb(es, [128, 512], F32, "tmp")
        stg = [S.sb(es, [128, 512], F32, "stg") for _ in range(4)]
        si = 0
        for tb in range(NB):
            xt = xts[tb % 2]
            hT = hTs[tb % 2]
            modnorm_block(S, C, (sq, rstd, tmp), x_dram, ls, tb * 512, xt, hT, 0)
            for ot in range(n_ot):
                m = min(128, ncols - ot * 128)
                ps = S.ps()
                for kc in range(8):
                    mm(S, ps, ps[0:m, :], wt, wt[:, kc, ot * 128:ot * 128 + m], hT, hT[:, kc, :], kc == 0, kc == 7)
                st = stg[si % 4]
                si += 1
                if ot % 2 == 0:
                    S.act(lambda st=st, ps=ps, m=m: nc.scalar.copy(out=st[0:m, :], in_=ps[0:m, :]), r=[ps], w=[st])
                else:
                    S.dve(lambda st=st, ps=ps, m=m: nc.vector.tensor_copy(out=st[0:m, :], in_=ps[0:m, :]), r=[ps], w=[st])
                S.dma("sp", pT_dram[ot * 128:ot * 128 + m, tb * 512:(tb + 1) * 512], st[0:m, :], r=[st], w=[pT_dram])
            for (c0, n, dst) in tok_major:
                for tt in range(4):
                    ps = S.ps()
                    for kc in range(8):
                        mm(S, ps, ps[:, 0:n], hT, hT[:, kc, tt * 128:(tt + 1) * 128], wt, wt[:, kc, c0:c0 + n],
                           kc == 0, kc == 7)
                    st = stg[si % 4]
                    si += 1
                    S.act(lambda st=st, ps=ps, n=n: nc.scalar.copy(out=st[:, 0:n], in_=ps[:, 0:n]), r=[ps], w=[st])
                    r0 = tb * 512 + tt * 128
                    S.dma("sp", dst[r0:r0 + 128, :], st[:, 0:n], r=[st], w=[dst])
        S.barrier()


def phase_outproj(S, C, x_dram, ls, w_dram, w_ap, oT_dram, xo_dram):
    nc = S.nc
    with ExitStack() as es:
        wt = S.sb(es, [128, 8, DM], BF16, "w_out")
        load_w_bf16(S, wt, w_ap, w_dram, DM, DM)
        xts = [S.sb(es, [128, 8, 512], F32, "xt") for _ in range(2)]
        ots = [S.sb(es, [128, 8, 512], BF16, "oT") for _ in range(2)]
        for tb in range(NB):
            xt = xts[tb % 2]
            ot_ = ots[tb % 2]
            sl = slice(tb * 512, (tb + 1) * 512)
            S.dma("sp", xt[:], x_dram[:, sl].rearrange("(kc p) t -> p kc t", p=128), r=[x_dram], w=[xt])
            S.dma("sp", ot_[:], oT_dram[:, sl].rearrange("(kc p) t -> p kc t", p=128), r=[oT_dram], w=[ot_])
            for oc in range(8):
                ps = S.ps()
                for kc in range(8):
                    mm(S, ps, ps[:, :], wt, wt[:, kc, oc * 128:(oc + 1) * 128], ot_, ot_[:, kc, :], kc == 0, kc == 7)
                g_ap = C.mods[:, ls * 24 + 16 + oc:ls * 24 + 17 + oc]
                S.dve(lambda ps=ps, oc=oc, g_ap=g_ap, xt=xt: nc.vector.scalar_tensor_tensor(
                    out=xt[:, oc, :], in0=ps[:, :], scalar=g_ap, in1=xt[:, oc, :], op0=ALU.mult, op1=ALU.add),
                    r=[ps, C.mods, xt], w=[xt])
            S.dma("sp", xo_dram[:, sl].rearrange("(kc p) t -> p kc t", p=128), xt[:], r=[xt], w=[xo_dram])
        S.barrier()


def phase_ffn(S, C, x_dram, ls, xo_dram, F, n_exp, wg_dram, wg_ap, wu_dram, wu_ap, wd_dram, wd_ap,
              router=None, TB=2048, CT=4, conv=True):
    nc = S.nc
    n_ft = F // 128
    chunks = [(c0, min(CT, n_ft - c0)) for c0 in range(0, n_ft, CT)]
    nsub = TB // 512
    WB = {}
    if conv:
        nq = 4 if n_ft % 4 == 0 else 2
        rq = DM // nq
        tq = n_ft // nq
        for e in range(n_exp):
            for q in range(nq):
                for nm, ap_fn, dr, shape, rows in (("g", wg_ap, wg_dram, [rq, F], slice(q * rq, (q + 1) * rq)),
                                                   ("u", wu_ap, wu_dram, [rq, F], slice(q * rq, (q + 1) * rq)),
                                                   ("d", wd_ap, wd_dram, [tq * 128, DM], slice(q * tq * 128, (q + 1) * tq * 128))):
                    b_ = S.dram(f"WB{nm}_{F}_{e}_{q}", shape, BF16)
                    WB[(nm, e, q)] = b_
                    S.dma("pool", b_[:, :], ap_fn(e)[rows, :], r=[dr], w=[b_])
    with ExitStack() as es:
        xt = S.sb(es, [128, 8, TB], F32, "xt")
        hT = S.sb(es, [128, 8, TB], BF16, "hT")
        sq = S.sb(es, [128, 8, 512], BF16, "sq")
        rstd = S.sb(es, [128, 512], F32, "rstd")
        tmp = S.sb(es, [128, 512], F32, "tmp")
        wgs = [S.sb(es, [128, 8, CT * 128], BF16, "wg") for _ in range(2)]
        wus = [S.sb(es, [128, 8, CT * 128], BF16, "wu") for _ in range(2)]
        wds = [S.sb(es, [128, CT, DM], BF16, "wd") for _ in range(2)]
        acts = [S.sb(es, [128, CT, 512], BF16, "act") for _ in range(max(2, nsub))]
        sgs = [S.sb(es, [128, 512], F32, "sg") for _ in range(2)]
        ytm = [S.sb(es, [128, 512], F32, "ytm") for _ in range(2)]
        if router is not None:
            hF = S.sb(es, [128, 8, 512], F32, "hF")
            wr = S.sb(es, [128, 8, 8], F32, "wr")
            rb = S.sb(es, [128, 8], F32, "rb")
            lg = S.sb(es, [128, 8], F32, "lg")
            mx = S.sb(es, [128, 8], F32, "mx")
            ex = S.sb(es, [128, 8], F32, "ex")
            den = S.sb(es, [128, 2], F32, "den")
            gw = S.sb(es, [128, 8], F32, "gw")
            gwT = S.sb(es, [8, TB], F32, "gwT")
            sel = S.sb(es, [8, 8 * 128], F32, "sel")
            Gs = [S.sb(es, [128, 512], F32, "G") for _ in range(nsub)]
            r_dram, r_ap, rb_dram, rb_ap = router
            S.dma("sp", wr[:], r_ap.rearrange("(kc p) n -> p kc n", p=128), r=[r_dram], w=[wr])
            S.dma("sp", rb[:], rb_ap, r=[rb_dram], w=[rb])
            S.pool(lambda: nc.gpsimd.memset(sel[:], 0.0), w=[sel])
            S.pool(lambda: nc.gpsimd.affine_select(out=sel[:].rearrange("k (e m) -> k e m", m=128),
                                                   in_=sel[:].rearrange("k (e m) -> k e m", m=128),
                                                   pattern=[[-1, 8], [0, 128]], compare_op=ALU.not_equal, fill=1.0,
                                                   base=0, channel_multiplier=1), r=[sel], w=[sel])
        wi = 0
        gi = 0
        ai = 0
        for tb in range(S_LEN // TB):
            for sub in range(nsub):
                modnorm_block(S, C, (sq, rstd, tmp), x_dram, ls, tb * TB + sub * 512, xt, hT, sub * 512,
                              want_f32=(hF if router is not None else None))
                if router is not None:
                    for tt in range(4):
                        ps = S.ps()
                        for kc in range(8):
                            mm(S, ps, ps[:, 0:8], hF, hF[:, kc, tt * 128:(tt + 1) * 128], wr, wr[:, kc, :], kc == 0, kc == 7)
                        S.dve(lambda ps=ps: nc.vector.tensor_tensor(out=lg[:], in0=ps[:, 0:8], in1=rb[:], op=ALU.add),
                              r=[ps, rb], w=[lg])
                        S.dve(lambda: nc.vector.max(out=mx[:], in_=lg[:]), r=[lg], w=[mx])
                        S.dve(lambda: nc.vector.tensor_scalar(out=ex[:], in0=lg[:], scalar1=mx[:, 0:1], scalar2=None,
                                                              op0=ALU.subtract), r=[lg, mx], w=[ex])
                        S.act(lambda: nc.scalar.activation(out=ex[:], in_=ex[:], func=AF.Exp), r=[ex], w=[ex])
                        S.dve(lambda: nc.vector.tensor_scalar(out=gw[:], in0=lg[:], scalar1=mx[:, 1:2], scalar2=None,
                                                              op0=ALU.is_ge), r=[lg, mx], w=[gw])
                        S.dve(lambda: nc.vector.tensor_tensor(out=gw[:], in0=gw[:], in1=ex[:], op=ALU.mult),
                              r=[gw, ex], w=[gw])
                        S.dve(lambda: nc.vector.reduce_sum(out=den[:, 0:1], in_=gw[:], axis=AX.X), r=[gw], w=[den])
                        S.dve(lambda: nc.vector.reciprocal(out=den[:, 1:2], in_=den[:, 0:1]), r=[den], w=[den])
                        S.dve(lambda: nc.vector.tensor_scalar(out=gw[:], in0=gw[:], scalar1=den[:, 1:2], scalar2=None,
                                                              op0=ALU.mult), r=[gw, den], w=[gw])
                        pt = S.ps()
                        S.pe(lambda pt=pt: nc.tensor.transpose(out=pt[0:8, 0:128], in_=gw[:, :], identity=C.ident[:, :]),
                             r=[gw, C.ident], w=[pt])
                        c0 = sub * 512 + tt * 128
                        S.act(lambda pt=pt, c0=c0: nc.scalar.copy(out=gwT[:, c0:c0 + 128], in_=pt[0:8, 0:128]),
                              r=[pt], w=[gwT])
            for e in range(n_exp):
                for (c0, ct) in chunks:
                    wg = wgs[wi % 2]
                    wu = wus[wi % 2]
                    wd = wds[wi % 2]
                    wi += 1
                    if conv:
                        kpq = rq // 128
                        for kc in range(8):
                            q, lk = kc // kpq, kc % kpq
                            for nm, wt_ in (("g", wg), ("u", wu)):
                                b_ = WB[(nm, e, q)]
                                S.dma("sp", wt_[:, kc, 0:ct * 128], b_[lk * 128:(lk + 1) * 128, c0 * 128:(c0 + ct) * 128],
                                      r=[b_], w=[wt_])
                        for f in range(ct):
                            q, lt = (c0 + f) // tq, (c0 + f) % tq
                            b_ = WB[("d", e, q)]
                            S.dma("sp", wd[:, f, :], b_[lt * 128:(lt + 1) * 128, :], r=[b_], w=[wd])
                    else:
                        load_w_bf16(S, wg, wg_ap(e), wg_dram, DM, F, n0=c0 * 128, ncols=ct * 128)
                        load_w_bf16(S, wu, wu_ap(e), wu_dram, DM, F, n0=c0 * 128, ncols=ct * 128)
                        S.dma("pool", wd[:, 0:ct, :], wd_ap(e)[c0 * 128:(c0 + ct) * 128, :].rearrange("(c p) n -> p c n", p=128),
                              r=[wd_dram], w=[wd])
                    sub_state = []
                    for sub in range(nsub):
                        ts = slice(sub * 512, (sub + 1) * 512)
                        G = None
                        if router is not None:
                            G = Gs[sub]
                        if router is not None and c0 == 0:
                            pg = S.ps()
                            mm(S, pg, pg[:, :], sel, sel[:, e * 128:(e + 1) * 128], gwT, gwT[:, ts], True, True)
                            S.act(lambda G=G, pg=pg: nc.scalar.copy(out=G[:], in_=pg[:, :]), r=[pg], w=[G])
                        act_t = acts[sub % len(acts)]
                        ai += 1
                        for f in range(ct):
                            pg = S.ps()
                            pu = S.ps()
                            for kc in range(8):
                                mm(S, pg, pg[:, :], wg, wg[:, kc, f * 128:(f + 1) * 128], hT, hT[:, kc, ts], kc == 0, kc == 7)
                            for kc in range(8):
                                mm(S, pu, pu[:, :], wu, wu[:, kc, f * 128:(f + 1) * 128], hT, hT[:, kc, ts], kc == 0, kc == 7)
                            sg = sgs[f % 2]
                            S.act(lambda sg=sg, pg=pg: nc.scalar.activation(out=sg[:], in_=pg[:, :], func=AF.Silu),
                                  r=[pg], w=[sg])
                            S.dve(lambda sg=sg, pu=pu, f=f, act_t=act_t: nc.vector.tensor_tensor(
                                out=act_t[:, f, :], in0=sg[:], in1=pu[:, :], op=ALU.mult), r=[sg, pu], w=[act_t])
                        sub_state.append((ts, G, act_t))
                    for (ts, G, act_t) in sub_state:
                        for oc in range(8):
                            py = S.ps()
                            for f in range(ct):
                                mm(S, py, py[:, :], wd, wd[:, f, oc * 128:(oc + 1) * 128], act_t, act_t[:, f, :],
                                   f == 0, f == ct - 1)
                            g_ap = C.mods[:, ls * 24 + 16 + oc:ls * 24 + 17 + oc]
                            if G is None:
                                S.dve(lambda py=py, oc=oc, g_ap=g_ap, ts=ts: nc.vector.scalar_tensor_tensor(
                                    out=xt[:, oc, ts], in0=py[:, :], scalar=g_ap, in1=xt[:, oc, ts], op0=ALU.mult,
                                    op1=ALU.add), r=[py, C.mods, xt], w=[xt])
                            else:
                                yt = ytm[oc % 2]
                                S.dve(lambda py=py, yt=yt, G=G: nc.vector.tensor_tensor(out=yt[:], in0=py[:, :], in1=G[:],
                                                                                        op=ALU.mult), r=[py, G], w=[yt])
                                S.dve(lambda yt=yt, oc=oc, g_ap=g_ap, ts=ts: nc.vector.scalar_tensor_tensor(
                                    out=xt[:, oc, ts], in0=yt[:], scalar=g_ap, in1=xt[:, oc, ts], op0=ALU.mult,
                                    op1=ALU.add), r=[yt, C.mods, xt], w=[xt])
            for kc in range(8):
                S.dma("sp", xo_dram[kc * 128:(kc + 1) * 128, tb * TB:(tb + 1) * TB], xt[:, kc, :], r=[xt], w=[xo_dram])
        S.barrier()


def phase_final(S, C, x_dram, out_dram):
    nc = S.nc
    fg = S.dram("final_gT", [128, 8], F32)
    with ExitStack() as es:
        g = S.sb(es, [128, 8], F32, "fg")
        S.dma("sp", g[:], fg[:, :], r=[fg], w=[g])
        xts = [S.sb(es, [128, 8, 512], F32, "xt") for _ in range(2)]
        sq = S.sb(es, [128, 8, 512], BF16, "sq")
        rstd = S.sb(es, [128, 512], F32, "rstd")
        for tb in range(NB):
            xt = xts[tb % 2]
            sl = slice(tb * 512, (tb + 1) * 512)
            S.dma("sp", xt[:], x_dram[:, sl].rearrange("(kc p) t -> p kc t", p=128), r=[x_dram], w=[xt])
            for kc in range(8):
                S.pool(lambda kc=kc, xt=xt: nc.gpsimd.tensor_tensor(out=sq[:, kc, :], in0=xt[:, kc, :], in1=xt[:, kc, :],
                                                                    op=ALU.mult), r=[xt], w=[sq])
            ps = S.ps()
            for kc in range(8):
                mm(S, ps, ps[:, :], C.ones_bf, C.ones_bf[:, :], sq, sq[:, kc, :], kc == 0, kc == 7)
            S.act(lambda ps=ps: nc.scalar.activation(out=rstd[:], in_=ps[:, :], func=AF.Sqrt, scale=1.0 / DM,
                                                     bias=C.eps[:, 0:1]), r=[ps, C.eps], w=[rstd])
            S.dve(lambda: nc.vector.reciprocal(out=rstd[:], in_=rstd[:]), r=[rstd], w=[rstd])
            for kc in range(8):
                S.dve(lambda kc=kc, xt=xt: nc.vector.scalar_tensor_tensor(
                    out=xt[:, kc, :], in0=xt[:, kc, :], scalar=g[:, kc:kc + 1], in1=rstd[:], op0=ALU.mult, op1=ALU.mult),
                    r=[xt, g, rstd], w=[xt])
            S.dma("sp", out_dram[:, sl].rearrange("(kc p) t -> p kc t", p=128), xt[:], r=[xt], w=[out_dram])
        S.barrier()


def common_inputs(inputs, b):
    f = np.float32
    m = {}
    m["xT"] = np.ascontiguousarray(np.asarray(inputs["x"][b], f).T)
    m["cT"] = np.ascontiguousarray(np.asarray(inputs["c"][b], f).reshape(8, 128).T)
    m["ada_w"] = np.asarray(inputs["ada_w"], f)
    m["ada_bT"] = np.ascontiguousarray(np.asarray(inputs["ada_b"], f).reshape(8, 24, 128).transpose(2, 0, 1).reshape(128, 192))
    m["norm_gT"] = np.ascontiguousarray(np.asarray(inputs["norm_g"], f).reshape(8, 8, 128).transpose(2, 0, 1).reshape(128, 64))
    m["final_gT"] = np.ascontiguousarray(np.asarray(inputs["final_g"], f).reshape(8, 128).T)
    m["pos64"] = np.ascontiguousarray(np.broadcast_to(np.asarray(inputs["positions"][b], np.int32)[None, :], (64, 8192)))
    inv = (1.0 / (10000.0 ** (np.arange(0, 64, 2, dtype=np.float32) / 64))).astype(f)
    m["inv_freq2"] = np.concatenate([inv, inv]).reshape(64, 1).astype(f)
    m["rope_sign"] = np.concatenate([-np.ones(32, f), np.ones(32, f)]).reshape(64, 1)
    m["ev_q_normT"] = np.ascontiguousarray(np.asarray(inputs["ev_q_norm"], f).reshape(2, 2, 128).transpose(0, 2, 1))
    m["ev_kv_normT"] = np.ascontiguousarray(np.asarray(inputs["ev_kv_norm"], f).reshape(2, 128, 1))
    m["ev_conv_wT"] = np.ascontiguousarray(np.asarray(inputs["ev_conv_w"], f).reshape(2, 4, 12, 128).transpose(0, 3, 2, 1))
    m["ev_a_log_b"] = np.ascontiguousarray(np.broadcast_to(np.asarray(inputs["ev_a_log"], f)[:, None, :], (2, 128, 4)))
    m["ev_dt_bias_b"] = np.ascontiguousarray(np.broadcast_to(np.asarray(inputs["ev_dt_bias"], f)[:, None, :], (2, 128, 4)))
    m["ev_dn_normT"] = np.ascontiguousarray(np.asarray(inputs["ev_dn_norm"], f).reshape(2, 128, 1))
    m["od_cmp_pos_kT"] = np.ascontiguousarray(np.asarray(inputs["od_cmp_pos_k"], f).transpose(0, 2, 1))
    m["od_cmp_pos_vT"] = np.ascontiguousarray(np.asarray(inputs["od_cmp_pos_v"], f).transpose(0, 2, 1))
    return m


def setup_attn_consts(S, C):
    nc = S.nc
    es = S.es
    C.ident_bf = S.sb(es, [128, 128], BF16, "ident_bf")
    S.dve(lambda: nc.vector.tensor_copy(out=C.ident_bf[:], in_=C.ident[:]), r=[C.ident], w=[C.ident_bf])
    C.tiny = S.sb(es, [128, 1], F32, "tiny")
    S.pool(lambda: nc.gpsimd.memset(C.tiny[:], 1e-30), w=[C.tiny])

    def mask_tile(name, base, cm, step, op=ALU.is_ge):
        t = S.sb(es, [128, 512], BF16, name)
        S.pool(lambda: nc.gpsimd.memset(t[:], 0.0), w=[t])
        S.pool(lambda: nc.gpsimd.affine_select(out=t[:], in_=t[:], pattern=[[step, 512]], compare_op=op, fill=NEG,
                                               base=base, channel_multiplier=cm), r=[t], w=[t])
        return t
    C.Mc = [mask_tile(f"Mc{j}", -128 * j, -1, 1) for j in range(4)]


def attn_chunk(S, C, c, bank, kparts, qparts, V, dv, kts, scale, pts, ot, rden, pi0=0):
    nc = S.nc
    po = S.psb[4 + bank % 2]
    pd = S.psb[6 + bank % 2]
    qs = slice(c * 512, (c + 1) * 512)
    n = len(kts)
    held = {}

    def st1(i):
        kt, nk, masks = kts[i]
        pst = S.ps4()
        n_mm = len(kparts) + len(masks)
        j = 0
        for (kb, rows), (qb, _) in zip(kparts, qparts):
            mm(S, pst, pst[0:nk, :], kb, kb[0:rows, kt * 128:kt * 128 + nk], qb, qb[0:rows, qs], j == 0, j == n_mm - 1)
            j += 1
        for (lb, lap, rb, rap) in masks:
            mm(S, pst, pst[0:nk, :], lb, lap, rb, rap, False, j == n_mm - 1)
            j += 1
        pt = pts[(pi0 + i) % len(pts)]
        S.act(lambda: nc.scalar.activation(out=pt[0:nk, :], in_=pst[0:nk, :], func=AF.Exp, scale=scale), r=[pst], w=[pt])
        held[i] = pt

    def st2(i):
        kt, nk, masks = kts[i]
        pt = held.pop(i)
        mm(S, po, po[0:dv, :], V, V[0:nk, kt, :], pt, pt[0:nk, :], i == 0, i == n - 1)
        mm(S, pd, pd[0:dv, :], C.ones_bf, C.ones_bf[0:nk, 0:dv], pt, pt[0:nk, :], i == 0, i == n - 1)

    D = min(2, len(pts) - 1)
    for i in range(n + D):
        if i < n:
            st1(i)
        if i >= D:
            st2(i - D)
    S.dve(lambda: nc.vector.tensor_scalar(out=rden[0:dv, :], in0=pd[0:dv, :], scalar1=C.tiny[0:dv, 0:1],
                                          scalar2=None, op0=ALU.max), r=[pd, C.tiny], w=[rden])
    S.dve(lambda: nc.vector.reciprocal(out=rden[0:dv, :], in_=rden[0:dv, :]), r=[rden], w=[rden])
    S.dve(lambda: nc.vector.tensor_tensor(out=ot[0:dv, :], in0=po[0:dv, :], in1=rden[0:dv, :], op=ALU.mult),
          r=[po, rden], w=[ot])
    return pi0 + n


def attn_head(S, C, es, kparts, qparts, V, dv, kts_fn, scale, out_cb, pts, otiles, rden):
    pi = 0
    for c in range(16):
        ot = otiles[c % len(otiles)]
        pi = attn_chunk(S, C, c, c, kparts, qparts, V, dv, kts_fn(c), scale, pts, ot, rden, pi)
        out_cb(c, ot)


def phase_mla(S, C, pT, j, oT_dram):
    nc = S.nc
    R_CQ, R_CKV, R_KR = 2056, 2312, 2440
    w_uq = S.dram("ev_w_uq", [2, 256, 768], F32)
    w_ukv = S.dram("ev_w_ukv", [2, 128, 1024], F32)
    qn_d = S.dram("ev_q_normT", [2, 128, 2], F32)
    kvn_d = S.dram("ev_kv_normT", [2, 128, 1], F32)
    pos_d = S.dram("pos64", [64, 8192], I32)
    inv_d = S.dram("inv_freq2", [64, 1], F32)
    sgn_d = S.dram("rope_sign", [64, 1], F32)
    with ExitStack() as es:
        cos2 = S.sb(es, [64, 8192], BF16, "cos2")
        sin2 = S.sb(es, [64, 8192], BF16, "sin2s")
        cqn = S.sb(es, [128, 2, 8192], BF16, "cqn")
        ckvn = S.sb(es, [128, 8192], BF16, "ckvn")
        KR = S.sb(es, [64, 8192], BF16, "KR")
        wq = S.sb(es, [128, 2, 768], BF16, "wq")
        wqs = S.sb(es, [128, 2, 4, 64], BF16, "wqs")
        wkv = S.sb(es, [128, 1024], BF16, "wkv")
        qn = S.sb(es, [128, 2], F32, "qn")
        kvn = S.sb(es, [128, 1], F32, "kvn")
        inv = S.sb(es, [64, 1], F32, "inv")
        sgn = S.sb(es, [64, 1], F32, "sgn")
        negpi = S.sb(es, [64, 1], F32, "negpi")
        S.pool(lambda: nc.gpsimd.memset(negpi[:], -float(np.pi)), w=[negpi])
        S.dma("sp", qn[:], qn_d[j], r=[qn_d], w=[qn])
        S.dma("sp", kvn[:], kvn_d[j], r=[kvn_d], w=[kvn])
        S.dma("sp", inv[:], inv_d[:, :], r=[inv_d], w=[inv])
        S.dma("sp", sgn[:], sgn_d[:, :], r=[sgn_d], w=[sgn])
        for kc in range(2):
            S.dma("pool", wq[:, kc, :], w_uq[j, kc * 128:(kc + 1) * 128, :], r=[w_uq], w=[wq])
            for h in range(4):
                b0 = h * 192 + 128
                S.dma("pool", wqs[:, kc, h, 0:32], w_uq[j, kc * 128:(kc + 1) * 128, b0 + 32:b0 + 64], r=[w_uq], w=[wqs])
                S.dma("pool", wqs[:, kc, h, 32:64], w_uq[j, kc * 128:(kc + 1) * 128, b0:b0 + 32], r=[w_uq], w=[wqs])
        S.dma("pool", wkv[:], w_ukv[j, :, :], r=[w_ukv], w=[wkv])
        with ExitStack() as es2:
            posi = S.sb(es2, [64, 512], I32, "posi")
            ang = S.sb(es2, [64, 512], F32, "ang")
            u = S.sb(es2, [64, 512], F32, "u")
            ni = S.sb(es2, [64, 512], I32, "ni")
            nf = S.sb(es2, [64, 512], F32, "nf")
            cq = S.sb(es2, [128, 2, 512], F32, "cq")
            ckv = S.sb(es2, [128, 512], F32, "ckv")
            kr = S.sb(es2, [64, 512], F32, "kr")
            krs = S.sb(es2, [64, 512], F32, "krs")
            sq = S.sb(es2, [128, 3, 512], BF16, "sq")
            rstd = S.sb(es2, [128, 512], F32, "rstd")
            tmp = S.sb(es2, [128, 512], F32, "tmp")
            for tb in range(NB):
                ts = slice(tb * 512, (tb + 1) * 512)
                S.dma("sp", posi[:], pos_d[:, ts], r=[pos_d], w=[posi])
                S.dve(lambda: nc.vector.tensor_copy(out=ang[:], in_=posi[:]), r=[posi], w=[ang])
                S.dve(lambda: nc.vector.tensor_scalar(out=ang[:], in0=ang[:], scalar1=inv[:, 0:1], scalar2=None,
                                                      op0=ALU.mult), r=[ang, inv], w=[ang])
                for (dst, off, signed) in ((sin2, 0.5, True), (cos2, 0.75, False)):
                    S.dve(lambda off=off: nc.vector.tensor_scalar(out=u[:], in0=ang[:], scalar1=float(1.0 / (2 * np.pi)),
                                                                  scalar2=off, op0=ALU.mult, op1=ALU.add), r=[ang], w=[u])
                    S.dve(lambda: nc.vector.tensor_copy(out=ni[:], in_=u[:]), r=[u], w=[ni])
                    S.dve(lambda: nc.vector.tensor_copy(out=nf[:], in_=ni[:]), r=[ni], w=[nf])
                    S.dve(lambda: nc.vector.tensor_tensor(out=u[:], in0=u[:], in1=nf[:], op=ALU.subtract), r=[u, nf], w=[u])
                    S.dve(lambda: nc.vector.tensor_scalar(out=nf[:], in0=u[:], scalar1=0.0, scalar2=None, op0=ALU.is_lt),
                          r=[u], w=[nf])
                    S.dve(lambda: nc.vector.tensor_tensor(out=u[:], in0=u[:], in1=nf[:], op=ALU.add), r=[u, nf], w=[u])
                    S.act(lambda: nc.scalar.activation(out=u[:], in_=u[:], func=AF.Sin, scale=float(2 * np.pi),
                                                       bias=negpi[:, 0:1]), r=[u, negpi], w=[u])
                    if signed:
                        S.dve(lambda dst=dst, ts=ts: nc.vector.tensor_scalar(out=dst[:, ts], in0=u[:], scalar1=sgn[:, 0:1],
                                                                             scalar2=None, op0=ALU.mult), r=[u, sgn], w=[dst])
                    else:
                        S.dve(lambda dst=dst, ts=ts: nc.vector.tensor_copy(out=dst[:, ts], in_=u[:]), r=[u], w=[dst])
                S.dma("sp", cq[:], pT[R_CQ:R_CQ + 256, ts].rearrange("(kc p) t -> p kc t", p=128), r=[pT], w=[cq])
                S.dma("sp", ckv[:], pT[R_CKV:R_CKV + 128, ts], r=[pT], w=[ckv])
                S.dma("sp", kr[:], pT[R_KR:R_KR + 64, ts], r=[pT], w=[kr])
                S.dma("sp", krs[0:32, :], pT[R_KR + 32:R_KR + 64, ts], r=[pT], w=[krs])
                S.dma("sp", krs[32:64, :], pT[R_KR:R_KR + 32, ts], r=[pT], w=[krs])
                for kc in range(2):
                    S.pool(lambda kc=kc: nc.gpsimd.tensor_tensor(out=sq[:, kc, :], in0=cq[:, kc, :], in1=cq[:, kc, :],
                                                                 op=ALU.mult), r=[cq], w=[sq])
                S.pool(lambda: nc.gpsimd.tensor_tensor(out=sq[:, 2, :], in0=ckv[:], in1=ckv[:], op=ALU.mult), r=[ckv], w=[sq])
                ps = S.ps4()
                for kc in range(2):
                    mm(S, ps, ps[:, :], C.ones_bf, C.ones_bf[:, :], sq, sq[:, kc, :], kc == 0, kc == 1)
                S.act(lambda ps=ps: nc.scalar.activation(out=rstd[:], in_=ps[:, :], func=AF.Sqrt, scale=1.0 / 256,
                                                         bias=C.eps[:, 0:1]), r=[ps, C.eps], w=[rstd])
                S.dve(lambda: nc.vector.reciprocal(out=rstd[:], in_=rstd[:]), r=[rstd], w=[rstd])
                for kc in range(2):
                    S.dve(lambda kc=kc: nc.vector.tensor_tensor(out=tmp[:], in0=cq[:, kc, :], in1=rstd[:], op=ALU.mult),
                          r=[cq, rstd], w=[tmp])
                    S.dve(lambda kc=kc, ts=ts: nc.vector.tensor_scalar(out=cqn[:, kc, ts], in0=tmp[:], scalar1=qn[:, kc:kc + 1],
                                                                       scalar2=None, op0=ALU.mult), r=[tmp, qn], w=[cqn])
                ps = S.ps4()
                mm(S, ps, ps[:, :], C.ones_bf, C.ones_bf[:, :], sq, sq[:, 2, :], True, True)
                S.act(lambda ps=ps: nc.scalar.activation(out=rstd[:], in_=ps[:, :], func=AF.Sqrt, scale=1.0 / 128,
                                                         bias=C.eps[:, 0:1]), r=[ps, C.eps], w=[rstd])
                S.dve(lambda: nc.vector.reciprocal(out=rstd[:], in_=rstd[:]), r=[rstd], w=[rstd])
                S.dve(lambda: nc.vector.tensor_tensor(out=tmp[:], in0=ckv[:], in1=rstd[:], op=ALU.mult), r=[ckv, rstd], w=[tmp])
                S.dve(lambda ts=ts: nc.vector.tensor_scalar(out=ckvn[:, ts], in0=tmp[:], scalar1=kvn[:, 0:1], scalar2=None,
                                                            op0=ALU.mult), r=[tmp, kvn], w=[ckvn])
                S.dve(lambda ts=ts: nc.vector.tensor_tensor(out=kr[:], in0=kr[:], in1=cos2[:, ts], op=ALU.mult), r=[kr, cos2], w=[kr])
                S.dve(lambda ts=ts: nc.vector.tensor_tensor(out=krs[:], in0=krs[:], in1=sin2[:, ts], op=ALU.mult), r=[krs, sin2], w=[krs])
                S.dve(lambda ts=ts: nc.vector.tensor_tensor(out=KR[:, ts], in0=kr[:], in1=krs[:], op=ALU.add), r=[kr, krs], w=[KR])
        S.barrier()
        with ExitStack() as es3:
            QN = S.sb(es3, [128, 8192], BF16, "QN")
            QR = S.sb(es3, [64, 8192], BF16, "QR")
            KN = S.sb(es3, [128, 8192], BF16, "KN")
            V = S.sb(es3, [128, 64, 128], BF16, "V")
            pts = [S.sb(es3, [128, 512], BF16, "pt") for _ in range(4)]
            otiles = [S.sb(es3, [128, 512], BF16, "ot") for _ in range(2)]
            rden = S.sb(es3, [128, 512], F32, "rden")
            t1 = S.sb(es3, [64, 512], F32, "t1")
            t2 = S.sb(es3, [64, 512], F32, "t2")
            for h in range(4):
                for tb in range(NB):
                    ts = slice(tb * 512, (tb + 1) * 512)
                    ps = S.ps4()
                    for kc in range(2):
                        mm(S, ps, ps[:, :], wq, wq[:, kc, h * 192:h * 192 + 128], cqn, cqn[:, kc, ts], kc == 0, kc == 1)
                    S.act(lambda ps=ps, ts=ts: nc.scalar.copy(out=QN[:, ts], in_=ps[:, :]), r=[ps], w=[QN])
                    ps = S.ps4()
                    for kc in range(2):
                        mm(S, ps, ps[0:64, :], wq, wq[:, kc, h * 192 + 128:h * 192 + 192], cqn, cqn[:, kc, ts], kc == 0, kc == 1)
                    ps2 = S.ps4()
                    for kc in range(2):
                        mm(S, ps2, ps2[0:64, :], wqs, wqs[:, kc, h, :], cqn, cqn[:, kc, ts], kc == 0, kc == 1)
                    S.dve(lambda ps=ps, ts=ts: nc.vector.tensor_tensor(out=t1[:], in0=ps[0:64, :], in1=cos2[:, ts], op=ALU.mult),
                          r=[ps, cos2], w=[t1])
                    S.dve(lambda ps2=ps2, ts=ts: nc.vector.tensor_tensor(out=t2[:], in0=ps2[0:64, :], in1=sin2[:, ts], op=ALU.mult),
                          r=[ps2, sin2], w=[t2])
                    S.dve(lambda ts=ts: nc.vector.tensor_tensor(out=QR[:, ts], in0=t1[:], in1=t2[:], op=ALU.add), r=[t1, t2], w=[QR])
                    ps = S.ps4()
                    mm(S, ps, ps[:, :], wkv, wkv[:, h * 256:h * 256 + 128], ckvn, ckvn[:, ts], True, True)
                    S.act(lambda ps=ps, ts=ts: nc.scalar.copy(out=KN[:, ts], in_=ps[:, :]), r=[ps], w=[KN])
                    ps = S.ps4()
                    for tt in range(4):
                        mm(S, ps, ps[:, tt * 128:(tt + 1) * 128], ckvn, ckvn[:, tb * 512 + tt * 128:tb * 512 + (tt + 1) * 128],
                           wkv, wkv[:, h * 256 + 128:h * 256 + 256], True, True)
                    S.act(lambda ps=ps, tb=tb: nc.scalar.copy(out=V[:, tb * 4:(tb + 1) * 4, :],
                                                              in_=ps[:, :].rearrange("p (t d) -> p t d", d=128)), r=[ps], w=[V])

                def kts_fn(c):
                    out = []
                    for kt in range(4 * c + 4):
                        masks = []
                        if kt >= 4 * c:
                            m = C.Mc[kt - 4 * c]
                            masks = [(C.ident_bf, C.ident_bf[:, :], m, m[:, :])]
                        out.append((kt, 128, masks))
                    return out

                def out_cb(c, ot, h=h):
                    S.dma("sp", oT_dram[512 + h * 128:512 + (h + 1) * 128, c * 512:(c + 1) * 512], ot[:, :], r=[ot], w=[oT_dram])

                attn_head(S, C, es3, [(KN, 128), (KR, 64)], [(QN, 128), (QR, 64)], V, 128, kts_fn, 192 ** -0.5, out_cb,
                          pts, otiles, rden)
        S.barrier()


def phase_dn(S, C, pT, j, oT_dram):
    nc = S.nc
    cw_d = S.dram("ev_conv_wT", [2, 128, 12, 4], F32)
    al_d = S.dram("ev_a_log_b", [2, 128, 4], F32)
    dt_d = S.dram("ev_dt_bias_b", [2, 128, 4], F32)
    gn_d = S.dram("ev_dn_normT", [2, 128, 1], F32)
    with ExitStack() as es:
        cw = S.sb(es, [128, 12, 4], F32, "cw")
        al = S.sb(es, [128, 4], F32, "al")
        dtb = S.sb(es, [128, 4], F32, "dtb")
        gn = S.sb(es, [128, 1], F32, "gn")
        S.dma("sp", cw[:], cw_d[j], r=[cw_d], w=[cw])
        S.dma("sp", al[:], al_d[j], r=[al_d], w=[al])
        S.dma("sp", dtb[:], dt_d[j], r=[dt_d], w=[dtb])
        S.dma("sp", gn[:], gn_d[j], r=[gn_d], w=[gn])
        S.act(lambda: nc.scalar.activation(out=al[:], in_=al[:], func=AF.Exp), r=[al], w=[al])
        UT = S.sb(es, [128, 128], F32, "UT")
        S.pool(lambda: nc.gpsimd.memset(UT[:], 1.0), w=[UT])
        S.pool(lambda: nc.gpsimd.affine_select(out=UT[:], in_=UT[:], pattern=[[1, 128]], compare_op=ALU.is_ge, fill=0.0,
                                               base=0, channel_multiplier=-1), r=[UT], w=[UT])
        PM1 = S.sb(es, [128, 128], F32, "PM1")
        S.pool(lambda: nc.gpsimd.memset(PM1[:], 0.0), w=[PM1])
        S.pool(lambda: nc.gpsimd.affine_select(out=PM1[:], in_=PM1[:], pattern=[[-1, 128]], compare_op=ALU.is_gt, fill=1e5,
                                               base=0, channel_multiplier=1), r=[PM1], w=[PM1])
        NM2 = S.sb(es, [128, 128], F32, "NM2")
        S.pool(lambda: nc.gpsimd.memset(NM2[:], 0.0), w=[NM2])
        S.pool(lambda: nc.gpsimd.affine_select(out=NM2[:], in_=NM2[:], pattern=[[1, 128]], compare_op=ALU.is_ge, fill=-1e5,
                                               base=0, channel_multiplier=-1), r=[NM2], w=[NM2])
        GV = {n_: [S.sb(es, [64, 128] if n_ in ("braw", "araw") else [128, 64], F32, n_) for _ in range(4)]
              for n_ in ("braw", "araw", "beta", "g", "gc", "ngc", "glast", "alast", "etail", "bexpg", "nbeta")}
        Sts = [S.sb(es, [128, 128], F32, "state") for _ in range(4)]
        NS = 4
        def mk(name, shape=(128, 128), dt=F32):
            return [S.sb(es, list(shape), dt, name) for _ in range(NS)]
        raw = {n: mk("raw" + n, (128, 131)) for n in "qkv"}
        cv = {n: mk("cv" + n) for n in "qkv"}
        rn = mk("rn")
        qT = mk("qT"); kT = mk("kT"); ktok = mk("ktok"); vtok = mk("vtok")
        dgc = mk("dgc"); Dm = mk("Dm"); DiT = mk("DiT"); egr = mk("egr")
        Nm = mk("Nm"); NmT = mk("NmT"); M2 = mk("M2"); M2T = mk("M2T"); RT = mk("RT")
        qkT = mk("qkT"); qgT = mk("qgT"); vb = mk("vb"); kbg = mk("kbg"); ktl = mk("ktl")
        u = mk("u"); wT = mk("wT"); vnew = mk("vnew"); osb = mk("osb"); osq = mk("osq"); zt = mk("zt")
        obf = mk("obf", (128, 128), BF16)

        def evac(dst, ps, eng="act"):
            if eng == "act":
                S.act(lambda: nc.scalar.copy(out=dst[:], in_=ps[:, 0:128]), r=[ps], w=[dst])
            else:
                S.dve(lambda: nc.vector.tensor_copy(out=dst[:], in_=ps[:, 0:128]), r=[ps], w=[dst])

        def head_fn(h):
            braw, araw, beta, g, gc, ngc, glast, alast, etail, bexpg, nbeta = (GV[n_][h] for n_ in (
                "braw", "araw", "beta", "g", "gc", "ngc", "glast", "alast", "etail", "bexpg", "nbeta"))
            St = Sts[h]
            S.dma("sp", braw[:], pT[2048 + h, :].rearrange("(t p) -> t p", p=128), r=[pT], w=[braw])
            S.dma("sp", araw[:], pT[2052 + h, :].rearrange("(t p) -> t p", p=128), r=[pT], w=[araw])
            ps = S.ps()
            S.pe(lambda ps=ps: nc.tensor.transpose(out=ps[:, 0:64], in_=braw[:, :], identity=C.ident[0:64, 0:64]),
                 r=[braw, C.ident], w=[ps])
            S.act(lambda ps=ps: nc.scalar.activation(out=beta[:], in_=ps[:, 0:64], func=AF.Sigmoid), r=[ps], w=[beta])
            ps = S.ps()
            S.pe(lambda ps=ps: nc.tensor.transpose(out=ps[:, 0:64], in_=araw[:, :], identity=C.ident[0:64, 0:64]),
                 r=[araw, C.ident], w=[ps])
            S.act(lambda ps=ps, h=h: nc.scalar.activation(out=g[:], in_=ps[:, 0:64], func=AF.Exp, bias=dtb[:, h:h + 1]),
                  r=[ps, dtb], w=[g])
            S.dve(lambda: nc.vector.tensor_scalar(out=g[:], in0=g[:], scalar1=1.0, scalar2=None, op0=ALU.add), r=[g], w=[g])
            S.act(lambda: nc.scalar.activation(out=g[:], in_=g[:], func=AF.Ln), r=[g], w=[g])
            S.dve(lambda h=h: nc.vector.tensor_scalar(out=g[:], in0=g[:], scalar1=al[:, h:h + 1], scalar2=-1.0, op0=ALU.mult,
                                                      op1=ALU.mult), r=[g, al], w=[g])
            ps = S.ps()
            mm(S, ps, ps[:, 0:64], UT, UT[:, :], g, g[:, :], True, True)
            S.act(lambda ps=ps: nc.scalar.copy(out=gc[:], in_=ps[:, 0:64]), r=[ps], w=[gc])
            S.dve(lambda: nc.vector.tensor_scalar(out=ngc[:], in0=gc[:], scalar1=-1.0, scalar2=None, op0=ALU.mult), r=[gc], w=[ngc])
            ps = S.ps()
            mm(S, ps, ps[:, 0:64], C.ones_f, C.ones_f[:, :], g, g[:, :], True, True)
            S.act(lambda ps=ps: nc.scalar.copy(out=glast[:], in_=ps[:, 0:64]), r=[ps], w=[glast])
            S.act(lambda: nc.scalar.activation(out=alast[:], in_=glast[:], func=AF.Exp), r=[glast], w=[alast])
            S.dve(lambda: nc.vector.tensor_tensor(out=etail[:], in0=glast[:], in1=gc[:], op=ALU.subtract), r=[glast, gc], w=[etail])
            S.act(lambda: nc.scalar.activation(out=etail[:], in_=etail[:], func=AF.Exp), r=[etail], w=[etail])
            S.act(lambda: nc.scalar.activation(out=bexpg[:], in_=gc[:], func=AF.Exp), r=[gc], w=[bexpg])
            S.dve(lambda: nc.vector.tensor_tensor(out=bexpg[:], in0=bexpg[:], in1=beta[:], op=ALU.mult), r=[bexpg, beta], w=[bexpg])
            S.dve(lambda: nc.vector.tensor_scalar(out=nbeta[:], in0=beta[:], scalar1=-1.0, scalar2=None, op0=ALU.mult),
                  r=[beta], w=[nbeta])
            S.dve(lambda: nc.vector.memset(St[:], 0.0), w=[St])
            for T in range(NT):
                s = h
                t0 = T * 128
                for ci, n in enumerate("qkv"):
                    rw = raw[n][s]
                    row0 = ci * 512 + h * 128
                    if T == 0:
                        S.dve(lambda rw=rw: nc.vector.memset(rw[:, 0:3], 0.0), w=[rw])
                        S.dma("sp", rw[:, 3:131], pT[row0:row0 + 128, 0:128], r=[pT], w=[rw])
                    else:
                        S.dma("sp", rw[:, :], pT[row0:row0 + 128, t0 - 3:t0 + 128], r=[pT], w=[rw])
                    c_ = cv[n][s]
                    ft = ci * 4 + h
                    S.dve(lambda rw=rw, c_=c_, ft=ft: nc.vector.tensor_scalar(out=c_[:], in0=rw[:, 0:128], scalar1=cw[:, ft, 0:1],
                                                                              scalar2=None, op0=ALU.mult), r=[rw, cw], w=[c_])
                    for k in range(1, 4):
                        S.dve(lambda rw=rw, c_=c_, ft=ft, k=k: nc.vector.scalar_tensor_tensor(
                            out=c_[:], in0=rw[:, k:k + 128], scalar=cw[:, ft, k:k + 1], in1=c_[:], op0=ALU.mult, op1=ALU.add),
                            r=[rw, cw, c_], w=[c_])
                    S.act(lambda c_=c_: nc.scalar.activation(out=c_[:], in_=c_[:], func=AF.Silu), r=[c_], w=[c_])
                for n, dst, mul in (("q", qT[s], 128 ** -0.5), ("k", kT[s], 1.0)):
                    c_ = cv[n][s]
                    S.pool(lambda c_=c_: nc.gpsimd.tensor_tensor(out=rn[s][:], in0=c_[:], in1=c_[:], op=ALU.mult), r=[c_], w=[rn[s]])
                    ps = S.ps()
                    mm(S, ps, ps[:, 0:128], C.ones_f, C.ones_f[:, :], rn[s], rn[s][:, :], True, True)
                    S.act(lambda ps=ps: nc.scalar.activation(out=rn[s][:], in_=ps[:, 0:128], func=AF.Sqrt, bias=C.eps[:, 0:1]),
                          r=[ps, C.eps], w=[rn[s]])
                    S.dve(lambda: nc.vector.reciprocal(out=rn[s][:], in_=rn[s][:]), r=[rn[s]], w=[rn[s]])
                    S.dve(lambda c_=c_, dst=dst, mul=mul: nc.vector.scalar_tensor_tensor(
                        out=dst[:], in0=c_[:], scalar=mul, in1=rn[s][:], op0=ALU.mult, op1=ALU.mult), r=[c_, rn[s]], w=[dst])
                ps = S.ps()
                S.pe(lambda ps=ps: nc.tensor.transpose(out=ps[:, 0:128], in_=kT[s][:, :], identity=C.ident[:, :]),
                     r=[kT[s], C.ident], w=[ps])
                evac(ktok[s], ps)
                ps = S.ps()
                S.pe(lambda ps=ps: nc.tensor.transpose(out=ps[:, 0:128], in_=cv["v"][s][:, :], identity=C.ident[:, :]),
                     r=[cv["v"][s], C.ident], w=[ps])
                evac(vtok[s], ps, "dve")
                S.dve(lambda: nc.vector.tensor_scalar(out=dgc[s][:], in0=C.ident[:], scalar1=gc[:, T:T + 1], scalar2=None,
                                                      op0=ALU.mult), r=[C.ident, gc], w=[dgc[s]])
                p1 = S.ps()
                mm(S, p1, p1[:, 0:128], C.ones_f, C.ones_f[:, :], dgc[s], dgc[s][:, :], True, False)
                mm(S, p1, p1[:, 0:128], C.ident, C.ident[:, :], PM1, PM1[:, :], False, True)
                S.act(lambda p1=p1: nc.scalar.activation(out=Dm[s][:], in_=p1[:, 0:128], func=AF.Exp, scale=-1.0,
                                                         bias=gc[:, T:T + 1]), r=[p1, gc], w=[Dm[s]])
                p2 = S.ps()
                mm(S, p2, p2[:, 0:128], C.ones_f, C.ones_f[:, :], dgc[s], dgc[s][:, :], True, False)
                mm(S, p2, p2[:, 0:128], C.ident, C.ident[:, :], NM2, NM2[:, :], False, True)
                S.act(lambda p2=p2: nc.scalar.activation(out=DiT[s][:], in_=p2[:, 0:128], func=AF.Exp, scale=1.0,
                                                         bias=ngc[:, T:T + 1]), r=[p2, ngc], w=[DiT[s]])
                p3 = S.ps()
                mm(S, p3, p3[:, 0:128], C.ones_f, C.ones_f[:, :], dgc[s], dgc[s][:, :], True, True)
                S.act(lambda p3=p3: nc.scalar.activation(out=egr[s][:], in_=p3[:, 0:128], func=AF.Exp), r=[p3], w=[egr[s]])
                pg = S.ps()
                mm(S, pg, pg[:, 0:128], kT[s], kT[s][:, :], kT[s], kT[s][:, :], True, True)
                S.dve(lambda pg=pg: nc.vector.scalar_tensor_tensor(out=Nm[s][:], in0=pg[:, 0:128], scalar=nbeta[:, T:T + 1],
                                                                   in1=Dm[s][:], op0=ALU.mult, op1=ALU.mult),
                      r=[pg, nbeta, Dm[s]], w=[Nm[s]])
                ps = S.ps()
                S.pe(lambda ps=ps: nc.tensor.transpose(out=ps[:, 0:128], in_=Nm[s][:, :], identity=C.ident[:, :]),
                     r=[Nm[s], C.ident], w=[ps])
                evac(NmT[s], ps)
                S.dve(lambda: nc.vector.tensor_tensor(out=RT[s][:], in0=NmT[s][:], in1=C.ident[:], op=ALU.add),
                      r=[NmT[s], C.ident], w=[RT[s]])
                Mc_, McT = Nm[s], NmT[s]
                Mn, MnT = M2[s], M2T[s]
                for lvl in range(6):
                    pa = S.ps()
                    mm(S, pa, pa[:, 0:128], McT, McT[:, :], Mc_, Mc_[:, :], True, True)
                    pb = S.ps()
                    mm(S, pb, pb[:, 0:128], Mc_, Mc_[:, :], McT, McT[:, :], True, True)
                    evac(Mn, pa, "act")
                    evac(MnT, pb, "dve")
                    pr = S.ps()
                    mm(S, pr, pr[:, 0:128], Mn, Mn[:, :], RT[s], RT[s][:, :], True, True)
                    S.dve(lambda pr=pr: nc.vector.tensor_tensor(out=RT[s][:], in0=RT[s][:], in1=pr[:, 0:128], op=ALU.add),
                          r=[RT[s], pr], w=[RT[s]])
                    Mc_, McT, Mn, MnT = Mn, MnT, Mc_, McT
                S.dve(lambda: nc.vector.tensor_scalar(out=vb[s][:], in0=vtok[s][:], scalar1=beta[:, T:T + 1], scalar2=None,
                                                      op0=ALU.mult), r=[vtok[s], beta], w=[vb[s]])
                S.dve(lambda: nc.vector.tensor_scalar(out=kbg[s][:], in0=ktok[s][:], scalar1=bexpg[:, T:T + 1], scalar2=None,
                                                      op0=ALU.mult), r=[ktok[s], bexpg], w=[kbg[s]])
                S.pool(lambda: nc.gpsimd.tensor_scalar(out=ktl[s][:], in0=ktok[s][:], scalar1=etail[:, T:T + 1], scalar2=None,
                                                       op0=ALU.mult), r=[ktok[s], etail], w=[ktl[s]])
                pu = S.ps()
                mm(S, pu, pu[:, 0:128], RT[s], RT[s][:, :], vb[s], vb[s][:, :], True, True)
                evac(u[s], pu, "act")
                pw = S.ps()
                mm(S, pw, pw[:, 0:128], kbg[s], kbg[s][:, :], RT[s], RT[s][:, :], True, True)
                evac(wT[s], pw, "act")
                pq = S.ps()
                mm(S, pq, pq[:, 0:128], kT[s], kT[s][:, :], qT[s], qT[s][:, :], True, True)
                S.dve(lambda pq=pq: nc.vector.tensor_tensor(out=qkT[s][:], in0=pq[:, 0:128], in1=DiT[s][:], op=ALU.mult),
                      r=[pq, DiT[s]], w=[qkT[s]])
                S.pool(lambda: nc.gpsimd.tensor_tensor(out=qgT[s][:], in0=qT[s][:], in1=egr[s][:], op=ALU.mult),
                       r=[qT[s], egr[s]], w=[qgT[s]])
                pv = S.ps()
                mm(S, pv, pv[:, 0:128], wT[s], wT[s][:, :], St, St[:, :], True, True)
                S.dve(lambda pv=pv: nc.vector.tensor_tensor(out=vnew[s][:], in0=u[s][:], in1=pv[:, 0:128], op=ALU.subtract),
                      r=[u[s], pv], w=[vnew[s]])
                po = S.ps()
                mm(S, po, po[:, 0:128], St, St[:, :], qgT[s], qgT[s][:, :], True, False)
                mm(S, po, po[:, 0:128], vnew[s], vnew[s][:, :], qkT[s], qkT[s][:, :], False, True)
                pS = S.ps()
                mm(S, pS, pS[:, 0:128], ktl[s], ktl[s][:, :], vnew[s], vnew[s][:, :], True, True)
                S.dve(lambda pS=pS: nc.vector.scalar_tensor_tensor(out=St[:], in0=St[:], scalar=alast[:, T:T + 1],
                                                                   in1=pS[:, 0:128], op0=ALU.mult, op1=ALU.add),
                      r=[St, alast, pS], w=[St])
                evac(osb[s], po, "act")
                S.pool(lambda: nc.gpsimd.tensor_tensor(out=osq[s][:], in0=osb[s][:], in1=osb[s][:], op=ALU.mult), r=[osb[s]], w=[osq[s]])
                pn = S.ps()
                mm(S, pn, pn[:, 0:128], C.ones_f, C.ones_f[:, :], osq[s], osq[s][:, :], True, True)
                S.act(lambda pn=pn: nc.scalar.activation(out=osq[s][:], in_=pn[:, 0:128], func=AF.Sqrt, scale=1.0 / 128,
                                                         bias=C.eps[:, 0:1]), r=[pn, C.eps], w=[osq[s]])
                S.dve(lambda: nc.vector.reciprocal(out=osq[s][:], in_=osq[s][:]), r=[osq[s]], w=[osq[s]])
                S.dma("sp", zt[s][:], pT[1536 + h * 128:1536 + (h + 1) * 128, t0:t0 + 128], r=[pT], w=[zt[s]])
                S.act(lambda: nc.scalar.activation(out=zt[s][:], in_=zt[s][:], func=AF.Silu), r=[zt[s]], w=[zt[s]])
                S.dve(lambda: nc.vector.scalar_tensor_tensor(out=osb[s][:], in0=osb[s][:], scalar=gn[:, 0:1], in1=osq[s][:],
                                                             op0=ALU.mult, op1=ALU.mult), r=[osb[s], gn, osq[s]], w=[osb[s]])
                S.dve(lambda: nc.vector.tensor_tensor(out=obf[s][:], in0=osb[s][:], in1=zt[s][:], op=ALU.mult),
                      r=[osb[s], zt[s]], w=[obf[s]])
                S.dma("sp", oT_dram[h * 128:(h + 1) * 128, t0:t0 + 128], obf[s][:], r=[obf[s]], w=[oT_dram])
        Interleaver(S).run([(lambda h=h: head_fn(h)) for h in range(4)])
        S.barrier()


def phase_nsa(S, C, pT, vs_tok, vw_tok, j, oT_dram):
    nc = S.nc
    R_Q, R_KC, R_VC, R_KS, R_KW, R_GL = 0, 1024, 1280, 1536, 2048, 2560
    posk_d = S.dram("od_cmp_pos_kT", [2, 64, 32], F32)
    posv_d = S.dram("od_cmp_pos_vT", [2, 64, 32], F32)
    k1_d = S.dram("od_cmp_k1", [2, 2048, 256], F32)
    k2_d = S.dram("od_cmp_k2", [2, 256, 64], F32)
    v1_d = S.dram("od_cmp_v1", [2, 2048, 256], F32)
    v2_d = S.dram("od_cmp_v2", [2, 256, 64], F32)
    SC = 0.125
    with ExitStack() as es:
        KC = [S.sb(es, [64, 512], BF16, "KC") for _ in range(4)]
        for g in range(4):
            S.pool(lambda g=g: nc.gpsimd.memset(KC[g][:], 0.0), w=[KC[g]])
        VC = [S.sb(es, [128, 4, 64], BF16, "VC") for _ in range(4)]
        with ExitStack() as e1:
            src = S.sb(e1, [64, 8192], BF16, "csrc")
            w1 = S.sb(e1, [64, 32, 256], BF16, "w1")
            w2 = S.sb(e1, [128, 2, 64], BF16, "w2")
            posT = S.sb(e1, [64, 32], BF16, "posT")
            c1 = S.sb(e1, [128, 2], F32, "c1")
            h1 = S.sb(e1, [128, 2, 512], BF16, "h1")
            for which, (pos_d, a_d, b_d, row0) in enumerate(((posk_d, k1_d, k2_d, R_KC), (posv_d, v1_d, v2_d, R_VC))):
                S.dma("pool", w1[:], a_d[j].rearrange("(l d) n -> d l n", d=64), r=[a_d], w=[w1])
                S.dma("pool", w2[:], b_d[j].rearrange("(c p) n -> p c n", p=128), r=[b_d], w=[w2])
                S.dma("pool", posT[:], pos_d[j], r=[pos_d], w=[posT])
                for ncx in range(2):
                    ps = S.ps()
                    for l in range(32):
                        mm(S, ps, ps[:, 0:1], w1, w1[:, l, ncx * 128:(ncx + 1) * 128], posT, posT[:, l:l + 1], l == 0, l == 31)
                    S.act(lambda ps=ps, ncx=ncx: nc.scalar.copy(out=c1[:, ncx:ncx + 1], in_=ps[:, 0:1]), r=[ps], w=[c1])
                for g in range(4):
                    S.dma("pool", src[:], pT[row0 + g * 64:row0 + (g + 1) * 64, :], r=[pT], w=[src])
                    for ncx in range(2):
                        ps = S.ps()
                        for l in range(32):
                            mm(S, ps, ps[:, 0:511], w1, w1[:, l, ncx * 128:(ncx + 1) * 128], src,
                               src[:, l:l + 16 * 510 + 1:16], l == 0, l == 31)
                        S.act(lambda ps=ps, ncx=ncx: nc.scalar.activation(out=h1[:, ncx, 0:511], in_=ps[:, 0:511], func=AF.Silu,
                                                                          bias=c1[:, ncx:ncx + 1]), r=[ps, c1], w=[h1])
                    if which == 0:
                        ps = S.ps()
                        for ncx in range(2):
                            mm(S, ps, ps[0:64, 0:511], w2, w2[:, ncx, :], h1, h1[:, ncx, 0:511], ncx == 0, ncx == 1)
                        S.act(lambda ps=ps, g=g: nc.scalar.copy(out=KC[g][:, 0:511], in_=ps[0:64, 0:511]), r=[ps], w=[KC[g]])
                    else:
                        for nt in range(4):
                            rows = 128 if nt < 3 else 127
                            ps = S.ps()
                            for ncx in range(2):
                                mm(S, ps, ps[0:rows, 0:64], h1, h1[:, ncx, nt * 128:nt * 128 + rows], w2, w2[:, ncx, :],
                                   ncx == 0, ncx == 1)
                            S.act(lambda ps=ps, g=g, nt=nt, rows=rows: nc.scalar.copy(out=VC[g][0:rows, nt, :], in_=ps[0:rows, 0:64]),
                                  r=[ps], w=[VC[g]])
        S.barrier()
        negselT = S.sb(es, [128, 8192], BF16, "negselT")
        Wm = S.sb(es, [128, 16], F32, "Wm")
        Wm0 = S.sb(es, [128, 16], F32, "Wm0")
        for (t_, b_) in ((Wm, 97), (Wm0, -31)):
            S.pool(lambda t_=t_: nc.gpsimd.memset(t_[:], 0.0), w=[t_])
            S.pool(lambda t_=t_, b_=b_: nc.gpsimd.affine_select(out=t_[:], in_=t_[:], pattern=[[-16, 16]], compare_op=ALU.is_ge,
                                                                fill=NEG, base=b_, channel_multiplier=1), r=[t_], w=[t_])
        Ebig = S.sb(es, [128, 8192], BF16, "Ebig")
        Sel48 = S.sb(es, [48, 48, 64], F32, "Sel48")
        S.pool(lambda: nc.gpsimd.memset(Sel48[:], 0.0), w=[Sel48])
        S.pool(lambda: nc.gpsimd.affine_select(out=Sel48[:], in_=Sel48[:], pattern=[[-1, 48], [0, 64]],
                                               compare_op=ALU.not_equal, fill=1.0, base=0, channel_multiplier=1),
               r=[Sel48], w=[Sel48])
        S.pool(lambda: nc.gpsimd.memset(Ebig[:], 0.0), w=[Ebig])
        ebv = Ebig[:].rearrange("p (b x) -> p b x", x=64)
        S.pool(lambda: nc.gpsimd.affine_select(out=ebv, in_=ebv, pattern=[[-1, 128], [0, 64]], compare_op=ALU.not_equal,
                                               fill=1.0, base=0, channel_multiplier=1), r=[Ebig], w=[Ebig])
        for g in range(4):
            with ExitStack() as e2:
                QT4 = [S.sb(e2, [64, 8192], BF16, "QT4") for _ in range(4)]
                for hg in range(4):
                    r0 = R_Q + (g * 4 + hg) * 64
                    S.dma("pool", QT4[hg][:], pT[r0:r0 + 64, :], r=[pT], w=[QT4[hg]])
                scs = [S.sb(e2, [128, 512], F32, "sc") for _ in range(2)]
                pp = S.sb(e2, [128, 516], F32, "pp")
                rs = S.sb(e2, [128, 2], F32, "rs")
                imp = S.sb(e2, [128, 128], F32, "imp")
                imp2 = S.sb(e2, [128, 128], F32, "imp2")
                mx = S.sb(e2, [128, 8], F32, "mx")
                S.dve(lambda: nc.vector.memset(pp[:], 0.0), w=[pp])
                for T in range(NT):
                    for hg in range(4):
                        ps = S.ps()
                        mm(S, ps, ps[:, 0:512], QT4[hg], QT4[hg][:, T * 128:(T + 1) * 128], KC[g], KC[g][:, 0:512], True, True)
                        sc = scs[hg % 2]
                        S.act(lambda ps=ps, sc=sc: nc.scalar.activation(out=sc[:], in_=ps[:, 0:512], func=AF.Copy, scale=SC),
                              r=[ps], w=[sc])
                        w0 = max(8 * T - 8, 0)
                        w1_ = min(8 * T + 8, 512)
                        wm_ap = Wm0[:, 0:8] if T == 0 else Wm[:, 0:w1_ - w0]
                        S.pool(lambda sc=sc, w0=w0, w1_=w1_, wm_ap=wm_ap: nc.gpsimd.tensor_tensor(
                            out=sc[:, w0:w1_], in0=sc[:, w0:w1_], in1=wm_ap, op=ALU.add), r=[sc, Wm, Wm0], w=[sc])
                        if w1_ < 512:
                            S.pool(lambda sc=sc, w1_=w1_: nc.gpsimd.memset(sc[:, w1_:512], NEG), r=[sc], w=[sc])
                        S.act(lambda sc=sc: nc.scalar.activation(out=sc[:], in_=sc[:], func=AF.Exp, accum_out=rs[:, 0:1]),
                              r=[sc], w=[sc, rs])
                        S.dve(lambda: nc.vector.tensor_scalar(out=rs[:, 1:2], in0=rs[:, 0:1], scalar1=C.tiny[:, 0:1], scalar2=None,
                                                              op0=ALU.max), r=[rs, C.tiny], w=[rs])
                        S.dve(lambda: nc.vector.reciprocal(out=rs[:, 1:2], in_=rs[:, 1:2]), r=[rs], w=[rs])
                        if hg == 0:
                            S.dve(lambda sc=sc: nc.vector.tensor_scalar(out=pp[:, 1:513], in0=sc[:], scalar1=rs[:, 1:2], scalar2=None,
                                                                        op0=ALU.mult), r=[sc, rs], w=[pp])
                        else:
                            S.dve(lambda sc=sc: nc.vector.scalar_tensor_tensor(out=pp[:, 1:513], in0=sc[:], scalar=rs[:, 1:2],
                                                                               in1=pp[:, 1:513], op0=ALU.mult, op1=ALU.add),
                                  r=[sc, rs, pp], w=[pp])
                    a = pp[:, 0:512].rearrange("p (j f) -> p j f", f=4)
                    e_ = pp[:, 4:516].rearrange("p (j f) -> p j f", f=4)
                    S.dve(lambda a=a: nc.vector.tensor_scalar(out=imp[:], in0=a[:, :, 0], scalar1=0.5, scalar2=None, op0=ALU.mult),
                          r=[pp], w=[imp])
                    for f in (1, 2, 3):
                        S.dve(lambda a=a, f=f: nc.vector.tensor_tensor(out=imp[:], in0=imp[:], in1=a[:, :, f], op=ALU.add),
                              r=[pp, imp], w=[imp])
                    S.dve(lambda e_=e_: nc.vector.scalar_tensor_tensor(out=imp[:], in0=e_[:, :, 0], scalar=0.5, in1=imp[:],
                                                                       op0=ALU.mult, op1=ALU.add), r=[pp, imp], w=[imp])
                    for half in range(2):
                        cur = 2 * T + half
                        hs = slice(half * 64, half * 64 + 64)
                        if cur + 1 < 128:
                            S.pool(lambda hs=hs, cur=cur: nc.gpsimd.memset(imp[hs, cur + 1:128], -1.0), r=[imp], w=[imp])
                        lo = max(cur - 1, 0)
                        S.pool(lambda hs=hs, lo=lo, cur=cur: nc.gpsimd.memset(imp[hs, lo:cur + 1], 1e9), r=[imp], w=[imp])
                    S.pool(lambda: nc.gpsimd.memset(imp[:, 0:1], 1e9), r=[imp], w=[imp])
                    S.dve(lambda: nc.vector.max(out=mx[:], in_=imp[:]), r=[imp], w=[mx])
                    S.dve(lambda: nc.vector.match_replace(out=imp2[:], in_to_replace=mx[:], in_values=imp[:], imm_value=-2.0),
                          r=[mx, imp], w=[imp2])
                    S.dve(lambda: nc.vector.max(out=mx[:], in_=imp2[:]), r=[imp2], w=[mx])
                    S.dve(lambda: nc.vector.tensor_scalar(out=imp2[:], in0=imp[:], scalar1=mx[:, 7:8], scalar2=None, op0=ALU.is_ge),
                          r=[imp, mx], w=[imp2])
                    S.dve(lambda: nc.vector.tensor_scalar(out=imp2[:], in0=imp2[:], scalar1=-1.0, scalar2=-NEG, op0=ALU.add,
                                                          op1=ALU.mult), r=[imp2], w=[imp2])
                    ps = S.ps()
                    S.pe(lambda ps=ps: nc.tensor.transpose(out=ps[:, 0:128], in_=imp2[:, :], identity=C.ident[:, :]),
                         r=[imp2, C.ident], w=[ps])
                    S.act(lambda ps=ps, T=T: nc.scalar.copy(out=negselT[:, T * 128:(T + 1) * 128], in_=ps[:, 0:128]),
                          r=[ps], w=[negselT])
            S.barrier()
            with ExitStack() as e3:
                ksT = S.sb(e3, [64, 8192], BF16, "ksT")
                kwT = S.sb(e3, [64, 8192], BF16, "kwT")
                VS = S.sb(e3, [128, 64, 64], BF16, "VS")
                VW = S.sb(e3, [128, 64, 64], BF16, "VW")
                QT = S.sb(e3, [64, 8192], BF16, "QT")
                gates = S.sb(e3, [48, 8192], F32, "gates")
                pts = [S.sb(e3, [128, 512], BF16, "pt") for _ in range(3)]
                ots = [S.sb(e3, [64, 512], F32, "ot") for _ in range(2)]
                rden = S.sb(e3, [64, 512], F32, "rden")
                acc = S.sb(e3, [64, 512], F32, "acc")
                tmpo = S.sb(e3, [64, 512], F32, "tmpo")
                obf = [S.sb(e3, [64, 512], BF16, "obf") for _ in range(2)]
                S.dma("pool", ksT[:], pT[R_KS + g * 64:R_KS + (g + 1) * 64, :], r=[pT], w=[ksT])
                S.dma("pool", kwT[:], pT[R_KW + g * 64:R_KW + (g + 1) * 64, :], r=[pT], w=[kwT])
                for q4 in range(4):
                    tsl = slice(q4 * 16, (q4 + 1) * 16)
                    rsl = slice(q4 * 2048, (q4 + 1) * 2048)
                    S.dma("pool", VS[:, tsl, :], vs_tok[rsl, g * 64:(g + 1) * 64].rearrange("(t p) d -> p t d", p=128),
                          r=[vs_tok], w=[VS])
                    S.dma("pool", VW[:, tsl, :], vw_tok[rsl, g * 64:(g + 1) * 64].rearrange("(t p) d -> p t d", p=128),
                          r=[vw_tok], w=[VW])
                S.dma("sp", gates[:], pT[R_GL:R_GL + 48, :], r=[pT], w=[gates])
                S.act(lambda: nc.scalar.activation(out=gates[:], in_=gates[:], func=AF.Sigmoid), r=[gates], w=[gates])
                pi = 0
                bank = 0
                for hg in range(4):
                    head = g * 4 + hg
                    S.dma("pool", QT[:], pT[R_Q + head * 64:R_Q + (head + 1) * 64, :], r=[pT], w=[QT])
                    for c in range(16):
                        qs = slice(c * 512, (c + 1) * 512)
                        for br in range(3):
                            if br == 0:
                                kts = []
                                for nt in range(4):
                                    D = c - 4 * nt
                                    if D < 0:
                                        continue
                                    nk = 128 if nt < 3 else 127
                                    masks = [(C.ident_bf, C.ident_bf[:, 0:nk], C.Mk[D], C.Mk[D][:, :])] if D <= 4 else []
                                    kts.append((nt, nk, masks))
                                kp, V_ = [(KC[g], 64)], VC[g]
                            elif br == 1:
                                kts = []
                                for kt in range(4 * c + 4):
                                    masks = [(Ebig, Ebig[:, kt * 128:(kt + 1) * 128], negselT, negselT[:, qs])]
                                    if kt >= 4 * c:
                                        m_ = C.Mc[kt - 4 * c]
                                        masks.append((C.ident_bf, C.ident_bf[:, :], m_, m_[:, :]))
                                    kts.append((kt, 128, masks))
                                kp, V_ = [(ksT, 64)], VS
                            else:
                                kts = []
                                for jj in range(-4, 4):
                                    kt = 4 * c + jj
                                    if kt < 0:
                                        continue
                                    m_ = C.Mw[-jj] if jj < 0 else C.Mc[jj]
                                    kts.append((kt, 128, [(C.ident_bf, C.ident_bf[:, :], m_, m_[:, :])]))
                                kp, V_ = [(kwT, 64)], VW
                            ot = ots[bank % 2]
                            pi = attn_chunk(S, C, c, bank, kp, [(QT, 64)], V_, 64, kts, SC, pts, ot, rden, pi)
                            bank += 1
                            pg = S.ps4()
                            mm(S, pg, pg[0:64, :], Sel48, Sel48[:, head * 3 + br, :], gates, gates[:, qs], True, True)
                            if br == 0:
                                S.dve(lambda ot=ot, pg=pg: nc.vector.tensor_tensor(out=acc[:], in0=ot[:], in1=pg[0:64, :], op=ALU.mult),
                                      r=[ot, pg], w=[acc])
                            else:
                                S.dve(lambda ot=ot, pg=pg: nc.vector.tensor_tensor(out=tmpo[:], in0=ot[:], in1=pg[0:64, :], op=ALU.mult),
                                      r=[ot, pg], w=[tmpo])
                                S.pool(lambda: nc.gpsimd.tensor_tensor(out=acc[:], in0=acc[:], in1=tmpo[:], op=ALU.add),
                                       r=[acc, tmpo], w=[acc])
                        ob = obf[c % 2]
                        S.act(lambda ob=ob: nc.scalar.copy(out=ob[:], in_=acc[:]), r=[acc], w=[ob])
                        S.dma("sp", oT_dram[head * 64:(head + 1) * 64, qs], ob[:], r=[ob], w=[oT_dram])
            S.barrier()
        S.barrier()


def phase_nsa2(S, C, pT, vs_tok, vw_tok, j, oT_dram):
    nc = S.nc
    R_Q, R_KC, R_VC, R_KS, R_KW, R_GL = 0, 1024, 1280, 1536, 2048, 2560
    posk_d = S.dram("od_cmp_pos_kT", [2, 64, 32], F32)
    posv_d = S.dram("od_cmp_pos_vT", [2, 64, 32], F32)
    k1_d = S.dram("od_cmp_k1", [2, 2048, 256], F32)
    k2_d = S.dram("od_cmp_k2", [2, 256, 64], F32)
    v1_d = S.dram("od_cmp_v1", [2, 2048, 256], F32)
    v2_d = S.dram("od_cmp_v2", [2, 256, 64], F32)
    SC = 0.125
    with ExitStack() as es:
        KC = [S.sb(es, [64, 512], BF16, "KC") for _ in range(4)]
        for g in range(4):
            S.pool(lambda g=g: nc.gpsimd.memset(KC[g][:], 0.0), w=[KC[g]])
        VC = [S.sb(es, [128, 4, 64], BF16, "VC") for _ in range(4)]
        with ExitStack() as e1:
            src = S.sb(e1, [64, 8192], BF16, "csrc")
            w1 = S.sb(e1, [64, 32, 256], BF16, "w1")
            w2 = S.sb(e1, [128, 2, 64], BF16, "w2")
            posT = S.sb(e1, [64, 32], BF16, "posT")
            c1 = S.sb(e1, [128, 2], F32, "c1")
            h1 = S.sb(e1, [128, 2, 512], BF16, "h1")
            for which, (pos_d, a_d, b_d, row0) in enumerate(((posk_d, k1_d, k2_d, R_KC), (posv_d, v1_d, v2_d, R_VC))):
                S.dma("pool", w1[:], a_d[j].rearrange("(l d) n -> d l n", d=64), r=[a_d], w=[w1])
                S.dma("pool", w2[:], b_d[j].rearrange("(c p) n -> p c n", p=128), r=[b_d], w=[w2])
                S.dma("pool", posT[:], pos_d[j], r=[pos_d], w=[posT])
                for ncx in range(2):
                    ps = S.ps()
                    for l in range(32):
                        mm(S, ps, ps[:, 0:1], w1, w1[:, l, ncx * 128:(ncx + 1) * 128], posT, posT[:, l:l + 1], l == 0, l == 31)
                    S.act(lambda ps=ps, ncx=ncx: nc.scalar.copy(out=c1[:, ncx:ncx + 1], in_=ps[:, 0:1]), r=[ps], w=[c1])
                for g in range(4):
                    S.dma("pool", src[:], pT[row0 + g * 64:row0 + (g + 1) * 64, :], r=[pT], w=[src])
                    for ncx in range(2):
                        ps = S.ps()
                        for l in range(32):
                            mm(S, ps, ps[:, 0:511], w1, w1[:, l, ncx * 128:(ncx + 1) * 128], src,
                               src[:, l:l + 16 * 510 + 1:16], l == 0, l == 31)
                        S.act(lambda ps=ps, ncx=ncx: nc.scalar.activation(out=h1[:, ncx, 0:511], in_=ps[:, 0:511], func=AF.Silu,
                                                                          bias=c1[:, ncx:ncx + 1]), r=[ps, c1], w=[h1])
                    if which == 0:
                        ps = S.ps()
                        for ncx in range(2):
                            mm(S, ps, ps[0:64, 0:511], w2, w2[:, ncx, :], h1, h1[:, ncx, 0:511], ncx == 0, ncx == 1)
                        S.act(lambda ps=ps, g=g: nc.scalar.copy(out=KC[g][:, 0:511], in_=ps[0:64, 0:511]), r=[ps], w=[KC[g]])
                    else:
                        for nt in range(4):
                            rows = 128 if nt < 3 else 127
                            ps = S.ps()
                            for ncx in range(2):
                                mm(S, ps, ps[0:rows, 0:64], h1, h1[:, ncx, nt * 128:nt * 128 + rows], w2, w2[:, ncx, :],
                                   ncx == 0, ncx == 1)
                            S.act(lambda ps=ps, g=g, nt=nt, rows=rows: nc.scalar.copy(out=VC[g][0:rows, nt, :], in_=ps[0:rows, 0:64]),
                                  r=[ps], w=[VC[g]])
        S.barrier()
        sel01T = S.sb(es, [128, 8192], BF16, "sel01T")
        Wm = S.sb(es, [128, 16], F32, "Wm")
        Wm0 = S.sb(es, [128, 16], F32, "Wm0")
        for (t_, b_) in ((Wm, 97), (Wm0, -31)):
            S.pool(lambda t_=t_: nc.gpsimd.memset(t_[:], 0.0), w=[t_])
            S.pool(lambda t_=t_, b_=b_: nc.gpsimd.affine_select(out=t_[:], in_=t_[:], pattern=[[-16, 16]], compare_op=ALU.is_ge,
                                                                fill=NEG, base=b_, channel_multiplier=1), r=[t_], w=[t_])
        Ebig = S.sb(es, [128, 8192], BF16, "Ebig")
        def m01(name, base, cm, step):
            t = S.sb(es, [128, 512], BF16, name)
            S.pool(lambda: nc.gpsimd.memset(t[:], 1.0), w=[t])
            S.pool(lambda: nc.gpsimd.affine_select(out=t[:], in_=t[:], pattern=[[step, 512]], compare_op=ALU.is_ge, fill=0.0,
                                                   base=base, channel_multiplier=cm), r=[t], w=[t])
            return t
        Mc01 = [m01(f"Mc01{j_}", -128 * j_, -1, 1) for j_ in range(4)]
        Mw01 = [None] + [m01(f"Mw01{m_}", 511 - 128 * m_, 1, -1) for m_ in range(1, 5)]
        Mk01 = [m01(f"Mk01{d_}", 512 * d_ - 31, -16, 1) for d_ in range(5)]
        GS = S.dram("GS", [48, 8192], F32)
        with ExitStack() as eg:
            gt = S.sb(eg, [48, 8192], F32, "gt")
            S.dma("sp", gt[:], pT[R_GL:R_GL + 48, :], r=[pT], w=[gt])
            S.act(lambda: nc.scalar.activation(out=gt[:], in_=gt[:], func=AF.Sigmoid), r=[gt], w=[gt])
            S.dma("sp", GS[:, :], gt[:], r=[gt], w=[GS])
            S.barrier()
        S.pool(lambda: nc.gpsimd.memset(Ebig[:], 0.0), w=[Ebig])
        ebv = Ebig[:].rearrange("p (b x) -> p b x", x=64)
        S.pool(lambda: nc.gpsimd.affine_select(out=ebv, in_=ebv, pattern=[[-1, 128], [0, 64]], compare_op=ALU.not_equal,
                                               fill=1.0, base=0, channel_multiplier=1), r=[Ebig], w=[Ebig])
        for g in range(4):
            with ExitStack() as e2:
                QT4 = [S.sb(e2, [64, 8192], BF16, "QT4") for _ in range(4)]
                for hg in range(4):
                    r0 = R_Q + (g * 4 + hg) * 64
                    S.dma("pool", QT4[hg][:], pT[r0:r0 + 64, :], r=[pT], w=[QT4[hg]])
                NTI = 4
                BS = []
                for _ in range(NTI):
                    BS.append(dict(scs=[S.sb(e2, [128, 512], F32, "sc") for _ in range(2)],
                                   pp=S.sb(e2, [128, 516], F32, "pp"), rs=S.sb(e2, [128, 2], F32, "rs"),
                                   imp=S.sb(e2, [128, 128], F32, "imp"), imp2=S.sb(e2, [128, 128], F32, "imp2"),
                                   mx=S.sb(e2, [128, 8], F32, "mx")))
                    S.dve(lambda: nc.vector.memset(BS[-1]["pp"][:], 0.0), w=[BS[-1]["pp"]])

                def sel_gen(T, B):
                    scs, pp, rs, imp, imp2, mx = B["scs"], B["pp"], B["rs"], B["imp"], B["imp2"], B["mx"]
                    for hg in range(4):
                        ps = S.ps()
                        mm(S, ps, ps[:, 0:512], QT4[hg], QT4[hg][:, T * 128:(T + 1) * 128], KC[g], KC[g][:, 0:512], True, True)
                        sc = scs[hg % 2]
                        S.act(lambda: nc.scalar.activation(out=sc[:], in_=ps[:, 0:512], func=AF.Copy, scale=SC), r=[ps], w=[sc])
                        yield
                        w0 = max(8 * T - 8, 0)
                        w1_ = min(8 * T + 8, 512)
                        wm_ap = Wm0[:, 0:8] if T == 0 else Wm[:, 0:w1_ - w0]
                        S.pool(lambda: nc.gpsimd.tensor_tensor(out=sc[:, w0:w1_], in0=sc[:, w0:w1_], in1=wm_ap, op=ALU.add),
                               r=[sc, Wm, Wm0], w=[sc])
                        if w1_ < 512:
                            S.pool(lambda: nc.gpsimd.memset(sc[:, w1_:512], NEG), r=[sc], w=[sc])
                        yield
                        S.act(lambda: nc.scalar.activation(out=sc[:], in_=sc[:], func=AF.Exp, accum_out=rs[:, 0:1]),
                              r=[sc], w=[sc, rs])
                        yield
                        S.dve(lambda: nc.vector.tensor_scalar(out=rs[:, 1:2], in0=rs[:, 0:1], scalar1=C.tiny[:, 0:1], scalar2=None,
                                                              op0=ALU.max), r=[rs, C.tiny], w=[rs])
                        S.dve(lambda: nc.vector.reciprocal(out=rs[:, 1:2], in_=rs[:, 1:2]), r=[rs], w=[rs])
                        if hg == 0:
                            S.dve(lambda: nc.vector.tensor_scalar(out=pp[:, 1:513], in0=sc[:], scalar1=rs[:, 1:2], scalar2=None,
                                                                  op0=ALU.mult), r=[sc, rs], w=[pp])
                        else:
                            S.dve(lambda: nc.vector.scalar_tensor_tensor(out=pp[:, 1:513], in0=sc[:], scalar=rs[:, 1:2],
                                                                         in1=pp[:, 1:513], op0=ALU.mult, op1=ALU.add),
                                  r=[sc, rs, pp], w=[pp])
                        yield
                    a_ = pp[:, 0:512].rearrange("p (j f) -> p j f", f=4)
                    e_ = pp[:, 4:516].rearrange("p (j f) -> p j f", f=4)
                    S.dve(lambda: nc.vector.tensor_scalar(out=imp[:], in0=a_[:, :, 0], scalar1=0.5, scalar2=None, op0=ALU.mult),
                          r=[pp], w=[imp])
                    for f in (1, 2, 3):
                        S.dve(lambda f=f: nc.vector.tensor_tensor(out=imp[:], in0=imp[:], in1=a_[:, :, f], op=ALU.add),
                              r=[pp, imp], w=[imp])
                    S.dve(lambda: nc.vector.scalar_tensor_tensor(out=imp[:], in0=e_[:, :, 0], scalar=0.5, in1=imp[:],
                                                                 op0=ALU.mult, op1=ALU.add), r=[pp, imp], w=[imp])
                    yield
                    for half in range(2):
                        cur = 2 * T + half
                        hs = slice(half * 64, half * 64 + 64)
                        if cur + 1 < 128:
                            S.pool(lambda hs=hs, cur=cur: nc.gpsimd.memset(imp[hs, cur + 1:128], -1.0), r=[imp], w=[imp])
                        S.pool(lambda hs=hs, cur=cur: nc.gpsimd.memset(imp[hs, cur:cur + 1], 2e9), r=[imp], w=[imp])
                        if cur >= 1:
                            S.pool(lambda hs=hs, cur=cur: nc.gpsimd.memset(imp[hs, cur - 1:cur], 1e9), r=[imp], w=[imp])
                    S.pool(lambda: nc.gpsimd.memset(imp[:, 0:1], 3e9), r=[imp], w=[imp])
                    yield
                    S.dve(lambda: nc.vector.max(out=mx[:], in_=imp[:]), r=[imp], w=[mx])
                    S.dve(lambda: nc.vector.match_replace(out=imp2[:], in_to_replace=mx[:], in_values=imp[:], imm_value=-2.0),
                          r=[mx, imp], w=[imp2])
                    S.dve(lambda: nc.vector.max(out=mx[:], in_=imp2[:]), r=[imp2], w=[mx])
                    S.dve(lambda: nc.vector.tensor_scalar(out=imp2[:], in0=imp[:], scalar1=mx[:, 7:8], scalar2=None, op0=ALU.is_ge),
                          r=[imp, mx], w=[imp2])
                    yield
                    ps = S.ps()
                    S.pe(lambda: nc.tensor.transpose(out=ps[:, 0:128], in_=imp2[:, :], identity=C.ident[:, :]),
                         r=[imp2, C.ident], w=[ps])
                    S.act(lambda: nc.scalar.copy(out=sel01T[:, T * 128:(T + 1) * 128], in_=ps[:, 0:128]), r=[ps], w=[sel01T])
                    yield

                for T0 in range(0, NT, NTI):
                    gens = [sel_gen(T0 + i_, BS[i_]) for i_ in range(NTI)]
                    alive = list(gens)
                    while alive:
                        nxt = []
                        for g_ in alive:
                            try:
                                next(g_)
                                nxt.append(g_)
                            except StopIteration:
                                pass
                        alive = nxt
            S.barrier()
            with ExitStack() as e3:
                ksT2 = S.sb(e3, [128, 8192], BF16, "ksT2")
                kwT2 = S.sb(e3, [128, 8192], BF16, "kwT2")
                KC2 = S.sb(e3, [128, 512], BF16, "KC2")
                VSa = S.sb(e3, [128, 64, 65], BF16, "VSa")
                VWa = S.sb(e3, [128, 64, 65], BF16, "VWa")
                VCa = S.sb(e3, [128, 4, 65], BF16, "VCa")
                QTp = S.sb(e3, [128, 2, 8192], BF16, "QTp")
                Sel12 = S.sb(e3, [48, 12, 64], F32, "Sel12")
                NPT = 6
                pts = [S.sb(e3, [128, 512], BF16, "pt") for _ in range(NPT)]
                mks = [S.sb(e3, [128, 512], BF16, "mk") for _ in range(3)]
                gch = [S.sb(e3, [48, 512], F32, "gch") for _ in range(2)]
                acc = [S.sb(e3, [64, 512], F32, "acc") for _ in range(4)]
                T1 = [S.sb(e3, [64, 512], F32, "t1") for _ in range(4)]
                T2 = [S.sb(e3, [65, 512], F32, "t2") for _ in range(4)]
                obf = [S.sb(e3, [64, 512], BF16, "obf") for _ in range(2)]
                S.pool(lambda: nc.gpsimd.memset(Sel12[:], 0.0), w=[Sel12])
                S.pool(lambda g=g: nc.gpsimd.affine_select(out=Sel12[:], in_=Sel12[:], pattern=[[-1, 12], [0, 64]],
                                                           compare_op=ALU.not_equal, fill=1.0, base=-12 * g, channel_multiplier=1),
                       r=[Sel12], w=[Sel12])
                for hf in range(2):
                    ps_ = slice(hf * 64, hf * 64 + 64)
                    S.dma("pool", ksT2[ps_, :], pT[R_KS + g * 64:R_KS + (g + 1) * 64, :], r=[pT], w=[ksT2])
                    S.dma("pool", kwT2[ps_, :], pT[R_KW + g * 64:R_KW + (g + 1) * 64, :], r=[pT], w=[kwT2])
                    S.dma("sp", KC2[ps_, :], KC[g][:, :], r=[KC[g]], w=[KC2])
                for hg in range(4):
                    r0 = R_Q + (g * 4 + hg) * 64
                    S.dma("pool", QTp[(hg % 2) * 64:(hg % 2) * 64 + 64, hg // 2, :], pT[r0:r0 + 64, :], r=[pT], w=[QTp])
                for va in (VSa, VWa, VCa):
                    S.pool(lambda va=va: nc.gpsimd.memset(va[:], 1.0), w=[va])
                S.pool(lambda: nc.gpsimd.tensor_copy(out=VCa[:, :, 0:64], in_=VC[g][:, :, :]), r=[VC[g], VCa], w=[VCa])
                for q4 in range(4):
                    tsl = slice(q4 * 16, (q4 + 1) * 16)
                    rsl = slice(q4 * 2048, (q4 + 1) * 2048)
                    S.dma("pool", VSa[:, tsl, 0:64], vs_tok[rsl, g * 64:(g + 1) * 64].rearrange("(t p) d -> p t d", p=128),
                          r=[vs_tok], w=[VSa])
                    S.dma("pool", VWa[:, tsl, 0:64], vw_tok[rsl, g * 64:(g + 1) * 64].rearrange("(t p) d -> p t d", p=128),
                          r=[vw_tok], w=[VWa])
                cnt = {"pt": 0, "mk": 0, "ps": 0, "mul": 0}

                def ps3():
                    b_ = S.psb[cnt["ps"] % 4]
                    cnt["ps"] += 1
                    return b_

                for c in range(16):
                    qs = slice(c * 512, (c + 1) * 512)
                    gc_ = gch[c % 2]
                    S.dma("sp", gc_[:], GS[:, qs], r=[GS], w=[gc_])
                    for br in range(3):
                        items = []
                        if br == 0:
                            for nt in range(4):
                                D = c - 4 * nt
                                if D < 0:
                                    continue
                                items.append((nt, 128 if nt < 3 else 127, "const", Mk01[D] if D <= 4 else None))
                            kk, Va = KC2, VCa
                        elif br == 1:
                            for kt in range(4 * c + 4):
                                items.append((kt, 128, "sel", Mc01[kt - 4 * c] if kt >= 4 * c else None))
                            kk, Va = ksT2, VSa
                        else:
                            for jj in range(-4, 4):
                                kt = 4 * c + jj
                                if kt < 0:
                                    continue
                                items.append((kt, 128, "const", Mw01[-jj] if jj < 0 else Mc01[jj]))
                            kk, Va = kwT2, VWa
                        work = [(ii, hg) for ii in range(len(items)) for hg in range(4)]
                        state = {}

                        def stage1(w_):
                            ii, hg = w_
                            kt, nk, kind, arg = items[ii]
                            if kind == "sel" and hg == 0:
                                pm = ps3()
                                mm(S, pm, pm[0:nk, :], Ebig, Ebig[:, kt * 128:kt * 128 + nk], sel01T, sel01T[:, qs], True, True)
                                mk = mks[cnt["mk"] % 3]
                                cnt["mk"] += 1
                                if arg is None:
                                    S.act(lambda: nc.scalar.copy(out=mk[0:nk, :], in_=pm[0:nk, :]), r=[pm], w=[mk])
                                else:
                                    S.dve(lambda: nc.vector.tensor_tensor(out=mk[0:nk, :], in0=pm[0:nk, :], in1=arg[0:nk, :],
                                                                          op=ALU.mult), r=[pm, arg], w=[mk])
                                state[("mk", ii)] = mk
                            mask = state[("mk", ii)] if kind == "sel" else arg
                            pb_ = (hg % 2) * 64
                            pst = ps3()
                            mm(S, pst, pst[0:nk, :], kk, kk[pb_:pb_ + 64, kt * 128:kt * 128 + nk], QTp, QTp[pb_:pb_ + 64, hg // 2, qs],
                               True, True)
                            pt = pts[cnt["pt"] % NPT]
                            cnt["pt"] += 1
                            S.act(lambda: nc.scalar.activation(out=pt[0:nk, :], in_=pst[0:nk, :], func=AF.Exp, scale=SC),
                                  r=[pst], w=[pt])
                            if mask is not None:
                                cnt["mul"] += 1
                                if cnt["mul"] % 2 == 0:
                                    S.dve(lambda: nc.vector.tensor_tensor(out=pt[0:nk, :], in0=pt[0:nk, :], in1=mask[0:nk, :],
                                                                          op=ALU.mult), r=[pt, mask], w=[pt])
                                else:
                                    S.pool(lambda: nc.gpsimd.tensor_tensor(out=pt[0:nk, :], in0=pt[0:nk, :], in1=mask[0:nk, :],
                                                                           op=ALU.mult), r=[pt, mask], w=[pt])
                            state[("pt", ii, hg)] = pt

                        def stage2(w_):
                            ii, hg = w_
                            kt, nk, kind, arg = items[ii]
                            pt = state.pop(("pt", ii, hg))
                            po = S.psb[4 + hg]
                            mm(S, po, po[0:65, :], Va, Va[0:nk, kt, :], pt, pt[0:nk, :], ii == 0, ii == len(items) - 1)

                        DEPTH = 3
                        for i_ in range(len(work) + DEPTH):
                            if i_ < len(work):
                                stage1(work[i_])
                            if i_ >= DEPTH:
                                stage2(work[i_ - DEPTH])
                        def norm_fn(hg, br=br):
                            po = S.psb[4 + hg]
                            pb_ = S.psb[hg]
                            t1, t2 = T1[hg], T2[hg]
                            S.dve(lambda: nc.vector.tensor_scalar(out=t2[64:65, :], in0=po[64:65, :], scalar1=C.tiny[64:65, 0:1],
                                                                  scalar2=None, op0=ALU.max), r=[po, C.tiny], w=[t2])
                            S.dve(lambda: nc.vector.reciprocal(out=t2[64:65, :], in_=t2[64:65, :]), r=[t2], w=[t2])
                            mm(S, pb_, pb_[0:64, :], C.ones_f, C.ones_f[64:65, 0:64], t2, t2[64:65, :], True, True)
                            S.act(lambda: nc.scalar.copy(out=t1[:], in_=po[0:64, :]), r=[po], w=[t1])
                            S.dve(lambda: nc.vector.tensor_tensor(out=t1[:], in0=t1[:], in1=pb_[0:64, :], op=ALU.mult),
                                  r=[t1, pb_], w=[t1])
                            mm(S, pb_, pb_[0:64, :], Sel12, Sel12[:, hg * 3 + br, :], gc_, gc_[:, :], True, True)
                            if br == 0:
                                S.dve(lambda: nc.vector.tensor_tensor(out=acc[hg][:], in0=t1[:], in1=pb_[0:64, :], op=ALU.mult),
                                      r=[t1, pb_], w=[acc[hg]])
                            else:
                                S.dve(lambda: nc.vector.tensor_tensor(out=t2[0:64, :], in0=t1[:], in1=pb_[0:64, :], op=ALU.mult),
                                      r=[t1, pb_], w=[t2])
                                S.pool(lambda: nc.gpsimd.tensor_tensor(out=acc[hg][:], in0=acc[hg][:], in1=t2[0:64, :], op=ALU.add),
                                       r=[acc[hg], t2], w=[acc[hg]])

                        Interleaver(S).run([(lambda hg=hg: norm_fn(hg)) for hg in range(4)])
                    for hg in range(4):
                        head_ = g * 4 + hg
                        ob = obf[hg % 2]
                        S.act(lambda ob=ob, hg=hg: nc.scalar.copy(out=ob[:], in_=acc[hg][:]), r=[acc[hg]], w=[ob])
                        S.dma("sp", oT_dram[head_ * 64:(head_ + 1) * 64, qs], ob[:], r=[ob], w=[oT_dram])
            S.barrier()
        S.barrier()


W_NAMES = ["ev_w_in", "ev_w_uq", "ev_w_ukv", "ev_w_out", "ev_ff_gate", "ev_ff_up", "ev_ff_down",
           "od_w_in", "od_cmp_k1", "od_cmp_k2", "od_cmp_v1", "od_cmp_v2", "od_w_out", "od_router",
           "od_moe_w1", "od_moe_w3", "od_moe_w2"]
L_NAMES = ["xT", "cT", "ada_w", "ada_bT", "norm_gT", "final_gT", "pos64", "inv_freq2", "rope_sign", "ev_q_normT",
           "ev_kv_normT", "ev_conv_wT", "ev_a_log_b", "ev_dt_bias_b", "ev_dn_normT", "od_cmp_pos_kT", "od_cmp_pos_vT",
           "od_router_b_b"]


def build_program(n_layers=4):
    nc = bass.Bass("TRN2", target_bir_lowering=False)
    with ExitStack() as es:
        S = Sched(nc, es, ext_in=W_NAMES + L_NAMES, ext_out=["outT"])
        C = Ctx()
        S.init_psum()
        setup_consts(S, C)
        setup_attn_consts(S, C)
        phase_ada(S, C, 4)
        xT = S.dram("xT", [1024, 8192], F32)
        XA = S.dram("XA", [1024, 8192], F32)
        XB = S.dram("XB", [1024, 8192], F32)
        PT = S.dram("PT", [2608, 8192], F32)
        OT = S.dram("OT", [1024, 8192], BF16)
        VSd = S.dram("VS", [8192, 256], F32)
        VWd = S.dram("VW", [8192, 256], F32)
        outT = S.dram("outT", [1024, 8192], F32)
        ev_w_in = S.dram("ev_w_in", [2, 1024, 2504], F32)
        ev_w_out = S.dram("ev_w_out", [2, 1024, 1024], F32)
        wg = S.dram("ev_ff_gate", [2, 1024, 2816], F32)
        wu = S.dram("ev_ff_up", [2, 1024, 2816], F32)
        wd = S.dram("ev_ff_down", [2, 2816, 1024], F32)
        od_w_in = S.dram("od_w_in", [2, 1024, 2608], F32)
        od_w_out = S.dram("od_w_out", [2, 1024, 1024], F32)
        rt = S.dram("od_router", [2, 1024, 8], F32)
        rtb = S.dram("od_router_b_b", [2, 128, 8], F32)
        m1 = S.dram("od_moe_w1", [2, 8, 1024, 3584], F32)
        m3 = S.dram("od_moe_w3", [2, 8, 1024, 3584], F32)
        m2 = S.dram("od_moe_w2", [2, 8, 3584, 1024], F32)
        x_cur = xT
        for layer in range(n_layers):
            j = layer // 2
            ls = layer * 2
            if layer % 2 == 0:
                phase_inproj(S, C, x_cur, ls, ev_w_in, ev_w_in[j], 2504, PT)
                phase_dn(S, C, PT, j, OT)
                phase_mla(S, C, PT, j, OT)
                phase_outproj(S, C, x_cur, ls, ev_w_out, ev_w_out[j], OT, XA)
                phase_ffn(S, C, XA, ls + 1, XB, 2816, 1, wg, lambda e, j=j: wg[j], wu, lambda e, j=j: wu[j],
                          wd, lambda e, j=j: wd[j], TB=1024)
            else:
                phase_inproj(S, C, x_cur, ls, od_w_in, od_w_in[j], 2608, PT, tok_major=[(1792, 256, VSd), (2304, 256, VWd)])
                phase_nsa2(S, C, PT, VSd, VWd, j, OT)
                phase_outproj(S, C, x_cur, ls, od_w_out, od_w_out[j], OT, XA)
                phase_ffn(S, C, XA, ls + 1, XB, 3584, 8, m1, lambda e, j=j: m1[j, e], m3, lambda e, j=j: m3[j, e],
                          m2, lambda e, j=j: m2[j, e], router=(rt, rt[j], rtb, rtb[j]), TB=1024)
            x_cur = XB
        phase_final(S, C, x_cur, outT)
        S.finish()
    return nc, S


def kernel(**inputs):
    f = np.float32
    nc, _ = build_program()
    shared = {k: np.ascontiguousarray(np.asarray(inputs[k], f)) for k in W_NAMES}
    in_maps = []
    for core in range(8):
        b = core % 4
        m = common_inputs(inputs, b)
        m["od_router_b_b"] = np.ascontiguousarray(np.broadcast_to(np.asarray(inputs["od_router_b"], f)[:, None, :], (2, 128, 8)))
        d = dict(shared)
        for k in L_NAMES:
            d[k] = m[k]
        in_maps.append(d)
    res = run_bass_kernel_spmd(nc, in_maps, core_ids=list(range(8)))
    out = np.stack([np.ascontiguousarray(res.results[b]["outT"].T) for b in range(4)], axis=0)
    return out.astype(np.float32)
```
